# Optimizing a Trainium2 kernel written in Bass

```python
import math
import jax
import jax.numpy as jnp
from jax import lax
import numpy as np


D_MODEL = 1024
BATCH = 4
SEQ = 4096
DEPTH = 2

GRID_W = 64
CTX_LEN = 256

N_MOD = 6
NORM_EPS = 1e-6
NEG_INF = -1e30

ATT_HEADS = 8
ATT_KV_HEADS = 2
ATT_GROUP = ATT_HEADS // ATT_KV_HEADS
HEAD_DIM = 64
ATT_WIDTH = ATT_HEADS * HEAD_DIM
KV_WIDTH = ATT_KV_HEADS * HEAD_DIM
WINDOW = 128
ATT_BLOCK = 128
ROPE_BASE = 10000.0
ROPE_PAIRS_PER_AXIS = HEAD_DIM // 4

HY_WIDTH = 512
HY_ORDER = 2
HY_N_PROJ = HY_ORDER + 1
HY_SHORT = 3
HY_BANDS = 16
HY_EMB = 1 + 2 * HY_BANDS
HY_FILTER_HIDDEN = 64
HY_DECAY_TARGET = 1e-2
HY_FAST_RATE = -math.log(HY_DECAY_TARGET) / 0.3
HY_SLOW_RATE = -math.log(HY_DECAY_TARGET) / 1.5

S5_WIDTH = 512
S5_GROUP = 16
S5_GROUPS = S5_WIDTH // S5_GROUP
S5_STATE = 64
S5_DT_MIN = 1e-3
S5_DT_MAX = 1e-1

N_BRANCH = 3

PEER_HEADS = 8
PEER_KEYS = 128
PEER_EXPERTS = PEER_KEYS * PEER_KEYS
PEER_HALF = 128
PEER_QDIM = 2 * PEER_HALF
PEER_TOPK = 16
PEER_BLOCK = 128

COL_Q = 0
COL_K = COL_Q + ATT_WIDTH
COL_V = COL_K + KV_WIDTH
COL_S5 = COL_V + KV_WIDTH
COL_HY = COL_S5 + S5_WIDTH
COL_GATE = COL_HY + HY_N_PROJ * HY_WIDTH
IN_WIDTH = COL_GATE + N_BRANCH * D_MODEL

kernel_name = 'hybrid_diffusion_trunk'


def rms_norm(x, g):
    xf = x.astype(jnp.float32)
    y = xf * lax.rsqrt(jnp.mean(xf * xf, axis=-1, keepdims=True) + NORM_EPS)
    return (y * g.astype(jnp.float32)).astype(x.dtype)


def modulate(h, shift, scale):
    return h * (1 + scale) + shift


def axial_rope_tables(L):
    rows_n = L // GRID_W
    row = jnp.repeat(jnp.arange(rows_n), GRID_W).astype(jnp.float32)
    col = jnp.tile(jnp.arange(GRID_W), rows_n).astype(jnp.float32)
    inv = jnp.power(ROPE_BASE, -jnp.arange(ROPE_PAIRS_PER_AXIS, dtype=jnp.float32) / ROPE_PAIRS_PER_AXIS)
    ang = jnp.concatenate([row[:, None] * inv, col[:, None] * inv], axis=-1)
    return jnp.cos(ang), jnp.sin(ang)


def apply_rope(t, cos, sin):
    tf = t.astype(jnp.float32)
    t1, t2 = jnp.split(tf, 2, axis=-1)
    c = cos[None, :, None, :]
    s = sin[None, :, None, :]
    return jnp.concatenate([t1 * c - t2 * s, t1 * s + t2 * c], axis=-1).astype(t.dtype)


def latent_window_attention(q, k, v, kc, vc, sink):
    B, L = q.shape[:2]
    C = kc.shape[1]
    nb = L // ATT_BLOCK
    scale = HEAD_DIM ** -0.5
    qb = q.reshape(B, nb, ATT_BLOCK, ATT_KV_HEADS, ATT_GROUP, HEAD_DIM)
    pad = ((0, 0), (ATT_BLOCK, ATT_BLOCK), (0, 0), (0, 0))

    def band(t):
        tp = jnp.pad(t, pad).reshape(B, nb + 2, ATT_BLOCK, ATT_KV_HEADS, HEAD_DIM)
        return jnp.concatenate([tp[:, :-2], tp[:, 1:-1], tp[:, 2:]], axis=2)

    kb = band(k)
    vb = band(v)
    s_loc = jnp.einsum('bnqhgd,bnkhd->bnhgqk', qb, kb).astype(jnp.float32) * scale
    s_ctx = jnp.einsum('bnqhgd,bchd->bnhgqc', qb, kc).astype(jnp.float32) * scale
    blk = jnp.arange(nb)[:, None, None] * ATT_BLOCK
    qpos = blk + jnp.arange(ATT_BLOCK)[None, :, None]
    kpos = blk - ATT_BLOCK + jnp.arange(3 * ATT_BLOCK)[None, None, :]
    valid = (jnp.abs(kpos - qpos) <= WINDOW) & (kpos >= 0) & (kpos < L)
    s_loc = jnp.where(valid[None, :, None, None], s_loc, NEG_INF)
    s_sink = jnp.broadcast_to(sink.astype(jnp.float32).reshape(1, 1, ATT_KV_HEADS, ATT_GROUP, 1, 1),
                              s_ctx.shape[:-1] + (1,))
    p = jax.nn.softmax(jnp.concatenate([s_sink, s_ctx, s_loc], axis=-1), axis=-1).astype(v.dtype)
    p_ctx = p[..., 1:1 + C]
    p_loc = p[..., 1 + C:]
    o = (jnp.einsum('bnhgqc,bchd->bnqhgd', p_ctx, vc)
         + jnp.einsum('bnhgqk,bnkhd->bnqhgd', p_loc, vb))
    return o.reshape(B, L, ATT_WIDTH)


def context_attention(qc, kc, vc, sink):
    B, C = qc.shape[:2]
    qg = qc.reshape(B, C, ATT_KV_HEADS, ATT_GROUP, HEAD_DIM)
    s = jnp.einsum('bqhgd,bkhd->bhgqk', qg, kc).astype(jnp.float32) * (HEAD_DIM ** -0.5)
    s_sink = jnp.broadcast_to(sink.astype(jnp.float32).reshape(1, ATT_KV_HEADS, ATT_GROUP, 1, 1),
                              s.shape[:-1] + (1,))
    p = jax.nn.softmax(jnp.concatenate([s_sink, s], axis=-1), axis=-1)[..., 1:].astype(vc.dtype)
    o = jnp.einsum('bhgqk,bkhd->bqhgd', p, vc)
    return o.reshape(B, C, ATT_WIDTH)


def short_conv(z, w, b):
    L = z.shape[1]
    half = HY_SHORT // 2
    zp = jnp.pad(z, ((0, 0), (half, half), (0, 0)))
    out = b
    for j in range(HY_SHORT):
        out = out + zp[:, j:j + L] * w[j]
    return out


def hyena_filters(L, lp):
    f32 = jnp.float32
    t = jnp.arange(L, dtype=f32)
    tn = t / L
    bands = jnp.arange(1, HY_BANDS + 1, dtype=f32)
    ang = 2.0 * math.pi * tn[:, None] * bands[None, :]
    z = jnp.concatenate([tn[:, None], jnp.cos(ang), jnp.sin(ang)], axis=-1)
    hdn = jnp.sin(lp['hy_freq1'].astype(f32) * (z @ lp['hy_w1'].astype(f32) + lp['hy_b1'].astype(f32)))
    hdn = jnp.sin(lp['hy_freq2'].astype(f32) * (hdn @ lp['hy_w2'].astype(f32) + lp['hy_b2'].astype(f32)))
    filt = hdn @ lp['hy_w3'].astype(f32)
    rate = jnp.linspace(HY_FAST_RATE, HY_SLOW_RATE, HY_WIDTH, dtype=f32)
    tw = jnp.linspace(0.0, 1.0, L, dtype=f32)
    window = jnp.exp(-tw[:, None] * rate[None, :])
    return filt[:, :HY_WIDTH] * window, filt[:, HY_WIDTH:] * window


def bidirectional_fftconv(u, h_fwd, h_bwd):
    L = u.shape[1]
    n = 2 * L
    kern = jnp.concatenate([h_fwd, jnp.zeros_like(h_fwd[:1]), h_bwd[1:][::-1]], axis=0)
    kf = jnp.fft.rfft(kern, n=n, axis=0)
    uf = jnp.fft.rfft(u.astype(jnp.float32), n=n, axis=1)
    return jnp.fft.irfft(uf * kf[None], n=n, axis=1)[:, :L]


def hyena_mixer(z, lp):
    L = z.shape[1]
    z = short_conv(z, lp['hy_short_w'], lp['hy_short_b'])
    x0, x1, v = jnp.split(z, HY_N_PROJ, axis=-1)
    h_fwd, h_bwd = hyena_filters(L, lp)
    v = v * x1
    y = bidirectional_fftconv(v, h_fwd, h_bwd).astype(z.dtype) + v * lp['hy_bias']
    return y * x0


def s5_discretize(lp, d):
    f32 = jnp.float32
    lam = lax.complex(jnp.minimum(lp['s5_a_re'][d].astype(f32), -1e-4), lp['s5_a_im'][d].astype(f32))
    dt = jnp.exp(lp['s5_log_dt'][d].astype(f32))[:, None]
    abar = jnp.exp(lam * dt)
    b = lax.complex(lp['s5_b_re'][d].astype(f32), lp['s5_b_im'][d].astype(f32))
    bbar = ((abar - 1.0) / lam)[..., None] * b
    cmat = lax.complex(lp['s5_c_re'][d].astype(f32), lp['s5_c_im'][d].astype(f32))
    return abar, bbar, cmat


def s5_drive(u, bbar):
    B, L, _ = u.shape
    ug = u.astype(jnp.float32).reshape(B, L, S5_GROUPS, S5_GROUP).astype(jnp.complex64)
    return jnp.einsum('blgh,gph->blgp', ug, bbar)


def s5_scan(bu, abar, h0, reverse):
    if h0 is not None:
        edge = -1 if reverse else 0
        bu = bu.at[:, edge].add(abar * h0)
    a = jnp.broadcast_to(abar, bu.shape)

    def combine(e1, e2):
        a1, b1 = e1
        a2, b2 = e2
        return a1 * a2, a2 * b1 + b2

    _, h = lax.associative_scan(combine, (a, bu), reverse=reverse, axis=1)
    return h


def s5_readout(h_fwd, h_bwd, c_fwd, c_bwd, u, lp):
    B, L, _ = u.shape
    y = (jnp.einsum('blgp,ghp->blgh', h_fwd, c_fwd)
         + jnp.einsum('blgp,ghp->blgh', h_bwd, c_bwd)).real.reshape(B, L, S5_WIDTH)
    y = (y + lp['s5_d'].astype(jnp.float32) * u.astype(jnp.float32)).astype(u.dtype)
    y = jax.nn.gelu(y)
    return y * jax.nn.sigmoid(y @ lp['s5_glu_w'] + lp['s5_glu_b'])


def s5_mixer(u, uc, lp, need_ctx):
    af, bf, cf = s5_discretize(lp, 0)
    ab, bb, cb = s5_discretize(lp, 1)
    hcf = s5_scan(s5_drive(uc, bf), af, None, False)
    hcb = s5_scan(s5_drive(uc, bb), ab, None, True)
    hf = s5_scan(s5_drive(u, bf), af, hcf[:, -1], False)
    hb = s5_scan(s5_drive(u, bb), ab, hcb[:, 0], True)
    y = s5_readout(hf, hb, cf, cb, u, lp)
    yc = s5_readout(hcf, hcb, cf, cb, uc, lp) if need_ctx else None
    return y, yc


def branch_merge(att, hy, s5, gate_logits, lp):
    g = jax.nn.sigmoid(gate_logits.astype(jnp.float32)).astype(att.dtype)
    ga, gh, gs = jnp.split(g, N_BRANCH, axis=-1)
    m = ga * (att @ lp['br_attn_w']) + gh * (hy @ lp['br_hyena_w']) + gs * (s5 @ lp['br_s5_w'])
    return m @ lp['out_w']


def token_mixer(h, hc, rope_cos, rope_sin, lp, need_ctx):
    B, L, _ = h.shape
    C = hc.shape[1]
    w_in = lp['in_w']
    proj = h @ w_in
    q = apply_rope(proj[..., COL_Q:COL_K].reshape(B, L, ATT_HEADS, HEAD_DIM), rope_cos, rope_sin)
    k = apply_rope(proj[..., COL_K:COL_V].reshape(B, L, ATT_KV_HEADS, HEAD_DIM), rope_cos, rope_sin)
    v = proj[..., COL_V:COL_S5].reshape(B, L, ATT_KV_HEADS, HEAD_DIM)
    u_s5 = proj[..., COL_S5:COL_HY]
    z_hy = proj[..., COL_HY:COL_GATE]
    gates = proj[..., COL_GATE:] + lp['gate_b']
    lo = COL_Q if need_ctx else COL_K
    hi = IN_WIDTH if need_ctx else COL_HY
    projc = hc @ w_in[:, lo:hi]
    kc = projc[..., COL_K - lo:COL_V - lo].reshape(B, C, ATT_KV_HEADS, HEAD_DIM)
    vc = projc[..., COL_V - lo:COL_S5 - lo].reshape(B, C, ATT_KV_HEADS, HEAD_DIM)
    uc_s5 = projc[..., COL_S5 - lo:COL_HY - lo]
    att = latent_window_attention(q, k, v, kc, vc, lp['attn_sink'])
    hy = hyena_mixer(z_hy, lp)
    s5, s5c = s5_mixer(u_s5, uc_s5, lp, need_ctx)
    out = branch_merge(att, hy, s5, gates, lp)
    if not need_ctx:
        return out, None
    qc = projc[..., COL_Q - lo:COL_K - lo].reshape(B, C, ATT_HEADS, HEAD_DIM)
    att_c = context_attention(qc, kc, vc, lp['attn_sink'])
    hy_c = hyena_mixer(projc[..., COL_HY - lo:COL_GATE - lo], lp)
    gates_c = projc[..., COL_GATE - lo:] + lp['gate_b']
    out_c = branch_merge(att_c, hy_c, s5c, gates_c, lp)
    return out, out_c


def peer_ffn(x, wq, sub_keys, u_tab, v_tab):
    B, L, D = x.shape
    nb = L // PEER_BLOCK
    xb = jnp.swapaxes(x.reshape(B, nb, PEER_BLOCK, D), 0, 1)
    kk = PEER_TOPK * PEER_TOPK

    def block(xc):
        q = (xc @ wq).reshape(B, PEER_BLOCK, PEER_HEADS, 2, PEER_HALF)
        s = jnp.einsum('bthsk,hsnk->bthsn', q, sub_keys).astype(jnp.float32)
        s_top, i_top = lax.top_k(s, PEER_TOPK)
        cand = (s_top[..., 0, :, None] + s_top[..., 1, None, :]).reshape(B, PEER_BLOCK, PEER_HEADS, kk)
        cand_id = (i_top[..., 0, :, None] * PEER_KEYS + i_top[..., 1, None, :]).reshape(B, PEER_BLOCK, PEER_HEADS, kk)
        best, pos = lax.top_k(cand, PEER_TOPK)
        expert = jnp.take_along_axis(cand_id, pos, axis=-1)
        gate = jax.nn.softmax(best, axis=-1)
        act = jax.nn.gelu(jnp.einsum('btd,bthkd->bthk', xc, u_tab[expert]).astype(jnp.float32))
        w = (gate * act).astype(xc.dtype)
        return jnp.einsum('bthk,bthkd->btd', w, v_tab[expert])

    return jnp.swapaxes(lax.map(block, xb), 0, 1).reshape(B, L, D)


def setup_inputs(seed: int = 0) -> dict:
    key = jax.random.key(seed)
    keys = iter(jax.random.split(key, 64))
    f32 = jnp.float32

    def nrm(shape, std):
        return std * jax.random.normal(next(keys), shape, f32)

    def near_one(shape):
        return 1.0 + nrm(shape, 0.02)

    L = DEPTH
    hy_cols = HY_N_PROJ * HY_WIDTH
    a_im0 = math.pi * jnp.arange(S5_STATE, dtype=f32)
    return {
        'x': nrm((BATCH, SEQ, D_MODEL), 1.0),
        'c': nrm((BATCH, D_MODEL), 1.0),
        'ctx': nrm((BATCH, CTX_LEN, D_MODEL), 1.0),
        'c_ctx': nrm((D_MODEL,), 1.0),
        'mod_w': nrm((L, D_MODEL, N_MOD * D_MODEL), 0.5 * D_MODEL ** -0.5),
        'mod_b': nrm((L, N_MOD * D_MODEL), 0.02),
        'norm1_g': near_one((L, D_MODEL)),
        'norm2_g': near_one((L, D_MODEL)),
        'in_w': nrm((L, D_MODEL, IN_WIDTH), D_MODEL ** -0.5),
        'gate_b': nrm((L, N_BRANCH * D_MODEL), 0.02),
        'attn_sink': nrm((L, ATT_HEADS), 0.5),
        'hy_short_w': nrm((L, HY_SHORT, hy_cols), HY_SHORT ** -0.5),
        'hy_short_b': nrm((L, hy_cols), 0.02),
        'hy_w1': nrm((L, HY_EMB, HY_FILTER_HIDDEN), HY_EMB ** -0.5),
        'hy_b1': nrm((L, HY_FILTER_HIDDEN), 0.1),
        'hy_freq1': 1.0 + nrm((L, HY_FILTER_HIDDEN), 0.1),
        'hy_w2': nrm((L, HY_FILTER_HIDDEN, HY_FILTER_HIDDEN), HY_FILTER_HIDDEN ** -0.5),
        'hy_b2': nrm((L, HY_FILTER_HIDDEN), 0.1),
        'hy_freq2': 1.0 + nrm((L, HY_FILTER_HIDDEN), 0.1),
        'hy_w3': nrm((L, HY_FILTER_HIDDEN, 2 * HY_WIDTH), 0.01),
        'hy_bias': nrm((L, HY_WIDTH), 0.5),
        's5_a_re': -0.5 + nrm((L, 2, S5_GROUPS, S5_STATE), 0.01),
        's5_a_im': a_im0 + nrm((L, 2, S5_GROUPS, S5_STATE), 0.01),
        's5_log_dt': jax.random.uniform(next(keys), (L, 2, S5_GROUPS), f32, math.log(S5_DT_MIN), math.log(S5_DT_MAX)),
        's5_b_re': nrm((L, 2, S5_GROUPS, S5_STATE, S5_GROUP), (2 * S5_GROUP) ** -0.5),
        's5_b_im': nrm((L, 2, S5_GROUPS, S5_STATE, S5_GROUP), (2 * S5_GROUP) ** -0.5),
        's5_c_re': nrm((L, 2, S5_GROUPS, S5_GROUP, S5_STATE), S5_STATE ** -0.5),
        's5_c_im': nrm((L, 2, S5_GROUPS, S5_GROUP, S5_STATE), S5_STATE ** -0.5),
        's5_d': nrm((L, S5_WIDTH), 1.0),
        's5_glu_w': nrm((L, S5_WIDTH, S5_WIDTH), S5_WIDTH ** -0.5),
        's5_glu_b': nrm((L, S5_WIDTH), 0.02),
        'br_attn_w': nrm((L, ATT_WIDTH, D_MODEL), ATT_WIDTH ** -0.5),
        'br_hyena_w': nrm((L, HY_WIDTH, D_MODEL), HY_WIDTH ** -0.5),
        'br_s5_w': nrm((L, S5_WIDTH, D_MODEL), S5_WIDTH ** -0.5),
        'out_w': nrm((L, D_MODEL, D_MODEL), D_MODEL ** -0.5),
        'peer_wq': nrm((L, D_MODEL, PEER_HEADS * PEER_QDIM), D_MODEL ** -0.5),
        'peer_keys': nrm((L, PEER_HEADS, 2, PEER_KEYS, PEER_HALF), PEER_HALF ** -0.5),
        'peer_u': nrm((L, PEER_EXPERTS, D_MODEL), D_MODEL ** -0.5),
        'peer_v': nrm((L, PEER_EXPERTS, D_MODEL), PEER_HEADS ** -0.5),
        'final_g': near_one((D_MODEL,)),
    }


def reference(x, c, ctx, c_ctx, mod_w, mod_b, norm1_g, norm2_g, in_w, gate_b, attn_sink,
              hy_short_w, hy_short_b, hy_w1, hy_b1, hy_freq1, hy_w2, hy_b2, hy_freq2, hy_w3, hy_bias,
              s5_a_re, s5_a_im, s5_log_dt, s5_b_re, s5_b_im, s5_c_re, s5_c_im, s5_d, s5_glu_w, s5_glu_b,
              br_attn_w, br_hyena_w, br_s5_w, out_w, peer_wq, peer_keys, peer_u, peer_v, final_g):
    L = x.shape[1]
    rope_cos, rope_sin = axial_rope_tables(L)
    xc = ctx
    silu_c = jax.nn.silu(c)
    silu_cc = jax.nn.silu(c_ctx)
    for layer in range(DEPTH):
        need_ctx = layer < DEPTH - 1
        lp = dict(in_w=in_w[layer], gate_b=gate_b[layer], attn_sink=attn_sink[layer],
                  hy_short_w=hy_short_w[layer], hy_short_b=hy_short_b[layer],
                  hy_w1=hy_w1[layer], hy_b1=hy_b1[layer], hy_freq1=hy_freq1[layer],
                  hy_w2=hy_w2[layer], hy_b2=hy_b2[layer], hy_freq2=hy_freq2[layer],
                  hy_w3=hy_w3[layer], hy_bias=hy_bias[layer],
                  s5_a_re=s5_a_re[layer], s5_a_im=s5_a_im[layer], s5_log_dt=s5_log_dt[layer],
                  s5_b_re=s5_b_re[layer], s5_b_im=s5_b_im[layer],
                  s5_c_re=s5_c_re[layer], s5_c_im=s5_c_im[layer], s5_d=s5_d[layer],
                  s5_glu_w=s5_glu_w[layer], s5_glu_b=s5_glu_b[layer],
                  br_attn_w=br_attn_w[layer], br_hyena_w=br_hyena_w[layer], br_s5_w=br_s5_w[layer],
                  out_w=out_w[layer])
        mod = (silu_c @ mod_w[layer] + mod_b[layer])[:, None, :]
        sh1, sc1, g1, sh2, sc2, g2 = jnp.split(mod, N_MOD, axis=-1)
        n_c = (N_MOD if need_ctx else 2) * D_MODEL
        mc = jnp.split(silu_cc @ mod_w[layer][:, :n_c] + mod_b[layer][:n_c], n_c // D_MODEL)
        h = modulate(rms_norm(x, norm1_g[layer]), sh1, sc1)
        hc = modulate(rms_norm(xc, norm1_g[layer]), mc[0], mc[1])
        y, yc = token_mixer(h, hc, rope_cos, rope_sin, lp, need_ctx)
        x = x + g1 * y
        h = modulate(rms_norm(x, norm2_g[layer]), sh2, sc2)
        x = x + g2 * peer_ffn(h, peer_wq[layer], peer_keys[layer], peer_u[layer], peer_v[layer])
        if need_ctx:
            xc = xc + mc[2] * yc
            hc = modulate(rms_norm(xc, norm2_g[layer]), mc[3], mc[4])
            xc = xc + mc[5] * peer_ffn(hc, peer_wq[layer], peer_keys[layer], peer_u[layer], peer_v[layer])
    return rms_norm(x, final_g)
```

```python
import math
import numpy as np
from contextlib import ExitStack
import concourse.bass as bass
import concourse.mybir as mybir
from concourse.bass_utils import run_bass_kernel_spmd

F32 = mybir.dt.float32
I32 = mybir.dt.int32
U32 = mybir.dt.uint32
AF = mybir.ActivationFunctionType
ALU = mybir.AluOpType
AX = mybir.AxisListType

D = 1024
L = 4096
C = 256
NT = L + C
DEPTH = 2
IN_W = 5888
NBLK = [(0, 256)] + [(256 + 512 * i, 512) for i in range(8)]


class Res:
    __slots__ = ("name", "w", "r", "ep")

    def __init__(self, name):
        self.name = name
        self.w = None
        self.r = {}
        self.ep = 0


class Tile(Res):
    __slots__ = ("t",)

    def __init__(self, name, t):
        Res.__init__(self, name)
        self.t = t

    def __getitem__(self, k):
        return self.t[k]


class Eng:
    def __init__(self, name, e, sem):
        self.name = name
        self.e = e
        self.sem = sem
        self.cnt = 0
        self.known = {}


class K:
    SAME_ENGINE_SYNC = True
    ROTATE_AT = 20000

    def __init__(self, nc, n_dma_slots=24):
        self.nc = nc
        self._sems = []
        self.es = ExitStack()
        self.epoch = 1
        self.uid = 0
        self.eng = {}
        for nm, e in (("pe", nc.tensor), ("act", nc.scalar), ("dve", nc.vector),
                      ("pool", nc.gpsimd), ("sp", nc.sync)):
            self.eng[nm] = Eng(nm, e, self.es.enter_context(nc.semaphore("sem_" + nm)))
        self.slots = []
        for i in range(n_dma_slots):
            s = self.es.enter_context(nc.semaphore("dsem%d" % i))
            self.slots.append([s, 0])
        self.slot_i = 0
        self.phase_stack = None
        self.psum = []
        for i in range(8):
            t = self.es.enter_context(nc.psum_tensor("ps%d" % i, [128, 512], F32))
            self.psum.append(Tile("ps%d" % i, t))
        self.ps_i = 0
        self.n_ins = 0
        self.n_rot = 0
        self._sems = []

    def next_ps(self):
        p = self.psum[self.ps_i]
        self.ps_i = (self.ps_i + 1) % 8
        return p

    def sb(self, name, shape, dtype=F32, persistent=False):
        self.uid += 1
        st = self.es if (persistent or self.phase_stack is None) else self.phase_stack
        t = st.enter_context(self.nc.sbuf_tensor("%s_%d" % (name, self.uid), list(shape), dtype))
        return Tile(name, t)

    def pool(self, name, shape, n, dtype=F32):
        tiles = [self.sb("%s%d" % (name, i), shape, dtype) for i in range(n)]
        st = {"i": 0}

        def nxt():
            t = tiles[st["i"]]
            st["i"] = (st["i"] + 1) % n
            return t
        return nxt

    def dram(self, name, shape, dtype=F32, kind="Internal"):
        t = self.nc.dram_tensor(name, list(shape), dtype, kind=kind)
        return Tile(name, t.ap())

    def phase_begin(self):
        assert self.phase_stack is None
        self.phase_stack = ExitStack()

    def phase_end(self):
        self.barrier()
        self.phase_stack.close()
        self.phase_stack = None

    def _key(self, sem):
        for i, s_ in enumerate(self._sems):
            if s_ is sem:
                return i
        self._sems.append(sem)
        return len(self._sems) - 1

    def _wait(self, E, ev):
        sem, val = ev
        k = self._key(sem)
        if E.known.get(k, 0) >= val:
            return
        E.e.wait_ge(sem, val)
        E.known[k] = val
        self.n_ins += 1

    def _deps(self, E, reads, writes, is_pe=False):
        evs = []
        for r in reads:
            if r.ep == self.epoch and r.w is not None:
                evs.append(r.w)
        for w in writes:
            if w.ep == self.epoch:
                if w.w is not None:
                    evs.append(w.w)
                evs.extend(w.r.values())
        for ev in evs:
            if ev[0] is E.sem and (is_pe or not self.SAME_ENGINE_SYNC):
                continue
            self._wait(E, ev)

    def _record(self, ev, reads, writes):
        for r in reads:
            if r.ep != self.epoch:
                r.ep = self.epoch
                r.w = None
                r.r = {}
            r.r[self._key(ev[0])] = ev
        for w in writes:
            w.ep = self.epoch
            w.w = ev
            w.r = {}

    def op(self, eng, fn, reads=(), writes=()):
        E = self.eng[eng]
        self._deps(E, reads, writes, is_pe=(eng == "pe"))
        ins = fn(E.e)
        E.cnt += 1
        ins.then_inc(E.sem, 1)
        self.n_ins += 1
        self._record((E.sem, E.cnt), reads, writes)
        return ins

    def dma(self, out_ap, in_ap, reads=(), writes=(), q="sp", fn=None):
        E = self.eng[q]
        self._deps(E, reads, writes)
        slot = self.slots[self.slot_i]
        self.slot_i = (self.slot_i + 1) % len(self.slots)
        if slot[1] > 0:
            self._wait(E, (slot[0], slot[1]))
        ins = E.e.dma_start(out=out_ap, in_=in_ap) if fn is None else fn(E.e)
        slot[1] += 16
        ins.then_inc(slot[0], 16)
        self.n_ins += 1
        self._record((slot[0], slot[1]), reads, writes)
        return ins

    def barrier(self):
        evs = []
        for E in self.eng.values():
            if E.cnt > 0:
                evs.append((E.sem, E.cnt))
        for s in self.slots:
            if s[1] > 0:
                evs.append((s[0], s[1]))
        for E in self.eng.values():
            for ev in evs:
                if ev[0] is E.sem:
                    continue
                self._wait(E, ev)
        self.epoch += 1
        for E in self.eng.values():
            if E.cnt > self.ROTATE_AT:
                self.n_rot += 1
                E.sem = self.es.enter_context(self.nc.semaphore("sem_%s_r%d" % (E.name, self.n_rot)))
                E.cnt = 0
        for s in self.slots:
            if s[1] > self.ROTATE_AT:
                self.n_rot += 1
                s[0] = self.es.enter_context(self.nc.semaphore("dsem_r%d" % self.n_rot))
                s[1] = 0

    def finish(self):
        self.barrier()
        self.es.close()

    def mm(self, ps, out_ap, lt, lhsT_ap, rt, rhs_ap, start, stop):
        return self.op("pe", lambda e: e.matmul(out_ap, lhsT_ap, rhs_ap, start=start, stop=stop),
                       reads=[lt, rt], writes=[ps])


def _rot_perm():
    perm = []
    for hd in range(10):
        for i in range(64):
            perm.append(hd * 64 + (i + 32) % 64)
    return np.array(perm)


def _rope_tables():
    row = np.repeat(np.arange(L // 64), 64).astype(np.float32)
    col = np.tile(np.arange(64), L // 64).astype(np.float32)
    inv = np.power(np.float32(10000.0), -np.arange(16, dtype=np.float32) / 16).astype(np.float32)
    ang = np.concatenate([row[:, None] * inv, col[:, None] * inv], axis=-1)
    cos = np.cos(ang).astype(np.float32)
    sin = np.sin(ang).astype(np.float32)
    cosT = np.ones((64, NT), np.float32)
    sinT = np.zeros((64, NT), np.float32)
    cosT[:32, C:] = cos.T
    cosT[32:, C:] = cos.T
    sinT[:32, C:] = -sin.T
    sinT[32:, C:] = sin.T
    return np.concatenate([cosT, cosT], 0), np.concatenate([sinT, sinT], 0)


def _pk(v, n):
    return np.ascontiguousarray(np.asarray(v, np.float32).reshape(n, 128).T)


def _prep(inp, b):
    m = {}
    m["xT"] = np.ascontiguousarray(inp["x"][b].T)
    m["ctxT"] = np.ascontiguousarray(inp["ctx"][b].T)
    cv = np.stack([_pk(inp["c"][b], 8), _pk(inp["c_ctx"], 8)], axis=-1)
    m["cvec"] = np.ascontiguousarray(cv.reshape(128, 16))
    perm = _rot_perm()
    cosT, sinT = _rope_tables()
    m["ropec"] = cosT
    qq = np.arange(128)[:, None]
    kk = np.arange(128)[None, :]
    mk = np.zeros((128, 256), np.float32)
    mk[:, :128] = np.where(kk >= qq, 0.0, -1e30)
    mk[:, 128:] = np.where(kk <= qq, 0.0, -1e30)
    m["amask"] = mk
    m["final_g"] = _pk(inp["final_g"], 8)
    csel = np.zeros((128, 255), np.float32)
    csel[:, 127] = 1.0
    m["csel"] = csel
    m["iota16"] = np.ascontiguousarray(np.broadcast_to(np.arange(16, dtype=np.float32)[None, :], (128, 16)))
    for nm, Ls in (("L", L), ("C", C)):
        t = np.arange(Ls, dtype=np.float32)
        tn = t / np.float32(Ls)
        bands = np.arange(1, 17, dtype=np.float32)
        ang = np.float32(2.0 * math.pi) * tn[:, None] * bands[None, :]
        zf = np.concatenate([tn[:, None], np.cos(ang), np.sin(ang)], axis=-1).astype(np.float32)
        m["hyz" + nm] = np.ascontiguousarray(zf.T)
        tw = np.linspace(0.0, 1.0, Ls, dtype=np.float32)
        m["hytw" + nm] = np.ascontiguousarray((-tw).reshape(Ls // 128, 128).T)
    fast = -math.log(1e-2) / 0.3
    slow = -math.log(1e-2) / 1.5
    rate = np.linspace(fast, slow, 512, dtype=np.float32)
    m["hyrate"] = np.ascontiguousarray(np.broadcast_to(rate[None, :], (128, 512)))
    m["ropes"] = sinT
    for l in range(DEPTH):
        m["mod_w%d" % l] = np.ascontiguousarray(inp["mod_w"][l])
        m["mod_b%d" % l] = _pk(inp["mod_b"][l], 48)
        m["n1g%d" % l] = _pk(inp["norm1_g"][l], 8)
        m["in_w%d" % l] = np.ascontiguousarray(inp["in_w"][l])
        m["in_wr%d" % l] = np.ascontiguousarray(inp["in_w"][l][:, perm])
        m["gate_b%d" % l] = _pk(inp["gate_b"][l], 24)
        m["hy_sw%d" % l] = np.ascontiguousarray(np.asarray(inp["hy_short_w"][l], np.float32).reshape(3, 12, 128).transpose(2, 1, 0))
        m["hy_sb%d" % l] = _pk(inp["hy_short_b"][l], 12)
        m["hy_w1%d" % l] = np.ascontiguousarray(inp["hy_w1"][l])
        m["hy_w2%d" % l] = np.ascontiguousarray(inp["hy_w2"][l])
        m["hy_w3%d" % l] = np.ascontiguousarray(inp["hy_w3"][l])
        m["hy_fb%d" % l] = np.ascontiguousarray(np.stack([inp["hy_b1"][l], inp["hy_freq1"][l], inp["hy_b2"][l], inp["hy_freq2"][l]], 1).astype(np.float32))
        m["hy_bias%d" % l] = _pk(inp["hy_bias"][l], 4)
        for nm_ in ("s5_a_re", "s5_a_im"):
            m["%s%d" % (nm_, l)] = np.ascontiguousarray(np.asarray(inp[nm_][l], np.float32).reshape(2, 16, 128).transpose(2, 0, 1))
        m["s5_ldt%d" % l] = np.ascontiguousarray(np.repeat(np.asarray(inp["s5_log_dt"][l], np.float32), 64, axis=1).reshape(2, 16, 128).transpose(2, 0, 1))
        for nm_ in ("s5_b_re", "s5_b_im"):
            m["%s%d" % (nm_, l)] = np.ascontiguousarray(np.asarray(inp[nm_][l], np.float32).reshape(2, 2048, 16))
        for nm_ in ("s5_c_re", "s5_c_im"):
            m["%s%d" % (nm_, l)] = np.ascontiguousarray(np.asarray(inp[nm_][l], np.float32).transpose(0, 1, 3, 2).reshape(2, 2048, 16))
        m["s5_d%d" % l] = np.ascontiguousarray(np.asarray(inp["s5_d"][l], np.float32).reshape(16, 32).T)
        m["s5_gw%d" % l] = np.ascontiguousarray(inp["s5_glu_w"][l])
        m["s5_gb%d" % l] = _pk(inp["s5_glu_b"][l], 4)
        for nm_ in ("br_attn_w", "br_hyena_w", "br_s5_w", "out_w"):
            m["%s%d" % (nm_, l)] = np.ascontiguousarray(inp[nm_][l])
        m["n2g%d" % l] = _pk(inp["norm2_g"][l], 8)
        m["peer_wq%d" % l] = np.ascontiguousarray(inp["peer_wq"][l])
        m["peer_kT%d" % l] = np.ascontiguousarray(np.asarray(inp["peer_keys"][l], np.float32).transpose(0, 1, 3, 2).reshape(16, 128, 128))
        m["peer_u%d" % l] = np.ascontiguousarray(inp["peer_u"][l])
        m["peer_v%d" % l] = np.ascontiguousarray(inp["peer_v"][l])
        m["sink%d" % l] = np.ascontiguousarray(np.broadcast_to(np.asarray(inp["attn_sink"][l], np.float32)[None, :], (128, 8)))
    return m


def build(dbg=False, n_layers=DEPTH, stop_after=None, start_at=None):
    nc = bass.Bass("TRN2", target_bir_lowering=False)
    k = K(nc)
    okind = "ExternalOutput" if dbg else "Internal"
    ein = lambda name, shape, dt=F32: k.dram(name, shape, dt, kind="ExternalInput")
    xT_in = ein("xT", [D, L])
    ctxT_in = ein("ctxT", [D, C])
    cvec_in = ein("cvec", [128, 16])
    ropec_in = ein("ropec", [128, NT])
    ropes_in = ein("ropes", [128, NT])
    amask_in = ein("amask", [128, 256])
    finalg_in = ein("final_g", [128, 8])
    csel_in = ein("csel", [128, 255])
    iota16_in = ein("iota16", [128, 16])
    hyz_in = {"L": ein("hyzL", [33, L]), "C": ein("hyzC", [33, C])}
    hytw_in = {"L": ein("hytwL", [128, L // 128]), "C": ein("hytwC", [128, C // 128])}
    hyrate_in = ein("hyrate", [128, 512])
    W = []
    for l in range(DEPTH):
        W.append(dict(mod_w=ein("mod_w%d" % l, [D, 6 * D]), mod_b=ein("mod_b%d" % l, [128, 48]),
                      n1g=ein("n1g%d" % l, [128, 8]), hy_sw=ein("hy_sw%d" % l, [128, 12, 3]), hy_sb=ein("hy_sb%d" % l, [128, 12]),
                      hy_w1=ein("hy_w1%d" % l, [33, 64]), hy_w2=ein("hy_w2%d" % l, [64, 64]), hy_w3=ein("hy_w3%d" % l, [64, 1024]),
                      hy_fb=ein("hy_fb%d" % l, [64, 4]),
                      n2g=ein("n2g%d" % l, [128, 8]), peer_wq=ein("peer_wq%d" % l, [D, 2048]), peer_kT=ein("peer_kT%d" % l, [16, 128, 128]),
                      peer_u=ein("peer_u%d" % l, [16384, D]), peer_v=ein("peer_v%d" % l, [16384, D]),
                      br_attn_w=ein("br_attn_w%d" % l, [512, D]), br_hyena_w=ein("br_hyena_w%d" % l, [512, D]), br_s5_w=ein("br_s5_w%d" % l, [512, D]),
                      out_w=ein("out_w%d" % l, [D, D]),
                      s5_a_re=ein("s5_a_re%d" % l, [128, 2, 16]), s5_a_im=ein("s5_a_im%d" % l, [128, 2, 16]), s5_ldt=ein("s5_ldt%d" % l, [128, 2, 16]),
                      s5_b_re=ein("s5_b_re%d" % l, [2, 2048, 16]), s5_b_im=ein("s5_b_im%d" % l, [2, 2048, 16]),
                      s5_c_re=ein("s5_c_re%d" % l, [2, 2048, 16]), s5_c_im=ein("s5_c_im%d" % l, [2, 2048, 16]),
                      s5_d=ein("s5_d%d" % l, [32, 16]), s5_gw=ein("s5_gw%d" % l, [512, 512]), s5_gb=ein("s5_gb%d" % l, [128, 4]), hy_bias=ein("hy_bias%d" % l, [128, 4]), sink=ein("sink%d" % l, [128, 8]), in_w=ein("in_w%d" % l, [D, IN_W]),
                      in_wr=ein("in_wr%d" % l, [D, 640]), gate_b=ein("gate_b%d" % l, [128, 24])))
    XT = k.dram("XT", [D, NT])
    MODT = k.dram("MODT", [128, 96], kind=okind)
    QT = k.dram("QT", [512, NT], kind=okind)
    KT = k.dram("KT", [128, NT], kind=okind)
    VTOK = k.dram("VTOK", [NT, 128], kind=okind)
    US5 = k.dram("US5", [512, NT], kind=okind)
    ZHY = k.dram("ZHY", [1536, NT], kind=okind)
    GATE = k.dram("GATE", [3072, NT], kind=okind)
    HT = k.dram("HT", [D, NT], kind=okind)
    MIX = k.dram("MIX", [D, NT], kind=okind)
    XDBG = k.dram("XDBG", [D, NT], kind=okind)
    S5Y = k.dram("S5Y", [512, NT], kind=okind)
    S5O = k.dram("S5O", [512, NT], kind=okind)
    HY = k.dram("HY", [512, NT], kind=okind)
    DFT = {}
    for nm, Ls in (("L", L), ("C", C)):
        DFT[nm] = {m_: k.dram("DFT_%s_%s" % (nm, m_), [Ls, Ls]) for m_ in ("CK", "SK", "CS", "SS")}
    HSDd, KRId, VXTd, VXFd, X0Fd = {}, {}, {}, {}, {}
    for nm, Ls in (("L", L), ("C", C)):
        HSDd[nm] = k.dram("HSD" + nm, [2, Ls, 512])
        KRId[nm] = k.dram("KRI" + nm, [2, Ls, 512])
        VXTd[nm] = k.dram("VXT" + nm, [Ls, 512])
        VXFd[nm] = k.dram("VXF" + nm, [512, Ls])
        X0Fd[nm] = k.dram("X0F" + nm, [512, Ls])
    ATT = k.dram("ATT", [512, NT], kind=okind)
    out = k.dram("out", [D, L], kind="ExternalOutput")

    ones = k.sb("ones", [128, 128], persistent=True)
    k.op("pool", lambda e: e.memset(ones[:], 1.0), writes=[ones])
    eps_t = k.sb("eps", [128, 1], persistent=True)
    k.op("pool", lambda e: e.memset(eps_t[:], 1e-6), writes=[eps_t])
    ident = k.sb("ident", [128, 128], persistent=True)
    k.op("pool", lambda e: e.memset(ident[:], 0.0), writes=[ident])
    k.op("pool", lambda e: e.affine_select(out=ident[:], in_=ident[:], pattern=[[-1, 128]],
                                           compare_op=ALU.not_equal, fill=1.0, base=0, channel_multiplier=1),
         reads=[ident], writes=[ident])

    k.phase_begin()
    stage_p = k.pool("stage", [128, NT], 2)
    for kc in range(8):
        st = stage_p()
        k.dma(st[:, 0:C], ctxT_in[kc * 128:(kc + 1) * 128, :], reads=[ctxT_in], writes=[st])
        k.dma(st[:, C:NT], xT_in[kc * 128:(kc + 1) * 128, :], reads=[xT_in], writes=[st])
        k.dma(XT[kc * 128:(kc + 1) * 128, :], st[:], reads=[st], writes=[XT])
    k.phase_end()


    k.phase_begin()
    negpi = k.sb("negpi", [128, 1])
    k.op("pool", lambda e: e.memset(negpi[:], -math.pi), writes=[negpi])
    colv_p = k.pool("colv", [128, 4096], 1, I32)
    pm_p = k.pool("pm", [128, 1], 2, I32)
    ri_p = k.pool("ri", [128, 4096], 2, I32)
    rj_p = k.pool("rj", [128, 4096], 2, I32)
    rf_p = k.pool("rf", [128, 4096], 2)
    go_p = k.pool("go", [128, 4096], 3)
    for nm, Ls in (("L", L), ("C", C)):
        N4 = 8 * Ls
        colv = colv_p()
        k.op("pool", lambda e, colv=colv: e.iota(colv[:, 0:Ls], pattern=[[2, Ls]], base=1, channel_multiplier=0), writes=[colv])
        for tt in range(Ls // 128):
            for shifted in (0, 1):
                pm = pm_p()
                k.op("pool", lambda e, pm=pm: e.iota(pm[:], pattern=[[0, 1]], base=2 * tt * 128 + shifted, channel_multiplier=2), writes=[pm])
                ri = ri_p()
                k.op("dve", lambda e, ri=ri, pm=pm: e.tensor_tensor(out=ri[:, 0:Ls], in0=colv[:, 0:Ls], in1=pm[:, 0:1].to_broadcast([128, Ls]), op=ALU.mult),
                     reads=[colv, pm], writes=[ri])
                for trig in ("S", "C"):
                    rj = rj_p()
                    if trig == "S":
                        k.op("dve", lambda e, ri=ri, rj=rj: e.tensor_single_scalar(rj[:, 0:Ls], ri[:, 0:Ls], N4 - 1, op=ALU.bitwise_and), reads=[ri], writes=[rj])
                    else:
                        k.op("dve", lambda e, ri=ri, rj=rj: e.tensor_single_scalar(rj[:, 0:Ls], ri[:, 0:Ls], N4 // 4, op=ALU.add), reads=[ri], writes=[rj])
                        k.op("dve", lambda e, rj=rj: e.tensor_single_scalar(rj[:, 0:Ls], rj[:, 0:Ls], N4 - 1, op=ALU.bitwise_and), reads=[rj], writes=[rj])
                    rf = rf_p()
                    k.op("pool", lambda e, rf=rf, rj=rj: e.tensor_copy(rf[:, 0:Ls], rj[:, 0:Ls]), reads=[rj], writes=[rf])
                    go = go_p()
                    k.op("act", lambda e, go=go, rf=rf: e.activation(out=go[:, 0:Ls], in_=rf[:, 0:Ls], func=AF.Sin, bias=negpi[:, 0:1], scale=2.0 * math.pi / N4),
                         reads=[rf, negpi], writes=[go])
                    dst = DFT[nm][trig + ("S" if shifted else "K")]
                    k.dma(dst[tt * 128:(tt + 1) * 128, :], go[:, 0:Ls], reads=[go], writes=[dst])
    k.phase_end()

    ORDER = ["0", "A", "B", "C", "D", "E", "F", "G"]
    skip = (lambda ph: start_at is not None and ORDER.index(ph) < ORDER.index(start_at))
    for l in range(n_layers):
        w = W[l]
        need_ctx = l < DEPTH - 1
        k.phase_begin()
        cv = k.sb("cv", [128, 16])
        k.dma(cv[:], cvec_in[:, :], reads=[cvec_in], writes=[cv])
        sl = k.sb("silu", [128, 16])
        k.op("act", lambda e: e.activation(out=sl[:], in_=cv[:], func=AF.Silu), reads=[cv], writes=[sl])
        mb = k.sb("mb", [128, 48])
        k.dma(mb[:], w["mod_b"][:, :], reads=[w["mod_b"]], writes=[mb])
        modt = k.sb("modt", [128, 96])
        mwv = w["mod_w"].t.rearrange("(kc p) n -> p kc n", p=128)
        mw_p = k.pool("mw", [128, 8, 1024], 2)
        for fg in range(6):
            mw = mw_p()
            for kc in range(8):
                k.dma(mw[:, kc, :], mwv[:, kc, fg * 1024:(fg + 1) * 1024], reads=[w["mod_w"]], writes=[mw])
            ps = k.next_ps()
            for fc in range(8):
                for kc in range(8):
                    k.mm(ps, ps[:, fc * 2:fc * 2 + 2], mw, mw[:, kc, fc * 128:(fc + 1) * 128],
                         sl, sl[:, kc * 2:kc * 2 + 2], kc == 0, kc == 7)
            for j in range(2):
                pv = ps.t[:, 0:16].rearrange("p (f j) -> p f j", j=2)[:, :, j]
                ov = modt.t[:, fg * 16:(fg + 1) * 16].rearrange("p (f j) -> p f j", j=2)[:, :, j]
                k.op("dve", lambda e, pv=pv, ov=ov: e.tensor_tensor(out=ov, in0=pv, in1=mb[:, fg * 8:(fg + 1) * 8], op=ALU.add),
                     reads=[ps, mb], writes=[modt])
        k.dma(MODT[:, :], modt[:], reads=[modt], writes=[MODT])
        k.phase_end()
        if stop_after == "A":
            break

        k.phase_begin()
        modt = k.sb("modt", [128, 96])
        k.dma(modt[:], MODT[:, :], reads=[MODT], writes=[modt])
        g1n = k.sb("g1n", [128, 8])
        k.dma(g1n[:], w["n1g"][:, :], reads=[w["n1g"]], writes=[g1n])
        gb = k.sb("gb", [128, 24])
        k.dma(gb[:], w["gate_b"][:, :], reads=[w["gate_b"]], writes=[gb])
        m3 = modt.t[:, :].rearrange("p (f j) -> p f j", j=2)
        Acoef = k.sb("Acoef", [128, 8, 2])
        for j in range(2):
            k.op("dve", lambda e, j=j: e.scalar_tensor_tensor(out=Acoef[:, :, j], in0=m3[:, 8:16, j], scalar=1.0, in1=g1n[:],
                                                             op0=ALU.add, op1=ALU.mult), reads=[modt, g1n], writes=[Acoef])
        iwv = w["in_w"].t.rearrange("(kc p) n -> p kc n", p=128)
        irv = w["in_wr"].t.rearrange("(kc p) n -> p kc n", p=128)
        groups = [("q", 0, 512), ("k", 512, 128), ("v", 640, 128), ("s5", 768, 512)]
        groups += [("hy", 1280 + 512 * i, 512) for i in range(3)] + [("gate", 2816 + 512 * i, 512) for i in range(6)]
        xb_p = k.pool("xb", [128, 8, 512], 1)
        sq_p = k.pool("sq", [128, 8, 512], 1)
        hb_p = k.pool("hb", [128, 8, 512], 1)
        wt_p = k.pool("wt", [128, 8, 512], 4)
        wr_p = k.pool("wr", [128, 8, 512], 2)
        rstd_p = k.pool("rstd", [128, 512], 1)
        rc_p = k.pool("rc", [128, 512], 2)
        rs_p = k.pool("rs", [128, 512], 2)
        ev_p = k.pool("ev", [128, 512], 4)
        e2_p = k.pool("e2", [128, 512], 2)
        vt_p = k.pool("vt", [128, 128], 2)
        for bi, (t0, nt) in enumerate(NBLK):
            j = 1 if bi == 0 else 0
            xb = xb_p()
            for kc in range(8):
                k.dma(xb[:, kc, 0:nt], XT[kc * 128:(kc + 1) * 128, t0:t0 + nt], reads=[XT], writes=[xb])
            sq = sq_p()
            k.op("act", lambda e: e.activation(out=sq[:, :, 0:nt], in_=xb[:, :, 0:nt], func=AF.Square), reads=[xb], writes=[sq])
            pss = k.next_ps()
            for kc in range(8):
                k.mm(pss, pss[:, 0:nt], ones, ones[:], sq, sq[:, kc, 0:nt], kc == 0, kc == 7)
            rstd = rstd_p()
            k.op("act", lambda e: e.activation(out=rstd[:, 0:nt], in_=pss[:, 0:nt], func=AF.Sqrt, bias=eps_t[:, 0:1], scale=1.0 / D),
                 reads=[pss, eps_t], writes=[rstd])
            k.op("dve", lambda e: e.reciprocal(rstd[:, 0:nt], rstd[:, 0:nt]), reads=[rstd], writes=[rstd])
            hb = hb_p()
            for kc in range(8):
                k.op("dve", lambda e, kc=kc: e.tensor_tensor(out=hb[:, kc, 0:nt], in0=xb[:, kc, 0:nt], in1=rstd[:, 0:nt], op=ALU.mult),
                     reads=[xb, rstd], writes=[hb])
                k.op("pool", lambda e, kc=kc: e.tensor_scalar(hb[:, kc, 0:nt], hb[:, kc, 0:nt], Acoef[:, kc, j:j + 1], m3[:, kc, j:j + 1],
                                                              op0=ALU.mult, op1=ALU.add), reads=[hb, Acoef, modt], writes=[hb])
            if dbg:
                for kc in range(8):
                    k.dma(HT[kc * 128:(kc + 1) * 128, t0:t0 + nt], hb[:, kc, 0:nt], reads=[hb], writes=[HT])
            rc = rc_p()
            rs = rs_p()
            k.dma(rc[:, 0:nt], ropec_in[:, t0:t0 + nt], reads=[ropec_in], writes=[rc])
            k.dma(rs[:, 0:nt], ropes_in[:, t0:t0 + nt], reads=[ropes_in], writes=[rs])
            for (kind, c0, ncol) in groups:
                wt = wt_p()
                for kc in range(8):
                    k.dma(wt[:, kc, 0:ncol], iwv[:, kc, c0:c0 + ncol], reads=[w["in_w"]], writes=[wt])
                if kind in ("q", "k"):
                    wr = wr_p()
                    for kc in range(8):
                        k.dma(wr[:, kc, 0:ncol], irv[:, kc, c0:c0 + ncol], reads=[w["in_wr"]], writes=[wr])
                if kind == "v":
                    for ts in range(nt // 128):
                        ps = k.next_ps()
                        for kc in range(8):
                            k.mm(ps, ps[:, 0:128], hb, hb[:, kc, ts * 128:(ts + 1) * 128], wt, wt[:, kc, 0:128], kc == 0, kc == 7)
                        vt = vt_p()
                        k.op("act", lambda e, ps=ps, vt=vt: e.copy(vt[:], ps[:, 0:128]), reads=[ps], writes=[vt])
                        k.dma(VTOK[t0 + ts * 128:t0 + (ts + 1) * 128, :], vt[:], reads=[vt], writes=[VTOK])
                    continue
                for cc in range(ncol // 128):
                    ps = k.next_ps()
                    for kc in range(8):
                        k.mm(ps, ps[:, 0:nt], wt, wt[:, kc, cc * 128:(cc + 1) * 128], hb, hb[:, kc, 0:nt], kc == 0, kc == 7)
                    ev = ev_p()
                    col = c0 + cc * 128
                    if kind in ("q", "k"):
                        ps2 = k.next_ps()
                        for kc in range(8):
                            k.mm(ps2, ps2[:, 0:nt], wr, wr[:, kc, cc * 128:(cc + 1) * 128], hb, hb[:, kc, 0:nt], kc == 0, kc == 7)
                        e2 = e2_p()
                        k.op("dve", lambda e, ps=ps, ev=ev: e.tensor_tensor(out=ev[:, 0:nt], in0=ps[:, 0:nt], in1=rc[:, 0:nt], op=ALU.mult),
                             reads=[ps, rc], writes=[ev])
                        k.op("dve", lambda e, ps2=ps2, e2=e2: e.tensor_tensor(out=e2[:, 0:nt], in0=ps2[:, 0:nt], in1=rs[:, 0:nt], op=ALU.mult),
                             reads=[ps2, rs], writes=[e2])
                        k.op("pool", lambda e, ev=ev, e2=e2: e.tensor_tensor(out=ev[:, 0:nt], in0=ev[:, 0:nt], in1=e2[:, 0:nt], op=ALU.add),
                             reads=[ev, e2], writes=[ev])
                        if kind == "q":
                            k.op("act", lambda e, ev=ev: e.mul(ev[:, 0:nt], ev[:, 0:nt], 0.125), reads=[ev], writes=[ev])
                            dst = QT[col:col + 128, t0:t0 + nt]
                            dres = QT
                        else:
                            dst = KT[0:128, t0:t0 + nt]
                            dres = KT
                    elif kind == "gate":
                        gi = (col - 2816) // 128
                        k.op("act", lambda e, ps=ps, ev=ev, gi=gi: e.activation(out=ev[:, 0:nt], in_=ps[:, 0:nt], func=AF.Sigmoid, bias=gb[:, gi:gi + 1]),
                             reads=[ps, gb], writes=[ev])
                        dst = GATE[col - 2816:col - 2816 + 128, t0:t0 + nt]
                        dres = GATE
                    else:
                        if cc % 2 == 0:
                            k.op("act", lambda e, ps=ps, ev=ev: e.copy(ev[:, 0:nt], ps[:, 0:nt]), reads=[ps], writes=[ev])
                        else:
                            k.op("dve", lambda e, ps=ps, ev=ev: e.tensor_copy(ev[:, 0:nt], ps[:, 0:nt]), reads=[ps], writes=[ev])
                        if kind == "s5":
                            dst = US5[col - 768:col - 768 + 128, t0:t0 + nt]
                            dres = US5
                        else:
                            dst = ZHY[col - 1280:col - 1280 + 128, t0:t0 + nt]
                            dres = ZHY
                    k.dma(dst, ev[:, 0:nt], reads=[ev], writes=[dres])
        k.phase_end()
        if stop_after == "B":
            break

        need_ctx = l < DEPTH - 1
        k.phase_begin()
        amask = k.sb("amask", [128, 256])
        k.dma(amask[:], amask_in[:, :], reads=[amask_in], writes=[amask])
        sinkb = k.sb("sinkb", [128, 8])
        k.dma(sinkb[:], w["sink"][:, :], reads=[w["sink"]], writes=[sinkb])
        kT_p = k.pool("kT", [64, NT], 1)
        v_p = k.pool("vtk", [128, NT // 128, 64], 1)
        qT_p = k.pool("qT", [64, NT], 2)
        ao_p = k.pool("ao", [64, NT], 2)
        S_p = k.pool("S", [128, 640], 3)
        PT_p = k.pool("PT", [128, 5, 128], 2)
        st_p = k.pool("stat", [128, 8], 4)
        vview = VTOK.t.rearrange("(n p) c -> p n c", p=128)
        for kv in range(2):
            kT = kT_p()
            k.dma(kT[:], KT[kv * 64:(kv + 1) * 64, :], reads=[KT], writes=[kT])
            vt = v_p()
            k.dma(vt[:], vview[:, :, kv * 64:(kv + 1) * 64], reads=[VTOK], writes=[vt])
            for hg in range(4):
                hd = kv * 4 + hg
                qT = qT_p()
                k.dma(qT[:], QT[hd * 64:(hd + 1) * 64, :], reads=[QT], writes=[qT])
                ao = ao_p()
                blocks = ([("c", 0), ("c", 1)] if need_ctx else []) + [("l", n) for n in range(32)]
                for (bt, n) in blocks:
                    q0 = n * 128 if bt == "c" else C + n * 128
                    if bt == "c":
                        kts = []
                    else:
                        kts = [j for j in (n - 1, n, n + 1) if 0 <= j < 32]
                    nloc = len(kts) * 128
                    wtot = 256 + nloc
                    S = S_p()
                    psc = k.next_ps()
                    k.mm(psc, psc[:, 0:256], qT, qT[:, q0:q0 + 128], kT, kT[:, 0:256], True, True)
                    k.op("act", lambda e, S=S, psc=psc: e.copy(S[:, 0:256], psc[:, 0:256]), reads=[psc], writes=[S])
                    if nloc:
                        psl = k.next_ps()
                        k0 = C + kts[0] * 128
                        k.mm(psl, psl[:, 0:nloc], qT, qT[:, q0:q0 + 128], kT, kT[:, k0:k0 + nloc], True, True)
                        for ji, j in enumerate(kts):
                            dst = S[:, 256 + ji * 128:256 + (ji + 1) * 128]
                            src = psl[:, ji * 128:(ji + 1) * 128]
                            if j == n:
                                k.op("dve", lambda e, dst=dst, src=src: e.tensor_copy(dst, src), reads=[psl], writes=[S])
                            else:
                                mko = 0 if j < n else 128
                                k.op("dve", lambda e, dst=dst, src=src, mko=mko: e.tensor_tensor(out=dst, in0=src, in1=amask[:, mko:mko + 128], op=ALU.add),
                                     reads=[psl, amask], writes=[S])
                    stt = st_p()
                    k.op("dve", lambda e, S=S, stt=stt: e.tensor_reduce(out=stt[:, 0:1], in_=S[:, 0:wtot], axis=AX.X, op=ALU.max), reads=[S], writes=[stt])
                    k.op("dve", lambda e, stt=stt: e.tensor_tensor(out=stt[:, 0:1], in0=stt[:, 0:1], in1=sinkb[:, hd:hd + 1], op=ALU.max), reads=[stt, sinkb], writes=[stt])
                    k.op("dve", lambda e, stt=stt: e.tensor_scalar(stt[:, 1:2], stt[:, 0:1], -1.0, None, op0=ALU.mult), reads=[stt], writes=[stt])
                    k.op("act", lambda e, S=S, stt=stt: e.activation(out=S[:, 0:wtot], in_=S[:, 0:wtot], func=AF.Exp, bias=stt[:, 1:2], accum_out=stt[:, 2:3]),
                         reads=[S, stt], writes=[S, stt])
                    k.op("act", lambda e, stt=stt: e.activation(out=stt[:, 3:4], in_=sinkb[:, hd:hd + 1], func=AF.Exp, bias=stt[:, 1:2]), reads=[stt, sinkb], writes=[stt])
                    k.op("dve", lambda e, stt=stt: e.tensor_tensor(out=stt[:, 4:5], in0=stt[:, 2:3], in1=stt[:, 3:4], op=ALU.add), reads=[stt], writes=[stt])
                    k.op("dve", lambda e, stt=stt: e.reciprocal(stt[:, 5:6], stt[:, 4:5]), reads=[stt], writes=[stt])
                    k.op("dve", lambda e, S=S, stt=stt: e.tensor_scalar(S[:, 0:wtot], S[:, 0:wtot], stt[:, 5:6], None, op0=ALU.mult), reads=[S, stt], writes=[S])
                    nkt = wtot // 128
                    PT = PT_p()
                    for ti in range(nkt):
                        pst = k.next_ps()
                        k.op("pe", lambda e, pst=pst, S=S, ti=ti: e.transpose(pst[:, 0:128], S[:, ti * 128:(ti + 1) * 128], ident[:]),
                             reads=[S, ident], writes=[pst])
                        if ti % 2 == 0:
                            k.op("act", lambda e, PT=PT, pst=pst, ti=ti: e.copy(PT[:, ti, :], pst[:, 0:128]), reads=[pst], writes=[PT])
                        else:
                            k.op("dve", lambda e, PT=PT, pst=pst, ti=ti: e.tensor_copy(PT[:, ti, :], pst[:, 0:128]), reads=[pst], writes=[PT])
                    pso = k.next_ps()
                    vtiles = [0, 1] + [2 + j for j in kts]
                    for ti, vti in enumerate(vtiles):
                        k.mm(pso, pso[0:64, 0:128], vt, vt[:, vti, :], PT, PT[:, ti, :], ti == 0, ti == nkt - 1)
                    k.op("act", lambda e, ao=ao, pso=pso, q0=q0: e.copy(ao[:, q0:q0 + 128], pso[0:64, 0:128]), reads=[pso], writes=[ao])
                lo = 0 if need_ctx else C
                k.dma(ATT[hd * 64:(hd + 1) * 64, lo:NT], ao[:, lo:NT], reads=[ao], writes=[ATT])
        k.phase_end()
        if stop_after == "C":
            break

        k.phase_begin()
        fb = k.sb("hy_fb", [64, 4])
        k.dma(fb[:], w["hy_fb"][:, :], reads=[w["hy_fb"]], writes=[fb])
        fbb = k.sb("hy_fbb", [64, 2])
        k.op("dve", lambda e: e.tensor_tensor(out=fbb[:, 0:1], in0=fb[:, 0:1], in1=fb[:, 1:2], op=ALU.mult), reads=[fb], writes=[fbb])
        k.op("dve", lambda e: e.tensor_tensor(out=fbb[:, 1:2], in0=fb[:, 2:3], in1=fb[:, 3:4], op=ALU.mult), reads=[fb], writes=[fbb])
        w1 = k.sb("hy_w1", [33, 64])
        k.dma(w1[:], w["hy_w1"][:, :], reads=[w["hy_w1"]], writes=[w1])
        w2 = k.sb("hy_w2", [64, 64])
        k.dma(w2[:], w["hy_w2"][:, :], reads=[w["hy_w2"]], writes=[w2])
        w3 = k.sb("hy_w3", [64, 1024])
        k.dma(w3[:], w["hy_w3"][:, :], reads=[w["hy_w3"]], writes=[w3])
        rate = k.sb("hy_rate", [128, 512])
        k.dma(rate[:], hyrate_in[:, :], reads=[hyrate_in], writes=[rate])
        sw = k.sb("hy_sw", [128, 12, 3])
        k.dma(sw[:], w["hy_sw"][:, :, :], reads=[w["hy_sw"]], writes=[sw])
        sbias = k.sb("hy_sb", [128, 12])
        k.dma(sbias[:], w["hy_sb"][:, :], reads=[w["hy_sb"]], writes=[sbias])
        hbias = k.sb("hy_bias", [128, 4])
        k.dma(hbias[:], w["hy_bias"][:, :], reads=[w["hy_bias"]], writes=[hbias])
        m0 = k.sb("m0", [128, 1])
        k.op("pool", lambda e: e.memset(m0[:], 1.0), writes=[m0])
        k.op("pool", lambda e: e.affine_select(out=m0[:], in_=m0[:], pattern=[[0, 1]], compare_op=ALU.not_equal, fill=0.0, base=0, channel_multiplier=1),
             reads=[m0], writes=[m0])
        negpi = k.sb("negpi2", [128, 1])
        k.op("pool", lambda e: e.memset(negpi[:], -math.pi), writes=[negpi])

        def sin_mlp(dst_t, dst, ps, fcol, bcol, n, tmp_p, tmpi_p):
            a = tmp_p()
            k.op("act", lambda e: e.activation(out=a[0:64, 0:n], in_=ps[0:64, 0:n], func=AF.Identity, bias=fbb[:, bcol:bcol + 1], scale=fb[:, fcol:fcol + 1]),
                 reads=[ps, fb, fbb], writes=[a])
            ki = tmpi_p()
            k.op("dve", lambda e: e.tensor_scalar(ki[0:64, 0:n], a[0:64, 0:n], 1.0 / (2.0 * math.pi), None, op0=ALU.mult), reads=[a], writes=[ki])
            kf = tmp_p()
            k.op("dve", lambda e: e.tensor_copy(kf[0:64, 0:n], ki[0:64, 0:n]), reads=[ki], writes=[kf])
            k.op("dve", lambda e: e.scalar_tensor_tensor(out=a[0:64, 0:n], in0=kf[0:64, 0:n], scalar=-2.0 * math.pi, in1=a[0:64, 0:n], op0=ALU.mult, op1=ALU.add),
                 reads=[kf, a], writes=[a])
            k.op("dve", lambda e: e.tensor_scalar(a[0:64, 0:n], a[0:64, 0:n], 3.1415925, -3.1415925, op0=ALU.min, op1=ALU.max), reads=[a], writes=[a])
            k.op("act", lambda e: e.activation(out=dst, in_=a[0:64, 0:n], func=AF.Sin), reads=[a], writes=[dst_t])

        seqs = [("L", L, C)] + ([("C", C, 0)] if need_ctx else [])
        tmp_p = k.pool("hy_tmp", [128, 512], 3)
        tmpi_p = k.pool("hy_tmpi", [128, 512], 2, I32)
        h2T_p = k.pool("hy_h2T", [64, 512], 2)
        h1T_p = k.pool("hy_h1T", [64, 512], 2)
        zf_p = k.pool("hy_zf", [33, 512], 2)
        tw_p = k.pool("hy_tw", [128, 32], 1)
        win_p = k.pool("hy_win", [128, 512], 2)
        hfb_p = k.pool("hy_hfb", [128, 2, 512], 2)
        hsd_p = k.pool("hy_hsd", [128, 2, 512], 2)
        zc_p = k.pool("hy_zc", [128, 3, 512 + 2], 2)
        zo_p = k.pool("hy_zo", [128, 3, 512], 2)
        vxt_p = k.pool("hy_vxt", [128, 512], 2)
        for (nm, Ls, toff) in seqs:
            HSD, VXT, VXF, X0F = HSDd[nm], VXTd[nm], VXFd[nm], X0Fd[nm]
            ntile = Ls // 128
            nb = max(1, Ls // 512)
            bw = min(512, Ls)
            tw = tw_p()
            k.dma(tw[:, 0:ntile], hytw_in[nm][:, :], reads=[hytw_in[nm]], writes=[tw])
            for bi in range(nb):
                zf = zf_p()
                k.dma(zf[:, 0:bw], hyz_in[nm][:, bi * bw:(bi + 1) * bw], reads=[hyz_in[nm]], writes=[zf])
                ps1 = k.next_ps()
                k.mm(ps1, ps1[0:64, 0:bw], w1, w1[:, :], zf, zf[:, 0:bw], True, True)
                h1T = h1T_p()
                sin_mlp(h1T, h1T[:, 0:bw], ps1, 1, 0, bw, tmp_p, tmpi_p)
                ps2 = k.next_ps()
                k.mm(ps2, ps2[0:64, 0:bw], w2, w2[:, :], h1T, h1T[:, 0:bw], True, True)
                h2T = h2T_p()
                sin_mlp(h2T, h2T[:, 0:bw], ps2, 3, 1, bw, tmp_p, tmpi_p)
                for ts in range(bw // 128):
                    tt = bi * (bw // 128) + ts
                    win = win_p()
                    k.op("act", lambda e, win=win, tt=tt: e.activation(out=win[:], in_=rate[:], func=AF.Exp, scale=tw[:, tt:tt + 1]), reads=[rate, tw], writes=[win])
                    hfb = hfb_p()
                    for d in range(2):
                        psf = k.next_ps()
                        k.mm(psf, psf[:, :], h2T, h2T[:, ts * 128:(ts + 1) * 128], w3, w3[:, d * 512:(d + 1) * 512], True, True)
                        k.op("dve", lambda e, hfb=hfb, psf=psf, win=win, d=d: e.tensor_tensor(out=hfb[:, d, :], in0=psf[:, :], in1=win[:], op=ALU.mult),
                             reads=[psf, win], writes=[hfb])
                    if tt == 0:
                        k.op("dve", lambda e, hfb=hfb: e.tensor_scalar(hfb[:, 1, :], hfb[:, 1, :], m0[:, 0:1], None, op0=ALU.mult), reads=[hfb, m0], writes=[hfb])
                    hsd = hsd_p()
                    k.op("dve", lambda e, hsd=hsd, hfb=hfb: e.tensor_tensor(out=hsd[:, 0, :], in0=hfb[:, 0, :], in1=hfb[:, 1, :], op=ALU.add), reads=[hfb], writes=[hsd])
                    k.op("pool", lambda e, hsd=hsd, hfb=hfb: e.tensor_tensor(out=hsd[:, 1, :], in0=hfb[:, 0, :], in1=hfb[:, 1, :], op=ALU.subtract), reads=[hfb], writes=[hsd])
                    for d in range(2):
                        k.dma(HSD[d, tt * 128:(tt + 1) * 128, :], hsd[:, d, :], reads=[hsd], writes=[HSD])
            for cc in range(4):
                for bi in range(nb):
                    zc = zc_p()
                    k.op("pool", lambda e, zc=zc: e.memset(zc[:], 0.0), writes=[zc])
                    lo = bi * bw
                    a0 = max(lo - 1, 0)
                    a1 = min(lo + bw + 1, Ls)
                    for pj in range(3):
                        r0 = pj * 512 + cc * 128
                        k.dma(zc[:, pj, (a0 - (lo - 1)):(a1 - (lo - 1))], ZHY[r0:r0 + 128, toff + a0:toff + a1], reads=[ZHY], writes=[zc])
                    zo = zo_p()
                    for pj in range(3):
                        ci = pj * 4 + cc
                        k.op("dve", lambda e, zo=zo, zc=zc, pj=pj, ci=ci: e.tensor_scalar(zo[:, pj, 0:bw], zc[:, pj, 0:bw], sw[:, ci, 0:1], sbias[:, ci:ci + 1], op0=ALU.mult, op1=ALU.add),
                             reads=[zc, sw, sbias], writes=[zo])
                        for tap in (1, 2):
                            k.op("dve", lambda e, zo=zo, zc=zc, pj=pj, ci=ci, tap=tap: e.scalar_tensor_tensor(out=zo[:, pj, 0:bw], in0=zc[:, pj, tap:tap + bw], scalar=sw[:, ci, tap:tap + 1],
                                                                                                                in1=zo[:, pj, 0:bw], op0=ALU.mult, op1=ALU.add), reads=[zc, sw, zo], writes=[zo])
                    k.op("pool", lambda e, zo=zo: e.tensor_tensor(out=zo[:, 2, 0:bw], in0=zo[:, 2, 0:bw], in1=zo[:, 1, 0:bw], op=ALU.mult), reads=[zo], writes=[zo])
                    k.dma(X0F[cc * 128:(cc + 1) * 128, lo:lo + bw], zo[:, 0, 0:bw], reads=[zo], writes=[X0F])
                    k.dma(VXF[cc * 128:(cc + 1) * 128, lo:lo + bw], zo[:, 2, 0:bw], reads=[zo], writes=[VXF])
                    for ts in range(bw // 128):
                        pst = k.next_ps()
                        k.op("pe", lambda e, pst=pst, zo=zo, ts=ts: e.transpose(pst[:, 0:128], zo[:, 2, ts * 128:(ts + 1) * 128], ident[:]), reads=[zo, ident], writes=[pst])
                        vxt = vxt_p()
                        k.op("act", lambda e, vxt=vxt, pst=pst: e.copy(vxt[:, 0:128], pst[:, 0:128]), reads=[pst], writes=[vxt])
                        t0_ = lo + ts * 128
                        k.dma(VXT[t0_:t0_ + 128, cc * 128:(cc + 1) * 128], vxt[:, 0:128], reads=[vxt], writes=[VXT])
        k.phase_end()
        for (nm, Ls, toff) in seqs:
            k.phase_begin()
            HSD, KRI, VXT, VXF, X0F = HSDd[nm], KRId[nm], VXTd[nm], VXFd[nm], X0Fd[nm]
            ntile = Ls // 128
            nb = max(1, Ls // 512)
            bw = min(512, Ls)
            hbias = k.sb("hy_bias", [128, 4])
            k.dma(hbias[:], w["hy_bias"][:, :], reads=[w["hy_bias"]], writes=[hbias])
            Mx = DFT[nm]
            src_p = k.pool("hy_src_" + nm, [128, ntile, 256], 1)
            Y_p = k.pool("hy_Y_" + nm, [128, ntile, 2, 256], 1)
            slab_p = k.pool("hy_slab_" + nm, [128, min(8, ntile), 2, 128], 2)
            kri_p = k.pool("hy_kri_" + nm, [128, 2, 256], 2)
            pw_p = k.pool("hy_pw_" + nm, [128, 4, 256], 2)
            mt_p = k.pool("hy_mt_" + nm, [128, 2, 512], 3)
            ep_p = k.pool("hy_ep_" + nm, [128, 3, 512], 2)
            tg = min(8, ntile)
            for stage in ("kern", "conv"):
                for hh in range(2):
                    cs = slice(hh * 256, (hh + 1) * 256)
                    if stage == "kern":
                        mats = (Mx["CK"], Mx["SK"])
                        for d in range(2):
                            src = src_p()
                            k.dma(src[:, :, :], HSD[d, 0:Ls, cs].rearrange("(n p) c -> p n c", p=128), reads=[HSD], writes=[src])
                            for ft in range(ntile):
                                psK = k.next_ps()
                                for g in range(ntile // tg):
                                    slab = slab_p()
                                    k.dma(slab[:, :, 0, :], mats[d][g * tg * 128:(g + 1) * tg * 128, ft * 128:(ft + 1) * 128].rearrange("(n p) f -> p n f", p=128),
                                          reads=[mats[d]], writes=[slab])
                                    for ti in range(tg):
                                        tt = g * tg + ti
                                        k.mm(psK, psK[:, 0:256], slab, slab[:, ti, 0, :], src, src[:, tt, :], tt == 0, tt == ntile - 1)
                                kri = kri_p()
                                k.op("act", lambda e, kri=kri, psK=psK: e.copy(kri[:, 0, :], psK[:, 0:256]), reads=[psK], writes=[kri])
                                k.dma(KRI[d, ft * 128:(ft + 1) * 128, cs], kri[:, 0, :], reads=[kri], writes=[KRI])
                        continue
                    mats = (Mx["CS"], Mx["SS"])
                    src = src_p()
                    k.dma(src[:, :, :], VXT[0:Ls, cs].rearrange("(n p) c -> p n c", p=128), reads=[VXT], writes=[src])
                    Y = Y_p()
                    for ft in range(ntile):
                        psR = k.next_ps()
                        psI = k.next_ps()
                        for g in range(ntile // tg):
                            slab = slab_p()
                            for d in range(2):
                                k.dma(slab[:, :, d, :], mats[d][g * tg * 128:(g + 1) * tg * 128, ft * 128:(ft + 1) * 128].rearrange("(n p) f -> p n f", p=128),
                                      reads=[mats[d]], writes=[slab])
                            for ti in range(tg):
                                tt = g * tg + ti
                                k.mm(psR, psR[:, 0:256], slab, slab[:, ti, 0, :], src, src[:, tt, :], tt == 0, tt == ntile - 1)
                                k.mm(psI, psI[:, 0:256], slab, slab[:, ti, 1, :], src, src[:, tt, :], tt == 0, tt == ntile - 1)
                        kri = kri_p()
                        for d in range(2):
                            k.dma(kri[:, d, :], KRI[d, ft * 128:(ft + 1) * 128, cs], reads=[KRI], writes=[kri])
                        pw = pw_p()
                        k.op("dve", lambda e, pw=pw, psR=psR, kri=kri: e.tensor_tensor(out=pw[:, 0, :], in0=psR[:, 0:256], in1=kri[:, 0, :], op=ALU.mult), reads=[psR, kri], writes=[pw])
                        k.op("dve", lambda e, pw=pw, psI=psI, kri=kri: e.tensor_tensor(out=pw[:, 1, :], in0=psI[:, 0:256], in1=kri[:, 1, :], op=ALU.mult), reads=[psI, kri], writes=[pw])
                        k.op("dve", lambda e, pw=pw, psR=psR, kri=kri: e.tensor_tensor(out=pw[:, 2, :], in0=psR[:, 0:256], in1=kri[:, 1, :], op=ALU.mult), reads=[psR, kri], writes=[pw])
                        k.op("dve", lambda e, pw=pw, psI=psI, kri=kri: e.tensor_tensor(out=pw[:, 3, :], in0=psI[:, 0:256], in1=kri[:, 0, :], op=ALU.mult), reads=[psI, kri], writes=[pw])
                        k.op("pool", lambda e, Y=Y, pw=pw, ft=ft: e.tensor_tensor(out=Y[:, ft, 0, :], in0=pw[:, 0, :], in1=pw[:, 1, :], op=ALU.subtract), reads=[pw], writes=[Y])
                        k.op("pool", lambda e, Y=Y, pw=pw, ft=ft: e.tensor_tensor(out=Y[:, ft, 1, :], in0=pw[:, 2, :], in1=pw[:, 3, :], op=ALU.add), reads=[pw], writes=[Y])
                    if stage == "conv":
                        Nfft = 2 * Ls
                        for nbk in range(nb):
                            pso = [k.next_ps(), k.next_ps()]
                            for ft in range(ntile):
                                mt = mt_p()
                                for d in range(2):
                                    k.dma(mt[:, d, 0:bw], mats[d][ft * 128:(ft + 1) * 128, nbk * bw:(nbk + 1) * bw], reads=[mats[d]], writes=[mt])
                                for c2 in range(2):
                                    for d in range(2):
                                        k.mm(pso[c2], pso[c2][:, 0:bw], Y, Y[:, ft, d, c2 * 128:(c2 + 1) * 128], mt, mt[:, d, 0:bw], ft == 0 and d == 0, ft == ntile - 1 and d == 1)
                            for c2 in range(2):
                                ch = hh * 2 + c2
                                ep = ep_p()
                                k.dma(ep[:, 0, 0:bw], VXF[ch * 128:(ch + 1) * 128, nbk * bw:(nbk + 1) * bw], reads=[VXF], writes=[ep])
                                k.dma(ep[:, 1, 0:bw], X0F[ch * 128:(ch + 1) * 128, nbk * bw:(nbk + 1) * bw], reads=[X0F], writes=[ep])
                                k.op("dve", lambda e, ep=ep, ch=ch: e.tensor_scalar(ep[:, 0, 0:bw], ep[:, 0, 0:bw], hbias[:, ch:ch + 1], None, op0=ALU.mult), reads=[ep, hbias], writes=[ep])
                                k.op("dve", lambda e, ep=ep, c2=c2: e.scalar_tensor_tensor(out=ep[:, 2, 0:bw], in0=pso[c2][:, 0:bw], scalar=-2.0 / Nfft, in1=ep[:, 0, 0:bw], op0=ALU.mult, op1=ALU.add),
                                     reads=[pso[c2], ep], writes=[ep])
                                k.op("pool", lambda e, ep=ep: e.tensor_tensor(out=ep[:, 2, 0:bw], in0=ep[:, 2, 0:bw], in1=ep[:, 1, 0:bw], op=ALU.mult), reads=[ep], writes=[ep])
                                k.dma(HY[ch * 128:(ch + 1) * 128, toff + nbk * bw:toff + (nbk + 1) * bw], ep[:, 2, 0:bw], reads=[ep], writes=[HY])
                k.barrier()
            k.phase_end()
        if stop_after == "D":
            break

        k.phase_begin()
        TWO_PI = 2.0 * math.pi
        P32 = [128, 2, 16]
        a_re = k.sb("a_re", P32); a_im = k.sb("a_im", P32); ldt = k.sb("ldt", P32)
        for t_, src_ in ((a_re, w["s5_a_re"]), (a_im, w["s5_a_im"]), (ldt, w["s5_ldt"])):
            k.dma(t_[:], src_[:, :, :], reads=[src_], writes=[t_])
        prm = k.sb("s5prm", [128, 12, 32])
        fl = lambda t_: t_.t[:, :, :].rearrange("p a b -> p (a b)")
        R_ = lambda i: prm[:, i, :]
        ki32 = k.sb("ki32", [128, 32], I32)
        kf32 = k.sb("kf32", [128, 32])
        hpi = k.sb("hpi", [128, 1])
        k.op("pool", lambda e: e.memset(hpi[:], 0.0), writes=[hpi])

        def reduce_angle(dst, src, shift):
            k.op("dve", lambda e: e.tensor_scalar(R_(8), src, shift, None, op0=ALU.add), reads=[prm], writes=[prm])
            k.op("dve", lambda e: e.tensor_scalar(ki32[:], R_(8), 1.0 / TWO_PI, None, op0=ALU.mult), reads=[prm], writes=[ki32])
            k.op("dve", lambda e: e.tensor_copy(kf32[:], ki32[:]), reads=[ki32], writes=[kf32])
            k.op("dve", lambda e: e.scalar_tensor_tensor(out=dst, in0=kf32[:], scalar=-TWO_PI, in1=R_(8), op0=ALU.mult, op1=ALU.add), reads=[kf32, prm], writes=[prm])
            k.op("dve", lambda e: e.tensor_scalar(dst, dst, 3.1415925, -3.1415925, op0=ALU.min, op1=ALU.max), reads=[prm], writes=[prm])

        k.op("dve", lambda e: e.tensor_scalar(R_(0), fl(a_re), -1e-4, None, op0=ALU.min), reads=[a_re], writes=[prm])
        k.op("act", lambda e: e.activation(out=R_(1), in_=fl(ldt), func=AF.Exp), reads=[ldt], writes=[prm])
        k.op("dve", lambda e: e.tensor_tensor(out=R_(9), in0=R_(0), in1=R_(1), op=ALU.mult), reads=[prm], writes=[prm])
        k.op("act", lambda e: e.activation(out=R_(3), in_=R_(9), func=AF.Exp), reads=[prm], writes=[prm])
        k.op("dve", lambda e: e.tensor_tensor(out=R_(10), in0=fl(a_im), in1=R_(1), op=ALU.mult), reads=[prm, a_im], writes=[prm])
        reduce_angle(R_(2), R_(10), 0.0)
        k.op("act", lambda e: e.activation(out=R_(5), in_=R_(2), func=AF.Sin), reads=[prm], writes=[prm])
        reduce_angle(R_(11), R_(10), math.pi / 2.0)
        k.op("act", lambda e: e.activation(out=R_(4), in_=R_(11), func=AF.Sin), reads=[prm], writes=[prm])
        k.op("dve", lambda e: e.tensor_tensor(out=R_(8), in0=R_(3), in1=R_(4), op=ALU.mult), reads=[prm], writes=[prm])
        k.op("dve", lambda e: e.tensor_scalar(R_(8), R_(8), -1.0, None, op0=ALU.add), reads=[prm], writes=[prm])
        k.op("dve", lambda e: e.tensor_tensor(out=R_(9), in0=R_(3), in1=R_(5), op=ALU.mult), reads=[prm], writes=[prm])
        k.op("dve", lambda e: e.tensor_tensor(out=R_(10), in0=R_(0), in1=R_(0), op=ALU.mult), reads=[prm], writes=[prm])
        k.op("dve", lambda e: e.tensor_tensor(out=R_(11), in0=fl(a_im), in1=fl(a_im), op=ALU.mult), reads=[a_im], writes=[prm])
        k.op("dve", lambda e: e.tensor_tensor(out=R_(10), in0=R_(10), in1=R_(11), op=ALU.add), reads=[prm], writes=[prm])
        k.op("dve", lambda e: e.reciprocal(R_(10), R_(10)), reads=[prm], writes=[prm])
        k.op("dve", lambda e: e.tensor_tensor(out=R_(6), in0=R_(8), in1=R_(0), op=ALU.mult), reads=[prm], writes=[prm])
        k.op("dve", lambda e: e.tensor_tensor(out=R_(11), in0=R_(9), in1=fl(a_im), op=ALU.mult), reads=[prm, a_im], writes=[prm])
        k.op("dve", lambda e: e.tensor_tensor(out=R_(6), in0=R_(6), in1=R_(11), op=ALU.add), reads=[prm], writes=[prm])
        k.op("dve", lambda e: e.tensor_tensor(out=R_(6), in0=R_(6), in1=R_(10), op=ALU.mult), reads=[prm], writes=[prm])
        k.op("dve", lambda e: e.tensor_tensor(out=R_(7), in0=R_(9), in1=R_(0), op=ALU.mult), reads=[prm], writes=[prm])
        k.op("dve", lambda e: e.tensor_tensor(out=R_(11), in0=R_(8), in1=fl(a_im), op=ALU.mult), reads=[prm, a_im], writes=[prm])
        k.op("dve", lambda e: e.tensor_tensor(out=R_(7), in0=R_(7), in1=R_(11), op=ALU.subtract), reads=[prm], writes=[prm])
        k.op("dve", lambda e: e.tensor_tensor(out=R_(7), in0=R_(7), in1=R_(10), op=ALU.mult), reads=[prm], writes=[prm])

        tio = k.sb("tio", [128, 513])
        k.op("pool", lambda e: e.iota(tio[:], pattern=[[1, 513]], base=0, channel_multiplier=0, allow_small_or_imprecise_dtypes=True), writes=[tio])
        s5d = k.sb("s5d", [32, 16])
        k.dma(s5d[:], w["s5_d"][:, :], reads=[w["s5_d"]], writes=[s5d])
        chunks = [(0, 256)] + [(256 + 512 * i, 512) for i in range(8)]
        order = {0: list(range(9)), 1: [0] + list(range(8, 0, -1))}
        u_p = k.pool("s5u", [32, NT], 2)
        ya_p = k.pool("s5ya", [32, NT], 4)
        bc_p = k.pool("s5bc", [128, 4, 16], 2)
        B2_p = k.pool("s5B2", [128, 2, 32], 2)
        BT_p = k.pool("s5BT", [32, 2, 128], 2)
        CL_p = k.pool("s5CL", [128, 2, 32], 2)
        tb_p = k.pool("s5tb", [128, 2, 513], 2)
        ang_p = k.pool("s5ang", [128, 513], 4)
        angi_p = k.pool("s5angi", [128, 513], 2, I32)
        wk_p = k.pool("s5wk", [128, 512], 18)
        g_p = k.pool("s5g", [128, 2, 512], 4)
        h_p = k.pool("s5h", [128, 2, 512], 4)
        ini_p = k.pool("s5ini", [128, 4], 6)
        yo_p = k.pool("s5yo", [32, 512], 3)

        def rev(t_, p0, p1, a, n):
            b_ = t_.t[p0:p1, a + n - 1:a + n]
            return bass.AP(tensor=b_.tensor, offset=b_.offset, ap=[list(b_.ap[0]), [-1, n]])

        for st in range(16):
            u = u_p()
            k.dma(u[:], US5[st * 32:(st + 1) * 32, :], reads=[US5], writes=[u])
            yad = [ya_p(), ya_p()]
            ctxd = {}
            for d in range(2):
                col = d * 16 + st
                bc = bc_p()
                for i_, nm_ in enumerate(("s5_b_re", "s5_b_im", "s5_c_re", "s5_c_im")):
                    k.dma(bc[:, i_, :], w[nm_][d, st * 128:(st + 1) * 128, :], reads=[w[nm_]], writes=[bc])
                B2 = B2_p()
                CL = CL_p()
                k.op("pool", lambda e, B2=B2: e.memset(B2[:], 0.0), writes=[B2])
                k.op("pool", lambda e, CL=CL: e.memset(CL[:], 0.0), writes=[CL])
                wk = wk_p()
                for gl in range(2):
                    ps_ = slice(gl * 64, (gl + 1) * 64)
                    cs_ = slice(gl * 16, (gl + 1) * 16)
                    kr = prm[ps_, 6, col:col + 1]
                    kim = prm[ps_, 7, col:col + 1]
                    k.op("dve", lambda e, wk=wk, bc=bc, ps_=ps_, kim=kim: e.tensor_scalar(wk[ps_, 0:16], bc[ps_, 1, :], kim, None, op0=ALU.mult), reads=[bc, prm], writes=[wk])
                    k.op("dve", lambda e, wk=wk, bc=bc, ps_=ps_, kim=kim: e.tensor_scalar(wk[ps_, 16:32], bc[ps_, 0, :], kim, None, op0=ALU.mult), reads=[bc, prm], writes=[wk])
                    k.op("dve", lambda e, B2=B2, wk=wk, bc=bc, ps_=ps_, cs_=cs_, kr=kr: e.scalar_tensor_tensor(out=B2[ps_, 0, cs_], in0=bc[ps_, 0, :], scalar=kr, in1=wk[ps_, 0:16], op0=ALU.mult, op1=ALU.subtract),
                         reads=[bc, prm, wk], writes=[B2])
                    k.op("dve", lambda e, B2=B2, wk=wk, bc=bc, ps_=ps_, cs_=cs_, kr=kr: e.scalar_tensor_tensor(out=B2[ps_, 1, cs_], in0=bc[ps_, 1, :], scalar=kr, in1=wk[ps_, 16:32], op0=ALU.mult, op1=ALU.add),
                         reads=[bc, prm, wk], writes=[B2])
                    k.op("dve", lambda e, CL=CL, bc=bc, ps_=ps_, cs_=cs_: e.tensor_copy(CL[ps_, 0, cs_], bc[ps_, 2, :]), reads=[bc], writes=[CL])
                    k.op("dve", lambda e, CL=CL, bc=bc, ps_=ps_, cs_=cs_: e.tensor_scalar(CL[ps_, 1, cs_], bc[ps_, 3, :], -1.0, None, op0=ALU.mult), reads=[bc], writes=[CL])
                BT = BT_p()
                for ri in range(2):
                    pst = k.next_ps()
                    k.op("pe", lambda e, pst=pst, B2=B2, ri=ri: e.transpose(pst[0:32, 0:128], B2[:, ri, :], ident[:]), reads=[B2, ident], writes=[pst])
                    k.op("act", lambda e, BT=BT, pst=pst, ri=ri: e.copy(BT[:, ri, :], pst[0:32, 0:128]), reads=[pst], writes=[BT])
                tb = tb_p()
                for ti_, shift in ((1, 0.0), (0, math.pi / 2.0)):
                    ang = ang_p()
                    k.op("dve", lambda e, ang=ang, shift=shift: e.tensor_scalar(ang[:], tio[:], prm[:, 2, col:col + 1], shift, op0=ALU.mult, op1=ALU.add), reads=[tio, prm], writes=[ang])
                    angi = angi_p()
                    k.op("dve", lambda e, ang=ang, angi=angi: e.tensor_scalar(angi[:], ang[:], 1.0 / TWO_PI, None, op0=ALU.mult), reads=[ang], writes=[angi])
                    angf = ang_p()
                    k.op("pool", lambda e, angf=angf, angi=angi: e.tensor_copy(angf[:], angi[:]), reads=[angi], writes=[angf])
                    k.op("dve", lambda e, ang=ang, angf=angf: e.scalar_tensor_tensor(out=ang[:], in0=angf[:], scalar=-TWO_PI, in1=ang[:], op0=ALU.mult, op1=ALU.add), reads=[angf, ang], writes=[ang])
                    k.op("dve", lambda e, ang=ang: e.tensor_scalar(ang[:], ang[:], 3.1415925, -3.1415925, op0=ALU.min, op1=ALU.max), reads=[ang], writes=[ang])
                    k.op("act", lambda e, tb=tb, ang=ang, ti_=ti_: e.activation(out=tb[:, ti_, :], in_=ang[:], func=AF.Sin), reads=[ang], writes=[tb])
                ctxd[d] = dict(col=col, BT=BT, CL=CL, tb=tb, rho_b=prm[:, 3, col:col + 1], prev=None)
            for step in range(9):
                for d in range(2):
                    cx = ctxd[d]
                    col, BT, CL, tb, rho_b, prev, ya = cx["col"], cx["BT"], cx["CL"], cx["tb"], cx["rho_b"], cx["prev"], yad[d]
                    ci = order[d][step]
                    c0, n = chunks[ci]
                    rhs_u = u[:, c0:c0 + n] if d == 0 else rev(u, 0, 32, c0, n)
                    pbr = k.next_ps()
                    pbi = k.next_ps()
                    k.mm(pbr, pbr[:, 0:n], BT, BT[:, 0, :], u, rhs_u, True, True)
                    k.mm(pbi, pbi[:, 0:n], BT, BT[:, 1, :], u, rhs_u, True, True)
                    cosT = tb[:, 0, 0:n]
                    sinT = tb[:, 1, 0:n]
                    w0, w1, w2, w3_ = wk_p(), wk_p(), wk_p(), wk_p()
                    k.op("dve", lambda e, w0=w0, pbr=pbr, cosT=cosT: e.tensor_tensor(out=w0[:, 0:n], in0=pbr[:, 0:n], in1=cosT, op=ALU.mult), reads=[pbr, tb], writes=[w0])
                    k.op("dve", lambda e, w1=w1, pbi=pbi, sinT=sinT: e.tensor_tensor(out=w1[:, 0:n], in0=pbi[:, 0:n], in1=sinT, op=ALU.mult), reads=[pbi, tb], writes=[w1])
                    k.op("dve", lambda e, w2=w2, pbi=pbi, cosT=cosT: e.tensor_tensor(out=w2[:, 0:n], in0=pbi[:, 0:n], in1=cosT, op=ALU.mult), reads=[pbi, tb], writes=[w2])
                    k.op("dve", lambda e, w3_=w3_, pbr=pbr, sinT=sinT: e.tensor_tensor(out=w3_[:, 0:n], in0=pbr[:, 0:n], in1=sinT, op=ALU.mult), reads=[pbr, tb], writes=[w3_])
                    k.op("pool", lambda e, w0=w0, w1=w1: e.tensor_tensor(out=w0[:, 0:n], in0=w0[:, 0:n], in1=w1[:, 0:n], op=ALU.add), reads=[w0, w1], writes=[w0])
                    k.op("pool", lambda e, w2=w2, w3_=w3_: e.tensor_tensor(out=w2[:, 0:n], in0=w2[:, 0:n], in1=w3_[:, 0:n], op=ALU.subtract), reads=[w2, w3_], writes=[w2])
                    ini = ini_p()
                    if prev is None:
                        k.op("pool", lambda e, ini=ini: e.memset(ini[:], 0.0), writes=[ini])
                    else:
                        gp, npv = prev
                        cr = tb[:, 0, npv:npv + 1]
                        sr = tb[:, 1, npv:npv + 1]
                        k.op("dve", lambda e, ini=ini, gp=gp, npv=npv, cr=cr: e.tensor_tensor(out=ini[:, 2:3], in0=gp[:, 0, npv - 1:npv], in1=cr, op=ALU.mult), reads=[gp, tb], writes=[ini])
                        k.op("dve", lambda e, ini=ini, gp=gp, npv=npv, sr=sr: e.tensor_tensor(out=ini[:, 3:4], in0=gp[:, 1, npv - 1:npv], in1=sr, op=ALU.mult), reads=[gp, tb], writes=[ini])
                        k.op("dve", lambda e, ini=ini: e.tensor_tensor(out=ini[:, 0:1], in0=ini[:, 2:3], in1=ini[:, 3:4], op=ALU.subtract), reads=[ini], writes=[ini])
                        k.op("dve", lambda e, ini=ini, gp=gp, npv=npv, sr=sr: e.tensor_tensor(out=ini[:, 2:3], in0=gp[:, 0, npv - 1:npv], in1=sr, op=ALU.mult), reads=[gp, tb], writes=[ini])
                        k.op("dve", lambda e, ini=ini, gp=gp, npv=npv, cr=cr: e.tensor_tensor(out=ini[:, 3:4], in0=gp[:, 1, npv - 1:npv], in1=cr, op=ALU.mult), reads=[gp, tb], writes=[ini])
                        k.op("dve", lambda e, ini=ini: e.tensor_tensor(out=ini[:, 1:2], in0=ini[:, 2:3], in1=ini[:, 3:4], op=ALU.add), reads=[ini], writes=[ini])
                    g = g_p()
                    k.op("dve", lambda e, g=g, w0=w0, ini=ini: e.tensor_tensor_scan(g[:, 0, 0:n], rho_b.to_broadcast([128, n]), w0[:, 0:n], ini[:, 0:1], op0=ALU.mult, op1=ALU.add),
                         reads=[prm, w0, ini], writes=[g])
                    k.op("dve", lambda e, g=g, w2=w2, ini=ini: e.tensor_tensor_scan(g[:, 1, 0:n], rho_b.to_broadcast([128, n]), w2[:, 0:n], ini[:, 1:2], op0=ALU.mult, op1=ALU.add),
                         reads=[prm, w2, ini], writes=[g])
                    prev = (g, n)
                    cx["prev"] = prev
                    h = h_p()
                    x0_, x1_, x2_, x3_ = wk_p(), wk_p(), wk_p(), wk_p()
                    k.op("pool", lambda e, x0_=x0_, g=g, cosT=cosT: e.tensor_tensor(out=x0_[:, 0:n], in0=g[:, 0, 0:n], in1=cosT, op=ALU.mult), reads=[g, tb], writes=[x0_])
                    k.op("pool", lambda e, x1_=x1_, g=g, sinT=sinT: e.tensor_tensor(out=x1_[:, 0:n], in0=g[:, 1, 0:n], in1=sinT, op=ALU.mult), reads=[g, tb], writes=[x1_])
                    k.op("dve", lambda e, x2_=x2_, g=g, sinT=sinT: e.tensor_tensor(out=x2_[:, 0:n], in0=g[:, 0, 0:n], in1=sinT, op=ALU.mult), reads=[g, tb], writes=[x2_])
                    k.op("dve", lambda e, x3_=x3_, g=g, cosT=cosT: e.tensor_tensor(out=x3_[:, 0:n], in0=g[:, 1, 0:n], in1=cosT, op=ALU.mult), reads=[g, tb], writes=[x3_])
                    k.op("pool", lambda e, h=h, x0_=x0_, x1_=x1_: e.tensor_tensor(out=h[:, 0, 0:n], in0=x0_[:, 0:n], in1=x1_[:, 0:n], op=ALU.subtract), reads=[x0_, x1_], writes=[h])
                    k.op("dve", lambda e, h=h, x2_=x2_, x3_=x3_: e.tensor_tensor(out=h[:, 1, 0:n], in0=x2_[:, 0:n], in1=x3_[:, 0:n], op=ALU.add), reads=[x2_, x3_], writes=[h])
                    py = k.next_ps()
                    for ri in range(2):
                        hb_ = h.t[:, ri, :]
                        if d == 0:
                            rhs_h = h[:, ri, 0:n]
                        else:
                            b_ = h.t[:, ri, n - 1:n]
                            rhs_h = bass.AP(tensor=b_.tensor, offset=b_.offset, ap=[list(b_.ap[0]), [-1, n]])
                        k.mm(py, py[0:32, 0:n], CL, CL[:, ri, :], h, rhs_h, ri == 0, ri == 1)
                    if d == 0:
                        k.op("act", lambda e, ya=ya, py=py, c0=c0, n=n: e.copy(ya[:, c0:c0 + n], py[0:32, 0:n]), reads=[py], writes=[ya])
                    else:
                        k.op("dve", lambda e, ya=ya, py=py, c0=c0, n=n: e.tensor_copy(ya[:, c0:c0 + n], py[0:32, 0:n]), reads=[py], writes=[ya])
            ya = yad[0]
            for (c0, n) in chunks:
                k.op("pool", lambda e, c0=c0, n=n: e.tensor_tensor(out=yad[0][:, c0:c0 + n], in0=yad[1][:, c0:c0 + n], in1=yad[0][:, c0:c0 + n], op=ALU.add), reads=[yad[0], yad[1]], writes=[yad[0]])
                yo = yo_p()
                k.op("dve", lambda e, yo=yo, c0=c0, n=n: e.scalar_tensor_tensor(out=yo[:, 0:n], in0=u[:, c0:c0 + n], scalar=s5d[:, st:st + 1], in1=ya[:, c0:c0 + n], op0=ALU.mult, op1=ALU.add),
                     reads=[u, s5d, ya], writes=[yo])
                t1 = yo_p()
                k.op("dve", lambda e, yo=yo, t1=t1, n=n: e.tensor_tensor(out=t1[:, 0:n], in0=yo[:, 0:n], in1=yo[:, 0:n], op=ALU.mult), reads=[yo], writes=[t1])
                k.op("dve", lambda e, t1=t1, n=n: e.tensor_scalar(t1[:, 0:n], t1[:, 0:n], 0.044715, 1.0, op0=ALU.mult, op1=ALU.add), reads=[t1], writes=[t1])
                k.op("dve", lambda e, yo=yo, t1=t1, n=n: e.tensor_tensor(out=t1[:, 0:n], in0=t1[:, 0:n], in1=yo[:, 0:n], op=ALU.mult), reads=[t1, yo], writes=[t1])
                k.op("act", lambda e, t1=t1, n=n: e.activation(out=t1[:, 0:n], in_=t1[:, 0:n], func=AF.Sigmoid, scale=2.0 * math.sqrt(2.0 / math.pi)), reads=[t1], writes=[t1])
                k.op("dve", lambda e, yo=yo, t1=t1, n=n: e.tensor_tensor(out=yo[:, 0:n], in0=yo[:, 0:n], in1=t1[:, 0:n], op=ALU.mult), reads=[t1, yo], writes=[yo])
                k.dma(S5Y[st * 32:(st + 1) * 32, c0:c0 + n], yo[:, 0:n], reads=[yo], writes=[S5Y])
        k.phase_end()
        k.phase_begin()
        gw = k.sb("s5gw", [128, 4, 512])
        k.dma(gw[:], w["s5_gw"].t.rearrange("(kc p) n -> p kc n", p=128), reads=[w["s5_gw"]], writes=[gw])
        ggb = k.sb("s5gb", [128, 4])
        k.dma(ggb[:], w["s5_gb"][:, :], reads=[w["s5_gb"]], writes=[ggb])
        yb_p = k.pool("s5yb", [128, 4, 512], 2)
        go_p2 = k.pool("s5go", [128, 512], 3)
        for (c0, n) in NBLK:
            yb = yb_p()
            k.dma(yb[:, :, 0:n], S5Y[:, c0:c0 + n].rearrange("(kc p) t -> p kc t", p=128), reads=[S5Y], writes=[yb])
            for mc_ in range(4):
                ps = k.next_ps()
                for kc in range(4):
                    k.mm(ps, ps[:, 0:n], gw, gw[:, kc, mc_ * 128:(mc_ + 1) * 128], yb, yb[:, kc, 0:n], kc == 0, kc == 3)
                go = go_p2()
                k.op("act", lambda e, go=go, ps=ps, mc_=mc_: e.activation(out=go[:, 0:n], in_=ps[:, 0:n], func=AF.Sigmoid, bias=ggb[:, mc_:mc_ + 1]), reads=[ps, ggb], writes=[go])
                k.op("dve", lambda e, go=go, yb=yb, mc_=mc_: e.tensor_tensor(out=go[:, 0:n], in0=go[:, 0:n], in1=yb[:, mc_, 0:n], op=ALU.mult), reads=[go, yb], writes=[go])
                k.dma(S5O[mc_ * 128:(mc_ + 1) * 128, c0:c0 + n], go[:, 0:n], reads=[go], writes=[S5O])
        k.phase_end()
        if stop_after == "E":
            break

        k.phase_begin()
        modt = k.sb("modtF", [128, 96])
        k.dma(modt[:], MODT[:, :], reads=[MODT], writes=[modt])
        m3 = modt.t[:, :].rearrange("p (f j) -> p f j", j=2)
        brw = []
        for nm_ in ("br_attn_w", "br_hyena_w", "br_s5_w"):
            t_ = k.sb(nm_, [128, 4, D])
            k.dma(t_[:], w[nm_].t.rearrange("(kc p) n -> p kc n", p=128), reads=[w[nm_]], writes=[t_])
            brw.append(t_)
        ow = k.sb("out_w", [128, 8, D])
        k.dma(ow[:], w["out_w"].t.rearrange("(kc p) n -> p kc n", p=128), reads=[w["out_w"]], writes=[ow])
        br_p = k.pool("brin", [128, 3, 4, 512], 1)
        gt_p = k.pool("gt", [128, 3, 512], 2)
        mm_p = k.pool("mmix", [128, 8, 512], 1)
        xb_p = k.pool("xbF", [128, 8, 512], 1)
        t_p = k.pool("tF", [128, 512], 4)
        xo_p = k.pool("xoF", [128, 512], 3)
        srcs = (ATT, HY, S5O)
        for bi, (c0, n) in enumerate(NBLK):
            if bi == 0 and not need_ctx:
                continue
            j = 1 if bi == 0 else 0
            br = br_p()
            for b_ in range(3):
                k.dma(br[:, b_, :, 0:n], srcs[b_][:, c0:c0 + n].rearrange("(kc p) t -> p kc t", p=128), reads=[srcs[b_]], writes=[br])
            xb = xb_p()
            k.dma(xb[:, :, 0:n], XT[:, c0:c0 + n].rearrange("(kc p) t -> p kc t", p=128), reads=[XT], writes=[xb])
            mx = mm_p()
            for fc in range(8):
                gt = gt_p()
                for b_ in range(3):
                    k.dma(gt[:, b_, 0:n], GATE[b_ * D + fc * 128:b_ * D + (fc + 1) * 128, c0:c0 + n], reads=[GATE], writes=[gt])
                pb = [k.next_ps() for _ in range(3)]
                for b_ in range(3):
                    for kc in range(4):
                        k.mm(pb[b_], pb[b_][:, 0:n], brw[b_], brw[b_][:, kc, fc * 128:(fc + 1) * 128], br, br[:, b_, kc, 0:n], kc == 0, kc == 3)
                ta, tb_, tc_ = t_p(), t_p(), t_p()
                k.op("dve", lambda e, ta=ta, pb=pb, gt=gt: e.tensor_tensor(out=ta[:, 0:n], in0=pb[0][:, 0:n], in1=gt[:, 0, 0:n], op=ALU.mult), reads=[pb[0], gt], writes=[ta])
                k.op("dve", lambda e, tb_=tb_, pb=pb, gt=gt: e.tensor_tensor(out=tb_[:, 0:n], in0=pb[1][:, 0:n], in1=gt[:, 1, 0:n], op=ALU.mult), reads=[pb[1], gt], writes=[tb_])
                k.op("dve", lambda e, tc_=tc_, pb=pb, gt=gt: e.tensor_tensor(out=tc_[:, 0:n], in0=pb[2][:, 0:n], in1=gt[:, 2, 0:n], op=ALU.mult), reads=[pb[2], gt], writes=[tc_])
                k.op("pool", lambda e, ta=ta, tb_=tb_: e.tensor_tensor(out=ta[:, 0:n], in0=ta[:, 0:n], in1=tb_[:, 0:n], op=ALU.add), reads=[ta, tb_], writes=[ta])
                k.op("pool", lambda e, mx=mx, ta=ta, tc_=tc_, fc=fc: e.tensor_tensor(out=mx[:, fc, 0:n], in0=ta[:, 0:n], in1=tc_[:, 0:n], op=ALU.add), reads=[ta, tc_], writes=[mx])
            for dc in range(8):
                po = k.next_ps()
                for fc in range(8):
                    k.mm(po, po[:, 0:n], ow, ow[:, fc, dc * 128:(dc + 1) * 128], mx, mx[:, fc, 0:n], fc == 0, fc == 7)
                xo = xo_p()
                if dbg:
                    mo = xo_p()
                    k.op("act", lambda e, mo=mo, po=po: e.copy(mo[:, 0:n], po[:, 0:n]), reads=[po], writes=[mo])
                    k.dma(MIX[dc * 128:(dc + 1) * 128, c0:c0 + n], mo[:, 0:n], reads=[mo], writes=[MIX])
                k.op("dve", lambda e, xo=xo, po=po, xb=xb, dc=dc: e.scalar_tensor_tensor(out=xo[:, 0:n], in0=po[:, 0:n], scalar=m3[:, 16 + dc, j:j + 1], in1=xb[:, dc, 0:n], op0=ALU.mult, op1=ALU.add),
                     reads=[po, modt, xb], writes=[xo])
                k.dma(XT[dc * 128:(dc + 1) * 128, c0:c0 + n], xo[:, 0:n], reads=[xo], writes=[XT])
                if dbg:
                    k.dma(XDBG[dc * 128:(dc + 1) * 128, c0:c0 + n], xo[:, 0:n], reads=[xo], writes=[XDBG])
        k.phase_end()
        if stop_after == "F":
            break

        k.phase_begin()
        modt = k.sb("modtG", [128, 96])
        k.dma(modt[:], MODT[:, :], reads=[MODT], writes=[modt])
        m3 = modt.t[:, :].rearrange("p (f j) -> p f j", j=2)
        g2n = k.sb("g2n", [128, 8])
        k.dma(g2n[:], w["n2g"][:, :], reads=[w["n2g"]], writes=[g2n])
        A2 = k.sb("A2", [128, 8, 2])
        for j in range(2):
            k.op("dve", lambda e, j=j: e.scalar_tensor_tensor(out=A2[:, :, j], in0=m3[:, 32:40, j], scalar=1.0, in1=g2n[:], op0=ALU.add, op1=ALU.mult), reads=[modt, g2n], writes=[A2])
        wq = k.sb("peer_wq", [128, 8, 2048])
        wqv = w["peer_wq"].t.rearrange("(kc p) n -> p kc n", p=128)
        for kc in range(8):
            k.dma(wq[:, kc, :], wqv[:, kc, :], reads=[w["peer_wq"]], writes=[wq])
        kT = k.sb("peer_kT", [128, 16, 128])
        k.dma(kT[:], w["peer_kT"].t.rearrange("j k n -> k j n"), reads=[w["peer_kT"]], writes=[kT])
        csel = k.sb("csel", [128, 255])
        k.dma(csel[:], csel_in[:, :], reads=[csel_in], writes=[csel])
        io16 = k.sb("io16", [128, 16])
        k.dma(io16[:], iota16_in[:, :], reads=[iota16_in], writes=[io16])
        xb_p = k.pool("xbG", [128, 8, 128], 1)
        hb_p = k.pool("hbG", [128, 8, 128], 1)
        sq_p = k.pool("sqG", [128, 8, 128], 1)
        sm_p = k.pool("smG", [128, 128], 6)
        qT_p = k.pool("qTG", [128, 16, 128], 1)
        S_p = k.pool("SG", [128, 16, 128], 1)
        SW_p = k.pool("SWG", [128, 128], 2)
        TV_p = k.pool("TVG", [128, 16, 16], 1)
        TI_p = k.pool("TIG", [128, 16, 16], 1, U32)
        TF_p = k.pool("TFG", [128, 16, 16], 1)
        cand_p = k.pool("candG", [128, 8, 256], 1)
        cw_p = k.pool("cwG", [128, 256], 2)
        BV_p = k.pool("BVG", [128, 8, 16], 1)
        BP_p = k.pool("BPG", [128, 8, 16], 1, U32)
        BI_p = k.pool("BIG", [128, 8, 16], 2, I32)
        oh_p = k.pool("ohG", [128, 16, 16], 2)
        IDF_p = k.pool("IDFG", [128, 128], 1)
        GT_p = k.pool("GTG", [128, 128], 1)
        idT_p = k.pool("idTG", [128, 128], 1, I32)
        gT_p = k.pool("gTG", [128, 128], 1)
        htok_p = k.pool("htokG", [128, D], 1)
        ug_p = k.pool("ugG", [128, D], 6)
        vg_p = k.pool("vgG", [128, D], 6)
        dots_p = k.pool("dotsG", [128, 128], 1)
        wts_p = k.pool("wtsG", [128, 128], 1)
        wt_p = k.pool("wtG", [128, 128], 3)
        otok_p = k.pool("otokG", [128, D], 1)
        xo_p = k.pool("xoG", [128, 128], 3)
        GELU_C = 2.0 * math.sqrt(2.0 / math.pi)

        def bc3(t_ap_base, strides):
            return bass.AP(tensor=t_ap_base.tensor, offset=t_ap_base.offset, ap=[list(t_ap_base.ap[0]), [strides[0], 8], [strides[1], 16], [strides[2], 16]])

        tiles = [(t0, 1 if t0 < C else 0) for t0 in range(0 if need_ctx else C, NT, 128)]
        for ti_, (t0, j) in enumerate(tiles):
            if ti_ > 0 and ti_ % 8 == 0:
                k.barrier()
            xb = xb_p()
            k.dma(xb[:], XT[:, t0:t0 + 128].rearrange("(kc p) t -> p kc t", p=128), reads=[XT], writes=[xb])
            sq = sq_p()
            k.op("act", lambda e, sq=sq, xb=xb: e.activation(out=sq[:], in_=xb[:], func=AF.Square), reads=[xb], writes=[sq])
            pss = k.next_ps()
            for kc in range(8):
                k.mm(pss, pss[:, 0:128], ones, ones[:], sq, sq[:, kc, :], kc == 0, kc == 7)
            rstd = sm_p()
            k.op("act", lambda e, rstd=rstd, pss=pss: e.activation(out=rstd[:], in_=pss[:, 0:128], func=AF.Sqrt, bias=eps_t[:, 0:1], scale=1.0 / D), reads=[pss, eps_t], writes=[rstd])
            k.op("dve", lambda e, rstd=rstd: e.reciprocal(rstd[:], rstd[:]), reads=[rstd], writes=[rstd])
            hb = hb_p()
            for kc in range(8):
                k.op("dve", lambda e, hb=hb, xb=xb, rstd=rstd, kc=kc: e.tensor_tensor(out=hb[:, kc, :], in0=xb[:, kc, :], in1=rstd[:], op=ALU.mult), reads=[xb, rstd], writes=[hb])
                k.op("pool", lambda e, hb=hb, kc=kc: e.tensor_scalar(hb[:, kc, :], hb[:, kc, :], A2[:, kc, j:j + 1], m3[:, 24 + kc, j:j + 1], op0=ALU.mult, op1=ALU.add), reads=[hb, A2, modt], writes=[hb])
            htok = htok_p()
            for kc in range(8):
                pst = k.next_ps()
                k.op("pe", lambda e, pst=pst, hb=hb, kc=kc: e.transpose(pst[:, 0:128], hb[:, kc, :], ident[:]), reads=[hb, ident], writes=[pst])
                k.op("act", lambda e, htok=htok, pst=pst, kc=kc: e.copy(htok[:, kc * 128:(kc + 1) * 128], pst[:, 0:128]), reads=[pst], writes=[htok])
            qT = qT_p()
            for jc in range(16):
                psq = k.next_ps()
                for kc in range(8):
                    k.mm(psq, psq[:, 0:128], wq, wq[:, kc, jc * 128:(jc + 1) * 128], hb, hb[:, kc, :], kc == 0, kc == 7)
                if jc % 2 == 0:
                    k.op("act", lambda e, qT=qT, psq=psq, jc=jc: e.copy(qT[:, jc, :], psq[:, 0:128]), reads=[psq], writes=[qT])
                else:
                    k.op("dve", lambda e, qT=qT, psq=psq, jc=jc: e.tensor_copy(qT[:, jc, :], psq[:, 0:128]), reads=[psq], writes=[qT])
            S = S_p()
            for jc in range(16):
                pss2 = k.next_ps()
                k.mm(pss2, pss2[:, 0:128], qT, qT[:, jc, :], kT, kT[:, jc, :], True, True)
                if jc % 2 == 0:
                    k.op("act", lambda e, S=S, pss2=pss2, jc=jc: e.copy(S[:, jc, :], pss2[:, 0:128]), reads=[pss2], writes=[S])
                else:
                    k.op("dve", lambda e, S=S, pss2=pss2, jc=jc: e.tensor_copy(S[:, jc, :], pss2[:, 0:128]), reads=[pss2], writes=[S])
            TV = TV_p(); TI = TI_p()
            for jc in range(16):
                SW = SW_p()
                k.op("dve", lambda e, TV=TV, S=S, jc=jc: e.max(out=TV[:, jc, 0:8], in_=S[:, jc, :]), reads=[S], writes=[TV])
                k.op("dve", lambda e, TI=TI, TV=TV, S=S, jc=jc: e.max_index(out=TI[:, jc, 0:8], in_max=TV[:, jc, 0:8], in_values=S[:, jc, :]), reads=[S, TV], writes=[TI])
                k.op("dve", lambda e, SW=SW, TV=TV, S=S, jc=jc: e.match_replace(out=SW[:], in_to_replace=TV[:, jc, 0:8], in_values=S[:, jc, :], imm_value=-1e30), reads=[S, TV], writes=[SW])
                k.op("dve", lambda e, TV=TV, SW=SW, jc=jc: e.max(out=TV[:, jc, 8:16], in_=SW[:]), reads=[SW], writes=[TV])
                k.op("dve", lambda e, TI=TI, TV=TV, SW=SW, jc=jc: e.max_index(out=TI[:, jc, 8:16], in_max=TV[:, jc, 8:16], in_values=SW[:]), reads=[SW, TV], writes=[TI])
            TF = TF_p()
            k.op("dve", lambda e, TF=TF, TI=TI: e.tensor_copy(TF[:], TI[:]), reads=[TI], writes=[TF])
            cand = cand_p()
            tvb = TV.t[:, 0:1, 0:1]
            cv4 = cand.t[:, :, :].rearrange("p h (a b) -> p h a b", b=16)
            in0 = bc3(TV.t[:, 0, 0:1], (32, 1, 0))
            in1 = bc3(TV.t[:, 1, 0:1], (32, 0, 1))
            k.op("dve", lambda e, cv4=cv4, in0=in0, in1=in1: e.tensor_tensor(out=cv4, in0=in0, in1=in1, op=ALU.add), reads=[TV], writes=[cand])
            BV = BV_p(); BP = BP_p()
            for h in range(8):
                cw = cw_p()
                k.op("dve", lambda e, BV=BV, cand=cand, h=h: e.max(out=BV[:, h, 0:8], in_=cand[:, h, :]), reads=[cand], writes=[BV])
                k.op("dve", lambda e, BP=BP, BV=BV, cand=cand, h=h: e.max_index(out=BP[:, h, 0:8], in_max=BV[:, h, 0:8], in_values=cand[:, h, :]), reads=[cand, BV], writes=[BP])
                k.op("dve", lambda e, cw=cw, BV=BV, cand=cand, h=h: e.match_replace(out=cw[:], in_to_replace=BV[:, h, 0:8], in_values=cand[:, h, :], imm_value=-1e30), reads=[cand, BV], writes=[cw])
                k.op("dve", lambda e, BV=BV, cw=cw, h=h: e.max(out=BV[:, h, 8:16], in_=cw[:]), reads=[cw], writes=[BV])
                k.op("dve", lambda e, BP=BP, BV=BV, cw=cw, h=h: e.max_index(out=BP[:, h, 8:16], in_max=BV[:, h, 8:16], in_values=cw[:]), reads=[cw, BV], writes=[BP])
            bpi = BP.t[:, :, :].bitcast(I32)
            Ai = BI_p(); Bi = BI_p()
            k.op("dve", lambda e, Ai=Ai, bpi=bpi: e.tensor_single_scalar(Ai[:], bpi, 4, op=ALU.arith_shift_right), reads=[BP], writes=[Ai])
            k.op("dve", lambda e, Bi=Bi, bpi=bpi: e.tensor_single_scalar(Bi[:], bpi, 15, op=ALU.bitwise_and), reads=[BP], writes=[Bi])
            Af = sm_p(); Bf = sm_p()
            k.op("dve", lambda e, Af=Af, Ai=Ai: e.tensor_copy(Af[:], Ai.t[:, :, :].rearrange("p h k -> p (h k)")), reads=[Ai], writes=[Af])
            k.op("dve", lambda e, Bf=Bf, Bi=Bi: e.tensor_copy(Bf[:], Bi.t[:, :, :].rearrange("p h k -> p (h k)")), reads=[Bi], writes=[Bf])
            IDF = IDF_p()
            isel = sm_p(); jsel = sm_p()
            for h in range(8):
                for (posf, seg, dst) in ((Af, 2 * h, isel), (Bf, 2 * h + 1, jsel)):
                    oh = oh_p()
                    pk = posf.t[:, h * 16:(h + 1) * 16]
                    pk3 = bass.AP(tensor=pk.tensor, offset=pk.offset, ap=[list(pk.ap[0]), [1, 16], [0, 16]])
                    io3 = bass.AP(tensor=io16.t[:, 0:16].tensor, offset=io16.t[:, 0:16].offset, ap=[list(io16.t[:, 0:16].ap[0]), [0, 16], [1, 16]])
                    tf_ = TF.t[:, seg, 0:16]
                    tf3 = bass.AP(tensor=tf_.tensor, offset=tf_.offset, ap=[list(tf_.ap[0]), [0, 16], [1, 16]])
                    k.op("dve", lambda e, oh=oh, pk3=pk3, io3=io3: e.tensor_tensor(out=oh[:], in0=pk3, in1=io3, op=ALU.is_equal), reads=[posf, io16], writes=[oh])
                    k.op("dve", lambda e, oh=oh, tf3=tf3: e.tensor_tensor(out=oh[:], in0=oh[:], in1=tf3, op=ALU.mult), reads=[oh, TF], writes=[oh])
                    k.op("dve", lambda e, oh=oh, dst=dst, h=h: e.tensor_reduce(out=dst[:, h * 16:(h + 1) * 16], in_=oh[:], axis=AX.X, op=ALU.add), reads=[oh], writes=[dst])
            k.op("dve", lambda e, IDF=IDF, isel=isel, jsel=jsel: e.scalar_tensor_tensor(out=IDF[:], in0=isel[:], scalar=128.0, in1=jsel[:], op0=ALU.mult, op1=ALU.add), reads=[isel, jsel], writes=[IDF])
            k.op("dve", lambda e, IDF=IDF: e.tensor_scalar(IDF[:], IDF[:], 16383.0, 0.0, op0=ALU.min, op1=ALU.max), reads=[IDF], writes=[IDF])
            GT = GT_p()
            gst = sm_p()
            for h in range(8):
                k.op("dve", lambda e, gst=gst, BV=BV, h=h: e.tensor_scalar(gst[:, h:h + 1], BV[:, h, 0:1], -1.0, None, op0=ALU.mult), reads=[BV], writes=[gst])
                k.op("act", lambda e, GT=GT, BV=BV, gst=gst, h=h: e.activation(out=GT[:, h * 16:(h + 1) * 16], in_=BV[:, h, :], func=AF.Exp, bias=gst[:, h:h + 1], accum_out=gst[:, 8 + h:9 + h]),
                     reads=[BV, gst], writes=[GT, gst])
            k.op("dve", lambda e, gst=gst: e.reciprocal(gst[:, 16:24], gst[:, 8:16]), reads=[gst], writes=[gst])
            for h in range(8):
                k.op("dve", lambda e, GT=GT, gst=gst, h=h: e.tensor_scalar(GT[:, h * 16:(h + 1) * 16], GT[:, h * 16:(h + 1) * 16], gst[:, 16 + h:17 + h], None, op0=ALU.mult), reads=[GT, gst], writes=[GT])
            idT = idT_p(); gT = gT_p()
            pst = k.next_ps()
            k.op("pe", lambda e, pst=pst, IDF=IDF: e.transpose(pst[:, 0:128], IDF[:], ident[:]), reads=[IDF, ident], writes=[pst])
            k.op("dve", lambda e, idT=idT, pst=pst: e.tensor_copy(idT[:], pst[:, 0:128]), reads=[pst], writes=[idT])
            pst2 = k.next_ps()
            k.op("pe", lambda e, pst2=pst2, GT=GT: e.transpose(pst2[:, 0:128], GT[:], ident[:]), reads=[GT, ident], writes=[pst2])
            k.op("act", lambda e, gT=gT, pst2=pst2: e.copy(gT[:], pst2[:, 0:128]), reads=[pst2], writes=[gT])
            dots = dots_p()
            for t in range(128):
                ug = ug_p()
                k.dma(None, None, reads=[idT, w["peer_u"]], writes=[ug], q="pool",
                      fn=lambda e, ug=ug, t=t: e.indirect_dma_start(out=ug[:], out_offset=None, in_=w["peer_u"][:, :],
                                                                  in_offset=bass.IndirectOffsetOnAxis(ap=idT[:, t:t + 1], axis=0)))
                pb0 = k.next_ps(); pb1 = k.next_ps()
                sel = ident[:, t:t + 1].to_broadcast([128, 128])
                k.mm(pb0, pb0[:, :], ident, sel, htok, htok[:, 0:512], True, True)
                k.mm(pb1, pb1[:, :], ident, sel, htok, htok[:, 512:1024], True, True)
                hrow = vg_p()
                k.op("act", lambda e, hrow=hrow, pb0=pb0: e.copy(hrow[:, 0:512], pb0[:, :]), reads=[pb0], writes=[hrow])
                k.op("act", lambda e, hrow=hrow, pb1=pb1: e.copy(hrow[:, 512:1024], pb1[:, :]), reads=[pb1], writes=[hrow])
                k.op("dve", lambda e, ug=ug, hrow=hrow, t=t: e.scalar_tensor_tensor(out=hrow[:], in0=ug[:], scalar=1.0, in1=hrow[:], op0=ALU.mult, op1=ALU.mult, accum_out=dots[:, t:t + 1]),
                     reads=[ug, hrow], writes=[hrow, dots])
            wts = wts_p()
            g1_ = sm_p()
            k.op("dve", lambda e, g1_=g1_: e.tensor_tensor(out=g1_[:], in0=dots[:], in1=dots[:], op=ALU.mult), reads=[dots], writes=[g1_])
            k.op("dve", lambda e, g1_=g1_: e.tensor_scalar(g1_[:], g1_[:], 0.044715, 1.0, op0=ALU.mult, op1=ALU.add), reads=[g1_], writes=[g1_])
            k.op("dve", lambda e, g1_=g1_: e.tensor_tensor(out=g1_[:], in0=g1_[:], in1=dots[:], op=ALU.mult), reads=[g1_, dots], writes=[g1_])
            k.op("act", lambda e, g1_=g1_: e.activation(out=g1_[:], in_=g1_[:], func=AF.Sigmoid, scale=GELU_C), reads=[g1_], writes=[g1_])
            k.op("dve", lambda e, g1_=g1_: e.tensor_tensor(out=g1_[:], in0=g1_[:], in1=dots[:], op=ALU.mult), reads=[g1_, dots], writes=[g1_])
            k.op("dve", lambda e, wts=wts, g1_=g1_, gT=gT: e.tensor_tensor(out=wts[:], in0=g1_[:], in1=gT[:], op=ALU.mult), reads=[g1_, gT], writes=[wts])
            po0 = k.next_ps(); po1 = k.next_ps()
            for t in range(128):
                vg = vg_p()
                k.dma(None, None, reads=[idT, w["peer_v"]], writes=[vg], q="pool",
                      fn=lambda e, vg=vg, t=t: e.indirect_dma_start(out=vg[:], out_offset=None, in_=w["peer_v"][:, :],
                                                                  in_offset=bass.IndirectOffsetOnAxis(ap=idT[:, t:t + 1], axis=0)))
                wt_ = wt_p()
                k.op("dve", lambda e, wt_=wt_, t=t: e.tensor_scalar(wt_[:], csel[:, 127 - t:255 - t], wts[:, t:t + 1], None, op0=ALU.mult), reads=[csel, wts], writes=[wt_])
                k.mm(po0, po0[:, :], wt_, wt_[:], vg, vg[:, 0:512], t == 0, t == 127)
                k.mm(po1, po1[:, :], wt_, wt_[:], vg, vg[:, 512:1024], t == 0, t == 127)
            otok = otok_p()
            k.op("act", lambda e, otok=otok, po0=po0: e.copy(otok[:, 0:512], po0[:, :]), reads=[po0], writes=[otok])
            k.op("dve", lambda e, otok=otok, po1=po1: e.tensor_copy(otok[:, 512:1024], po1[:, :]), reads=[po1], writes=[otok])
            for dc in range(8):
                pst = k.next_ps()
                k.op("pe", lambda e, pst=pst, otok=otok, dc=dc: e.transpose(pst[:, 0:128], otok[:, dc * 128:(dc + 1) * 128], ident[:]), reads=[otok, ident], writes=[pst])
                xo = xo_p()
                k.op("dve", lambda e, xo=xo, pst=pst, xb=xb, dc=dc: e.scalar_tensor_tensor(out=xo[:], in0=pst[:, 0:128], scalar=m3[:, 40 + dc, j:j + 1], in1=xb[:, dc, :], op0=ALU.mult, op1=ALU.add),
                     reads=[pst, modt, xb], writes=[xo])
                k.dma(XT[dc * 128:(dc + 1) * 128, t0:t0 + 128], xo[:], reads=[xo], writes=[XT])
        k.phase_end()
        if stop_after == "G":
            break

    if stop_after is None:
        k.phase_begin()
        fg = k.sb("final_g", [128, 8])
        k.dma(fg[:], finalg_in[:, :], reads=[finalg_in], writes=[fg])
        xb_p = k.pool("xbN", [128, 8, 512], 2)
        sq_p = k.pool("sqN", [128, 8, 512], 1)
        rs_p = k.pool("rsN", [128, 512], 2)
        yo_p = k.pool("yoN", [128, 512], 4)
        for bi in range(8):
            t0 = C + bi * 512
            xb = xb_p()
            k.dma(xb[:], XT[:, t0:t0 + 512].rearrange("(kc p) t -> p kc t", p=128), reads=[XT], writes=[xb])
            sq = sq_p()
            k.op("act", lambda e, sq=sq, xb=xb: e.activation(out=sq[:], in_=xb[:], func=AF.Square), reads=[xb], writes=[sq])
            pss = k.next_ps()
            for kc in range(8):
                k.mm(pss, pss[:, :], ones, ones[:], sq, sq[:, kc, :], kc == 0, kc == 7)
            rstd = rs_p()
            k.op("act", lambda e, rstd=rstd, pss=pss: e.activation(out=rstd[:], in_=pss[:, :], func=AF.Sqrt, bias=eps_t[:, 0:1], scale=1.0 / D), reads=[pss, eps_t], writes=[rstd])
            k.op("dve", lambda e, rstd=rstd: e.reciprocal(rstd[:], rstd[:]), reads=[rstd], writes=[rstd])
            for kc in range(8):
                yo = yo_p()
                k.op("dve", lambda e, yo=yo, xb=xb, rstd=rstd, kc=kc: e.scalar_tensor_tensor(out=yo[:], in0=xb[:, kc, :], scalar=fg[:, kc:kc + 1], in1=rstd[:], op0=ALU.mult, op1=ALU.mult),
                     reads=[xb, fg, rstd], writes=[yo])
                k.dma(out[kc * 128:(kc + 1) * 128, bi * 512:(bi + 1) * 512], yo[:], reads=[yo], writes=[out])
        k.phase_end()
    else:
        k.phase_begin()
        fin_p = k.pool("fin", [128, L], 2)
        for kc in range(8):
            st = fin_p()
            k.dma(st[:], XT[kc * 128:(kc + 1) * 128, C:NT], reads=[XT], writes=[st])
            k.dma(out[kc * 128:(kc + 1) * 128, :], st[:], reads=[st], writes=[out])
        k.phase_end()
    k.finish()
    return nc, k


def kernel(**inputs):
    inp = {kk: np.asarray(v) for kk, v in inputs.items()}
    nc, _ = build()
    in_maps = [_prep(inp, b) for b in range(4)]
    res = run_bass_kernel_spmd(nc, in_maps, core_ids=list(range(4)))
    return np.stack([np.ascontiguousarray(res.results[b]["out"].T) for b in range(4)], 0).astype(np.float32)
```

```python
import math
import numpy as np
from contextlib import ExitStack
import concourse.bass as bass
import concourse.mybir as mybir
from concourse.bass_utils import run_bass_kernel_spmd

F32 = mybir.dt.float32
I32 = mybir.dt.int32
U32 = mybir.dt.uint32
AF = mybir.ActivationFunctionType
ALU = mybir.AluOpType
AX = mybir.AxisListType

D = 1024
L = 4096
C = 256
NT = L + C
DEPTH = 2
IN_W = 5888
NBLK = [(0, 256)] + [(256 + 512 * i, 512) for i in range(8)]


class Res:
    __slots__ = ("name", "w", "r", "ep")

    def __init__(self, name):
        self.name = name
        self.w = None
        self.r = {}
        self.ep = 0


class Tile(Res):
    __slots__ = ("t",)

    def __init__(self, name, t):
        Res.__init__(self, name)
        self.t = t

    def __getitem__(self, k):
        return self.t[k]


class Eng:
    def __init__(self, name, e, sem):
        self.name = name
        self.e = e
        self.sem = sem
        self.cnt = 0
        self.known = {}


class K:
    SAME_ENGINE_SYNC = True
    ROTATE_AT = 20000

    def __init__(self, nc, n_dma_slots=24):
        self.nc = nc
        self._sems = []
        self.es = ExitStack()
        self.epoch = 1
        self.uid = 0
        self.eng = {}
        for nm, e in (("pe", nc.tensor), ("act", nc.scalar), ("dve", nc.vector),
                      ("pool", nc.gpsimd), ("sp", nc.sync)):
            self.eng[nm] = Eng(nm, e, self.es.enter_context(nc.semaphore("sem_" + nm)))
        self.slots = []
        for i in range(n_dma_slots):
            s = self.es.enter_context(nc.semaphore("dsem%d" % i))
            self.slots.append([s, 0])
        self.slot_i = 0
        self.phase_stack = None
        self.psum = []
        for i in range(8):
            t = self.es.enter_context(nc.psum_tensor("ps%d" % i, [128, 512], F32))
            self.psum.append(Tile("ps%d" % i, t))
        self.ps_i = 0
        self.n_ins = 0
        self.n_rot = 0
        self._sems = []

    def next_ps(self):
        p = self.psum[self.ps_i]
        self.ps_i = (self.ps_i + 1) % 8
        return p

    def sb(self, name, shape, dtype=F32, persistent=False):
        self.uid += 1
        st = self.es if (persistent or self.phase_stack is None) else self.phase_stack
        t = st.enter_context(self.nc.sbuf_tensor("%s_%d" % (name, self.uid), list(shape), dtype))
        return Tile(name, t)

    def pool(self, name, shape, n, dtype=F32):
        tiles = [self.sb("%s%d" % (name, i), shape, dtype) for i in range(n)]
        st = {"i": 0}

        def nxt():
            t = tiles[st["i"]]
            st["i"] = (st["i"] + 1) % n
            return t
        return nxt

    def dram(self, name, shape, dtype=F32, kind="Internal"):
        t = self.nc.dram_tensor(name, list(shape), dtype, kind=kind)
        return Tile(name, t.ap())

    def phase_begin(self):
        assert self.phase_stack is None
        self.phase_stack = ExitStack()

    def phase_end(self):
        self.barrier()
        self.phase_stack.close()
        self.phase_stack = None

    def _key(self, sem):
        for i, s_ in enumerate(self._sems):
            if s_ is sem:
                return i
        self._sems.append(sem)
        return len(self._sems) - 1

    def _wait(self, E, ev):
        sem, val = ev
        k = self._key(sem)
        if E.known.get(k, 0) >= val:
            return
        E.e.wait_ge(sem, val)
        E.known[k] = val
        self.n_ins += 1

    def _deps(self, E, reads, writes, is_pe=False):
        evs = []
        for r in reads:
            if r.ep == self.epoch and r.w is not None:
                evs.append(r.w)
        for w in writes:
            if w.ep == self.epoch:
                if w.w is not None:
                    evs.append(w.w)
                evs.extend(w.r.values())
        for ev in evs:
            if ev[0] is E.sem and (is_pe or not self.SAME_ENGINE_SYNC):
                continue
            self._wait(E, ev)

    def _record(self, ev, reads, writes):
        for r in reads:
            if r.ep != self.epoch:
                r.ep = self.epoch
                r.w = None
                r.r = {}
            r.r[self._key(ev[0])] = ev
        for w in writes:
            w.ep = self.epoch
            w.w = ev
            w.r = {}

    def op(self, eng, fn, reads=(), writes=()):
        E = self.eng[eng]
        self._deps(E, reads, writes, is_pe=(eng == "pe"))
        ins = fn(E.e)
        E.cnt += 1
        ins.then_inc(E.sem, 1)
        self.n_ins += 1
        self._record((E.sem, E.cnt), reads, writes)
        return ins

    def dma(self, out_ap, in_ap, reads=(), writes=(), q="sp", fn=None):
        E = self.eng[q]
        self._deps(E, reads, writes)
        slot = self.slots[self.slot_i]
        self.slot_i = (self.slot_i + 1) % len(self.slots)
        if slot[1] > 0:
            self._wait(E, (slot[0], slot[1]))
        ins = E.e.dma_start(out=out_ap, in_=in_ap) if fn is None else fn(E.e)
        slot[1] += 16
        ins.then_inc(slot[0], 16)
        self.n_ins += 1
        self._record((slot[0], slot[1]), reads, writes)
        return ins

    def barrier(self):
        evs = []
        for E in self.eng.values():
            if E.cnt > 0:
                evs.append((E.sem, E.cnt))
        for s in self.slots:
            if s[1] > 0:
                evs.append((s[0], s[1]))
        for E in self.eng.values():
            for ev in evs:
                if ev[0] is E.sem:
                    continue
                self._wait(E, ev)
        self.epoch += 1
        for E in self.eng.values():
            if E.cnt > self.ROTATE_AT:
                self.n_rot += 1
                E.sem = self.es.enter_context(self.nc.semaphore("sem_%s_r%d" % (E.name, self.n_rot)))
                E.cnt = 0
        for s in self.slots:
            if s[1] > self.ROTATE_AT:
                self.n_rot += 1
                s[0] = self.es.enter_context(self.nc.semaphore("dsem_r%d" % self.n_rot))
                s[1] = 0

    def finish(self):
        self.barrier()
        self.es.close()

    def mm(self, ps, out_ap, lt, lhsT_ap, rt, rhs_ap, start, stop):
        return self.op("pe", lambda e: e.matmul(out_ap, lhsT_ap, rhs_ap, start=start, stop=stop),
                       reads=[lt, rt], writes=[ps])


def _rot_perm():
    perm = []
    for hd in range(10):
        for i in range(64):
            perm.append(hd * 64 + (i + 32) % 64)
    return np.array(perm)


def _rope_tables():
    row = np.repeat(np.arange(L // 64), 64).astype(np.float32)
    col = np.tile(np.arange(64), L // 64).astype(np.float32)
    inv = np.power(np.float32(10000.0), -np.arange(16, dtype=np.float32) / 16).astype(np.float32)
    ang = np.concatenate([row[:, None] * inv, col[:, None] * inv], axis=-1)
    cos = np.cos(ang).astype(np.float32)
    sin = np.sin(ang).astype(np.float32)
    cosT = np.ones((64, NT), np.float32)
    sinT = np.zeros((64, NT), np.float32)
    cosT[:32, C:] = cos.T
    cosT[32:, C:] = cos.T
    sinT[:32, C:] = -sin.T
    sinT[32:, C:] = sin.T
    return np.concatenate([cosT, cosT], 0), np.concatenate([sinT, sinT], 0)


def _pk(v, n):
    return np.ascontiguousarray(np.asarray(v, np.float32).reshape(n, 128).T)


def _prep(inp, b):
    m = {}
    m["xT"] = np.ascontiguousarray(inp["x"][b].T)
    m["ctxT"] = np.ascontiguousarray(inp["ctx"][b].T)
    cv = np.stack([_pk(inp["c"][b], 8), _pk(inp["c_ctx"], 8)], axis=-1)
    m["cvec"] = np.ascontiguousarray(cv.reshape(128, 16))
    perm = _rot_perm()
    cosT, sinT = _rope_tables()
    m["ropec"] = cosT
    qq = np.arange(128)[:, None]
    kk = np.arange(128)[None, :]
    mk = np.zeros((128, 256), np.float32)
    mk[:, :128] = np.where(kk >= qq, 0.0, -1e30)
    mk[:, 128:] = np.where(kk <= qq, 0.0, -1e30)
    m["amask"] = mk
    m["final_g"] = _pk(inp["final_g"], 8)
    csel = np.zeros((128, 255), np.float32)
    csel[:, 127] = 1.0
    m["csel"] = csel
    m["iota16"] = np.ascontiguousarray(np.broadcast_to(np.arange(16, dtype=np.float32)[None, :], (128, 16)))
    for nm, Ls in (("L", L), ("C", C)):
        t = np.arange(Ls, dtype=np.float32)
        tn = t / np.float32(Ls)
        bands = np.arange(1, 17, dtype=np.float32)
        ang = np.float32(2.0 * math.pi) * tn[:, None] * bands[None, :]
        zf = np.concatenate([tn[:, None], np.cos(ang), np.sin(ang)], axis=-1).astype(np.float32)
        m["hyz" + nm] = np.ascontiguousarray(zf.T)
        tw = np.linspace(0.0, 1.0, Ls, dtype=np.float32)
        m["hytw" + nm] = np.ascontiguousarray((-tw).reshape(Ls // 128, 128).T)
    fast = -math.log(1e-2) / 0.3
    slow = -math.log(1e-2) / 1.5
    rate = np.linspace(fast, slow, 512, dtype=np.float32)
    m["hyrate"] = np.ascontiguousarray(np.broadcast_to(rate[None, :], (128, 512)))
    m["ropes"] = sinT
    for l in range(DEPTH):
        m["mod_w%d" % l] = np.ascontiguousarray(inp["mod_w"][l])
        m["mod_b%d" % l] = _pk(inp["mod_b"][l], 48)
        m["n1g%d" % l] = _pk(inp["norm1_g"][l], 8)
        m["in_w%d" % l] = np.ascontiguousarray(inp["in_w"][l])
        m["in_wr%d" % l] = np.ascontiguousarray(inp["in_w"][l][:, perm])
        m["gate_b%d" % l] = _pk(inp["gate_b"][l], 24)
        m["hy_sw%d" % l] = np.ascontiguousarray(np.asarray(inp["hy_short_w"][l], np.float32).reshape(3, 12, 128).transpose(2, 1, 0))
        m["hy_sb%d" % l] = _pk(inp["hy_short_b"][l], 12)
        m["hy_w1%d" % l] = np.ascontiguousarray(inp["hy_w1"][l])
        m["hy_w2%d" % l] = np.ascontiguousarray(inp["hy_w2"][l])
        m["hy_w3%d" % l] = np.ascontiguousarray(inp["hy_w3"][l])
        m["hy_fb%d" % l] = np.ascontiguousarray(np.stack([inp["hy_b1"][l], inp["hy_freq1"][l], inp["hy_b2"][l], inp["hy_freq2"][l]], 1).astype(np.float32))
        m["hy_bias%d" % l] = _pk(inp["hy_bias"][l], 4)
        for nm_ in ("s5_a_re", "s5_a_im"):
            m["%s%d" % (nm_, l)] = np.ascontiguousarray(np.asarray(inp[nm_][l], np.float32).reshape(2, 16, 128).transpose(2, 0, 1))
        m["s5_ldt%d" % l] = np.ascontiguousarray(np.repeat(np.asarray(inp["s5_log_dt"][l], np.float32), 64, axis=1).reshape(2, 16, 128).transpose(2, 0, 1))
        for nm_ in ("s5_b_re", "s5_b_im"):
            m["%s%d" % (nm_, l)] = np.ascontiguousarray(np.asarray(inp[nm_][l], np.float32).reshape(2, 2048, 16))
        for nm_ in ("s5_c_re", "s5_c_im"):
            m["%s%d" % (nm_, l)] = np.ascontiguousarray(np.asarray(inp[nm_][l], np.float32).transpose(0, 1, 3, 2).reshape(2, 2048, 16))
        m["s5_d%d" % l] = np.ascontiguousarray(np.asarray(inp["s5_d"][l], np.float32).reshape(16, 32).T)
        m["s5_gw%d" % l] = np.ascontiguousarray(inp["s5_glu_w"][l])
        m["s5_gb%d" % l] = _pk(inp["s5_glu_b"][l], 4)
        for nm_ in ("br_attn_w", "br_hyena_w", "br_s5_w", "out_w"):
            m["%s%d" % (nm_, l)] = np.ascontiguousarray(inp[nm_][l])
        m["n2g%d" % l] = _pk(inp["norm2_g"][l], 8)
        m["peer_wq%d" % l] = np.ascontiguousarray(inp["peer_wq"][l])
        m["peer_kT%d" % l] = np.ascontiguousarray(np.asarray(inp["peer_keys"][l], np.float32).transpose(0, 1, 3, 2).reshape(16, 128, 128))
        m["peer_u%d" % l] = np.ascontiguousarray(inp["peer_u"][l])
        m["peer_v%d" % l] = np.ascontiguousarray(inp["peer_v"][l])
        m["sink%d" % l] = np.ascontiguousarray(np.broadcast_to(np.asarray(inp["attn_sink"][l], np.float32)[None, :], (128, 8)))
    return m


def build(dbg=False, n_layers=DEPTH, stop_after=None, start_at=None):
    nc = bass.Bass("TRN2", target_bir_lowering=False)
    k = K(nc)
    okind = "ExternalOutput" if dbg else "Internal"
    ein = lambda name, shape, dt=F32: k.dram(name, shape, dt, kind="ExternalInput")
    xT_in = ein("xT", [D, L])
    ctxT_in = ein("ctxT", [D, C])
    cvec_in = ein("cvec", [128, 16])
    ropec_in = ein("ropec", [128, NT])
    ropes_in = ein("ropes", [128, NT])
    amask_in = ein("amask", [128, 256])
    finalg_in = ein("final_g", [128, 8])
    csel_in = ein("csel", [128, 255])
    iota16_in = ein("iota16", [128, 16])
    hyz_in = {"L": ein("hyzL", [33, L]), "C": ein("hyzC", [33, C])}
    hytw_in = {"L": ein("hytwL", [128, L // 128]), "C": ein("hytwC", [128, C // 128])}
    hyrate_in = ein("hyrate", [128, 512])
    W = []
    for l in range(DEPTH):
        W.append(dict(mod_w=ein("mod_w%d" % l, [D, 6 * D]), mod_b=ein("mod_b%d" % l, [128, 48]),
                      n1g=ein("n1g%d" % l, [128, 8]), hy_sw=ein("hy_sw%d" % l, [128, 12, 3]), hy_sb=ein("hy_sb%d" % l, [128, 12]),
                      hy_w1=ein("hy_w1%d" % l, [33, 64]), hy_w2=ein("hy_w2%d" % l, [64, 64]), hy_w3=ein("hy_w3%d" % l, [64, 1024]),
                      hy_fb=ein("hy_fb%d" % l, [64, 4]),
                      n2g=ein("n2g%d" % l, [128, 8]), peer_wq=ein("peer_wq%d" % l, [D, 2048]), peer_kT=ein("peer_kT%d" % l, [16, 128, 128]),
                      peer_u=ein("peer_u%d" % l, [16384, D]), peer_v=ein("peer_v%d" % l, [16384, D]),
                      br_attn_w=ein("br_attn_w%d" % l, [512, D]), br_hyena_w=ein("br_hyena_w%d" % l, [512, D]), br_s5_w=ein("br_s5_w%d" % l, [512, D]),
                      out_w=ein("out_w%d" % l, [D, D]),
                      s5_a_re=ein("s5_a_re%d" % l, [128, 2, 16]), s5_a_im=ein("s5_a_im%d" % l, [128, 2, 16]), s5_ldt=ein("s5_ldt%d" % l, [128, 2, 16]),
                      s5_b_re=ein("s5_b_re%d" % l, [2, 2048, 16]), s5_b_im=ein("s5_b_im%d" % l, [2, 2048, 16]),
                      s5_c_re=ein("s5_c_re%d" % l, [2, 2048, 16]), s5_c_im=ein("s5_c_im%d" % l, [2, 2048, 16]),
                      s5_d=ein("s5_d%d" % l, [32, 16]), s5_gw=ein("s5_gw%d" % l, [512, 512]), s5_gb=ein("s5_gb%d" % l, [128, 4]), hy_bias=ein("hy_bias%d" % l, [128, 4]), sink=ein("sink%d" % l, [128, 8]), in_w=ein("in_w%d" % l, [D, IN_W]),
                      in_wr=ein("in_wr%d" % l, [D, 640]), gate_b=ein("gate_b%d" % l, [128, 24])))
    XT = k.dram("XT", [D, NT])
    MODT = k.dram("MODT", [128, 96], kind=okind)
    QT = k.dram("QT", [512, NT], kind=okind)
    KT = k.dram("KT", [128, NT], kind=okind)
    VTOK = k.dram("VTOK", [NT, 128], kind=okind)
    US5 = k.dram("US5", [512, NT], kind=okind)
    ZHY = k.dram("ZHY", [1536, NT], kind=okind)
    GATE = k.dram("GATE", [3072, NT], kind=okind)
    HT = k.dram("HT", [D, NT], kind=okind)
    MIX = k.dram("MIX", [D, NT], kind=okind)
    XDBG = k.dram("XDBG", [D, NT], kind=okind)
    S5Y = k.dram("S5Y", [512, NT], kind=okind)
    S5O = k.dram("S5O", [512, NT], kind=okind)
    HY = k.dram("HY", [512, NT], kind=okind)
    DFT = {}
    for nm, Ls in (("L", L), ("C", C)):
        DFT[nm] = {m_: k.dram("DFT_%s_%s" % (nm, m_), [Ls, Ls]) for m_ in ("CK", "SK", "CS", "SS")}
    HSDd, KRId, VXTd, VXFd, X0Fd = {}, {}, {}, {}, {}
    for nm, Ls in (("L", L), ("C", C)):
        HSDd[nm] = k.dram("HSD" + nm, [2, Ls, 512])
        KRId[nm] = k.dram("KRI" + nm, [2, Ls, 512])
        VXTd[nm] = k.dram("VXT" + nm, [Ls, 512])
        VXFd[nm] = k.dram("VXF" + nm, [512, Ls])
        X0Fd[nm] = k.dram("X0F" + nm, [512, Ls])
    ATT = k.dram("ATT", [512, NT], kind=okind)
    out = k.dram("out", [D, L], kind="ExternalOutput")

    ones = k.sb("ones", [128, 128], persistent=True)
    k.op("pool", lambda e: e.memset(ones[:], 1.0), writes=[ones])
    eps_t = k.sb("eps", [128, 1], persistent=True)
    k.op("pool", lambda e: e.memset(eps_t[:], 1e-6), writes=[eps_t])
    ident = k.sb("ident", [128, 128], persistent=True)
    k.op("pool", lambda e: e.memset(ident[:], 0.0), writes=[ident])
    k.op("pool", lambda e: e.affine_select(out=ident[:], in_=ident[:], pattern=[[-1, 128]],
                                           compare_op=ALU.not_equal, fill=1.0, base=0, channel_multiplier=1),
         reads=[ident], writes=[ident])

    k.phase_begin()
    stage_p = k.pool("stage", [128, NT], 2)
    for kc in range(8):
        st = stage_p()
        k.dma(st[:, 0:C], ctxT_in[kc * 128:(kc + 1) * 128, :], reads=[ctxT_in], writes=[st])
        k.dma(st[:, C:NT], xT_in[kc * 128:(kc + 1) * 128, :], reads=[xT_in], writes=[st])
        k.dma(XT[kc * 128:(kc + 1) * 128, :], st[:], reads=[st], writes=[XT])
    k.phase_end()


    k.phase_begin()
    negpi = k.sb("negpi", [128, 1])
    k.op("pool", lambda e: e.memset(negpi[:], -math.pi), writes=[negpi])
    colv_p = k.pool("colv", [128, 4096], 1, I32)
    pm_p = k.pool("pm", [128, 1], 2, I32)
    ri_p = k.pool("ri", [128, 4096], 2, I32)
    rj_p = k.pool("rj", [128, 4096], 2, I32)
    rf_p = k.pool("rf", [128, 4096], 2)
    go_p = k.pool("go", [128, 4096], 3)
    for nm, Ls in (("L", L), ("C", C)):
        N4 = 8 * Ls
        colv = colv_p()
        k.op("pool", lambda e, colv=colv: e.iota(colv[:, 0:Ls], pattern=[[2, Ls]], base=1, channel_multiplier=0), writes=[colv])
        for tt in range(Ls // 128):
            for shifted in (0, 1):
                pm = pm_p()
                k.op("pool", lambda e, pm=pm: e.iota(pm[:], pattern=[[0, 1]], base=2 * tt * 128 + shifted, channel_multiplier=2), writes=[pm])
                ri = ri_p()
                k.op("dve", lambda e, ri=ri, pm=pm: e.tensor_tensor(out=ri[:, 0:Ls], in0=colv[:, 0:Ls], in1=pm[:, 0:1].to_broadcast([128, Ls]), op=ALU.mult),
                     reads=[colv, pm], writes=[ri])
                for trig in ("S", "C"):
                    rj = rj_p()
                    if trig == "S":
                        k.op("dve", lambda e, ri=ri, rj=rj: e.tensor_single_scalar(rj[:, 0:Ls], ri[:, 0:Ls], N4 - 1, op=ALU.bitwise_and), reads=[ri], writes=[rj])
                    else:
                        k.op("dve", lambda e, ri=ri, rj=rj: e.tensor_single_scalar(rj[:, 0:Ls], ri[:, 0:Ls], N4 // 4, op=ALU.add), reads=[ri], writes=[rj])
                        k.op("dve", lambda e, rj=rj: e.tensor_single_scalar(rj[:, 0:Ls], rj[:, 0:Ls], N4 - 1, op=ALU.bitwise_and), reads=[rj], writes=[rj])
                    rf = rf_p()
                    k.op("pool", lambda e, rf=rf, rj=rj: e.tensor_copy(rf[:, 0:Ls], rj[:, 0:Ls]), reads=[rj], writes=[rf])
                    go = go_p()
                    k.op("act", lambda e, go=go, rf=rf: e.activation(out=go[:, 0:Ls], in_=rf[:, 0:Ls], func=AF.Sin, bias=negpi[:, 0:1], scale=2.0 * math.pi / N4),
                         reads=[rf, negpi], writes=[go])
                    dst = DFT[nm][trig + ("S" if shifted else "K")]
                    k.dma(dst[tt * 128:(tt + 1) * 128, :], go[:, 0:Ls], reads=[go], writes=[dst])
    k.phase_end()

    ORDER = ["0", "A", "B", "C", "D", "E", "F", "G"]
    skip = (lambda ph: start_at is not None and ORDER.index(ph) < ORDER.index(start_at))
    for l in range(n_layers):
        w = W[l]
        need_ctx = l < DEPTH - 1
        k.phase_begin()
        cv = k.sb("cv", [128, 16])
        k.dma(cv[:], cvec_in[:, :], reads=[cvec_in], writes=[cv])
        sl = k.sb("silu", [128, 16])
        k.op("act", lambda e: e.activation(out=sl[:], in_=cv[:], func=AF.Silu), reads=[cv], writes=[sl])
        mb = k.sb("mb", [128, 48])
        k.dma(mb[:], w["mod_b"][:, :], reads=[w["mod_b"]], writes=[mb])
        modt = k.sb("modt", [128, 96])
        mwv = w["mod_w"].t.rearrange("(kc p) n -> p kc n", p=128)
        mw_p = k.pool("mw", [128, 8, 1024], 2)
        for fg in range(6):
            mw = mw_p()
            for kc in range(8):
                k.dma(mw[:, kc, :], mwv[:, kc, fg * 1024:(fg + 1) * 1024], reads=[w["mod_w"]], writes=[mw])
            ps = k.next_ps()
            for fc in range(8):
                for kc in range(8):
                    k.mm(ps, ps[:, fc * 2:fc * 2 + 2], mw, mw[:, kc, fc * 128:(fc + 1) * 128],
                         sl, sl[:, kc * 2:kc * 2 + 2], kc == 0, kc == 7)
            for j in range(2):
                pv = ps.t[:, 0:16].rearrange("p (f j) -> p f j", j=2)[:, :, j]
                ov = modt.t[:, fg * 16:(fg + 1) * 16].rearrange("p (f j) -> p f j", j=2)[:, :, j]
                k.op("dve", lambda e, pv=pv, ov=ov: e.tensor_tensor(out=ov, in0=pv, in1=mb[:, fg * 8:(fg + 1) * 8], op=ALU.add),
                     reads=[ps, mb], writes=[modt])
        k.dma(MODT[:, :], modt[:], reads=[modt], writes=[MODT])
        k.phase_end()
        if stop_after == "A":
            break

        k.phase_begin()
        modt = k.sb("modt", [128, 96])
        k.dma(modt[:], MODT[:, :], reads=[MODT], writes=[modt])
        g1n = k.sb("g1n", [128, 8])
        k.dma(g1n[:], w["n1g"][:, :], reads=[w["n1g"]], writes=[g1n])
        gb = k.sb("gb", [128, 24])
        k.dma(gb[:], w["gate_b"][:, :], reads=[w["gate_b"]], writes=[gb])
        m3 = modt.t[:, :].rearrange("p (f j) -> p f j", j=2)
        Acoef = k.sb("Acoef", [128, 8, 2])
        for j in range(2):
            k.op("dve", lambda e, j=j: e.scalar_tensor_tensor(out=Acoef[:, :, j], in0=m3[:, 8:16, j], scalar=1.0, in1=g1n[:],
                                                             op0=ALU.add, op1=ALU.mult), reads=[modt, g1n], writes=[Acoef])
        iwv = w["in_w"].t.rearrange("(kc p) n -> p kc n", p=128)
        irv = w["in_wr"].t.rearrange("(kc p) n -> p kc n", p=128)
        groups = [("q", 0, 512), ("k", 512, 128), ("v", 640, 128), ("s5", 768, 512)]
        groups += [("hy", 1280 + 512 * i, 512) for i in range(3)] + [("gate", 2816 + 512 * i, 512) for i in range(6)]
        xb_p = k.pool("xb", [128, 8, 512], 1)
        sq_p = k.pool("sq", [128, 8, 512], 1)
        hb_p = k.pool("hb", [128, 8, 512], 1)
        wt_p = k.pool("wt", [128, 8, 512], 4)
        wr_p = k.pool("wr", [128, 8, 512], 2)
        rstd_p = k.pool("rstd", [128, 512], 1)
        rc_p = k.pool("rc", [128, 512], 2)
        rs_p = k.pool("rs", [128, 512], 2)
        ev_p = k.pool("ev", [128, 512], 4)
        e2_p = k.pool("e2", [128, 512], 2)
        vt_p = k.pool("vt", [128, 128], 2)
        for bi, (t0, nt) in enumerate(NBLK):
            j = 1 if bi == 0 else 0
            xb = xb_p()
            for kc in range(8):
                k.dma(xb[:, kc, 0:nt], XT[kc * 128:(kc + 1) * 128, t0:t0 + nt], reads=[XT], writes=[xb])
            sq = sq_p()
            k.op("act", lambda e: e.activation(out=sq[:, :, 0:nt], in_=xb[:, :, 0:nt], func=AF.Square), reads=[xb], writes=[sq])
            pss = k.next_ps()
            for kc in range(8):
                k.mm(pss, pss[:, 0:nt], ones, ones[:], sq, sq[:, kc, 0:nt], kc == 0, kc == 7)
            rstd = rstd_p()
            k.op("act", lambda e: e.activation(out=rstd[:, 0:nt], in_=pss[:, 0:nt], func=AF.Sqrt, bias=eps_t[:, 0:1], scale=1.0 / D),
                 reads=[pss, eps_t], writes=[rstd])
            k.op("dve", lambda e: e.reciprocal(rstd[:, 0:nt], rstd[:, 0:nt]), reads=[rstd], writes=[rstd])
            hb = hb_p()
            for kc in range(8):
                k.op("dve", lambda e, kc=kc: e.tensor_tensor(out=hb[:, kc, 0:nt], in0=xb[:, kc, 0:nt], in1=rstd[:, 0:nt], op=ALU.mult),
                     reads=[xb, rstd], writes=[hb])
                k.op("pool", lambda e, kc=kc: e.tensor_scalar(hb[:, kc, 0:nt], hb[:, kc, 0:nt], Acoef[:, kc, j:j + 1], m3[:, kc, j:j + 1],
                                                              op0=ALU.mult, op1=ALU.add), reads=[hb, Acoef, modt], writes=[hb])
            if dbg:
                for kc in range(8):
                    k.dma(HT[kc * 128:(kc + 1) * 128, t0:t0 + nt], hb[:, kc, 0:nt], reads=[hb], writes=[HT])
            rc = rc_p()
            rs = rs_p()
            k.dma(rc[:, 0:nt], ropec_in[:, t0:t0 + nt], reads=[ropec_in], writes=[rc])
            k.dma(rs[:, 0:nt], ropes_in[:, t0:t0 + nt], reads=[ropes_in], writes=[rs])
            for (kind, c0, ncol) in groups:
                wt = wt_p()
                for kc in range(8):
                    k.dma(wt[:, kc, 0:ncol], iwv[:, kc, c0:c0 + ncol], reads=[w["in_w"]], writes=[wt])
                if kind in ("q", "k"):
                    wr = wr_p()
                    for kc in range(8):
                        k.dma(wr[:, kc, 0:ncol], irv[:, kc, c0:c0 + ncol], reads=[w["in_wr"]], writes=[wr])
                if kind == "v":
                    for ts in range(nt // 128):
                        ps = k.next_ps()
                        for kc in range(8):
                            k.mm(ps, ps[:, 0:128], hb, hb[:, kc, ts * 128:(ts + 1) * 128], wt, wt[:, kc, 0:128], kc == 0, kc == 7)
                        vt = vt_p()
                        k.op("act", lambda e, ps=ps, vt=vt: e.copy(vt[:], ps[:, 0:128]), reads=[ps], writes=[vt])
                        k.dma(VTOK[t0 + ts * 128:t0 + (ts + 1) * 128, :], vt[:], reads=[vt], writes=[VTOK])
                    continue
                for cc in range(ncol // 128):
                    ps = k.next_ps()
                    for kc in range(8):
                        k.mm(ps, ps[:, 0:nt], wt, wt[:, kc, cc * 128:(cc + 1) * 128], hb, hb[:, kc, 0:nt], kc == 0, kc == 7)
                    ev = ev_p()
                    col = c0 + cc * 128
                    if kind in ("q", "k"):
                        ps2 = k.next_ps()
                        for kc in range(8):
                            k.mm(ps2, ps2[:, 0:nt], wr, wr[:, kc, cc * 128:(cc + 1) * 128], hb, hb[:, kc, 0:nt], kc == 0, kc == 7)
                        e2 = e2_p()
                        k.op("dve", lambda e, ps=ps, ev=ev: e.tensor_tensor(out=ev[:, 0:nt], in0=ps[:, 0:nt], in1=rc[:, 0:nt], op=ALU.mult),
                             reads=[ps, rc], writes=[ev])
                        k.op("dve", lambda e, ps2=ps2, e2=e2: e.tensor_tensor(out=e2[:, 0:nt], in0=ps2[:, 0:nt], in1=rs[:, 0:nt], op=ALU.mult),
                             reads=[ps2, rs], writes=[e2])
                        k.op("pool", lambda e, ev=ev, e2=e2: e.tensor_tensor(out=ev[:, 0:nt], in0=ev[:, 0:nt], in1=e2[:, 0:nt], op=ALU.add),
                             reads=[ev, e2], writes=[ev])
                        if kind == "q":
                            k.op("act", lambda e, ev=ev: e.mul(ev[:, 0:nt], ev[:, 0:nt], 0.125), reads=[ev], writes=[ev])
                            dst = QT[col:col + 128, t0:t0 + nt]
                            dres = QT
                        else:
                            dst = KT[0:128, t0:t0 + nt]
                            dres = KT
                    elif kind == "gate":
                        gi = (col - 2816) // 128
                        k.op("act", lambda e, ps=ps, ev=ev, gi=gi: e.activation(out=ev[:, 0:nt], in_=ps[:, 0:nt], func=AF.Sigmoid, bias=gb[:, gi:gi + 1]),
                             reads=[ps, gb], writes=[ev])
                        dst = GATE[col - 2816:col - 2816 + 128, t0:t0 + nt]
                        dres = GATE
                    else:
                        if cc % 2 == 0:
                            k.op("act", lambda e, ps=ps, ev=ev: e.copy(ev[:, 0:nt], ps[:, 0:nt]), reads=[ps], writes=[ev])
                        else:
                            k.op("dve", lambda e, ps=ps, ev=ev: e.tensor_copy(ev[:, 0:nt], ps[:, 0:nt]), reads=[ps], writes=[ev])
                        if kind == "s5":
                            dst = US5[col - 768:col - 768 + 128, t0:t0 + nt]
                            dres = US5
                        else:
                            dst = ZHY[col - 1280:col - 1280 + 128, t0:t0 + nt]
                            dres = ZHY
                    k.dma(dst, ev[:, 0:nt], reads=[ev], writes=[dres])
        k.phase_end()
        if stop_after == "B":
            break

        need_ctx = l < DEPTH - 1
        k.phase_begin()
        amask = k.sb("amask", [128, 256])
        k.dma(amask[:], amask_in[:, :], reads=[amask_in], writes=[amask])
        sinkb = k.sb("sinkb", [128, 8])
        k.dma(sinkb[:], w["sink"][:, :], reads=[w["sink"]], writes=[sinkb])
        kT_p = k.pool("kT", [64, NT], 1)
        v_p = k.pool("vtk", [128, NT // 128, 64], 1)
        qT_p = k.pool("qT", [64, NT], 2)
        ao_p = k.pool("ao", [64, NT], 2)
        S_p = k.pool("S", [128, 640], 3)
        PT_p = k.pool("PT", [128, 5, 128], 2)
        st_p = k.pool("stat", [128, 8], 4)
        vview = VTOK.t.rearrange("(n p) c -> p n c", p=128)
        for kv in range(2):
            kT = kT_p()
            k.dma(kT[:], KT[kv * 64:(kv + 1) * 64, :], reads=[KT], writes=[kT])
            vt = v_p()
            k.dma(vt[:], vview[:, :, kv * 64:(kv + 1) * 64], reads=[VTOK], writes=[vt])
            for hg in range(4):
                hd = kv * 4 + hg
                qT = qT_p()
                k.dma(qT[:], QT[hd * 64:(hd + 1) * 64, :], reads=[QT], writes=[qT])
                ao = ao_p()
                blocks = ([("c", 0), ("c", 1)] if need_ctx else []) + [("l", n) for n in range(32)]
                for (bt, n) in blocks:
                    q0 = n * 128 if bt == "c" else C + n * 128
                    if bt == "c":
                        kts = []
                    else:
                        kts = [j for j in (n - 1, n, n + 1) if 0 <= j < 32]
                    nloc = len(kts) * 128
                    wtot = 256 + nloc
                    S = S_p()
                    psc = k.next_ps()
                    k.mm(psc, psc[:, 0:256], qT, qT[:, q0:q0 + 128], kT, kT[:, 0:256], True, True)
                    k.op("act", lambda e, S=S, psc=psc: e.copy(S[:, 0:256], psc[:, 0:256]), reads=[psc], writes=[S])
                    if nloc:
                        psl = k.next_ps()
                        k0 = C + kts[0] * 128
                        k.mm(psl, psl[:, 0:nloc], qT, qT[:, q0:q0 + 128], kT, kT[:, k0:k0 + nloc], True, True)
                        for ji, j in enumerate(kts):
                            dst = S[:, 256 + ji * 128:256 + (ji + 1) * 128]
                            src = psl[:, ji * 128:(ji + 1) * 128]
                            if j == n:
                                k.op("dve", lambda e, dst=dst, src=src: e.tensor_copy(dst, src), reads=[psl], writes=[S])
                            else:
                                mko = 0 if j < n else 128
                                k.op("dve", lambda e, dst=dst, src=src, mko=mko: e.tensor_tensor(out=dst, in0=src, in1=amask[:, mko:mko + 128], op=ALU.add),
                                     reads=[psl, amask], writes=[S])
                    stt = st_p()
                    k.op("dve", lambda e, S=S, stt=stt: e.tensor_reduce(out=stt[:, 0:1], in_=S[:, 0:wtot], axis=AX.X, op=ALU.max), reads=[S], writes=[stt])
                    k.op("dve", lambda e, stt=stt: e.tensor_tensor(out=stt[:, 0:1], in0=stt[:, 0:1], in1=sinkb[:, hd:hd + 1], op=ALU.max), reads=[stt, sinkb], writes=[stt])
                    k.op("dve", lambda e, stt=stt: e.tensor_scalar(stt[:, 1:2], stt[:, 0:1], -1.0, None, op0=ALU.mult), reads=[stt], writes=[stt])
                    k.op("act", lambda e, S=S, stt=stt: e.activation(out=S[:, 0:wtot], in_=S[:, 0:wtot], func=AF.Exp, bias=stt[:, 1:2], accum_out=stt[:, 2:3]),
                         reads=[S, stt], writes=[S, stt])
                    k.op("act", lambda e, stt=stt: e.activation(out=stt[:, 3:4], in_=sinkb[:, hd:hd + 1], func=AF.Exp, bias=stt[:, 1:2]), reads=[stt, sinkb], writes=[stt])
                    k.op("dve", lambda e, stt=stt: e.tensor_tensor(out=stt[:, 4:5], in0=stt[:, 2:3], in1=stt[:, 3:4], op=ALU.add), reads=[stt], writes=[stt])
                    k.op("dve", lambda e, stt=stt: e.reciprocal(stt[:, 5:6], stt[:, 4:5]), reads=[stt], writes=[stt])
                    k.op("dve", lambda e, S=S, stt=stt: e.tensor_scalar(S[:, 0:wtot], S[:, 0:wtot], stt[:, 5:6], None, op0=ALU.mult), reads=[S, stt], writes=[S])
                    nkt = wtot // 128
                    PT = PT_p()
                    for ti in range(nkt):
                        pst = k.next_ps()
                        k.op("pe", lambda e, pst=pst, S=S, ti=ti: e.transpose(pst[:, 0:128], S[:, ti * 128:(ti + 1) * 128], ident[:]),
                             reads=[S, ident], writes=[pst])
                        if ti % 2 == 0:
                            k.op("act", lambda e, PT=PT, pst=pst, ti=ti: e.copy(PT[:, ti, :], pst[:, 0:128]), reads=[pst], writes=[PT])
                        else:
                            k.op("dve", lambda e, PT=PT, pst=pst, ti=ti: e.tensor_copy(PT[:, ti, :], pst[:, 0:128]), reads=[pst], writes=[PT])
                    pso = k.next_ps()
                    vtiles = [0, 1] + [2 + j for j in kts]
                    for ti, vti in enumerate(vtiles):
                        k.mm(pso, pso[0:64, 0:128], vt, vt[:, vti, :], PT, PT[:, ti, :], ti == 0, ti == nkt - 1)
                    k.op("act", lambda e, ao=ao, pso=pso, q0=q0: e.copy(ao[:, q0:q0 + 128], pso[0:64, 0:128]), reads=[pso], writes=[ao])
                lo = 0 if need_ctx else C
                k.dma(ATT[hd * 64:(hd + 1) * 64, lo:NT], ao[:, lo:NT], reads=[ao], writes=[ATT])
        k.phase_end()
        if stop_after == "C":
            break

        k.phase_begin()
        fb = k.sb("hy_fb", [64, 4])
        k.dma(fb[:], w["hy_fb"][:, :], reads=[w["hy_fb"]], writes=[fb])
        fbb = k.sb("hy_fbb", [64, 2])
        k.op("dve", lambda e: e.tensor_tensor(out=fbb[:, 0:1], in0=fb[:, 0:1], in1=fb[:, 1:2], op=ALU.mult), reads=[fb], writes=[fbb])
        k.op("dve", lambda e: e.tensor_tensor(out=fbb[:, 1:2], in0=fb[:, 2:3], in1=fb[:, 3:4], op=ALU.mult), reads=[fb], writes=[fbb])
        w1 = k.sb("hy_w1", [33, 64])
        k.dma(w1[:], w["hy_w1"][:, :], reads=[w["hy_w1"]], writes=[w1])
        w2 = k.sb("hy_w2", [64, 64])
        k.dma(w2[:], w["hy_w2"][:, :], reads=[w["hy_w2"]], writes=[w2])
        w3 = k.sb("hy_w3", [64, 1024])
        k.dma(w3[:], w["hy_w3"][:, :], reads=[w["hy_w3"]], writes=[w3])
        rate = k.sb("hy_rate", [128, 512])
        k.dma(rate[:], hyrate_in[:, :], reads=[hyrate_in], writes=[rate])
        sw = k.sb("hy_sw", [128, 12, 3])
        k.dma(sw[:], w["hy_sw"][:, :, :], reads=[w["hy_sw"]], writes=[sw])
        sbias = k.sb("hy_sb", [128, 12])
        k.dma(sbias[:], w["hy_sb"][:, :], reads=[w["hy_sb"]], writes=[sbias])
        hbias = k.sb("hy_bias", [128, 4])
        k.dma(hbias[:], w["hy_bias"][:, :], reads=[w["hy_bias"]], writes=[hbias])
        m0 = k.sb("m0", [128, 1])
        k.op("pool", lambda e: e.memset(m0[:], 1.0), writes=[m0])
        k.op("pool", lambda e: e.affine_select(out=m0[:], in_=m0[:], pattern=[[0, 1]], compare_op=ALU.not_equal, fill=0.0, base=0, channel_multiplier=1),
             reads=[m0], writes=[m0])
        negpi = k.sb("negpi2", [128, 1])
        k.op("pool", lambda e: e.memset(negpi[:], -math.pi), writes=[negpi])

        def sin_mlp(dst_t, dst, ps, fcol, bcol, n, tmp_p, tmpi_p):
            a = tmp_p()
            k.op("act", lambda e: e.activation(out=a[0:64, 0:n], in_=ps[0:64, 0:n], func=AF.Identity, bias=fbb[:, bcol:bcol + 1], scale=fb[:, fcol:fcol + 1]),
                 reads=[ps, fb, fbb], writes=[a])
            ki = tmpi_p()
            k.op("dve", lambda e: e.tensor_scalar(ki[0:64, 0:n], a[0:64, 0:n], 1.0 / (2.0 * math.pi), None, op0=ALU.mult), reads=[a], writes=[ki])
            kf = tmp_p()
            k.op("dve", lambda e: e.tensor_copy(kf[0:64, 0:n], ki[0:64, 0:n]), reads=[ki], writes=[kf])
            k.op("dve", lambda e: e.scalar_tensor_tensor(out=a[0:64, 0:n], in0=kf[0:64, 0:n], scalar=-2.0 * math.pi, in1=a[0:64, 0:n], op0=ALU.mult, op1=ALU.add),
                 reads=[kf, a], writes=[a])
            k.op("dve", lambda e: e.tensor_scalar(a[0:64, 0:n], a[0:64, 0:n], 3.1415925, -3.1415925, op0=ALU.min, op1=ALU.max), reads=[a], writes=[a])
            k.op("act", lambda e: e.activation(out=dst, in_=a[0:64, 0:n], func=AF.Sin), reads=[a], writes=[dst_t])

        seqs = [("L", L, C)] + ([("C", C, 0)] if need_ctx else [])
        tmp_p = k.pool("hy_tmp", [128, 512], 3)
        tmpi_p = k.pool("hy_tmpi", [128, 512], 2, I32)
        h2T_p = k.pool("hy_h2T", [64, 512], 2)
        h1T_p = k.pool("hy_h1T", [64, 512], 2)
        zf_p = k.pool("hy_zf", [33, 512], 2)
        tw_p = k.pool("hy_tw", [128, 32], 1)
        win_p = k.pool("hy_win", [128, 512], 2)
        hfb_p = k.pool("hy_hfb", [128, 2, 512], 2)
        hsd_p = k.pool("hy_hsd", [128, 2, 512], 2)
        zc_p = k.pool("hy_zc", [128, 3, 512 + 2], 2)
        zo_p = k.pool("hy_zo", [128, 3, 512], 2)
        vxt_p = k.pool("hy_vxt", [128, 512], 2)
        for (nm, Ls, toff) in seqs:
            HSD, VXT, VXF, X0F = HSDd[nm], VXTd[nm], VXFd[nm], X0Fd[nm]
            ntile = Ls // 128
            nb = max(1, Ls // 512)
            bw = min(512, Ls)
            tw = tw_p()
            k.dma(tw[:, 0:ntile], hytw_in[nm][:, :], reads=[hytw_in[nm]], writes=[tw])
            for bi in range(nb):
                zf = zf_p()
                k.dma(zf[:, 0:bw], hyz_in[nm][:, bi * bw:(bi + 1) * bw], reads=[hyz_in[nm]], writes=[zf])
                ps1 = k.next_ps()
                k.mm(ps1, ps1[0:64, 0:bw], w1, w1[:, :], zf, zf[:, 0:bw], True, True)
                h1T = h1T_p()
                sin_mlp(h1T, h1T[:, 0:bw], ps1, 1, 0, bw, tmp_p, tmpi_p)
                ps2 = k.next_ps()
                k.mm(ps2, ps2[0:64, 0:bw], w2, w2[:, :], h1T, h1T[:, 0:bw], True, True)
                h2T = h2T_p()
                sin_mlp(h2T, h2T[:, 0:bw], ps2, 3, 1, bw, tmp_p, tmpi_p)
                for ts in range(bw // 128):
                    tt = bi * (bw // 128) + ts
                    win = win_p()
                    k.op("act", lambda e, win=win, tt=tt: e.activation(out=win[:], in_=rate[:], func=AF.Exp, scale=tw[:, tt:tt + 1]), reads=[rate, tw], writes=[win])
                    hfb = hfb_p()
                    for d in range(2):
                        psf = k.next_ps()
                        k.mm(psf, psf[:, :], h2T, h2T[:, ts * 128:(ts + 1) * 128], w3, w3[:, d * 512:(d + 1) * 512], True, True)
                        k.op("dve", lambda e, hfb=hfb, psf=psf, win=win, d=d: e.tensor_tensor(out=hfb[:, d, :], in0=psf[:, :], in1=win[:], op=ALU.mult),
                             reads=[psf, win], writes=[hfb])
                    if tt == 0:
                        k.op("dve", lambda e, hfb=hfb: e.tensor_scalar(hfb[:, 1, :], hfb[:, 1, :], m0[:, 0:1], None, op0=ALU.mult), reads=[hfb, m0], writes=[hfb])
                    hsd = hsd_p()
                    k.op("dve", lambda e, hsd=hsd, hfb=hfb: e.tensor_tensor(out=hsd[:, 0, :], in0=hfb[:, 0, :], in1=hfb[:, 1, :], op=ALU.add), reads=[hfb], writes=[hsd])
                    k.op("pool", lambda e, hsd=hsd, hfb=hfb: e.tensor_tensor(out=hsd[:, 1, :], in0=hfb[:, 0, :], in1=hfb[:, 1, :], op=ALU.subtract), reads=[hfb], writes=[hsd])
                    for d in range(2):
                        k.dma(HSD[d, tt * 128:(tt + 1) * 128, :], hsd[:, d, :], reads=[hsd], writes=[HSD])
            for cc in range(4):
                for bi in range(nb):
                    zc = zc_p()
                    k.op("pool", lambda e, zc=zc: e.memset(zc[:], 0.0), writes=[zc])
                    lo = bi * bw
                    a0 = max(lo - 1, 0)
                    a1 = min(lo + bw + 1, Ls)
                    for pj in range(3):
                        r0 = pj * 512 + cc * 128
                        k.dma(zc[:, pj, (a0 - (lo - 1)):(a1 - (lo - 1))], ZHY[r0:r0 + 128, toff + a0:toff + a1], reads=[ZHY], writes=[zc])
                    zo = zo_p()
                    for pj in range(3):
                        ci = pj * 4 + cc
                        k.op("dve", lambda e, zo=zo, zc=zc, pj=pj, ci=ci: e.tensor_scalar(zo[:, pj, 0:bw], zc[:, pj, 0:bw], sw[:, ci, 0:1], sbias[:, ci:ci + 1], op0=ALU.mult, op1=ALU.add),
                             reads=[zc, sw, sbias], writes=[zo])
                        for tap in (1, 2):
                            k.op("dve", lambda e, zo=zo, zc=zc, pj=pj, ci=ci, tap=tap: e.scalar_tensor_tensor(out=zo[:, pj, 0:bw], in0=zc[:, pj, tap:tap + bw], scalar=sw[:, ci, tap:tap + 1],
                                                                                                                in1=zo[:, pj, 0:bw], op0=ALU.mult, op1=ALU.add), reads=[zc, sw, zo], writes=[zo])
                    k.op("pool", lambda e, zo=zo: e.tensor_tensor(out=zo[:, 2, 0:bw], in0=zo[:, 2, 0:bw], in1=zo[:, 1, 0:bw], op=ALU.mult), reads=[zo], writes=[zo])
                    k.dma(X0F[cc * 128:(cc + 1) * 128, lo:lo + bw], zo[:, 0, 0:bw], reads=[zo], writes=[X0F])
                    k.dma(VXF[cc * 128:(cc + 1) * 128, lo:lo + bw], zo[:, 2, 0:bw], reads=[zo], writes=[VXF])
                    for ts in range(bw // 128):
                        pst = k.next_ps()
                        k.op("pe", lambda e, pst=pst, zo=zo, ts=ts: e.transpose(pst[:, 0:128], zo[:, 2, ts * 128:(ts + 1) * 128], ident[:]), reads=[zo, ident], writes=[pst])
                        vxt = vxt_p()
                        k.op("act", lambda e, vxt=vxt, pst=pst: e.copy(vxt[:, 0:128], pst[:, 0:128]), reads=[pst], writes=[vxt])
                        t0_ = lo + ts * 128
                        k.dma(VXT[t0_:t0_ + 128, cc * 128:(cc + 1) * 128], vxt[:, 0:128], reads=[vxt], writes=[VXT])
        k.phase_end()
        for (nm, Ls, toff) in seqs:
            k.phase_begin()
            HSD, KRI, VXT, VXF, X0F = HSDd[nm], KRId[nm], VXTd[nm], VXFd[nm], X0Fd[nm]
            ntile = Ls // 128
            nb = max(1, Ls // 512)
            bw = min(512, Ls)
            hbias = k.sb("hy_bias", [128, 4])
            k.dma(hbias[:], w["hy_bias"][:, :], reads=[w["hy_bias"]], writes=[hbias])
            Mx = DFT[nm]
            src_p = k.pool("hy_src_" + nm, [128, ntile, 256], 1)
            Y_p = k.pool("hy_Y_" + nm, [128, ntile, 2, 256], 1)
            slab_p = k.pool("hy_slab_" + nm, [128, min(8, ntile), 2, 128], 2)
            kri_p = k.pool("hy_kri_" + nm, [128, 2, 256], 2)
            pw_p = k.pool("hy_pw_" + nm, [128, 4, 256], 2)
            mt_p = k.pool("hy_mt_" + nm, [128, 2, 512], 3)
            ep_p = k.pool("hy_ep_" + nm, [128, 3, 512], 2)
            tg = min(8, ntile)
            for stage in ("kern", "conv"):
                for hh in range(2):
                    cs = slice(hh * 256, (hh + 1) * 256)
                    if stage == "kern":
                        mats = (Mx["CK"], Mx["SK"])
                        for d in range(2):
                            src = src_p()
                            k.dma(src[:, :, :], HSD[d, 0:Ls, cs].rearrange("(n p) c -> p n c", p=128), reads=[HSD], writes=[src])
                            for ft in range(ntile):
                                psK = k.next_ps()
                                for g in range(ntile // tg):
                                    slab = slab_p()
                                    k.dma(slab[:, :, 0, :], mats[d][g * tg * 128:(g + 1) * tg * 128, ft * 128:(ft + 1) * 128].rearrange("(n p) f -> p n f", p=128),
                                          reads=[mats[d]], writes=[slab])
                                    for ti in range(tg):
                                        tt = g * tg + ti
                                        k.mm(psK, psK[:, 0:256], slab, slab[:, ti, 0, :], src, src[:, tt, :], tt == 0, tt == ntile - 1)
                                kri = kri_p()
                                k.op("act", lambda e, kri=kri, psK=psK: e.copy(kri[:, 0, :], psK[:, 0:256]), reads=[psK], writes=[kri])
                                k.dma(KRI[d, ft * 128:(ft + 1) * 128, cs], kri[:, 0, :], reads=[kri], writes=[KRI])
                        continue
                    mats = (Mx["CS"], Mx["SS"])
                    src = src_p()
                    k.dma(src[:, :, :], VXT[0:Ls, cs].rearrange("(n p) c -> p n c", p=128), reads=[VXT], writes=[src])
                    Y = Y_p()
                    for ft in range(ntile):
                        psR = k.next_ps()
                        psI = k.next_ps()
                        for g in range(ntile // tg):
                            slab = slab_p()
                            for d in range(2):
                                k.dma(slab[:, :, d, :], mats[d][g * tg * 128:(g + 1) * tg * 128, ft * 128:(ft + 1) * 128].rearrange("(n p) f -> p n f", p=128),
                                      reads=[mats[d]], writes=[slab])
                            for ti in range(tg):
                                tt = g * tg + ti
                                k.mm(psR, psR[:, 0:256], slab, slab[:, ti, 0, :], src, src[:, tt, :], tt == 0, tt == ntile - 1)
                                k.mm(psI, psI[:, 0:256], slab, slab[:, ti, 1, :], src, src[:, tt, :], tt == 0, tt == ntile - 1)
                        kri = kri_p()
                        for d in range(2):
                            k.dma(kri[:, d, :], KRI[d, ft * 128:(ft + 1) * 128, cs], reads=[KRI], writes=[kri])
                        pw = pw_p()
                        k.op("dve", lambda e, pw=pw, psR=psR, kri=kri: e.tensor_tensor(out=pw[:, 0, :], in0=psR[:, 0:256], in1=kri[:, 0, :], op=ALU.mult), reads=[psR, kri], writes=[pw])
                        k.op("dve", lambda e, pw=pw, psI=psI, kri=kri: e.tensor_tensor(out=pw[:, 1, :], in0=psI[:, 0:256], in1=kri[:, 1, :], op=ALU.mult), reads=[psI, kri], writes=[pw])
                        k.op("dve", lambda e, pw=pw, psR=psR, kri=kri: e.tensor_tensor(out=pw[:, 2, :], in0=psR[:, 0:256], in1=kri[:, 1, :], op=ALU.mult), reads=[psR, kri], writes=[pw])
                        k.op("dve", lambda e, pw=pw, psI=psI, kri=kri: e.tensor_tensor(out=pw[:, 3, :], in0=psI[:, 0:256], in1=kri[:, 0, :], op=ALU.mult), reads=[psI, kri], writes=[pw])
                        k.op("pool", lambda e, Y=Y, pw=pw, ft=ft: e.tensor_tensor(out=Y[:, ft, 0, :], in0=pw[:, 0, :], in1=pw[:, 1, :], op=ALU.subtract), reads=[pw], writes=[Y])
                        k.op("pool", lambda e, Y=Y, pw=pw, ft=ft: e.tensor_tensor(out=Y[:, ft, 1, :], in0=pw[:, 2, :], in1=pw[:, 3, :], op=ALU.add), reads=[pw], writes=[Y])
                    if stage == "conv":
                        Nfft = 2 * Ls
                        for nbk in range(nb):
                            pso = [k.next_ps(), k.next_ps()]
                            for ft in range(ntile):
                                mt = mt_p()
                                for d in range(2):
                                    k.dma(mt[:, d, 0:bw], mats[d][ft * 128:(ft + 1) * 128, nbk * bw:(nbk + 1) * bw], reads=[mats[d]], writes=[mt])
                                for c2 in range(2):
                                    for d in range(2):
                                        k.mm(pso[c2], pso[c2][:, 0:bw], Y, Y[:, ft, d, c2 * 128:(c2 + 1) * 128], mt, mt[:, d, 0:bw], ft == 0 and d == 0, ft == ntile - 1 and d == 1)
                            for c2 in range(2):
                                ch = hh * 2 + c2
                                ep = ep_p()
                                k.dma(ep[:, 0, 0:bw], VXF[ch * 128:(ch + 1) * 128, nbk * bw:(nbk + 1) * bw], reads=[VXF], writes=[ep])
                                k.dma(ep[:, 1, 0:bw], X0F[ch * 128:(ch + 1) * 128, nbk * bw:(nbk + 1) * bw], reads=[X0F], writes=[ep])
                                k.op("dve", lambda e, ep=ep, ch=ch: e.tensor_scalar(ep[:, 0, 0:bw], ep[:, 0, 0:bw], hbias[:, ch:ch + 1], None, op0=ALU.mult), reads=[ep, hbias], writes=[ep])
                                k.op("dve", lambda e, ep=ep, c2=c2: e.scalar_tensor_tensor(out=ep[:, 2, 0:bw], in0=pso[c2][:, 0:bw], scalar=-2.0 / Nfft, in1=ep[:, 0, 0:bw], op0=ALU.mult, op1=ALU.add),
                                     reads=[pso[c2], ep], writes=[ep])
                                k.op("pool", lambda e, ep=ep: e.tensor_tensor(out=ep[:, 2, 0:bw], in0=ep[:, 2, 0:bw], in1=ep[:, 1, 0:bw], op=ALU.mult), reads=[ep], writes=[ep])
                                k.dma(HY[ch * 128:(ch + 1) * 128, toff + nbk * bw:toff + (nbk + 1) * bw], ep[:, 2, 0:bw], reads=[ep], writes=[HY])
                k.barrier()
            k.phase_end()
        if stop_after == "D":
            break

        k.phase_begin()
        TWO_PI = 2.0 * math.pi
        P32 = [128, 2, 16]
        a_re = k.sb("a_re", P32); a_im = k.sb("a_im", P32); ldt = k.sb("ldt", P32)
        for t_, src_ in ((a_re, w["s5_a_re"]), (a_im, w["s5_a_im"]), (ldt, w["s5_ldt"])):
            k.dma(t_[:], src_[:, :, :], reads=[src_], writes=[t_])
        prm = k.sb("s5prm", [128, 12, 32])
        fl = lambda t_: t_.t[:, :, :].rearrange("p a b -> p (a b)")
        R_ = lambda i: prm[:, i, :]
        ki32 = k.sb("ki32", [128, 32], I32)
        kf32 = k.sb("kf32", [128, 32])
        hpi = k.sb("hpi", [128, 1])
        k.op("pool", lambda e: e.memset(hpi[:], 0.0), writes=[hpi])

        def reduce_angle(dst, src, shift):
            k.op("dve", lambda e: e.tensor_scalar(R_(8), src, shift, None, op0=ALU.add), reads=[prm], writes=[prm])
            k.op("dve", lambda e: e.tensor_scalar(ki32[:], R_(8), 1.0 / TWO_PI, None, op0=ALU.mult), reads=[prm], writes=[ki32])
            k.op("dve", lambda e: e.tensor_copy(kf32[:], ki32[:]), reads=[ki32], writes=[kf32])
            k.op("dve", lambda e: e.scalar_tensor_tensor(out=dst, in0=kf32[:], scalar=-TWO_PI, in1=R_(8), op0=ALU.mult, op1=ALU.add), reads=[kf32, prm], writes=[prm])
            k.op("dve", lambda e: e.tensor_scalar(dst, dst, 3.1415925, -3.1415925, op0=ALU.min, op1=ALU.max), reads=[prm], writes=[prm])

        k.op("dve", lambda e: e.tensor_scalar(R_(0), fl(a_re), -1e-4, None, op0=ALU.min), reads=[a_re], writes=[prm])
        k.op("act", lambda e: e.activation(out=R_(1), in_=fl(ldt), func=AF.Exp), reads=[ldt], writes=[prm])
        k.op("dve", lambda e: e.tensor_tensor(out=R_(9), in0=R_(0), in1=R_(1), op=ALU.mult), reads=[prm], writes=[prm])
        k.op("act", lambda e: e.activation(out=R_(3), in_=R_(9), func=AF.Exp), reads=[prm], writes=[prm])
        k.op("dve", lambda e: e.tensor_tensor(out=R_(10), in0=fl(a_im), in1=R_(1), op=ALU.mult), reads=[prm, a_im], writes=[prm])
        reduce_angle(R_(2), R_(10), 0.0)
        k.op("act", lambda e: e.activation(out=R_(5), in_=R_(2), func=AF.Sin), reads=[prm], writes=[prm])
        reduce_angle(R_(11), R_(10), math.pi / 2.0)
        k.op("act", lambda e: e.activation(out=R_(4), in_=R_(11), func=AF.Sin), reads=[prm], writes=[prm])
        k.op("dve", lambda e: e.tensor_tensor(out=R_(8), in0=R_(3), in1=R_(4), op=ALU.mult), reads=[prm], writes=[prm])
        k.op("dve", lambda e: e.tensor_scalar(R_(8), R_(8), -1.0, None, op0=ALU.add), reads=[prm], writes=[prm])
        k.op("dve", lambda e: e.tensor_tensor(out=R_(9), in0=R_(3), in1=R_(5), op=ALU.mult), reads=[prm], writes=[prm])
        k.op("dve", lambda e: e.tensor_tensor(out=R_(10), in0=R_(0), in1=R_(0), op=ALU.mult), reads=[prm], writes=[prm])
        k.op("dve", lambda e: e.tensor_tensor(out=R_(11), in0=fl(a_im), in1=fl(a_im), op=ALU.mult), reads=[a_im], writes=[prm])
        k.op("dve", lambda e: e.tensor_tensor(out=R_(10), in0=R_(10), in1=R_(11), op=ALU.add), reads=[prm], writes=[prm])
        k.op("dve", lambda e: e.reciprocal(R_(10), R_(10)), reads=[prm], writes=[prm])
        k.op("dve", lambda e: e.tensor_tensor(out=R_(6), in0=R_(8), in1=R_(0), op=ALU.mult), reads=[prm], writes=[prm])
        k.op("dve", lambda e: e.tensor_tensor(out=R_(11), in0=R_(9), in1=fl(a_im), op=ALU.mult), reads=[prm, a_im], writes=[prm])
        k.op("dve", lambda e: e.tensor_tensor(out=R_(6), in0=R_(6), in1=R_(11), op=ALU.add), reads=[prm], writes=[prm])
        k.op("dve", lambda e: e.tensor_tensor(out=R_(6), in0=R_(6), in1=R_(10), op=ALU.mult), reads=[prm], writes=[prm])
        k.op("dve", lambda e: e.tensor_tensor(out=R_(7), in0=R_(9), in1=R_(0), op=ALU.mult), reads=[prm], writes=[prm])
        k.op("dve", lambda e: e.tensor_tensor(out=R_(11), in0=R_(8), in1=fl(a_im), op=ALU.mult), reads=[prm, a_im], writes=[prm])
        k.op("dve", lambda e: e.tensor_tensor(out=R_(7), in0=R_(7), in1=R_(11), op=ALU.subtract), reads=[prm], writes=[prm])
        k.op("dve", lambda e: e.tensor_tensor(out=R_(7), in0=R_(7), in1=R_(10), op=ALU.mult), reads=[prm], writes=[prm])

        tio = k.sb("tio", [128, 513])
        k.op("pool", lambda e: e.iota(tio[:], pattern=[[1, 513]], base=0, channel_multiplier=0, allow_small_or_imprecise_dtypes=True), writes=[tio])
        s5d = k.sb("s5d", [32, 16])
        k.dma(s5d[:], w["s5_d"][:, :], reads=[w["s5_d"]], writes=[s5d])
        chunks = [(0, 256)] + [(256 + 512 * i, 512) for i in range(8)]
        order = {0: list(range(9)), 1: [0] + list(range(8, 0, -1))}
        u_p = k.pool("s5u", [32, NT], 2)
        ya_p = k.pool("s5ya", [32, NT], 4)
        bc_p = k.pool("s5bc", [128, 4, 16], 2)
        B2_p = k.pool("s5B2", [128, 2, 32], 2)
        BT_p = k.pool("s5BT", [32, 2, 128], 2)
        CL_p = k.pool("s5CL", [128, 2, 32], 2)
        tb_p = k.pool("s5tb", [128, 2, 513], 2)
        ang_p = k.pool("s5ang", [128, 513], 4)
        angi_p = k.pool("s5angi", [128, 513], 2, I32)
        wk_p = k.pool("s5wk", [128, 512], 18)
        g_p = k.pool("s5g", [128, 2, 512], 4)
        h_p = k.pool("s5h", [128, 2, 512], 4)
        ini_p = k.pool("s5ini", [128, 4], 6)
        yo_p = k.pool("s5yo", [32, 512], 3)

        def rev(t_, p0, p1, a, n):
            b_ = t_.t[p0:p1, a + n - 1:a + n]
            return bass.AP(tensor=b_.tensor, offset=b_.offset, ap=[list(b_.ap[0]), [-1, n]])

        for st in range(16):
            u = u_p()
            k.dma(u[:], US5[st * 32:(st + 1) * 32, :], reads=[US5], writes=[u])
            yad = [ya_p(), ya_p()]
            ctxd = {}
            for d in range(2):
                col = d * 16 + st
                bc = bc_p()
                for i_, nm_ in enumerate(("s5_b_re", "s5_b_im", "s5_c_re", "s5_c_im")):
                    k.dma(bc[:, i_, :], w[nm_][d, st * 128:(st + 1) * 128, :], reads=[w[nm_]], writes=[bc])
                B2 = B2_p()
                CL = CL_p()
                k.op("pool", lambda e, B2=B2: e.memset(B2[:], 0.0), writes=[B2])
                k.op("pool", lambda e, CL=CL: e.memset(CL[:], 0.0), writes=[CL])
                wk = wk_p()
                for gl in range(2):
                    ps_ = slice(gl * 64, (gl + 1) * 64)
                    cs_ = slice(gl * 16, (gl + 1) * 16)
                    kr = prm[ps_, 6, col:col + 1]
                    kim = prm[ps_, 7, col:col + 1]
                    k.op("dve", lambda e, wk=wk, bc=bc, ps_=ps_, kim=kim: e.tensor_scalar(wk[ps_, 0:16], bc[ps_, 1, :], kim, None, op0=ALU.mult), reads=[bc, prm], writes=[wk])
                    k.op("dve", lambda e, wk=wk, bc=bc, ps_=ps_, kim=kim: e.tensor_scalar(wk[ps_, 16:32], bc[ps_, 0, :], kim, None, op0=ALU.mult), reads=[bc, prm], writes=[wk])
                    k.op("dve", lambda e, B2=B2, wk=wk, bc=bc, ps_=ps_, cs_=cs_, kr=kr: e.scalar_tensor_tensor(out=B2[ps_, 0, cs_], in0=bc[ps_, 0, :], scalar=kr, in1=wk[ps_, 0:16], op0=ALU.mult, op1=ALU.subtract),
                         reads=[bc, prm, wk], writes=[B2])
                    k.op("dve", lambda e, B2=B2, wk=wk, bc=bc, ps_=ps_, cs_=cs_, kr=kr: e.scalar_tensor_tensor(out=B2[ps_, 1, cs_], in0=bc[ps_, 1, :], scalar=kr, in1=wk[ps_, 16:32], op0=ALU.mult, op1=ALU.add),
                         reads=[bc, prm, wk], writes=[B2])
                    k.op("dve", lambda e, CL=CL, bc=bc, ps_=ps_, cs_=cs_: e.tensor_copy(CL[ps_, 0, cs_], bc[ps_, 2, :]), reads=[bc], writes=[CL])
                    k.op("dve", lambda e, CL=CL, bc=bc, ps_=ps_, cs_=cs_: e.tensor_scalar(CL[ps_, 1, cs_], bc[ps_, 3, :], -1.0, None, op0=ALU.mult), reads=[bc], writes=[CL])
                BT = BT_p()
                for ri in range(2):
                    pst = k.next_ps()
                    k.op("pe", lambda e, pst=pst, B2=B2, ri=ri: e.transpose(pst[0:32, 0:128], B2[:, ri, :], ident[:]), reads=[B2, ident], writes=[pst])
                    k.op("act", lambda e, BT=BT, pst=pst, ri=ri: e.copy(BT[:, ri, :], pst[0:32, 0:128]), reads=[pst], writes=[BT])
                tb = tb_p()
                for ti_, shift in ((1, 0.0), (0, math.pi / 2.0)):
                    ang = ang_p()
                    k.op("dve", lambda e, ang=ang, shift=shift: e.tensor_scalar(ang[:], tio[:], prm[:, 2, col:col + 1], shift, op0=ALU.mult, op1=ALU.add), reads=[tio, prm], writes=[ang])
                    angi = angi_p()
                    k.op("dve", lambda e, ang=ang, angi=angi: e.tensor_scalar(angi[:], ang[:], 1.0 / TWO_PI, None, op0=ALU.mult), reads=[ang], writes=[angi])
                    angf = ang_p()
                    k.op("pool", lambda e, angf=angf, angi=angi: e.tensor_copy(angf[:], angi[:]), reads=[angi], writes=[angf])
                    k.op("dve", lambda e, ang=ang, angf=angf: e.scalar_tensor_tensor(out=ang[:], in0=angf[:], scalar=-TWO_PI, in1=ang[:], op0=ALU.mult, op1=ALU.add), reads=[angf, ang], writes=[ang])
                    k.op("dve", lambda e, ang=ang: e.tensor_scalar(ang[:], ang[:], 3.1415925, -3.1415925, op0=ALU.min, op1=ALU.max), reads=[ang], writes=[ang])
                    k.op("act", lambda e, tb=tb, ang=ang, ti_=ti_: e.activation(out=tb[:, ti_, :], in_=ang[:], func=AF.Sin), reads=[ang], writes=[tb])
                ctxd[d] = dict(col=col, BT=BT, CL=CL, tb=tb, rho_b=prm[:, 3, col:col + 1], prev=None)
            def chunk_gen(d, step):
                cx = ctxd[d]
                col, BT, CL, tb, rho_b, prev, ya = cx["col"], cx["BT"], cx["CL"], cx["tb"], cx["rho_b"], cx["prev"], yad[d]
                ci = order[d][step]
                c0, n = chunks[ci]
                rhs_u = u[:, c0:c0 + n] if d == 0 else rev(u, 0, 32, c0, n)
                pbr = k.next_ps()
                pbi = k.next_ps()
                k.mm(pbr, pbr[:, 0:n], BT, BT[:, 0, :], u, rhs_u, True, True)
                yield
                k.mm(pbi, pbi[:, 0:n], BT, BT[:, 1, :], u, rhs_u, True, True)
                yield
                cosT = tb[:, 0, 0:n]
                sinT = tb[:, 1, 0:n]
                w0, w1, w2, w3_ = wk_p(), wk_p(), wk_p(), wk_p()
                k.op("dve", lambda e, w0=w0, pbr=pbr, cosT=cosT: e.tensor_tensor(out=w0[:, 0:n], in0=pbr[:, 0:n], in1=cosT, op=ALU.mult), reads=[pbr, tb], writes=[w0])
                yield
                k.op("dve", lambda e, w1=w1, pbi=pbi, sinT=sinT: e.tensor_tensor(out=w1[:, 0:n], in0=pbi[:, 0:n], in1=sinT, op=ALU.mult), reads=[pbi, tb], writes=[w1])
                yield
                k.op("dve", lambda e, w2=w2, pbi=pbi, cosT=cosT: e.tensor_tensor(out=w2[:, 0:n], in0=pbi[:, 0:n], in1=cosT, op=ALU.mult), reads=[pbi, tb], writes=[w2])
                yield
                k.op("dve", lambda e, w3_=w3_, pbr=pbr, sinT=sinT: e.tensor_tensor(out=w3_[:, 0:n], in0=pbr[:, 0:n], in1=sinT, op=ALU.mult), reads=[pbr, tb], writes=[w3_])
                yield
                k.op("pool", lambda e, w0=w0, w1=w1: e.tensor_tensor(out=w0[:, 0:n], in0=w0[:, 0:n], in1=w1[:, 0:n], op=ALU.add), reads=[w0, w1], writes=[w0])
                yield
                k.op("pool", lambda e, w2=w2, w3_=w3_: e.tensor_tensor(out=w2[:, 0:n], in0=w2[:, 0:n], in1=w3_[:, 0:n], op=ALU.subtract), reads=[w2, w3_], writes=[w2])
                yield
                ini = ini_p()
                if prev is None:
                    k.op("pool", lambda e, ini=ini: e.memset(ini[:], 0.0), writes=[ini])
                    yield
                else:
                    gp, npv = prev
                    cr = tb[:, 0, npv:npv + 1]
                    sr = tb[:, 1, npv:npv + 1]
                    k.op("dve", lambda e, ini=ini, gp=gp, npv=npv, cr=cr: e.tensor_tensor(out=ini[:, 2:3], in0=gp[:, 0, npv - 1:npv], in1=cr, op=ALU.mult), reads=[gp, tb], writes=[ini])
                    yield
                    k.op("dve", lambda e, ini=ini, gp=gp, npv=npv, sr=sr: e.tensor_tensor(out=ini[:, 3:4], in0=gp[:, 1, npv - 1:npv], in1=sr, op=ALU.mult), reads=[gp, tb], writes=[ini])
                    yield
                    k.op("dve", lambda e, ini=ini: e.tensor_tensor(out=ini[:, 0:1], in0=ini[:, 2:3], in1=ini[:, 3:4], op=ALU.subtract), reads=[ini], writes=[ini])
                    yield
                    k.op("dve", lambda e, ini=ini, gp=gp, npv=npv, sr=sr: e.tensor_tensor(out=ini[:, 2:3], in0=gp[:, 0, npv - 1:npv], in1=sr, op=ALU.mult), reads=[gp, tb], writes=[ini])
                    yield
                    k.op("dve", lambda e, ini=ini, gp=gp, npv=npv, cr=cr: e.tensor_tensor(out=ini[:, 3:4], in0=gp[:, 1, npv - 1:npv], in1=cr, op=ALU.mult), reads=[gp, tb], writes=[ini])
                    yield
                    k.op("dve", lambda e, ini=ini: e.tensor_tensor(out=ini[:, 1:2], in0=ini[:, 2:3], in1=ini[:, 3:4], op=ALU.add), reads=[ini], writes=[ini])
                    yield
                g = g_p()
                k.op("dve", lambda e, g=g, w0=w0, ini=ini: e.tensor_tensor_scan(g[:, 0, 0:n], rho_b.to_broadcast([128, n]), w0[:, 0:n], ini[:, 0:1], op0=ALU.mult, op1=ALU.add),
                     reads=[prm, w0, ini], writes=[g])
                yield
                k.op("dve", lambda e, g=g, w2=w2, ini=ini: e.tensor_tensor_scan(g[:, 1, 0:n], rho_b.to_broadcast([128, n]), w2[:, 0:n], ini[:, 1:2], op0=ALU.mult, op1=ALU.add),
                     reads=[prm, w2, ini], writes=[g])
                yield
                prev = (g, n)
                cx["prev"] = prev
                h = h_p()
                x0_, x1_, x2_, x3_ = wk_p(), wk_p(), wk_p(), wk_p()
                k.op("pool", lambda e, x0_=x0_, g=g, cosT=cosT: e.tensor_tensor(out=x0_[:, 0:n], in0=g[:, 0, 0:n], in1=cosT, op=ALU.mult), reads=[g, tb], writes=[x0_])
                yield
                k.op("pool", lambda e, x1_=x1_, g=g, sinT=sinT: e.tensor_tensor(out=x1_[:, 0:n], in0=g[:, 1, 0:n], in1=sinT, op=ALU.mult), reads=[g, tb], writes=[x1_])
                yield
                k.op("dve", lambda e, x2_=x2_, g=g, sinT=sinT: e.tensor_tensor(out=x2_[:, 0:n], in0=g[:, 0, 0:n], in1=sinT, op=ALU.mult), reads=[g, tb], writes=[x2_])
                yield
                k.op("dve", lambda e, x3_=x3_, g=g, cosT=cosT: e.tensor_tensor(out=x3_[:, 0:n], in0=g[:, 1, 0:n], in1=cosT, op=ALU.mult), reads=[g, tb], writes=[x3_])
                yield
                k.op("pool", lambda e, h=h, x0_=x0_, x1_=x1_: e.tensor_tensor(out=h[:, 0, 0:n], in0=x0_[:, 0:n], in1=x1_[:, 0:n], op=ALU.subtract), reads=[x0_, x1_], writes=[h])
                yield
                k.op("dve", lambda e, h=h, x2_=x2_, x3_=x3_: e.tensor_tensor(out=h[:, 1, 0:n], in0=x2_[:, 0:n], in1=x3_[:, 0:n], op=ALU.add), reads=[x2_, x3_], writes=[h])
                yield
                py = k.next_ps()
                for ri in range(2):
                    hb_ = h.t[:, ri, :]
                    if d == 0:
                        rhs_h = h[:, ri, 0:n]
                    else:
                        b_ = h.t[:, ri, n - 1:n]
                        rhs_h = bass.AP(tensor=b_.tensor, offset=b_.offset, ap=[list(b_.ap[0]), [-1, n]])
                    k.mm(py, py[0:32, 0:n], CL, CL[:, ri, :], h, rhs_h, ri == 0, ri == 1)
                    yield
                if d == 0:
                    k.op("act", lambda e, ya=ya, py=py, c0=c0, n=n: e.copy(ya[:, c0:c0 + n], py[0:32, 0:n]), reads=[py], writes=[ya])
                    yield
                else:
                    k.op("dve", lambda e, ya=ya, py=py, c0=c0, n=n: e.tensor_copy(ya[:, c0:c0 + n], py[0:32, 0:n]), reads=[py], writes=[ya])
                    yield
            for step in range(9):
                gens = [chunk_gen(0, step), chunk_gen(1, step)]
                while gens:
                    for g_ in list(gens):
                        try:
                            next(g_)
                        except StopIteration:
                            gens.remove(g_)
            ya = yad[0]
            for (c0, n) in chunks:
                k.op("pool", lambda e, c0=c0, n=n: e.tensor_tensor(out=yad[0][:, c0:c0 + n], in0=yad[1][:, c0:c0 + n], in1=yad[0][:, c0:c0 + n], op=ALU.add), reads=[yad[0], yad[1]], writes=[yad[0]])
                yo = yo_p()
                k.op("dve", lambda e, yo=yo, c0=c0, n=n: e.scalar_tensor_tensor(out=yo[:, 0:n], in0=u[:, c0:c0 + n], scalar=s5d[:, st:st + 1], in1=ya[:, c0:c0 + n], op0=ALU.mult, op1=ALU.add),
                     reads=[u, s5d, ya], writes=[yo])
                t1 = yo_p()
                k.op("dve", lambda e, yo=yo, t1=t1, n=n: e.tensor_tensor(out=t1[:, 0:n], in0=yo[:, 0:n], in1=yo[:, 0:n], op=ALU.mult), reads=[yo], writes=[t1])
                k.op("dve", lambda e, t1=t1, n=n: e.tensor_scalar(t1[:, 0:n], t1[:, 0:n], 0.044715, 1.0, op0=ALU.mult, op1=ALU.add), reads=[t1], writes=[t1])
                k.op("dve", lambda e, yo=yo, t1=t1, n=n: e.tensor_tensor(out=t1[:, 0:n], in0=t1[:, 0:n], in1=yo[:, 0:n], op=ALU.mult), reads=[t1, yo], writes=[t1])
                k.op("act", lambda e, t1=t1, n=n: e.activation(out=t1[:, 0:n], in_=t1[:, 0:n], func=AF.Sigmoid, scale=2.0 * math.sqrt(2.0 / math.pi)), reads=[t1], writes=[t1])
                k.op("dve", lambda e, yo=yo, t1=t1, n=n: e.tensor_tensor(out=yo[:, 0:n], in0=yo[:, 0:n], in1=t1[:, 0:n], op=ALU.mult), reads=[t1, yo], writes=[yo])
                k.dma(S5Y[st * 32:(st + 1) * 32, c0:c0 + n], yo[:, 0:n], reads=[yo], writes=[S5Y])
        k.phase_end()
        k.phase_begin()
        gw = k.sb("s5gw", [128, 4, 512])
        k.dma(gw[:], w["s5_gw"].t.rearrange("(kc p) n -> p kc n", p=128), reads=[w["s5_gw"]], writes=[gw])
        ggb = k.sb("s5gb", [128, 4])
        k.dma(ggb[:], w["s5_gb"][:, :], reads=[w["s5_gb"]], writes=[ggb])
        yb_p = k.pool("s5yb", [128, 4, 512], 2)
        go_p2 = k.pool("s5go", [128, 512], 3)
        for (c0, n) in NBLK:
            yb = yb_p()
            k.dma(yb[:, :, 0:n], S5Y[:, c0:c0 + n].rearrange("(kc p) t -> p kc t", p=128), reads=[S5Y], writes=[yb])
            for mc_ in range(4):
                ps = k.next_ps()
                for kc in range(4):
                    k.mm(ps, ps[:, 0:n], gw, gw[:, kc, mc_ * 128:(mc_ + 1) * 128], yb, yb[:, kc, 0:n], kc == 0, kc == 3)
                go = go_p2()
                k.op("act", lambda e, go=go, ps=ps, mc_=mc_: e.activation(out=go[:, 0:n], in_=ps[:, 0:n], func=AF.Sigmoid, bias=ggb[:, mc_:mc_ + 1]), reads=[ps, ggb], writes=[go])
                k.op("dve", lambda e, go=go, yb=yb, mc_=mc_: e.tensor_tensor(out=go[:, 0:n], in0=go[:, 0:n], in1=yb[:, mc_, 0:n], op=ALU.mult), reads=[go, yb], writes=[go])
                k.dma(S5O[mc_ * 128:(mc_ + 1) * 128, c0:c0 + n], go[:, 0:n], reads=[go], writes=[S5O])
        k.phase_end()
        if stop_after == "E":
            break

        k.phase_begin()
        modt = k.sb("modtF", [128, 96])
        k.dma(modt[:], MODT[:, :], reads=[MODT], writes=[modt])
        m3 = modt.t[:, :].rearrange("p (f j) -> p f j", j=2)
        brw = []
        for nm_ in ("br_attn_w", "br_hyena_w", "br_s5_w"):
            t_ = k.sb(nm_, [128, 4, D])
            k.dma(t_[:], w[nm_].t.rearrange("(kc p) n -> p kc n", p=128), reads=[w[nm_]], writes=[t_])
            brw.append(t_)
        ow = k.sb("out_w", [128, 8, D])
        k.dma(ow[:], w["out_w"].t.rearrange("(kc p) n -> p kc n", p=128), reads=[w["out_w"]], writes=[ow])
        br_p = k.pool("brin", [128, 3, 4, 512], 1)
        gt_p = k.pool("gt", [128, 3, 512], 2)
        mm_p = k.pool("mmix", [128, 8, 512], 1)
        xb_p = k.pool("xbF", [128, 8, 512], 1)
        t_p = k.pool("tF", [128, 512], 4)
        xo_p = k.pool("xoF", [128, 512], 3)
        srcs = (ATT, HY, S5O)
        for bi, (c0, n) in enumerate(NBLK):
            if bi == 0 and not need_ctx:
                continue
            j = 1 if bi == 0 else 0
            br = br_p()
            for b_ in range(3):
                k.dma(br[:, b_, :, 0:n], srcs[b_][:, c0:c0 + n].rearrange("(kc p) t -> p kc t", p=128), reads=[srcs[b_]], writes=[br])
            xb = xb_p()
            k.dma(xb[:, :, 0:n], XT[:, c0:c0 + n].rearrange("(kc p) t -> p kc t", p=128), reads=[XT], writes=[xb])
            mx = mm_p()
            for fc in range(8):
                gt = gt_p()
                for b_ in range(3):
                    k.dma(gt[:, b_, 0:n], GATE[b_ * D + fc * 128:b_ * D + (fc + 1) * 128, c0:c0 + n], reads=[GATE], writes=[gt])
                pb = [k.next_ps() for _ in range(3)]
                for b_ in range(3):
                    for kc in range(4):
                        k.mm(pb[b_], pb[b_][:, 0:n], brw[b_], brw[b_][:, kc, fc * 128:(fc + 1) * 128], br, br[:, b_, kc, 0:n], kc == 0, kc == 3)
                ta, tb_, tc_ = t_p(), t_p(), t_p()
                k.op("dve", lambda e, ta=ta, pb=pb, gt=gt: e.tensor_tensor(out=ta[:, 0:n], in0=pb[0][:, 0:n], in1=gt[:, 0, 0:n], op=ALU.mult), reads=[pb[0], gt], writes=[ta])
                k.op("dve", lambda e, tb_=tb_, pb=pb, gt=gt: e.tensor_tensor(out=tb_[:, 0:n], in0=pb[1][:, 0:n], in1=gt[:, 1, 0:n], op=ALU.mult), reads=[pb[1], gt], writes=[tb_])
                k.op("dve", lambda e, tc_=tc_, pb=pb, gt=gt: e.tensor_tensor(out=tc_[:, 0:n], in0=pb[2][:, 0:n], in1=gt[:, 2, 0:n], op=ALU.mult), reads=[pb[2], gt], writes=[tc_])
                k.op("pool", lambda e, ta=ta, tb_=tb_: e.tensor_tensor(out=ta[:, 0:n], in0=ta[:, 0:n], in1=tb_[:, 0:n], op=ALU.add), reads=[ta, tb_], writes=[ta])
                k.op("pool", lambda e, mx=mx, ta=ta, tc_=tc_, fc=fc: e.tensor_tensor(out=mx[:, fc, 0:n], in0=ta[:, 0:n], in1=tc_[:, 0:n], op=ALU.add), reads=[ta, tc_], writes=[mx])
            for dc in range(8):
                po = k.next_ps()
                for fc in range(8):
                    k.mm(po, po[:, 0:n], ow, ow[:, fc, dc * 128:(dc + 1) * 128], mx, mx[:, fc, 0:n], fc == 0, fc == 7)
                xo = xo_p()
                if dbg:
                    mo = xo_p()
                    k.op("act", lambda e, mo=mo, po=po: e.copy(mo[:, 0:n], po[:, 0:n]), reads=[po], writes=[mo])
                    k.dma(MIX[dc * 128:(dc + 1) * 128, c0:c0 + n], mo[:, 0:n], reads=[mo], writes=[MIX])
                k.op("dve", lambda e, xo=xo, po=po, xb=xb, dc=dc: e.scalar_tensor_tensor(out=xo[:, 0:n], in0=po[:, 0:n], scalar=m3[:, 16 + dc, j:j + 1], in1=xb[:, dc, 0:n], op0=ALU.mult, op1=ALU.add),
                     reads=[po, modt, xb], writes=[xo])
                k.dma(XT[dc * 128:(dc + 1) * 128, c0:c0 + n], xo[:, 0:n], reads=[xo], writes=[XT])
                if dbg:
                    k.dma(XDBG[dc * 128:(dc + 1) * 128, c0:c0 + n], xo[:, 0:n], reads=[xo], writes=[XDBG])
        k.phase_end()
        if stop_after == "F":
            break

        k.phase_begin()
        modt = k.sb("modtG", [128, 96])
        k.dma(modt[:], MODT[:, :], reads=[MODT], writes=[modt])
        m3 = modt.t[:, :].rearrange("p (f j) -> p f j", j=2)
        g2n = k.sb("g2n", [128, 8])
        k.dma(g2n[:], w["n2g"][:, :], reads=[w["n2g"]], writes=[g2n])
        A2 = k.sb("A2", [128, 8, 2])
        for j in range(2):
            k.op("dve", lambda e, j=j: e.scalar_tensor_tensor(out=A2[:, :, j], in0=m3[:, 32:40, j], scalar=1.0, in1=g2n[:], op0=ALU.add, op1=ALU.mult), reads=[modt, g2n], writes=[A2])
        wq = k.sb("peer_wq", [128, 8, 2048])
        wqv = w["peer_wq"].t.rearrange("(kc p) n -> p kc n", p=128)
        for kc in range(8):
            k.dma(wq[:, kc, :], wqv[:, kc, :], reads=[w["peer_wq"]], writes=[wq])
        kT = k.sb("peer_kT", [128, 16, 128])
        k.dma(kT[:], w["peer_kT"].t.rearrange("j k n -> k j n"), reads=[w["peer_kT"]], writes=[kT])
        csel = k.sb("csel", [128, 255])
        k.dma(csel[:], csel_in[:, :], reads=[csel_in], writes=[csel])
        io16 = k.sb("io16", [128, 16])
        k.dma(io16[:], iota16_in[:, :], reads=[iota16_in], writes=[io16])
        xb_p = k.pool("xbG", [128, 8, 128], 1)
        hb_p = k.pool("hbG", [128, 8, 128], 1)
        sq_p = k.pool("sqG", [128, 8, 128], 1)
        sm_p = k.pool("smG", [128, 128], 6)
        qT_p = k.pool("qTG", [128, 16, 128], 1)
        S_p = k.pool("SG", [128, 16, 128], 1)
        SW_p = k.pool("SWG", [128, 128], 2)
        TV_p = k.pool("TVG", [128, 16, 16], 1)
        TI_p = k.pool("TIG", [128, 16, 16], 1, U32)
        TF_p = k.pool("TFG", [128, 16, 16], 1)
        cand_p = k.pool("candG", [128, 8, 256], 1)
        cw_p = k.pool("cwG", [128, 256], 2)
        BV_p = k.pool("BVG", [128, 8, 16], 1)
        BP_p = k.pool("BPG", [128, 8, 16], 1, U32)
        BI_p = k.pool("BIG", [128, 8, 16], 2, I32)
        oh_p = k.pool("ohG", [128, 16, 16], 2)
        IDF_p = k.pool("IDFG", [128, 128], 1)
        GT_p = k.pool("GTG", [128, 128], 1)
        idT_p = k.pool("idTG", [128, 128], 1, I32)
        gT_p = k.pool("gTG", [128, 128], 1)
        htok_p = k.pool("htokG", [128, D], 1)
        ug_p = k.pool("ugG", [128, D], 6)
        vg_p = k.pool("vgG", [128, D], 6)
        dots_p = k.pool("dotsG", [128, 128], 1)
        wts_p = k.pool("wtsG", [128, 128], 1)
        wt_p = k.pool("wtG", [128, 128], 3)
        otok_p = k.pool("otokG", [128, D], 1)
        xo_p = k.pool("xoG", [128, 128], 3)
        GELU_C = 2.0 * math.sqrt(2.0 / math.pi)

        def bc3(t_ap_base, strides):
            return bass.AP(tensor=t_ap_base.tensor, offset=t_ap_base.offset, ap=[list(t_ap_base.ap[0]), [strides[0], 8], [strides[1], 16], [strides[2], 16]])

        tiles = [(t0, 1 if t0 < C else 0) for t0 in range(0 if need_ctx else C, NT, 128)]
        for ti_, (t0, j) in enumerate(tiles):
            if ti_ > 0 and ti_ % 8 == 0:
                k.barrier()
            xb = xb_p()
            k.dma(xb[:], XT[:, t0:t0 + 128].rearrange("(kc p) t -> p kc t", p=128), reads=[XT], writes=[xb])
            sq = sq_p()
            k.op("act", lambda e, sq=sq, xb=xb: e.activation(out=sq[:], in_=xb[:], func=AF.Square), reads=[xb], writes=[sq])
            pss = k.next_ps()
            for kc in range(8):
                k.mm(pss, pss[:, 0:128], ones, ones[:], sq, sq[:, kc, :], kc == 0, kc == 7)
            rstd = sm_p()
            k.op("act", lambda e, rstd=rstd, pss=pss: e.activation(out=rstd[:], in_=pss[:, 0:128], func=AF.Sqrt, bias=eps_t[:, 0:1], scale=1.0 / D), reads=[pss, eps_t], writes=[rstd])
            k.op("dve", lambda e, rstd=rstd: e.reciprocal(rstd[:], rstd[:]), reads=[rstd], writes=[rstd])
            hb = hb_p()
            for kc in range(8):
                k.op("dve", lambda e, hb=hb, xb=xb, rstd=rstd, kc=kc: e.tensor_tensor(out=hb[:, kc, :], in0=xb[:, kc, :], in1=rstd[:], op=ALU.mult), reads=[xb, rstd], writes=[hb])
                k.op("pool", lambda e, hb=hb, kc=kc: e.tensor_scalar(hb[:, kc, :], hb[:, kc, :], A2[:, kc, j:j + 1], m3[:, 24 + kc, j:j + 1], op0=ALU.mult, op1=ALU.add), reads=[hb, A2, modt], writes=[hb])
            htok = htok_p()
            for kc in range(8):
                pst = k.next_ps()
                k.op("pe", lambda e, pst=pst, hb=hb, kc=kc: e.transpose(pst[:, 0:128], hb[:, kc, :], ident[:]), reads=[hb, ident], writes=[pst])
                k.op("act", lambda e, htok=htok, pst=pst, kc=kc: e.copy(htok[:, kc * 128:(kc + 1) * 128], pst[:, 0:128]), reads=[pst], writes=[htok])
            qT = qT_p()
            for jc in range(16):
                psq = k.next_ps()
                for kc in range(8):
                    k.mm(psq, psq[:, 0:128], wq, wq[:, kc, jc * 128:(jc + 1) * 128], hb, hb[:, kc, :], kc == 0, kc == 7)
                if jc % 2 == 0:
                    k.op("act", lambda e, qT=qT, psq=psq, jc=jc: e.copy(qT[:, jc, :], psq[:, 0:128]), reads=[psq], writes=[qT])
                else:
                    k.op("dve", lambda e, qT=qT, psq=psq, jc=jc: e.tensor_copy(qT[:, jc, :], psq[:, 0:128]), reads=[psq], writes=[qT])
            S = S_p()
            for jc in range(16):
                pss2 = k.next_ps()
                k.mm(pss2, pss2[:, 0:128], qT, qT[:, jc, :], kT, kT[:, jc, :], True, True)
                if jc % 2 == 0:
                    k.op("act", lambda e, S=S, pss2=pss2, jc=jc: e.copy(S[:, jc, :], pss2[:, 0:128]), reads=[pss2], writes=[S])
                else:
                    k.op("dve", lambda e, S=S, pss2=pss2, jc=jc: e.tensor_copy(S[:, jc, :], pss2[:, 0:128]), reads=[pss2], writes=[S])
            TV = TV_p(); TI = TI_p()
            for jc in range(16):
                SW = SW_p()
                k.op("dve", lambda e, TV=TV, S=S, jc=jc: e.max(out=TV[:, jc, 0:8], in_=S[:, jc, :]), reads=[S], writes=[TV])
                k.op("dve", lambda e, TI=TI, TV=TV, S=S, jc=jc: e.max_index(out=TI[:, jc, 0:8], in_max=TV[:, jc, 0:8], in_values=S[:, jc, :]), reads=[S, TV], writes=[TI])
                k.op("dve", lambda e, SW=SW, TV=TV, S=S, jc=jc: e.match_replace(out=SW[:], in_to_replace=TV[:, jc, 0:8], in_values=S[:, jc, :], imm_value=-1e30), reads=[S, TV], writes=[SW])
                k.op("dve", lambda e, TV=TV, SW=SW, jc=jc: e.max(out=TV[:, jc, 8:16], in_=SW[:]), reads=[SW], writes=[TV])
                k.op("dve", lambda e, TI=TI, TV=TV, SW=SW, jc=jc: e.max_index(out=TI[:, jc, 8:16], in_max=TV[:, jc, 8:16], in_values=SW[:]), reads=[SW, TV], writes=[TI])
            TF = TF_p()
            k.op("dve", lambda e, TF=TF, TI=TI: e.tensor_copy(TF[:], TI[:]), reads=[TI], writes=[TF])
            cand = cand_p()
            tvb = TV.t[:, 0:1, 0:1]
            cv4 = cand.t[:, :, :].rearrange("p h (a b) -> p h a b", b=16)
            in0 = bc3(TV.t[:, 0, 0:1], (32, 1, 0))
            in1 = bc3(TV.t[:, 1, 0:1], (32, 0, 1))
            k.op("dve", lambda e, cv4=cv4, in0=in0, in1=in1: e.tensor_tensor(out=cv4, in0=in0, in1=in1, op=ALU.add), reads=[TV], writes=[cand])
            BV = BV_p(); BP = BP_p()
            for h in range(8):
                cw = cw_p()
                k.op("dve", lambda e, BV=BV, cand=cand, h=h: e.max(out=BV[:, h, 0:8], in_=cand[:, h, :]), reads=[cand], writes=[BV])
                k.op("dve", lambda e, BP=BP, BV=BV, cand=cand, h=h: e.max_index(out=BP[:, h, 0:8], in_max=BV[:, h, 0:8], in_values=cand[:, h, :]), reads=[cand, BV], writes=[BP])
                k.op("dve", lambda e, cw=cw, BV=BV, cand=cand, h=h: e.match_replace(out=cw[:], in_to_replace=BV[:, h, 0:8], in_values=cand[:, h, :], imm_value=-1e30), reads=[cand, BV], writes=[cw])
                k.op("dve", lambda e, BV=BV, cw=cw, h=h: e.max(out=BV[:, h, 8:16], in_=cw[:]), reads=[cw], writes=[BV])
                k.op("dve", lambda e, BP=BP, BV=BV, cw=cw, h=h: e.max_index(out=BP[:, h, 8:16], in_max=BV[:, h, 8:16], in_values=cw[:]), reads=[cw, BV], writes=[BP])
            bpi = BP.t[:, :, :].bitcast(I32)
            Ai = BI_p(); Bi = BI_p()
            k.op("dve", lambda e, Ai=Ai, bpi=bpi: e.tensor_single_scalar(Ai[:], bpi, 4, op=ALU.arith_shift_right), reads=[BP], writes=[Ai])
            k.op("dve", lambda e, Bi=Bi, bpi=bpi: e.tensor_single_scalar(Bi[:], bpi, 15, op=ALU.bitwise_and), reads=[BP], writes=[Bi])
            Af = sm_p(); Bf = sm_p()
            k.op("dve", lambda e, Af=Af, Ai=Ai: e.tensor_copy(Af[:], Ai.t[:, :, :].rearrange("p h k -> p (h k)")), reads=[Ai], writes=[Af])
            k.op("dve", lambda e, Bf=Bf, Bi=Bi: e.tensor_copy(Bf[:], Bi.t[:, :, :].rearrange("p h k -> p (h k)")), reads=[Bi], writes=[Bf])
            IDF = IDF_p()
            isel = sm_p(); jsel = sm_p()
            for h in range(8):
                for (posf, seg, dst) in ((Af, 2 * h, isel), (Bf, 2 * h + 1, jsel)):
                    oh = oh_p()
                    pk = posf.t[:, h * 16:(h + 1) * 16]
                    pk3 = bass.AP(tensor=pk.tensor, offset=pk.offset, ap=[list(pk.ap[0]), [1, 16], [0, 16]])
                    io3 = bass.AP(tensor=io16.t[:, 0:16].tensor, offset=io16.t[:, 0:16].offset, ap=[list(io16.t[:, 0:16].ap[0]), [0, 16], [1, 16]])
                    tf_ = TF.t[:, seg, 0:16]
                    tf3 = bass.AP(tensor=tf_.tensor, offset=tf_.offset, ap=[list(tf_.ap[0]), [0, 16], [1, 16]])
                    k.op("dve", lambda e, oh=oh, pk3=pk3, io3=io3: e.tensor_tensor(out=oh[:], in0=pk3, in1=io3, op=ALU.is_equal), reads=[posf, io16], writes=[oh])
                    k.op("dve", lambda e, oh=oh, tf3=tf3: e.tensor_tensor(out=oh[:], in0=oh[:], in1=tf3, op=ALU.mult), reads=[oh, TF], writes=[oh])
                    k.op("dve", lambda e, oh=oh, dst=dst, h=h: e.tensor_reduce(out=dst[:, h * 16:(h + 1) * 16], in_=oh[:], axis=AX.X, op=ALU.add), reads=[oh], writes=[dst])
            k.op("dve", lambda e, IDF=IDF, isel=isel, jsel=jsel: e.scalar_tensor_tensor(out=IDF[:], in0=isel[:], scalar=128.0, in1=jsel[:], op0=ALU.mult, op1=ALU.add), reads=[isel, jsel], writes=[IDF])
            k.op("dve", lambda e, IDF=IDF: e.tensor_scalar(IDF[:], IDF[:], 16383.0, 0.0, op0=ALU.min, op1=ALU.max), reads=[IDF], writes=[IDF])
            GT = GT_p()
            gst = sm_p()
            for h in range(8):
                k.op("dve", lambda e, gst=gst, BV=BV, h=h: e.tensor_scalar(gst[:, h:h + 1], BV[:, h, 0:1], -1.0, None, op0=ALU.mult), reads=[BV], writes=[gst])
                k.op("act", lambda e, GT=GT, BV=BV, gst=gst, h=h: e.activation(out=GT[:, h * 16:(h + 1) * 16], in_=BV[:, h, :], func=AF.Exp, bias=gst[:, h:h + 1], accum_out=gst[:, 8 + h:9 + h]),
                     reads=[BV, gst], writes=[GT, gst])
            k.op("dve", lambda e, gst=gst: e.reciprocal(gst[:, 16:24], gst[:, 8:16]), reads=[gst], writes=[gst])
            for h in range(8):
                k.op("dve", lambda e, GT=GT, gst=gst, h=h: e.tensor_scalar(GT[:, h * 16:(h + 1) * 16], GT[:, h * 16:(h + 1) * 16], gst[:, 16 + h:17 + h], None, op0=ALU.mult), reads=[GT, gst], writes=[GT])
            idT = idT_p(); gT = gT_p()
            pst = k.next_ps()
            k.op("pe", lambda e, pst=pst, IDF=IDF: e.transpose(pst[:, 0:128], IDF[:], ident[:]), reads=[IDF, ident], writes=[pst])
            k.op("dve", lambda e, idT=idT, pst=pst: e.tensor_copy(idT[:], pst[:, 0:128]), reads=[pst], writes=[idT])
            pst2 = k.next_ps()
            k.op("pe", lambda e, pst2=pst2, GT=GT: e.transpose(pst2[:, 0:128], GT[:], ident[:]), reads=[GT, ident], writes=[pst2])
            k.op("act", lambda e, gT=gT, pst2=pst2: e.copy(gT[:], pst2[:, 0:128]), reads=[pst2], writes=[gT])
            dots = dots_p()
            for t in range(128):
                ug = ug_p()
                k.dma(None, None, reads=[idT, w["peer_u"]], writes=[ug], q="pool",
                      fn=lambda e, ug=ug, t=t: e.indirect_dma_start(out=ug[:], out_offset=None, in_=w["peer_u"][:, :],
                                                                  in_offset=bass.IndirectOffsetOnAxis(ap=idT[:, t:t + 1], axis=0)))
                pb0 = k.next_ps(); pb1 = k.next_ps()
                sel = ident[:, t:t + 1].to_broadcast([128, 128])
                k.mm(pb0, pb0[:, :], ident, sel, htok, htok[:, 0:512], True, True)
                k.mm(pb1, pb1[:, :], ident, sel, htok, htok[:, 512:1024], True, True)
                hrow = vg_p()
                k.op("act", lambda e, hrow=hrow, pb0=pb0: e.copy(hrow[:, 0:512], pb0[:, :]), reads=[pb0], writes=[hrow])
                k.op("act", lambda e, hrow=hrow, pb1=pb1: e.copy(hrow[:, 512:1024], pb1[:, :]), reads=[pb1], writes=[hrow])
                k.op("dve", lambda e, ug=ug, hrow=hrow, t=t: e.scalar_tensor_tensor(out=hrow[:], in0=ug[:], scalar=1.0, in1=hrow[:], op0=ALU.mult, op1=ALU.mult, accum_out=dots[:, t:t + 1]),
                     reads=[ug, hrow], writes=[hrow, dots])
            wts = wts_p()
            g1_ = sm_p()
            k.op("dve", lambda e, g1_=g1_: e.tensor_tensor(out=g1_[:], in0=dots[:], in1=dots[:], op=ALU.mult), reads=[dots], writes=[g1_])
            k.op("dve", lambda e, g1_=g1_: e.tensor_scalar(g1_[:], g1_[:], 0.044715, 1.0, op0=ALU.mult, op1=ALU.add), reads=[g1_], writes=[g1_])
            k.op("dve", lambda e, g1_=g1_: e.tensor_tensor(out=g1_[:], in0=g1_[:], in1=dots[:], op=ALU.mult), reads=[g1_, dots], writes=[g1_])
            k.op("act", lambda e, g1_=g1_: e.activation(out=g1_[:], in_=g1_[:], func=AF.Sigmoid, scale=GELU_C), reads=[g1_], writes=[g1_])
            k.op("dve", lambda e, g1_=g1_: e.tensor_tensor(out=g1_[:], in0=g1_[:], in1=dots[:], op=ALU.mult), reads=[g1_, dots], writes=[g1_])
            k.op("dve", lambda e, wts=wts, g1_=g1_, gT=gT: e.tensor_tensor(out=wts[:], in0=g1_[:], in1=gT[:], op=ALU.mult), reads=[g1_, gT], writes=[wts])
            po0 = k.next_ps(); po1 = k.next_ps()
            for t in range(128):
                vg = vg_p()
                k.dma(None, None, reads=[idT, w["peer_v"]], writes=[vg], q="pool",
                      fn=lambda e, vg=vg, t=t: e.indirect_dma_start(out=vg[:], out_offset=None, in_=w["peer_v"][:, :],
                                                                  in_offset=bass.IndirectOffsetOnAxis(ap=idT[:, t:t + 1], axis=0)))
                wt_ = wt_p()
                k.op("dve", lambda e, wt_=wt_, t=t: e.tensor_scalar(wt_[:], csel[:, 127 - t:255 - t], wts[:, t:t + 1], None, op0=ALU.mult), reads=[csel, wts], writes=[wt_])
                k.mm(po0, po0[:, :], wt_, wt_[:], vg, vg[:, 0:512], t == 0, t == 127)
                k.mm(po1, po1[:, :], wt_, wt_[:], vg, vg[:, 512:1024], t == 0, t == 127)
            otok = otok_p()
            k.op("act", lambda e, otok=otok, po0=po0: e.copy(otok[:, 0:512], po0[:, :]), reads=[po0], writes=[otok])
            k.op("dve", lambda e, otok=otok, po1=po1: e.tensor_copy(otok[:, 512:1024], po1[:, :]), reads=[po1], writes=[otok])
            for dc in range(8):
                pst = k.next_ps()
                k.op("pe", lambda e, pst=pst, otok=otok, dc=dc: e.transpose(pst[:, 0:128], otok[:, dc * 128:(dc + 1) * 128], ident[:]), reads=[otok, ident], writes=[pst])
                xo = xo_p()
                k.op("dve", lambda e, xo=xo, pst=pst, xb=xb, dc=dc: e.scalar_tensor_tensor(out=xo[:], in0=pst[:, 0:128], scalar=m3[:, 40 + dc, j:j + 1], in1=xb[:, dc, :], op0=ALU.mult, op1=ALU.add),
                     reads=[pst, modt, xb], writes=[xo])
                k.dma(XT[dc * 128:(dc + 1) * 128, t0:t0 + 128], xo[:], reads=[xo], writes=[XT])
        k.phase_end()
        if stop_after == "G":
            break

    if stop_after is None:
        k.phase_begin()
        fg = k.sb("final_g", [128, 8])
        k.dma(fg[:], finalg_in[:, :], reads=[finalg_in], writes=[fg])
        xb_p = k.pool("xbN", [128, 8, 512], 2)
        sq_p = k.pool("sqN", [128, 8, 512], 1)
        rs_p = k.pool("rsN", [128, 512], 2)
        yo_p = k.pool("yoN", [128, 512], 4)
        for bi in range(8):
            t0 = C + bi * 512
            xb = xb_p()
            k.dma(xb[:], XT[:, t0:t0 + 512].rearrange("(kc p) t -> p kc t", p=128), reads=[XT], writes=[xb])
            sq = sq_p()
            k.op("act", lambda e, sq=sq, xb=xb: e.activation(out=sq[:], in_=xb[:], func=AF.Square), reads=[xb], writes=[sq])
            pss = k.next_ps()
            for kc in range(8):
                k.mm(pss, pss[:, :], ones, ones[:], sq, sq[:, kc, :], kc == 0, kc == 7)
            rstd = rs_p()
            k.op("act", lambda e, rstd=rstd, pss=pss: e.activation(out=rstd[:], in_=pss[:, :], func=AF.Sqrt, bias=eps_t[:, 0:1], scale=1.0 / D), reads=[pss, eps_t], writes=[rstd])
            k.op("dve", lambda e, rstd=rstd: e.reciprocal(rstd[:], rstd[:]), reads=[rstd], writes=[rstd])
            for kc in range(8):
                yo = yo_p()
                k.op("dve", lambda e, yo=yo, xb=xb, rstd=rstd, kc=kc: e.scalar_tensor_tensor(out=yo[:], in0=xb[:, kc, :], scalar=fg[:, kc:kc + 1], in1=rstd[:], op0=ALU.mult, op1=ALU.mult),
                     reads=[xb, fg, rstd], writes=[yo])
                k.dma(out[kc * 128:(kc + 1) * 128, bi * 512:(bi + 1) * 512], yo[:], reads=[yo], writes=[out])
        k.phase_end()
    else:
        k.phase_begin()
        fin_p = k.pool("fin", [128, L], 2)
        for kc in range(8):
            st = fin_p()
            k.dma(st[:], XT[kc * 128:(kc + 1) * 128, C:NT], reads=[XT], writes=[st])
            k.dma(out[kc * 128:(kc + 1) * 128, :], st[:], reads=[st], writes=[out])
        k.phase_end()
    k.finish()
    return nc, k


def kernel(**inputs):
    inp = {kk: np.asarray(v) for kk, v in inputs.items()}
    nc, _ = build()
    in_maps = [_prep(inp, b) for b in range(4)]
    res = run_bass_kernel_spmd(nc, in_maps, core_ids=list(range(4)))
    return np.stack([np.ascontiguousarray(res.results[b]["out"].T) for b in range(4)], 0).astype(np.float32)
```

```python
import math
import numpy as np
from contextlib import ExitStack
import concourse.bass as bass
import concourse.mybir as mybir
from concourse.bass_utils import run_bass_kernel_spmd

F32 = mybir.dt.float32
I32 = mybir.dt.int32
U32 = mybir.dt.uint32
AF = mybir.ActivationFunctionType
ALU = mybir.AluOpType
AX = mybir.AxisListType

D = 1024
L = 4096
C = 256
NT = L + C
DEPTH = 2
IN_W = 5888
NBLK = [(0, 256)] + [(256 + 512 * i, 512) for i in range(8)]


class Res:
    __slots__ = ("name", "w", "r", "ep")

    def __init__(self, name):
        self.name = name
        self.w = None
        self.r = {}
        self.ep = 0


class Tile(Res):
    __slots__ = ("t",)

    def __init__(self, name, t):
        Res.__init__(self, name)
        self.t = t

    def __getitem__(self, k):
        return self.t[k]


class Eng:
    def __init__(self, name, e, sem):
        self.name = name
        self.e = e
        self.sem = sem
        self.cnt = 0
        self.known = {}


class K:
    SAME_ENGINE_SYNC = True
    ROTATE_AT = 20000

    def __init__(self, nc, n_dma_slots=24):
        self.nc = nc
        self._sems = []
        self.es = ExitStack()
        self.epoch = 1
        self.uid = 0
        self.eng = {}
        for nm, e in (("pe", nc.tensor), ("act", nc.scalar), ("dve", nc.vector),
                      ("pool", nc.gpsimd), ("sp", nc.sync)):
            self.eng[nm] = Eng(nm, e, self.es.enter_context(nc.semaphore("sem_" + nm)))
        self.slots = []
        for i in range(n_dma_slots):
            s = self.es.enter_context(nc.semaphore("dsem%d" % i))
            self.slots.append([s, 0])
        self.slot_i = 0
        self.phase_stack = None
        self.psum = []
        for i in range(8):
            t = self.es.enter_context(nc.psum_tensor("ps%d" % i, [128, 512], F32))
            self.psum.append(Tile("ps%d" % i, t))
        self.ps_i = 0
        self.n_ins = 0
        self.n_rot = 0
        self._sems = []

    def next_ps(self):
        p = self.psum[self.ps_i]
        self.ps_i = (self.ps_i + 1) % 8
        return p

    def sb(self, name, shape, dtype=F32, persistent=False):
        self.uid += 1
        st = self.es if (persistent or self.phase_stack is None) else self.phase_stack
        t = st.enter_context(self.nc.sbuf_tensor("%s_%d" % (name, self.uid), list(shape), dtype))
        return Tile(name, t)

    def pool(self, name, shape, n, dtype=F32):
        tiles = [self.sb("%s%d" % (name, i), shape, dtype) for i in range(n)]
        st = {"i": 0}

        def nxt():
            t = tiles[st["i"]]
            st["i"] = (st["i"] + 1) % n
            return t
        return nxt

    def dram(self, name, shape, dtype=F32, kind="Internal"):
        t = self.nc.dram_tensor(name, list(shape), dtype, kind=kind)
        return Tile(name, t.ap())

    def phase_begin(self):
        assert self.phase_stack is None
        self.phase_stack = ExitStack()

    def phase_end(self):
        self.barrier()
        self.phase_stack.close()
        self.phase_stack = None

    def _key(self, sem):
        for i, s_ in enumerate(self._sems):
            if s_ is sem:
                return i
        self._sems.append(sem)
        return len(self._sems) - 1

    def _wait(self, E, ev):
        sem, val = ev
        k = self._key(sem)
        if E.known.get(k, 0) >= val:
            return
        E.e.wait_ge(sem, val)
        E.known[k] = val
        self.n_ins += 1

    def _deps(self, E, reads, writes, is_pe=False):
        evs = []
        for r in reads:
            if r.ep == self.epoch and r.w is not None:
                evs.append(r.w)
        for w in writes:
            if w.ep == self.epoch:
                if w.w is not None:
                    evs.append(w.w)
                evs.extend(w.r.values())
        for ev in evs:
            if ev[0] is E.sem and (is_pe or not self.SAME_ENGINE_SYNC):
                continue
            self._wait(E, ev)

    def _record(self, ev, reads, writes):
        for r in reads:
            if r.ep != self.epoch:
                r.ep = self.epoch
                r.w = None
                r.r = {}
            r.r[self._key(ev[0])] = ev
        for w in writes:
            w.ep = self.epoch
            w.w = ev
            w.r = {}

    def op(self, eng, fn, reads=(), writes=()):
        E = self.eng[eng]
        self._deps(E, reads, writes, is_pe=(eng == "pe"))
        ins = fn(E.e)
        E.cnt += 1
        ins.then_inc(E.sem, 1)
        self.n_ins += 1
        self._record((E.sem, E.cnt), reads, writes)
        return ins

    def dma(self, out_ap, in_ap, reads=(), writes=(), q="sp", fn=None):
        E = self.eng[q]
        self._deps(E, reads, writes)
        slot = self.slots[self.slot_i]
        self.slot_i = (self.slot_i + 1) % len(self.slots)
        if slot[1] > 0:
            self._wait(E, (slot[0], slot[1]))
        ins = E.e.dma_start(out=out_ap, in_=in_ap) if fn is None else fn(E.e)
        slot[1] += 16
        ins.then_inc(slot[0], 16)
        self.n_ins += 1
        self._record((slot[0], slot[1]), reads, writes)
        return ins

    def barrier(self):
        evs = []
        for E in self.eng.values():
            if E.cnt > 0:
                evs.append((E.sem, E.cnt))
        for s in self.slots:
            if s[1] > 0:
                evs.append((s[0], s[1]))
        for E in self.eng.values():
            for ev in evs:
                if ev[0] is E.sem:
                    continue
                self._wait(E, ev)
        self.epoch += 1
        for E in self.eng.values():
            if E.cnt > self.ROTATE_AT:
                self.n_rot += 1
                E.sem = self.es.enter_context(self.nc.semaphore("sem_%s_r%d" % (E.name, self.n_rot)))
                E.cnt = 0
        for s in self.slots:
            if s[1] > self.ROTATE_AT:
                self.n_rot += 1
                s[0] = self.es.enter_context(self.nc.semaphore("dsem_r%d" % self.n_rot))
                s[1] = 0

    def finish(self):
        self.barrier()
        self.es.close()

    def mm(self, ps, out_ap, lt, lhsT_ap, rt, rhs_ap, start, stop):
        return self.op("pe", lambda e: e.matmul(out_ap, lhsT_ap, rhs_ap, start=start, stop=stop),
                       reads=[lt, rt], writes=[ps])


def _rot_perm():
    perm = []
    for hd in range(10):
        for i in range(64):
            perm.append(hd * 64 + (i + 32) % 64)
    return np.array(perm)


def _rope_tables():
    row = np.repeat(np.arange(L // 64), 64).astype(np.float32)
    col = np.tile(np.arange(64), L // 64).astype(np.float32)
    inv = np.power(np.float32(10000.0), -np.arange(16, dtype=np.float32) / 16).astype(np.float32)
    ang = np.concatenate([row[:, None] * inv, col[:, None] * inv], axis=-1)
    cos = np.cos(ang).astype(np.float32)
    sin = np.sin(ang).astype(np.float32)
    cosT = np.ones((64, NT), np.float32)
    sinT = np.zeros((64, NT), np.float32)
    cosT[:32, C:] = cos.T
    cosT[32:, C:] = cos.T
    sinT[:32, C:] = -sin.T
    sinT[32:, C:] = sin.T
    return np.concatenate([cosT, cosT], 0), np.concatenate([sinT, sinT], 0)


def _pk(v, n):
    return np.ascontiguousarray(np.asarray(v, np.float32).reshape(n, 128).T)


def _prep(inp, b):
    m = {}
    m["xT"] = np.ascontiguousarray(inp["x"][b].T)
    m["ctxT"] = np.ascontiguousarray(inp["ctx"][b].T)
    cv = np.stack([_pk(inp["c"][b], 8), _pk(inp["c_ctx"], 8)], axis=-1)
    m["cvec"] = np.ascontiguousarray(cv.reshape(128, 16))
    perm = _rot_perm()
    cosT, sinT = _rope_tables()
    m["ropec"] = cosT
    qq = np.arange(128)[:, None]
    kk = np.arange(128)[None, :]
    mk = np.zeros((128, 256), np.float32)
    mk[:, :128] = np.where(kk >= qq, 0.0, -1e30)
    mk[:, 128:] = np.where(kk <= qq, 0.0, -1e30)
    m["amask"] = mk
    m["final_g"] = _pk(inp["final_g"], 8)
    csel = np.zeros((128, 255), np.float32)
    csel[:, 127] = 1.0
    m["csel"] = csel
    m["iota16"] = np.ascontiguousarray(np.broadcast_to(np.arange(16, dtype=np.float32)[None, :], (128, 16)))
    for nm, Ls in (("L", L), ("C", C)):
        t = np.arange(Ls, dtype=np.float32)
        tn = t / np.float32(Ls)
        bands = np.arange(1, 17, dtype=np.float32)
        ang = np.float32(2.0 * math.pi) * tn[:, None] * bands[None, :]
        zf = np.concatenate([tn[:, None], np.cos(ang), np.sin(ang)], axis=-1).astype(np.float32)
        m["hyz" + nm] = np.ascontiguousarray(zf.T)
        tw = np.linspace(0.0, 1.0, Ls, dtype=np.float32)
        m["hytw" + nm] = np.ascontiguousarray((-tw).reshape(Ls // 128, 128).T)
    fast = -math.log(1e-2) / 0.3
    slow = -math.log(1e-2) / 1.5
    rate = np.linspace(fast, slow, 512, dtype=np.float32)
    m["hyrate"] = np.ascontiguousarray(np.broadcast_to(rate[None, :], (128, 512)))
    m["ropes"] = sinT
    for l in range(DEPTH):
        m["mod_w%d" % l] = np.ascontiguousarray(inp["mod_w"][l])
        m["mod_b%d" % l] = _pk(inp["mod_b"][l], 48)
        m["n1g%d" % l] = _pk(inp["norm1_g"][l], 8)
        m["in_w%d" % l] = np.ascontiguousarray(inp["in_w"][l])
        m["in_wr%d" % l] = np.ascontiguousarray(inp["in_w"][l][:, perm])
        m["gate_b%d" % l] = _pk(inp["gate_b"][l], 24)
        m["hy_sw%d" % l] = np.ascontiguousarray(np.asarray(inp["hy_short_w"][l], np.float32).reshape(3, 12, 128).transpose(2, 1, 0))
        m["hy_sb%d" % l] = _pk(inp["hy_short_b"][l], 12)
        m["hy_w1%d" % l] = np.ascontiguousarray(inp["hy_w1"][l])
        m["hy_w2%d" % l] = np.ascontiguousarray(inp["hy_w2"][l])
        m["hy_w3%d" % l] = np.ascontiguousarray(inp["hy_w3"][l])
        m["hy_fb%d" % l] = np.ascontiguousarray(np.stack([inp["hy_b1"][l], inp["hy_freq1"][l], inp["hy_b2"][l], inp["hy_freq2"][l]], 1).astype(np.float32))
        m["hy_bias%d" % l] = _pk(inp["hy_bias"][l], 4)
        for nm_ in ("s5_a_re", "s5_a_im"):
            m["%s%d" % (nm_, l)] = np.ascontiguousarray(np.asarray(inp[nm_][l], np.float32).reshape(2, 16, 128).transpose(2, 0, 1))
        m["s5_ldt%d" % l] = np.ascontiguousarray(np.repeat(np.asarray(inp["s5_log_dt"][l], np.float32), 64, axis=1).reshape(2, 16, 128).transpose(2, 0, 1))
        for nm_ in ("s5_b_re", "s5_b_im"):
            m["%s%d" % (nm_, l)] = np.ascontiguousarray(np.asarray(inp[nm_][l], np.float32).reshape(2, 2048, 16))
        for nm_ in ("s5_c_re", "s5_c_im"):
            m["%s%d" % (nm_, l)] = np.ascontiguousarray(np.asarray(inp[nm_][l], np.float32).transpose(0, 1, 3, 2).reshape(2, 2048, 16))
        m["s5_d%d" % l] = np.ascontiguousarray(np.asarray(inp["s5_d"][l], np.float32).reshape(16, 32).T)
        m["s5_gw%d" % l] = np.ascontiguousarray(inp["s5_glu_w"][l])
        m["s5_gb%d" % l] = _pk(inp["s5_glu_b"][l], 4)
        for nm_ in ("br_attn_w", "br_hyena_w", "br_s5_w", "out_w"):
            m["%s%d" % (nm_, l)] = np.ascontiguousarray(inp[nm_][l])
        m["n2g%d" % l] = _pk(inp["norm2_g"][l], 8)
        m["peer_wq%d" % l] = np.ascontiguousarray(inp["peer_wq"][l])
        m["peer_kT%d" % l] = np.ascontiguousarray(np.asarray(inp["peer_keys"][l], np.float32).transpose(0, 1, 3, 2).reshape(16, 128, 128))
        m["peer_u%d" % l] = np.ascontiguousarray(inp["peer_u"][l])
        m["peer_v%d" % l] = np.ascontiguousarray(inp["peer_v"][l])
        m["sink%d" % l] = np.ascontiguousarray(np.broadcast_to(np.asarray(inp["attn_sink"][l], np.float32)[None, :], (128, 8)))
    return m


def build(dbg=False, n_layers=DEPTH, stop_after=None, start_at=None):
    nc = bass.Bass("TRN2", target_bir_lowering=False)
    k = K(nc)
    okind = "ExternalOutput" if dbg else "Internal"
    ein = lambda name, shape, dt=F32: k.dram(name, shape, dt, kind="ExternalInput")
    xT_in = ein("xT", [D, L])
    ctxT_in = ein("ctxT", [D, C])
    cvec_in = ein("cvec", [128, 16])
    ropec_in = ein("ropec", [128, NT])
    ropes_in = ein("ropes", [128, NT])
    amask_in = ein("amask", [128, 256])
    finalg_in = ein("final_g", [128, 8])
    csel_in = ein("csel", [128, 255])
    iota16_in = ein("iota16", [128, 16])
    hyz_in = {"L": ein("hyzL", [33, L]), "C": ein("hyzC", [33, C])}
    hytw_in = {"L": ein("hytwL", [128, L // 128]), "C": ein("hytwC", [128, C // 128])}
    hyrate_in = ein("hyrate", [128, 512])
    W = []
    for l in range(DEPTH):
        W.append(dict(mod_w=ein("mod_w%d" % l, [D, 6 * D]), mod_b=ein("mod_b%d" % l, [128, 48]),
                      n1g=ein("n1g%d" % l, [128, 8]), hy_sw=ein("hy_sw%d" % l, [128, 12, 3]), hy_sb=ein("hy_sb%d" % l, [128, 12]),
                      hy_w1=ein("hy_w1%d" % l, [33, 64]), hy_w2=ein("hy_w2%d" % l, [64, 64]), hy_w3=ein("hy_w3%d" % l, [64, 1024]),
                      hy_fb=ein("hy_fb%d" % l, [64, 4]),
                      n2g=ein("n2g%d" % l, [128, 8]), peer_wq=ein("peer_wq%d" % l, [D, 2048]), peer_kT=ein("peer_kT%d" % l, [16, 128, 128]),
                      peer_u=ein("peer_u%d" % l, [16384, D]), peer_v=ein("peer_v%d" % l, [16384, D]),
                      br_attn_w=ein("br_attn_w%d" % l, [512, D]), br_hyena_w=ein("br_hyena_w%d" % l, [512, D]), br_s5_w=ein("br_s5_w%d" % l, [512, D]),
                      out_w=ein("out_w%d" % l, [D, D]),
                      s5_a_re=ein("s5_a_re%d" % l, [128, 2, 16]), s5_a_im=ein("s5_a_im%d" % l, [128, 2, 16]), s5_ldt=ein("s5_ldt%d" % l, [128, 2, 16]),
                      s5_b_re=ein("s5_b_re%d" % l, [2, 2048, 16]), s5_b_im=ein("s5_b_im%d" % l, [2, 2048, 16]),
                      s5_c_re=ein("s5_c_re%d" % l, [2, 2048, 16]), s5_c_im=ein("s5_c_im%d" % l, [2, 2048, 16]),
                      s5_d=ein("s5_d%d" % l, [32, 16]), s5_gw=ein("s5_gw%d" % l, [512, 512]), s5_gb=ein("s5_gb%d" % l, [128, 4]), hy_bias=ein("hy_bias%d" % l, [128, 4]), sink=ein("sink%d" % l, [128, 8]), in_w=ein("in_w%d" % l, [D, IN_W]),
                      in_wr=ein("in_wr%d" % l, [D, 640]), gate_b=ein("gate_b%d" % l, [128, 24])))
    XT = k.dram("XT", [D, NT])
    MODT = k.dram("MODT", [128, 96], kind=okind)
    QT = k.dram("QT", [512, NT], kind=okind)
    KT = k.dram("KT", [128, NT], kind=okind)
    VTOK = k.dram("VTOK", [NT, 128], kind=okind)
    US5 = k.dram("US5", [512, NT], kind=okind)
    ZHY = k.dram("ZHY", [1536, NT], kind=okind)
    GATE = k.dram("GATE", [3072, NT], kind=okind)
    HT = k.dram("HT", [D, NT], kind=okind)
    MIX = k.dram("MIX", [D, NT], kind=okind)
    XDBG = k.dram("XDBG", [D, NT], kind=okind)
    S5Y = k.dram("S5Y", [512, NT], kind=okind)
    S5O = k.dram("S5O", [512, NT], kind=okind)
    HY = k.dram("HY", [512, NT], kind=okind)
    DFT = {}
    for nm, Ls in (("L", L), ("C", C)):
        DFT[nm] = {m_: k.dram("DFT_%s_%s" % (nm, m_), [Ls, Ls]) for m_ in ("CK", "SK", "CS", "SS")}
    HSDd, KRId, VXTd, VXFd, X0Fd = {}, {}, {}, {}, {}
    for nm, Ls in (("L", L), ("C", C)):
        HSDd[nm] = k.dram("HSD" + nm, [2, Ls, 512])
        KRId[nm] = k.dram("KRI" + nm, [2, Ls, 512])
        VXTd[nm] = k.dram("VXT" + nm, [Ls, 512])
        VXFd[nm] = k.dram("VXF" + nm, [512, Ls])
        X0Fd[nm] = k.dram("X0F" + nm, [512, Ls])
    ATT = k.dram("ATT", [512, NT], kind=okind)
    out = k.dram("out", [D, L], kind="ExternalOutput")

    ones = k.sb("ones", [128, 128], persistent=True)
    k.op("pool", lambda e: e.memset(ones[:], 1.0), writes=[ones])
    eps_t = k.sb("eps", [128, 1], persistent=True)
    k.op("pool", lambda e: e.memset(eps_t[:], 1e-6), writes=[eps_t])
    ident = k.sb("ident", [128, 128], persistent=True)
    k.op("pool", lambda e: e.memset(ident[:], 0.0), writes=[ident])
    k.op("pool", lambda e: e.affine_select(out=ident[:], in_=ident[:], pattern=[[-1, 128]],
                                           compare_op=ALU.not_equal, fill=1.0, base=0, channel_multiplier=1),
         reads=[ident], writes=[ident])

    k.phase_begin()
    stage_p = k.pool("stage", [128, NT], 2)
    for kc in range(8):
        st = stage_p()
        k.dma(st[:, 0:C], ctxT_in[kc * 128:(kc + 1) * 128, :], reads=[ctxT_in], writes=[st])
        k.dma(st[:, C:NT], xT_in[kc * 128:(kc + 1) * 128, :], reads=[xT_in], writes=[st])
        k.dma(XT[kc * 128:(kc + 1) * 128, :], st[:], reads=[st], writes=[XT])
    k.phase_end()


    k.phase_begin()
    negpi = k.sb("negpi", [128, 1])
    k.op("pool", lambda e: e.memset(negpi[:], -math.pi), writes=[negpi])
    colv_p = k.pool("colv", [128, 4096], 1, I32)
    pm_p = k.pool("pm", [128, 1], 2, I32)
    ri_p = k.pool("ri", [128, 4096], 2, I32)
    rj_p = k.pool("rj", [128, 4096], 2, I32)
    rf_p = k.pool("rf", [128, 4096], 2)
    go_p = k.pool("go", [128, 4096], 3)
    for nm, Ls in (("L", L), ("C", C)):
        N4 = 8 * Ls
        colv = colv_p()
        k.op("pool", lambda e, colv=colv: e.iota(colv[:, 0:Ls], pattern=[[2, Ls]], base=1, channel_multiplier=0), writes=[colv])
        for tt in range(Ls // 128):
            for shifted in (0, 1):
                pm = pm_p()
                k.op("pool", lambda e, pm=pm: e.iota(pm[:], pattern=[[0, 1]], base=2 * tt * 128 + shifted, channel_multiplier=2), writes=[pm])
                ri = ri_p()
                k.op("dve", lambda e, ri=ri, pm=pm: e.tensor_tensor(out=ri[:, 0:Ls], in0=colv[:, 0:Ls], in1=pm[:, 0:1].to_broadcast([128, Ls]), op=ALU.mult),
                     reads=[colv, pm], writes=[ri])
                for trig in ("S", "C"):
                    rj = rj_p()
                    if trig == "S":
                        k.op("dve", lambda e, ri=ri, rj=rj: e.tensor_single_scalar(rj[:, 0:Ls], ri[:, 0:Ls], N4 - 1, op=ALU.bitwise_and), reads=[ri], writes=[rj])
                    else:
                        k.op("dve", lambda e, ri=ri, rj=rj: e.tensor_single_scalar(rj[:, 0:Ls], ri[:, 0:Ls], N4 // 4, op=ALU.add), reads=[ri], writes=[rj])
                        k.op("dve", lambda e, rj=rj: e.tensor_single_scalar(rj[:, 0:Ls], rj[:, 0:Ls], N4 - 1, op=ALU.bitwise_and), reads=[rj], writes=[rj])
                    rf = rf_p()
                    k.op("pool", lambda e, rf=rf, rj=rj: e.tensor_copy(rf[:, 0:Ls], rj[:, 0:Ls]), reads=[rj], writes=[rf])
                    go = go_p()
                    k.op("act", lambda e, go=go, rf=rf: e.activation(out=go[:, 0:Ls], in_=rf[:, 0:Ls], func=AF.Sin, bias=negpi[:, 0:1], scale=2.0 * math.pi / N4),
                         reads=[rf, negpi], writes=[go])
                    dst = DFT[nm][trig + ("S" if shifted else "K")]
                    k.dma(dst[tt * 128:(tt + 1) * 128, :], go[:, 0:Ls], reads=[go], writes=[dst])
    k.phase_end()

    ORDER = ["0", "A", "B", "C", "D", "E", "F", "G"]
    skip = (lambda ph: start_at is not None and ORDER.index(ph) < ORDER.index(start_at))
    for l in range(n_layers):
        w = W[l]
        need_ctx = l < DEPTH - 1
        k.phase_begin()
        cv = k.sb("cv", [128, 16])
        k.dma(cv[:], cvec_in[:, :], reads=[cvec_in], writes=[cv])
        sl = k.sb("silu", [128, 16])
        k.op("act", lambda e: e.activation(out=sl[:], in_=cv[:], func=AF.Silu), reads=[cv], writes=[sl])
        mb = k.sb("mb", [128, 48])
        k.dma(mb[:], w["mod_b"][:, :], reads=[w["mod_b"]], writes=[mb])
        modt = k.sb("modt", [128, 96])
        mwv = w["mod_w"].t.rearrange("(kc p) n -> p kc n", p=128)
        mw_p = k.pool("mw", [128, 8, 1024], 2)
        for fg in range(6):
            mw = mw_p()
            for kc in range(8):
                k.dma(mw[:, kc, :], mwv[:, kc, fg * 1024:(fg + 1) * 1024], reads=[w["mod_w"]], writes=[mw])
            ps = k.next_ps()
            for fc in range(8):
                for kc in range(8):
                    k.mm(ps, ps[:, fc * 2:fc * 2 + 2], mw, mw[:, kc, fc * 128:(fc + 1) * 128],
                         sl, sl[:, kc * 2:kc * 2 + 2], kc == 0, kc == 7)
            for j in range(2):
                pv = ps.t[:, 0:16].rearrange("p (f j) -> p f j", j=2)[:, :, j]
                ov = modt.t[:, fg * 16:(fg + 1) * 16].rearrange("p (f j) -> p f j", j=2)[:, :, j]
                k.op("dve", lambda e, pv=pv, ov=ov: e.tensor_tensor(out=ov, in0=pv, in1=mb[:, fg * 8:(fg + 1) * 8], op=ALU.add),
                     reads=[ps, mb], writes=[modt])
        k.dma(MODT[:, :], modt[:], reads=[modt], writes=[MODT])
        k.phase_end()
        if stop_after == "A":
            break

        k.phase_begin()
        modt = k.sb("modt", [128, 96])
        k.dma(modt[:], MODT[:, :], reads=[MODT], writes=[modt])
        g1n = k.sb("g1n", [128, 8])
        k.dma(g1n[:], w["n1g"][:, :], reads=[w["n1g"]], writes=[g1n])
        gb = k.sb("gb", [128, 24])
        k.dma(gb[:], w["gate_b"][:, :], reads=[w["gate_b"]], writes=[gb])
        m3 = modt.t[:, :].rearrange("p (f j) -> p f j", j=2)
        Acoef = k.sb("Acoef", [128, 8, 2])
        for j in range(2):
            k.op("dve", lambda e, j=j: e.scalar_tensor_tensor(out=Acoef[:, :, j], in0=m3[:, 8:16, j], scalar=1.0, in1=g1n[:],
                                                             op0=ALU.add, op1=ALU.mult), reads=[modt, g1n], writes=[Acoef])
        iwv = w["in_w"].t.rearrange("(kc p) n -> p kc n", p=128)
        irv = w["in_wr"].t.rearrange("(kc p) n -> p kc n", p=128)
        groups = [("q", 0, 512), ("k", 512, 128), ("v", 640, 128), ("s5", 768, 512)]
        groups += [("hy", 1280 + 512 * i, 512) for i in range(3)] + [("gate", 2816 + 512 * i, 512) for i in range(6)]
        xb_p = k.pool("xb", [128, 8, 512], 1)
        sq_p = k.pool("sq", [128, 8, 512], 1)
        hb_p = k.pool("hb", [128, 8, 512], 1)
        wt_p = k.pool("wt", [128, 8, 512], 4)
        wr_p = k.pool("wr", [128, 8, 512], 2)
        rstd_p = k.pool("rstd", [128, 512], 1)
        rc_p = k.pool("rc", [128, 512], 2)
        rs_p = k.pool("rs", [128, 512], 2)
        ev_p = k.pool("ev", [128, 512], 4)
        e2_p = k.pool("e2", [128, 512], 2)
        vt_p = k.pool("vt", [128, 128], 2)
        for bi, (t0, nt) in enumerate(NBLK):
            j = 1 if bi == 0 else 0
            xb = xb_p()
            for kc in range(8):
                k.dma(xb[:, kc, 0:nt], XT[kc * 128:(kc + 1) * 128, t0:t0 + nt], reads=[XT], writes=[xb])
            sq = sq_p()
            k.op("act", lambda e: e.activation(out=sq[:, :, 0:nt], in_=xb[:, :, 0:nt], func=AF.Square), reads=[xb], writes=[sq])
            pss = k.next_ps()
            for kc in range(8):
                k.mm(pss, pss[:, 0:nt], ones, ones[:], sq, sq[:, kc, 0:nt], kc == 0, kc == 7)
            rstd = rstd_p()
            k.op("act", lambda e: e.activation(out=rstd[:, 0:nt], in_=pss[:, 0:nt], func=AF.Sqrt, bias=eps_t[:, 0:1], scale=1.0 / D),
                 reads=[pss, eps_t], writes=[rstd])
            k.op("dve", lambda e: e.reciprocal(rstd[:, 0:nt], rstd[:, 0:nt]), reads=[rstd], writes=[rstd])
            hb = hb_p()
            for kc in range(8):
                k.op("dve", lambda e, kc=kc: e.tensor_tensor(out=hb[:, kc, 0:nt], in0=xb[:, kc, 0:nt], in1=rstd[:, 0:nt], op=ALU.mult),
                     reads=[xb, rstd], writes=[hb])
                k.op("pool", lambda e, kc=kc: e.tensor_scalar(hb[:, kc, 0:nt], hb[:, kc, 0:nt], Acoef[:, kc, j:j + 1], m3[:, kc, j:j + 1],
                                                              op0=ALU.mult, op1=ALU.add), reads=[hb, Acoef, modt], writes=[hb])
            if dbg:
                for kc in range(8):
                    k.dma(HT[kc * 128:(kc + 1) * 128, t0:t0 + nt], hb[:, kc, 0:nt], reads=[hb], writes=[HT])
            rc = rc_p()
            rs = rs_p()
            k.dma(rc[:, 0:nt], ropec_in[:, t0:t0 + nt], reads=[ropec_in], writes=[rc])
            k.dma(rs[:, 0:nt], ropes_in[:, t0:t0 + nt], reads=[ropes_in], writes=[rs])
            for (kind, c0, ncol) in groups:
                wt = wt_p()
                for kc in range(8):
                    k.dma(wt[:, kc, 0:ncol], iwv[:, kc, c0:c0 + ncol], reads=[w["in_w"]], writes=[wt])
                if kind in ("q", "k"):
                    wr = wr_p()
                    for kc in range(8):
                        k.dma(wr[:, kc, 0:ncol], irv[:, kc, c0:c0 + ncol], reads=[w["in_wr"]], writes=[wr])
                if kind == "v":
                    for ts in range(nt // 128):
                        ps = k.next_ps()
                        for kc in range(8):
                            k.mm(ps, ps[:, 0:128], hb, hb[:, kc, ts * 128:(ts + 1) * 128], wt, wt[:, kc, 0:128], kc == 0, kc == 7)
                        vt = vt_p()
                        k.op("act", lambda e, ps=ps, vt=vt: e.copy(vt[:], ps[:, 0:128]), reads=[ps], writes=[vt])
                        k.dma(VTOK[t0 + ts * 128:t0 + (ts + 1) * 128, :], vt[:], reads=[vt], writes=[VTOK])
                    continue
                for cc in range(ncol // 128):
                    ps = k.next_ps()
                    for kc in range(8):
                        k.mm(ps, ps[:, 0:nt], wt, wt[:, kc, cc * 128:(cc + 1) * 128], hb, hb[:, kc, 0:nt], kc == 0, kc == 7)
                    ev = ev_p()
                    col = c0 + cc * 128
                    if kind in ("q", "k"):
                        ps2 = k.next_ps()
                        for kc in range(8):
                            k.mm(ps2, ps2[:, 0:nt], wr, wr[:, kc, cc * 128:(cc + 1) * 128], hb, hb[:, kc, 0:nt], kc == 0, kc == 7)
                        e2 = e2_p()
                        k.op("dve", lambda e, ps=ps, ev=ev: e.tensor_tensor(out=ev[:, 0:nt], in0=ps[:, 0:nt], in1=rc[:, 0:nt], op=ALU.mult),
                             reads=[ps, rc], writes=[ev])
                        k.op("dve", lambda e, ps2=ps2, e2=e2: e.tensor_tensor(out=e2[:, 0:nt], in0=ps2[:, 0:nt], in1=rs[:, 0:nt], op=ALU.mult),
                             reads=[ps2, rs], writes=[e2])
                        k.op("pool", lambda e, ev=ev, e2=e2: e.tensor_tensor(out=ev[:, 0:nt], in0=ev[:, 0:nt], in1=e2[:, 0:nt], op=ALU.add),
                             reads=[ev, e2], writes=[ev])
                        if kind == "q":
                            k.op("act", lambda e, ev=ev: e.mul(ev[:, 0:nt], ev[:, 0:nt], 0.125), reads=[ev], writes=[ev])
                            dst = QT[col:col + 128, t0:t0 + nt]
                            dres = QT
                        else:
                            dst = KT[0:128, t0:t0 + nt]
                            dres = KT
                    elif kind == "gate":
                        gi = (col - 2816) // 128
                        k.op("act", lambda e, ps=ps, ev=ev, gi=gi: e.activation(out=ev[:, 0:nt], in_=ps[:, 0:nt], func=AF.Sigmoid, bias=gb[:, gi:gi + 1]),
                             reads=[ps, gb], writes=[ev])
                        dst = GATE[col - 2816:col - 2816 + 128, t0:t0 + nt]
                        dres = GATE
                    else:
                        if cc % 2 == 0:
                            k.op("act", lambda e, ps=ps, ev=ev: e.copy(ev[:, 0:nt], ps[:, 0:nt]), reads=[ps], writes=[ev])
                        else:
                            k.op("dve", lambda e, ps=ps, ev=ev: e.tensor_copy(ev[:, 0:nt], ps[:, 0:nt]), reads=[ps], writes=[ev])
                        if kind == "s5":
                            dst = US5[col - 768:col - 768 + 128, t0:t0 + nt]
                            dres = US5
                        else:
                            dst = ZHY[col - 1280:col - 1280 + 128, t0:t0 + nt]
                            dres = ZHY
                    k.dma(dst, ev[:, 0:nt], reads=[ev], writes=[dres])
        k.phase_end()
        if stop_after == "B":
            break

        need_ctx = l < DEPTH - 1
        k.phase_begin()
        amask = k.sb("amask", [128, 256])
        k.dma(amask[:], amask_in[:, :], reads=[amask_in], writes=[amask])
        sinkb = k.sb("sinkb", [128, 8])
        k.dma(sinkb[:], w["sink"][:, :], reads=[w["sink"]], writes=[sinkb])
        kT_p = k.pool("kT", [64, NT], 1)
        v_p = k.pool("vtk", [128, NT // 128, 64], 1)
        qT_p = k.pool("qT", [64, NT], 2)
        ao_p = k.pool("ao", [64, NT], 2)
        S_p = k.pool("S", [128, 640], 4)
        PT_p = k.pool("PT", [128, 5, 128], 4)
        st_p = k.pool("stat", [128, 8], 6)
        vview = VTOK.t.rearrange("(n p) c -> p n c", p=128)
        for kv in range(2):
            kT = kT_p()
            k.dma(kT[:], KT[kv * 64:(kv + 1) * 64, :], reads=[KT], writes=[kT])
            vt = v_p()
            k.dma(vt[:], vview[:, :, kv * 64:(kv + 1) * 64], reads=[VTOK], writes=[vt])
            for hg in range(4):
                hd = kv * 4 + hg
                qT = qT_p()
                k.dma(qT[:], QT[hd * 64:(hd + 1) * 64, :], reads=[QT], writes=[qT])
                ao = ao_p()
                blocks = ([("c", 0), ("c", 1)] if need_ctx else []) + [("l", n) for n in range(32)]
                def unit_gen(bt, n):
                    q0 = n * 128 if bt == "c" else C + n * 128
                    if bt == "c":
                        kts = []
                    else:
                        kts = [j for j in (n - 1, n, n + 1) if 0 <= j < 32]
                    nloc = len(kts) * 128
                    wtot = 256 + nloc
                    S = S_p()
                    psc = k.next_ps()
                    k.mm(psc, psc[:, 0:256], qT, qT[:, q0:q0 + 128], kT, kT[:, 0:256], True, True)
                    yield
                    k.op("act", lambda e, S=S, psc=psc: e.copy(S[:, 0:256], psc[:, 0:256]), reads=[psc], writes=[S])
                    yield
                    if nloc:
                        psl = k.next_ps()
                        k0 = C + kts[0] * 128
                        k.mm(psl, psl[:, 0:nloc], qT, qT[:, q0:q0 + 128], kT, kT[:, k0:k0 + nloc], True, True)
                        yield
                        for ji, j in enumerate(kts):
                            dst = S[:, 256 + ji * 128:256 + (ji + 1) * 128]
                            src = psl[:, ji * 128:(ji + 1) * 128]
                            if j == n:
                                k.op("dve", lambda e, dst=dst, src=src: e.tensor_copy(dst, src), reads=[psl], writes=[S])
                                yield
                            else:
                                mko = 0 if j < n else 128
                                k.op("dve", lambda e, dst=dst, src=src, mko=mko: e.tensor_tensor(out=dst, in0=src, in1=amask[:, mko:mko + 128], op=ALU.add),
                                     reads=[psl, amask], writes=[S])
                                yield
                    stt = st_p()
                    k.op("dve", lambda e, S=S, stt=stt: e.tensor_reduce(out=stt[:, 0:1], in_=S[:, 0:wtot], axis=AX.X, op=ALU.max), reads=[S], writes=[stt])
                    yield
                    k.op("dve", lambda e, stt=stt: e.tensor_tensor(out=stt[:, 0:1], in0=stt[:, 0:1], in1=sinkb[:, hd:hd + 1], op=ALU.max), reads=[stt, sinkb], writes=[stt])
                    yield
                    k.op("dve", lambda e, stt=stt: e.tensor_scalar(stt[:, 1:2], stt[:, 0:1], -1.0, None, op0=ALU.mult), reads=[stt], writes=[stt])
                    yield
                    k.op("act", lambda e, S=S, stt=stt: e.activation(out=S[:, 0:wtot], in_=S[:, 0:wtot], func=AF.Exp, bias=stt[:, 1:2], accum_out=stt[:, 2:3]),
                         reads=[S, stt], writes=[S, stt])
                    yield
                    k.op("act", lambda e, stt=stt: e.activation(out=stt[:, 3:4], in_=sinkb[:, hd:hd + 1], func=AF.Exp, bias=stt[:, 1:2]), reads=[stt, sinkb], writes=[stt])
                    yield
                    k.op("dve", lambda e, stt=stt: e.tensor_tensor(out=stt[:, 4:5], in0=stt[:, 2:3], in1=stt[:, 3:4], op=ALU.add), reads=[stt], writes=[stt])
                    yield
                    k.op("dve", lambda e, stt=stt: e.reciprocal(stt[:, 5:6], stt[:, 4:5]), reads=[stt], writes=[stt])
                    yield
                    k.op("dve", lambda e, S=S, stt=stt: e.tensor_scalar(S[:, 0:wtot], S[:, 0:wtot], stt[:, 5:6], None, op0=ALU.mult), reads=[S, stt], writes=[S])
                    yield
                    nkt = wtot // 128
                    PT = PT_p()
                    for ti in range(nkt):
                        pst = k.next_ps()
                        k.op("pe", lambda e, pst=pst, S=S, ti=ti: e.transpose(pst[:, 0:128], S[:, ti * 128:(ti + 1) * 128], ident[:]),
                             reads=[S, ident], writes=[pst])
                        yield
                        if ti % 2 == 0:
                            k.op("act", lambda e, PT=PT, pst=pst, ti=ti: e.copy(PT[:, ti, :], pst[:, 0:128]), reads=[pst], writes=[PT])
                            yield
                        else:
                            k.op("dve", lambda e, PT=PT, pst=pst, ti=ti: e.tensor_copy(PT[:, ti, :], pst[:, 0:128]), reads=[pst], writes=[PT])
                            yield
                    pso = k.next_ps()
                    vtiles = [0, 1] + [2 + j for j in kts]
                    for ti, vti in enumerate(vtiles):
                        k.mm(pso, pso[0:64, 0:128], vt, vt[:, vti, :], PT, PT[:, ti, :], ti == 0, ti == nkt - 1)
                        yield
                    k.op("act", lambda e, ao=ao, pso=pso, q0=q0: e.copy(ao[:, q0:q0 + 128], pso[0:64, 0:128]), reads=[pso], writes=[ao])
                    yield
                for bi_ in range(0, len(blocks), 2):
                    gens = [unit_gen(*blk) for blk in blocks[bi_:bi_ + 2]]
                    while gens:
                        for g_ in list(gens):
                            try:
                                next(g_)
                            except StopIteration:
                                gens.remove(g_)
                lo = 0 if need_ctx else C
                k.dma(ATT[hd * 64:(hd + 1) * 64, lo:NT], ao[:, lo:NT], reads=[ao], writes=[ATT])
        k.phase_end()
        if stop_after == "C":
            break

        k.phase_begin()
        fb = k.sb("hy_fb", [64, 4])
        k.dma(fb[:], w["hy_fb"][:, :], reads=[w["hy_fb"]], writes=[fb])
        fbb = k.sb("hy_fbb", [64, 2])
        k.op("dve", lambda e: e.tensor_tensor(out=fbb[:, 0:1], in0=fb[:, 0:1], in1=fb[:, 1:2], op=ALU.mult), reads=[fb], writes=[fbb])
        k.op("dve", lambda e: e.tensor_tensor(out=fbb[:, 1:2], in0=fb[:, 2:3], in1=fb[:, 3:4], op=ALU.mult), reads=[fb], writes=[fbb])
        w1 = k.sb("hy_w1", [33, 64])
        k.dma(w1[:], w["hy_w1"][:, :], reads=[w["hy_w1"]], writes=[w1])
        w2 = k.sb("hy_w2", [64, 64])
        k.dma(w2[:], w["hy_w2"][:, :], reads=[w["hy_w2"]], writes=[w2])
        w3 = k.sb("hy_w3", [64, 1024])
        k.dma(w3[:], w["hy_w3"][:, :], reads=[w["hy_w3"]], writes=[w3])
        rate = k.sb("hy_rate", [128, 512])
        k.dma(rate[:], hyrate_in[:, :], reads=[hyrate_in], writes=[rate])
        sw = k.sb("hy_sw", [128, 12, 3])
        k.dma(sw[:], w["hy_sw"][:, :, :], reads=[w["hy_sw"]], writes=[sw])
        sbias = k.sb("hy_sb", [128, 12])
        k.dma(sbias[:], w["hy_sb"][:, :], reads=[w["hy_sb"]], writes=[sbias])
        hbias = k.sb("hy_bias", [128, 4])
        k.dma(hbias[:], w["hy_bias"][:, :], reads=[w["hy_bias"]], writes=[hbias])
        m0 = k.sb("m0", [128, 1])
        k.op("pool", lambda e: e.memset(m0[:], 1.0), writes=[m0])
        k.op("pool", lambda e: e.affine_select(out=m0[:], in_=m0[:], pattern=[[0, 1]], compare_op=ALU.not_equal, fill=0.0, base=0, channel_multiplier=1),
             reads=[m0], writes=[m0])
        negpi = k.sb("negpi2", [128, 1])
        k.op("pool", lambda e: e.memset(negpi[:], -math.pi), writes=[negpi])

        def sin_mlp(dst_t, dst, ps, fcol, bcol, n, tmp_p, tmpi_p):
            a = tmp_p()
            k.op("act", lambda e: e.activation(out=a[0:64, 0:n], in_=ps[0:64, 0:n], func=AF.Identity, bias=fbb[:, bcol:bcol + 1], scale=fb[:, fcol:fcol + 1]),
                 reads=[ps, fb, fbb], writes=[a])
            ki = tmpi_p()
            k.op("dve", lambda e: e.tensor_scalar(ki[0:64, 0:n], a[0:64, 0:n], 1.0 / (2.0 * math.pi), None, op0=ALU.mult), reads=[a], writes=[ki])
            kf = tmp_p()
            k.op("dve", lambda e: e.tensor_copy(kf[0:64, 0:n], ki[0:64, 0:n]), reads=[ki], writes=[kf])
            k.op("dve", lambda e: e.scalar_tensor_tensor(out=a[0:64, 0:n], in0=kf[0:64, 0:n], scalar=-2.0 * math.pi, in1=a[0:64, 0:n], op0=ALU.mult, op1=ALU.add),
                 reads=[kf, a], writes=[a])
            k.op("dve", lambda e: e.tensor_scalar(a[0:64, 0:n], a[0:64, 0:n], 3.1415925, -3.1415925, op0=ALU.min, op1=ALU.max), reads=[a], writes=[a])
            k.op("act", lambda e: e.activation(out=dst, in_=a[0:64, 0:n], func=AF.Sin), reads=[a], writes=[dst_t])

        seqs = [("L", L, C)] + ([("C", C, 0)] if need_ctx else [])
        tmp_p = k.pool("hy_tmp", [128, 512], 3)
        tmpi_p = k.pool("hy_tmpi", [128, 512], 2, I32)
        h2T_p = k.pool("hy_h2T", [64, 512], 2)
        h1T_p = k.pool("hy_h1T", [64, 512], 2)
        zf_p = k.pool("hy_zf", [33, 512], 2)
        tw_p = k.pool("hy_tw", [128, 32], 1)
        win_p = k.pool("hy_win", [128, 512], 2)
        hfb_p = k.pool("hy_hfb", [128, 2, 512], 2)
        hsd_p = k.pool("hy_hsd", [128, 2, 512], 2)
        zc_p = k.pool("hy_zc", [128, 3, 512 + 2], 2)
        zo_p = k.pool("hy_zo", [128, 3, 512], 2)
        vxt_p = k.pool("hy_vxt", [128, 512], 2)
        for (nm, Ls, toff) in seqs:
            HSD, VXT, VXF, X0F = HSDd[nm], VXTd[nm], VXFd[nm], X0Fd[nm]
            ntile = Ls // 128
            nb = max(1, Ls // 512)
            bw = min(512, Ls)
            tw = tw_p()
            k.dma(tw[:, 0:ntile], hytw_in[nm][:, :], reads=[hytw_in[nm]], writes=[tw])
            for bi in range(nb):
                zf = zf_p()
                k.dma(zf[:, 0:bw], hyz_in[nm][:, bi * bw:(bi + 1) * bw], reads=[hyz_in[nm]], writes=[zf])
                ps1 = k.next_ps()
                k.mm(ps1, ps1[0:64, 0:bw], w1, w1[:, :], zf, zf[:, 0:bw], True, True)
                h1T = h1T_p()
                sin_mlp(h1T, h1T[:, 0:bw], ps1, 1, 0, bw, tmp_p, tmpi_p)
                ps2 = k.next_ps()
                k.mm(ps2, ps2[0:64, 0:bw], w2, w2[:, :], h1T, h1T[:, 0:bw], True, True)
                h2T = h2T_p()
                sin_mlp(h2T, h2T[:, 0:bw], ps2, 3, 1, bw, tmp_p, tmpi_p)
                for ts in range(bw // 128):
                    tt = bi * (bw // 128) + ts
                    win = win_p()
                    k.op("act", lambda e, win=win, tt=tt: e.activation(out=win[:], in_=rate[:], func=AF.Exp, scale=tw[:, tt:tt + 1]), reads=[rate, tw], writes=[win])
                    hfb = hfb_p()
                    for d in range(2):
                        psf = k.next_ps()
                        k.mm(psf, psf[:, :], h2T, h2T[:, ts * 128:(ts + 1) * 128], w3, w3[:, d * 512:(d + 1) * 512], True, True)
                        k.op("dve", lambda e, hfb=hfb, psf=psf, win=win, d=d: e.tensor_tensor(out=hfb[:, d, :], in0=psf[:, :], in1=win[:], op=ALU.mult),
                             reads=[psf, win], writes=[hfb])
                    if tt == 0:
                        k.op("dve", lambda e, hfb=hfb: e.tensor_scalar(hfb[:, 1, :], hfb[:, 1, :], m0[:, 0:1], None, op0=ALU.mult), reads=[hfb, m0], writes=[hfb])
                    hsd = hsd_p()
                    k.op("dve", lambda e, hsd=hsd, hfb=hfb: e.tensor_tensor(out=hsd[:, 0, :], in0=hfb[:, 0, :], in1=hfb[:, 1, :], op=ALU.add), reads=[hfb], writes=[hsd])
                    k.op("pool", lambda e, hsd=hsd, hfb=hfb: e.tensor_tensor(out=hsd[:, 1, :], in0=hfb[:, 0, :], in1=hfb[:, 1, :], op=ALU.subtract), reads=[hfb], writes=[hsd])
                    for d in range(2):
                        k.dma(HSD[d, tt * 128:(tt + 1) * 128, :], hsd[:, d, :], reads=[hsd], writes=[HSD])
            for cc in range(4):
                for bi in range(nb):
                    zc = zc_p()
                    k.op("pool", lambda e, zc=zc: e.memset(zc[:], 0.0), writes=[zc])
                    lo = bi * bw
                    a0 = max(lo - 1, 0)
                    a1 = min(lo + bw + 1, Ls)
                    for pj in range(3):
                        r0 = pj * 512 + cc * 128
                        k.dma(zc[:, pj, (a0 - (lo - 1)):(a1 - (lo - 1))], ZHY[r0:r0 + 128, toff + a0:toff + a1], reads=[ZHY], writes=[zc])
                    zo = zo_p()
                    for pj in range(3):
                        ci = pj * 4 + cc
                        k.op("dve", lambda e, zo=zo, zc=zc, pj=pj, ci=ci: e.tensor_scalar(zo[:, pj, 0:bw], zc[:, pj, 0:bw], sw[:, ci, 0:1], sbias[:, ci:ci + 1], op0=ALU.mult, op1=ALU.add),
                             reads=[zc, sw, sbias], writes=[zo])
                        for tap in (1, 2):
                            k.op("dve", lambda e, zo=zo, zc=zc, pj=pj, ci=ci, tap=tap: e.scalar_tensor_tensor(out=zo[:, pj, 0:bw], in0=zc[:, pj, tap:tap + bw], scalar=sw[:, ci, tap:tap + 1],
                                                                                                                in1=zo[:, pj, 0:bw], op0=ALU.mult, op1=ALU.add), reads=[zc, sw, zo], writes=[zo])
                    k.op("pool", lambda e, zo=zo: e.tensor_tensor(out=zo[:, 2, 0:bw], in0=zo[:, 2, 0:bw], in1=zo[:, 1, 0:bw], op=ALU.mult), reads=[zo], writes=[zo])
                    k.dma(X0F[cc * 128:(cc + 1) * 128, lo:lo + bw], zo[:, 0, 0:bw], reads=[zo], writes=[X0F])
                    k.dma(VXF[cc * 128:(cc + 1) * 128, lo:lo + bw], zo[:, 2, 0:bw], reads=[zo], writes=[VXF])
                    for ts in range(bw // 128):
                        pst = k.next_ps()
                        k.op("pe", lambda e, pst=pst, zo=zo, ts=ts: e.transpose(pst[:, 0:128], zo[:, 2, ts * 128:(ts + 1) * 128], ident[:]), reads=[zo, ident], writes=[pst])
                        vxt = vxt_p()
                        k.op("act", lambda e, vxt=vxt, pst=pst: e.copy(vxt[:, 0:128], pst[:, 0:128]), reads=[pst], writes=[vxt])
                        t0_ = lo + ts * 128
                        k.dma(VXT[t0_:t0_ + 128, cc * 128:(cc + 1) * 128], vxt[:, 0:128], reads=[vxt], writes=[VXT])
        k.phase_end()
        for (nm, Ls, toff) in seqs:
            k.phase_begin()
            HSD, KRI, VXT, VXF, X0F = HSDd[nm], KRId[nm], VXTd[nm], VXFd[nm], X0Fd[nm]
            ntile = Ls // 128
            nb = max(1, Ls // 512)
            bw = min(512, Ls)
            hbias = k.sb("hy_bias", [128, 4])
            k.dma(hbias[:], w["hy_bias"][:, :], reads=[w["hy_bias"]], writes=[hbias])
            Mx = DFT[nm]
            src_p = k.pool("hy_src_" + nm, [128, ntile, 256], 1)
            Y_p = k.pool("hy_Y_" + nm, [128, ntile, 2, 256], 1)
            slab_p = k.pool("hy_slab_" + nm, [128, min(8, ntile), 2, 128], 2)
            kri_p = k.pool("hy_kri_" + nm, [128, 2, 256], 2)
            pw_p = k.pool("hy_pw_" + nm, [128, 4, 256], 2)
            mt_p = k.pool("hy_mt_" + nm, [128, 2, 512], 3)
            ep_p = k.pool("hy_ep_" + nm, [128, 3, 512], 2)
            tg = min(8, ntile)
            for stage in ("kern", "conv"):
                for hh in range(2):
                    cs = slice(hh * 256, (hh + 1) * 256)
                    if stage == "kern":
                        mats = (Mx["CK"], Mx["SK"])
                        for d in range(2):
                            src = src_p()
                            k.dma(src[:, :, :], HSD[d, 0:Ls, cs].rearrange("(n p) c -> p n c", p=128), reads=[HSD], writes=[src])
                            for ft in range(ntile):
                                psK = k.next_ps()
                                for g in range(ntile // tg):
                                    slab = slab_p()
                                    k.dma(slab[:, :, 0, :], mats[d][g * tg * 128:(g + 1) * tg * 128, ft * 128:(ft + 1) * 128].rearrange("(n p) f -> p n f", p=128),
                                          reads=[mats[d]], writes=[slab])
                                    for ti in range(tg):
                                        tt = g * tg + ti
                                        k.mm(psK, psK[:, 0:256], slab, slab[:, ti, 0, :], src, src[:, tt, :], tt == 0, tt == ntile - 1)
                                kri = kri_p()
                                k.op("act", lambda e, kri=kri, psK=psK: e.copy(kri[:, 0, :], psK[:, 0:256]), reads=[psK], writes=[kri])
                                k.dma(KRI[d, ft * 128:(ft + 1) * 128, cs], kri[:, 0, :], reads=[kri], writes=[KRI])
                        continue
                    mats = (Mx["CS"], Mx["SS"])
                    src = src_p()
                    k.dma(src[:, :, :], VXT[0:Ls, cs].rearrange("(n p) c -> p n c", p=128), reads=[VXT], writes=[src])
                    Y = Y_p()
                    for ft in range(ntile):
                        psR = k.next_ps()
                        psI = k.next_ps()
                        for g in range(ntile // tg):
                            slab = slab_p()
                            for d in range(2):
                                k.dma(slab[:, :, d, :], mats[d][g * tg * 128:(g + 1) * tg * 128, ft * 128:(ft + 1) * 128].rearrange("(n p) f -> p n f", p=128),
                                      reads=[mats[d]], writes=[slab])
                            for ti in range(tg):
                                tt = g * tg + ti
                                k.mm(psR, psR[:, 0:256], slab, slab[:, ti, 0, :], src, src[:, tt, :], tt == 0, tt == ntile - 1)
                                k.mm(psI, psI[:, 0:256], slab, slab[:, ti, 1, :], src, src[:, tt, :], tt == 0, tt == ntile - 1)
                        kri = kri_p()
                        for d in range(2):
                            k.dma(kri[:, d, :], KRI[d, ft * 128:(ft + 1) * 128, cs], reads=[KRI], writes=[kri])
                        pw = pw_p()
                        k.op("dve", lambda e, pw=pw, psR=psR, kri=kri: e.tensor_tensor(out=pw[:, 0, :], in0=psR[:, 0:256], in1=kri[:, 0, :], op=ALU.mult), reads=[psR, kri], writes=[pw])
                        k.op("dve", lambda e, pw=pw, psI=psI, kri=kri: e.tensor_tensor(out=pw[:, 1, :], in0=psI[:, 0:256], in1=kri[:, 1, :], op=ALU.mult), reads=[psI, kri], writes=[pw])
                        k.op("dve", lambda e, pw=pw, psR=psR, kri=kri: e.tensor_tensor(out=pw[:, 2, :], in0=psR[:, 0:256], in1=kri[:, 1, :], op=ALU.mult), reads=[psR, kri], writes=[pw])
                        k.op("dve", lambda e, pw=pw, psI=psI, kri=kri: e.tensor_tensor(out=pw[:, 3, :], in0=psI[:, 0:256], in1=kri[:, 0, :], op=ALU.mult), reads=[psI, kri], writes=[pw])
                        k.op("pool", lambda e, Y=Y, pw=pw, ft=ft: e.tensor_tensor(out=Y[:, ft, 0, :], in0=pw[:, 0, :], in1=pw[:, 1, :], op=ALU.subtract), reads=[pw], writes=[Y])
                        k.op("pool", lambda e, Y=Y, pw=pw, ft=ft: e.tensor_tensor(out=Y[:, ft, 1, :], in0=pw[:, 2, :], in1=pw[:, 3, :], op=ALU.add), reads=[pw], writes=[Y])
                    if stage == "conv":
                        Nfft = 2 * Ls
                        for nbk in range(nb):
                            pso = [k.next_ps(), k.next_ps()]
                            for ft in range(ntile):
                                mt = mt_p()
                                for d in range(2):
                                    k.dma(mt[:, d, 0:bw], mats[d][ft * 128:(ft + 1) * 128, nbk * bw:(nbk + 1) * bw], reads=[mats[d]], writes=[mt])
                                for c2 in range(2):
                                    for d in range(2):
                                        k.mm(pso[c2], pso[c2][:, 0:bw], Y, Y[:, ft, d, c2 * 128:(c2 + 1) * 128], mt, mt[:, d, 0:bw], ft == 0 and d == 0, ft == ntile - 1 and d == 1)
                            for c2 in range(2):
                                ch = hh * 2 + c2
                                ep = ep_p()
                                k.dma(ep[:, 0, 0:bw], VXF[ch * 128:(ch + 1) * 128, nbk * bw:(nbk + 1) * bw], reads=[VXF], writes=[ep])
                                k.dma(ep[:, 1, 0:bw], X0F[ch * 128:(ch + 1) * 128, nbk * bw:(nbk + 1) * bw], reads=[X0F], writes=[ep])
                                k.op("dve", lambda e, ep=ep, ch=ch: e.tensor_scalar(ep[:, 0, 0:bw], ep[:, 0, 0:bw], hbias[:, ch:ch + 1], None, op0=ALU.mult), reads=[ep, hbias], writes=[ep])
                                k.op("dve", lambda e, ep=ep, c2=c2: e.scalar_tensor_tensor(out=ep[:, 2, 0:bw], in0=pso[c2][:, 0:bw], scalar=-2.0 / Nfft, in1=ep[:, 0, 0:bw], op0=ALU.mult, op1=ALU.add),
                                     reads=[pso[c2], ep], writes=[ep])
                                k.op("pool", lambda e, ep=ep: e.tensor_tensor(out=ep[:, 2, 0:bw], in0=ep[:, 2, 0:bw], in1=ep[:, 1, 0:bw], op=ALU.mult), reads=[ep], writes=[ep])
                                k.dma(HY[ch * 128:(ch + 1) * 128, toff + nbk * bw:toff + (nbk + 1) * bw], ep[:, 2, 0:bw], reads=[ep], writes=[HY])
                k.barrier()
            k.phase_end()
        if stop_after == "D":
            break

        k.phase_begin()
        TWO_PI = 2.0 * math.pi
        P32 = [128, 2, 16]
        a_re = k.sb("a_re", P32); a_im = k.sb("a_im", P32); ldt = k.sb("ldt", P32)
        for t_, src_ in ((a_re, w["s5_a_re"]), (a_im, w["s5_a_im"]), (ldt, w["s5_ldt"])):
            k.dma(t_[:], src_[:, :, :], reads=[src_], writes=[t_])
        prm = k.sb("s5prm", [128, 12, 32])
        fl = lambda t_: t_.t[:, :, :].rearrange("p a b -> p (a b)")
        R_ = lambda i: prm[:, i, :]
        ki32 = k.sb("ki32", [128, 32], I32)
        kf32 = k.sb("kf32", [128, 32])
        hpi = k.sb("hpi", [128, 1])
        k.op("pool", lambda e: e.memset(hpi[:], 0.0), writes=[hpi])

        def reduce_angle(dst, src, shift):
            k.op("dve", lambda e: e.tensor_scalar(R_(8), src, shift, None, op0=ALU.add), reads=[prm], writes=[prm])
            k.op("dve", lambda e: e.tensor_scalar(ki32[:], R_(8), 1.0 / TWO_PI, None, op0=ALU.mult), reads=[prm], writes=[ki32])
            k.op("dve", lambda e: e.tensor_copy(kf32[:], ki32[:]), reads=[ki32], writes=[kf32])
            k.op("dve", lambda e: e.scalar_tensor_tensor(out=dst, in0=kf32[:], scalar=-TWO_PI, in1=R_(8), op0=ALU.mult, op1=ALU.add), reads=[kf32, prm], writes=[prm])
            k.op("dve", lambda e: e.tensor_scalar(dst, dst, 3.1415925, -3.1415925, op0=ALU.min, op1=ALU.max), reads=[prm], writes=[prm])

        k.op("dve", lambda e: e.tensor_scalar(R_(0), fl(a_re), -1e-4, None, op0=ALU.min), reads=[a_re], writes=[prm])
        k.op("act", lambda e: e.activation(out=R_(1), in_=fl(ldt), func=AF.Exp), reads=[ldt], writes=[prm])
        k.op("dve", lambda e: e.tensor_tensor(out=R_(9), in0=R_(0), in1=R_(1), op=ALU.mult), reads=[prm], writes=[prm])
        k.op("act", lambda e: e.activation(out=R_(3), in_=R_(9), func=AF.Exp), reads=[prm], writes=[prm])
        k.op("dve", lambda e: e.tensor_tensor(out=R_(10), in0=fl(a_im), in1=R_(1), op=ALU.mult), reads=[prm, a_im], writes=[prm])
        reduce_angle(R_(2), R_(10), 0.0)
        k.op("act", lambda e: e.activation(out=R_(5), in_=R_(2), func=AF.Sin), reads=[prm], writes=[prm])
        reduce_angle(R_(11), R_(10), math.pi / 2.0)
        k.op("act", lambda e: e.activation(out=R_(4), in_=R_(11), func=AF.Sin), reads=[prm], writes=[prm])
        k.op("dve", lambda e: e.tensor_tensor(out=R_(8), in0=R_(3), in1=R_(4), op=ALU.mult), reads=[prm], writes=[prm])
        k.op("dve", lambda e: e.tensor_scalar(R_(8), R_(8), -1.0, None, op0=ALU.add), reads=[prm], writes=[prm])
        k.op("dve", lambda e: e.tensor_tensor(out=R_(9), in0=R_(3), in1=R_(5), op=ALU.mult), reads=[prm], writes=[prm])
        k.op("dve", lambda e: e.tensor_tensor(out=R_(10), in0=R_(0), in1=R_(0), op=ALU.mult), reads=[prm], writes=[prm])
        k.op("dve", lambda e: e.tensor_tensor(out=R_(11), in0=fl(a_im), in1=fl(a_im), op=ALU.mult), reads=[a_im], writes=[prm])
        k.op("dve", lambda e: e.tensor_tensor(out=R_(10), in0=R_(10), in1=R_(11), op=ALU.add), reads=[prm], writes=[prm])
        k.op("dve", lambda e: e.reciprocal(R_(10), R_(10)), reads=[prm], writes=[prm])
        k.op("dve", lambda e: e.tensor_tensor(out=R_(6), in0=R_(8), in1=R_(0), op=ALU.mult), reads=[prm], writes=[prm])
        k.op("dve", lambda e: e.tensor_tensor(out=R_(11), in0=R_(9), in1=fl(a_im), op=ALU.mult), reads=[prm, a_im], writes=[prm])
        k.op("dve", lambda e: e.tensor_tensor(out=R_(6), in0=R_(6), in1=R_(11), op=ALU.add), reads=[prm], writes=[prm])
        k.op("dve", lambda e: e.tensor_tensor(out=R_(6), in0=R_(6), in1=R_(10), op=ALU.mult), reads=[prm], writes=[prm])
        k.op("dve", lambda e: e.tensor_tensor(out=R_(7), in0=R_(9), in1=R_(0), op=ALU.mult), reads=[prm], writes=[prm])
        k.op("dve", lambda e: e.tensor_tensor(out=R_(11), in0=R_(8), in1=fl(a_im), op=ALU.mult), reads=[prm, a_im], writes=[prm])
        k.op("dve", lambda e: e.tensor_tensor(out=R_(7), in0=R_(7), in1=R_(11), op=ALU.subtract), reads=[prm], writes=[prm])
        k.op("dve", lambda e: e.tensor_tensor(out=R_(7), in0=R_(7), in1=R_(10), op=ALU.mult), reads=[prm], writes=[prm])

        tio = k.sb("tio", [128, 513])
        k.op("pool", lambda e: e.iota(tio[:], pattern=[[1, 513]], base=0, channel_multiplier=0, allow_small_or_imprecise_dtypes=True), writes=[tio])
        s5d = k.sb("s5d", [32, 16])
        k.dma(s5d[:], w["s5_d"][:, :], reads=[w["s5_d"]], writes=[s5d])
        chunks = [(0, 256)] + [(256 + 512 * i, 512) for i in range(8)]
        order = {0: list(range(9)), 1: [0] + list(range(8, 0, -1))}
        u_p = k.pool("s5u", [32, NT], 2)
        ya_p = k.pool("s5ya", [32, NT], 4)
        bc_p = k.pool("s5bc", [128, 4, 16], 2)
        B2_p = k.pool("s5B2", [128, 2, 32], 2)
        BT_p = k.pool("s5BT", [32, 2, 128], 2)
        CL_p = k.pool("s5CL", [128, 2, 32], 2)
        tb_p = k.pool("s5tb", [128, 2, 513], 2)
        ang_p = k.pool("s5ang", [128, 513], 4)
        angi_p = k.pool("s5angi", [128, 513], 2, I32)
        wk_p = k.pool("s5wk", [128, 512], 18)
        g_p = k.pool("s5g", [128, 2, 512], 4)
        h_p = k.pool("s5h", [128, 2, 512], 4)
        ini_p = k.pool("s5ini", [128, 4], 6)
        yo_p = k.pool("s5yo", [32, 512], 3)

        def rev(t_, p0, p1, a, n):
            b_ = t_.t[p0:p1, a + n - 1:a + n]
            return bass.AP(tensor=b_.tensor, offset=b_.offset, ap=[list(b_.ap[0]), [-1, n]])

        for st in range(16):
            u = u_p()
            k.dma(u[:], US5[st * 32:(st + 1) * 32, :], reads=[US5], writes=[u])
            yad = [ya_p(), ya_p()]
            ctxd = {}
            for d in range(2):
                col = d * 16 + st
                bc = bc_p()
                for i_, nm_ in enumerate(("s5_b_re", "s5_b_im", "s5_c_re", "s5_c_im")):
                    k.dma(bc[:, i_, :], w[nm_][d, st * 128:(st + 1) * 128, :], reads=[w[nm_]], writes=[bc])
                B2 = B2_p()
                CL = CL_p()
                k.op("pool", lambda e, B2=B2: e.memset(B2[:], 0.0), writes=[B2])
                k.op("pool", lambda e, CL=CL: e.memset(CL[:], 0.0), writes=[CL])
                wk = wk_p()
                for gl in range(2):
                    ps_ = slice(gl * 64, (gl + 1) * 64)
                    cs_ = slice(gl * 16, (gl + 1) * 16)
                    kr = prm[ps_, 6, col:col + 1]
                    kim = prm[ps_, 7, col:col + 1]
                    k.op("dve", lambda e, wk=wk, bc=bc, ps_=ps_, kim=kim: e.tensor_scalar(wk[ps_, 0:16], bc[ps_, 1, :], kim, None, op0=ALU.mult), reads=[bc, prm], writes=[wk])
                    k.op("dve", lambda e, wk=wk, bc=bc, ps_=ps_, kim=kim: e.tensor_scalar(wk[ps_, 16:32], bc[ps_, 0, :], kim, None, op0=ALU.mult), reads=[bc, prm], writes=[wk])
                    k.op("dve", lambda e, B2=B2, wk=wk, bc=bc, ps_=ps_, cs_=cs_, kr=kr: e.scalar_tensor_tensor(out=B2[ps_, 0, cs_], in0=bc[ps_, 0, :], scalar=kr, in1=wk[ps_, 0:16], op0=ALU.mult, op1=ALU.subtract),
                         reads=[bc, prm, wk], writes=[B2])
                    k.op("dve", lambda e, B2=B2, wk=wk, bc=bc, ps_=ps_, cs_=cs_, kr=kr: e.scalar_tensor_tensor(out=B2[ps_, 1, cs_], in0=bc[ps_, 1, :], scalar=kr, in1=wk[ps_, 16:32], op0=ALU.mult, op1=ALU.add),
                         reads=[bc, prm, wk], writes=[B2])
                    k.op("dve", lambda e, CL=CL, bc=bc, ps_=ps_, cs_=cs_: e.tensor_copy(CL[ps_, 0, cs_], bc[ps_, 2, :]), reads=[bc], writes=[CL])
                    k.op("dve", lambda e, CL=CL, bc=bc, ps_=ps_, cs_=cs_: e.tensor_scalar(CL[ps_, 1, cs_], bc[ps_, 3, :], -1.0, None, op0=ALU.mult), reads=[bc], writes=[CL])
                BT = BT_p()
                for ri in range(2):
                    pst = k.next_ps()
                    k.op("pe", lambda e, pst=pst, B2=B2, ri=ri: e.transpose(pst[0:32, 0:128], B2[:, ri, :], ident[:]), reads=[B2, ident], writes=[pst])
                    k.op("act", lambda e, BT=BT, pst=pst, ri=ri: e.copy(BT[:, ri, :], pst[0:32, 0:128]), reads=[pst], writes=[BT])
                tb = tb_p()
                for ti_, shift in ((1, 0.0), (0, math.pi / 2.0)):
                    ang = ang_p()
                    k.op("dve", lambda e, ang=ang, shift=shift: e.tensor_scalar(ang[:], tio[:], prm[:, 2, col:col + 1], shift, op0=ALU.mult, op1=ALU.add), reads=[tio, prm], writes=[ang])
                    angi = angi_p()
                    k.op("dve", lambda e, ang=ang, angi=angi: e.tensor_scalar(angi[:], ang[:], 1.0 / TWO_PI, None, op0=ALU.mult), reads=[ang], writes=[angi])
                    angf = ang_p()
                    k.op("pool", lambda e, angf=angf, angi=angi: e.tensor_copy(angf[:], angi[:]), reads=[angi], writes=[angf])
                    k.op("dve", lambda e, ang=ang, angf=angf: e.scalar_tensor_tensor(out=ang[:], in0=angf[:], scalar=-TWO_PI, in1=ang[:], op0=ALU.mult, op1=ALU.add), reads=[angf, ang], writes=[ang])
                    k.op("dve", lambda e, ang=ang: e.tensor_scalar(ang[:], ang[:], 3.1415925, -3.1415925, op0=ALU.min, op1=ALU.max), reads=[ang], writes=[ang])
                    k.op("act", lambda e, tb=tb, ang=ang, ti_=ti_: e.activation(out=tb[:, ti_, :], in_=ang[:], func=AF.Sin), reads=[ang], writes=[tb])
                ctxd[d] = dict(col=col, BT=BT, CL=CL, tb=tb, rho_b=prm[:, 3, col:col + 1], prev=None)
            def chunk_gen(d, step):
                cx = ctxd[d]
                col, BT, CL, tb, rho_b, prev, ya = cx["col"], cx["BT"], cx["CL"], cx["tb"], cx["rho_b"], cx["prev"], yad[d]
                ci = order[d][step]
                c0, n = chunks[ci]
                rhs_u = u[:, c0:c0 + n] if d == 0 else rev(u, 0, 32, c0, n)
                pbr = k.next_ps()
                pbi = k.next_ps()
                k.mm(pbr, pbr[:, 0:n], BT, BT[:, 0, :], u, rhs_u, True, True)
                yield
                k.mm(pbi, pbi[:, 0:n], BT, BT[:, 1, :], u, rhs_u, True, True)
                yield
                cosT = tb[:, 0, 0:n]
                sinT = tb[:, 1, 0:n]
                w0, w1, w2, w3_ = wk_p(), wk_p(), wk_p(), wk_p()
                k.op("dve", lambda e, w0=w0, pbr=pbr, cosT=cosT: e.tensor_tensor(out=w0[:, 0:n], in0=pbr[:, 0:n], in1=cosT, op=ALU.mult), reads=[pbr, tb], writes=[w0])
                yield
                k.op("dve", lambda e, w1=w1, pbi=pbi, sinT=sinT: e.tensor_tensor(out=w1[:, 0:n], in0=pbi[:, 0:n], in1=sinT, op=ALU.mult), reads=[pbi, tb], writes=[w1])
                yield
                k.op("dve", lambda e, w2=w2, pbi=pbi, cosT=cosT: e.tensor_tensor(out=w2[:, 0:n], in0=pbi[:, 0:n], in1=cosT, op=ALU.mult), reads=[pbi, tb], writes=[w2])
                yield
                k.op("dve", lambda e, w3_=w3_, pbr=pbr, sinT=sinT: e.tensor_tensor(out=w3_[:, 0:n], in0=pbr[:, 0:n], in1=sinT, op=ALU.mult), reads=[pbr, tb], writes=[w3_])
                yield
                k.op("pool", lambda e, w0=w0, w1=w1: e.tensor_tensor(out=w0[:, 0:n], in0=w0[:, 0:n], in1=w1[:, 0:n], op=ALU.add), reads=[w0, w1], writes=[w0])
                yield
                k.op("pool", lambda e, w2=w2, w3_=w3_: e.tensor_tensor(out=w2[:, 0:n], in0=w2[:, 0:n], in1=w3_[:, 0:n], op=ALU.subtract), reads=[w2, w3_], writes=[w2])
                yield
                ini = ini_p()
                if prev is None:
                    k.op("pool", lambda e, ini=ini: e.memset(ini[:], 0.0), writes=[ini])
                    yield
                else:
                    gp, npv = prev
                    cr = tb[:, 0, npv:npv + 1]
                    sr = tb[:, 1, npv:npv + 1]
                    k.op("dve", lambda e, ini=ini, gp=gp, npv=npv, cr=cr: e.tensor_tensor(out=ini[:, 2:3], in0=gp[:, 0, npv - 1:npv], in1=cr, op=ALU.mult), reads=[gp, tb], writes=[ini])
                    yield
                    k.op("dve", lambda e, ini=ini, gp=gp, npv=npv, sr=sr: e.tensor_tensor(out=ini[:, 3:4], in0=gp[:, 1, npv - 1:npv], in1=sr, op=ALU.mult), reads=[gp, tb], writes=[ini])
                    yield
                    k.op("dve", lambda e, ini=ini: e.tensor_tensor(out=ini[:, 0:1], in0=ini[:, 2:3], in1=ini[:, 3:4], op=ALU.subtract), reads=[ini], writes=[ini])
                    yield
                    k.op("dve", lambda e, ini=ini, gp=gp, npv=npv, sr=sr: e.tensor_tensor(out=ini[:, 2:3], in0=gp[:, 0, npv - 1:npv], in1=sr, op=ALU.mult), reads=[gp, tb], writes=[ini])
                    yield
                    k.op("dve", lambda e, ini=ini, gp=gp, npv=npv, cr=cr: e.tensor_tensor(out=ini[:, 3:4], in0=gp[:, 1, npv - 1:npv], in1=cr, op=ALU.mult), reads=[gp, tb], writes=[ini])
                    yield
                    k.op("dve", lambda e, ini=ini: e.tensor_tensor(out=ini[:, 1:2], in0=ini[:, 2:3], in1=ini[:, 3:4], op=ALU.add), reads=[ini], writes=[ini])
                    yield
                g = g_p()
                k.op("dve", lambda e, g=g, w0=w0, ini=ini: e.tensor_tensor_scan(g[:, 0, 0:n], rho_b.to_broadcast([128, n]), w0[:, 0:n], ini[:, 0:1], op0=ALU.mult, op1=ALU.add),
                     reads=[prm, w0, ini], writes=[g])
                yield
                k.op("dve", lambda e, g=g, w2=w2, ini=ini: e.tensor_tensor_scan(g[:, 1, 0:n], rho_b.to_broadcast([128, n]), w2[:, 0:n], ini[:, 1:2], op0=ALU.mult, op1=ALU.add),
                     reads=[prm, w2, ini], writes=[g])
                yield
                prev = (g, n)
                cx["prev"] = prev
                h = h_p()
                x0_, x1_, x2_, x3_ = wk_p(), wk_p(), wk_p(), wk_p()
                k.op("pool", lambda e, x0_=x0_, g=g, cosT=cosT: e.tensor_tensor(out=x0_[:, 0:n], in0=g[:, 0, 0:n], in1=cosT, op=ALU.mult), reads=[g, tb], writes=[x0_])
                yield
                k.op("pool", lambda e, x1_=x1_, g=g, sinT=sinT: e.tensor_tensor(out=x1_[:, 0:n], in0=g[:, 1, 0:n], in1=sinT, op=ALU.mult), reads=[g, tb], writes=[x1_])
                yield
                k.op("dve", lambda e, x2_=x2_, g=g, sinT=sinT: e.tensor_tensor(out=x2_[:, 0:n], in0=g[:, 0, 0:n], in1=sinT, op=ALU.mult), reads=[g, tb], writes=[x2_])
                yield
                k.op("dve", lambda e, x3_=x3_, g=g, cosT=cosT: e.tensor_tensor(out=x3_[:, 0:n], in0=g[:, 1, 0:n], in1=cosT, op=ALU.mult), reads=[g, tb], writes=[x3_])
                yield
                k.op("pool", lambda e, h=h, x0_=x0_, x1_=x1_: e.tensor_tensor(out=h[:, 0, 0:n], in0=x0_[:, 0:n], in1=x1_[:, 0:n], op=ALU.subtract), reads=[x0_, x1_], writes=[h])
                yield
                k.op("dve", lambda e, h=h, x2_=x2_, x3_=x3_: e.tensor_tensor(out=h[:, 1, 0:n], in0=x2_[:, 0:n], in1=x3_[:, 0:n], op=ALU.add), reads=[x2_, x3_], writes=[h])
                yield
                py = k.next_ps()
                for ri in range(2):
                    hb_ = h.t[:, ri, :]
                    if d == 0:
                        rhs_h = h[:, ri, 0:n]
                    else:
                        b_ = h.t[:, ri, n - 1:n]
                        rhs_h = bass.AP(tensor=b_.tensor, offset=b_.offset, ap=[list(b_.ap[0]), [-1, n]])
                    k.mm(py, py[0:32, 0:n], CL, CL[:, ri, :], h, rhs_h, ri == 0, ri == 1)
                    yield
                if d == 0:
                    k.op("act", lambda e, ya=ya, py=py, c0=c0, n=n: e.copy(ya[:, c0:c0 + n], py[0:32, 0:n]), reads=[py], writes=[ya])
                    yield
                else:
                    k.op("dve", lambda e, ya=ya, py=py, c0=c0, n=n: e.tensor_copy(ya[:, c0:c0 + n], py[0:32, 0:n]), reads=[py], writes=[ya])
                    yield
            for step in range(9):
                gens = [chunk_gen(0, step), chunk_gen(1, step)]
                while gens:
                    for g_ in list(gens):
                        try:
                            next(g_)
                        except StopIteration:
                            gens.remove(g_)
            ya = yad[0]
            for (c0, n) in chunks:
                k.op("pool", lambda e, c0=c0, n=n: e.tensor_tensor(out=yad[0][:, c0:c0 + n], in0=yad[1][:, c0:c0 + n], in1=yad[0][:, c0:c0 + n], op=ALU.add), reads=[yad[0], yad[1]], writes=[yad[0]])
                yo = yo_p()
                k.op("dve", lambda e, yo=yo, c0=c0, n=n: e.scalar_tensor_tensor(out=yo[:, 0:n], in0=u[:, c0:c0 + n], scalar=s5d[:, st:st + 1], in1=ya[:, c0:c0 + n], op0=ALU.mult, op1=ALU.add),
                     reads=[u, s5d, ya], writes=[yo])
                t1 = yo_p()
                k.op("dve", lambda e, yo=yo, t1=t1, n=n: e.tensor_tensor(out=t1[:, 0:n], in0=yo[:, 0:n], in1=yo[:, 0:n], op=ALU.mult), reads=[yo], writes=[t1])
                k.op("dve", lambda e, t1=t1, n=n: e.tensor_scalar(t1[:, 0:n], t1[:, 0:n], 0.044715, 1.0, op0=ALU.mult, op1=ALU.add), reads=[t1], writes=[t1])
                k.op("dve", lambda e, yo=yo, t1=t1, n=n: e.tensor_tensor(out=t1[:, 0:n], in0=t1[:, 0:n], in1=yo[:, 0:n], op=ALU.mult), reads=[t1, yo], writes=[t1])
                k.op("act", lambda e, t1=t1, n=n: e.activation(out=t1[:, 0:n], in_=t1[:, 0:n], func=AF.Sigmoid, scale=2.0 * math.sqrt(2.0 / math.pi)), reads=[t1], writes=[t1])
                k.op("dve", lambda e, yo=yo, t1=t1, n=n: e.tensor_tensor(out=yo[:, 0:n], in0=yo[:, 0:n], in1=t1[:, 0:n], op=ALU.mult), reads=[t1, yo], writes=[yo])
                k.dma(S5Y[st * 32:(st + 1) * 32, c0:c0 + n], yo[:, 0:n], reads=[yo], writes=[S5Y])
        k.phase_end()
        k.phase_begin()
        gw = k.sb("s5gw", [128, 4, 512])
        k.dma(gw[:], w["s5_gw"].t.rearrange("(kc p) n -> p kc n", p=128), reads=[w["s5_gw"]], writes=[gw])
        ggb = k.sb("s5gb", [128, 4])
        k.dma(ggb[:], w["s5_gb"][:, :], reads=[w["s5_gb"]], writes=[ggb])
        yb_p = k.pool("s5yb", [128, 4, 512], 2)
        go_p2 = k.pool("s5go", [128, 512], 3)
        for (c0, n) in NBLK:
            yb = yb_p()
            k.dma(yb[:, :, 0:n], S5Y[:, c0:c0 + n].rearrange("(kc p) t -> p kc t", p=128), reads=[S5Y], writes=[yb])
            for mc_ in range(4):
                ps = k.next_ps()
                for kc in range(4):
                    k.mm(ps, ps[:, 0:n], gw, gw[:, kc, mc_ * 128:(mc_ + 1) * 128], yb, yb[:, kc, 0:n], kc == 0, kc == 3)
                go = go_p2()
                k.op("act", lambda e, go=go, ps=ps, mc_=mc_: e.activation(out=go[:, 0:n], in_=ps[:, 0:n], func=AF.Sigmoid, bias=ggb[:, mc_:mc_ + 1]), reads=[ps, ggb], writes=[go])
                k.op("dve", lambda e, go=go, yb=yb, mc_=mc_: e.tensor_tensor(out=go[:, 0:n], in0=go[:, 0:n], in1=yb[:, mc_, 0:n], op=ALU.mult), reads=[go, yb], writes=[go])
                k.dma(S5O[mc_ * 128:(mc_ + 1) * 128, c0:c0 + n], go[:, 0:n], reads=[go], writes=[S5O])
        k.phase_end()
        if stop_after == "E":
            break

        k.phase_begin()
        modt = k.sb("modtF", [128, 96])
        k.dma(modt[:], MODT[:, :], reads=[MODT], writes=[modt])
        m3 = modt.t[:, :].rearrange("p (f j) -> p f j", j=2)
        brw = []
        for nm_ in ("br_attn_w", "br_hyena_w", "br_s5_w"):
            t_ = k.sb(nm_, [128, 4, D])
            k.dma(t_[:], w[nm_].t.rearrange("(kc p) n -> p kc n", p=128), reads=[w[nm_]], writes=[t_])
            brw.append(t_)
        ow = k.sb("out_w", [128, 8, D])
        k.dma(ow[:], w["out_w"].t.rearrange("(kc p) n -> p kc n", p=128), reads=[w["out_w"]], writes=[ow])
        br_p = k.pool("brin", [128, 3, 4, 512], 1)
        gt_p = k.pool("gt", [128, 3, 512], 2)
        mm_p = k.pool("mmix", [128, 8, 512], 1)
        xb_p = k.pool("xbF", [128, 8, 512], 1)
        t_p = k.pool("tF", [128, 512], 4)
        xo_p = k.pool("xoF", [128, 512], 3)
        srcs = (ATT, HY, S5O)
        for bi, (c0, n) in enumerate(NBLK):
            if bi == 0 and not need_ctx:
                continue
            j = 1 if bi == 0 else 0
            br = br_p()
            for b_ in range(3):
                k.dma(br[:, b_, :, 0:n], srcs[b_][:, c0:c0 + n].rearrange("(kc p) t -> p kc t", p=128), reads=[srcs[b_]], writes=[br])
            xb = xb_p()
            k.dma(xb[:, :, 0:n], XT[:, c0:c0 + n].rearrange("(kc p) t -> p kc t", p=128), reads=[XT], writes=[xb])
            mx = mm_p()
            for fc in range(8):
                gt = gt_p()
                for b_ in range(3):
                    k.dma(gt[:, b_, 0:n], GATE[b_ * D + fc * 128:b_ * D + (fc + 1) * 128, c0:c0 + n], reads=[GATE], writes=[gt])
                pb = [k.next_ps() for _ in range(3)]
                for b_ in range(3):
                    for kc in range(4):
                        k.mm(pb[b_], pb[b_][:, 0:n], brw[b_], brw[b_][:, kc, fc * 128:(fc + 1) * 128], br, br[:, b_, kc, 0:n], kc == 0, kc == 3)
                ta, tb_, tc_ = t_p(), t_p(), t_p()
                k.op("dve", lambda e, ta=ta, pb=pb, gt=gt: e.tensor_tensor(out=ta[:, 0:n], in0=pb[0][:, 0:n], in1=gt[:, 0, 0:n], op=ALU.mult), reads=[pb[0], gt], writes=[ta])
                k.op("dve", lambda e, tb_=tb_, pb=pb, gt=gt: e.tensor_tensor(out=tb_[:, 0:n], in0=pb[1][:, 0:n], in1=gt[:, 1, 0:n], op=ALU.mult), reads=[pb[1], gt], writes=[tb_])
                k.op("dve", lambda e, tc_=tc_, pb=pb, gt=gt: e.tensor_tensor(out=tc_[:, 0:n], in0=pb[2][:, 0:n], in1=gt[:, 2, 0:n], op=ALU.mult), reads=[pb[2], gt], writes=[tc_])
                k.op("pool", lambda e, ta=ta, tb_=tb_: e.tensor_tensor(out=ta[:, 0:n], in0=ta[:, 0:n], in1=tb_[:, 0:n], op=ALU.add), reads=[ta, tb_], writes=[ta])
                k.op("pool", lambda e, mx=mx, ta=ta, tc_=tc_, fc=fc: e.tensor_tensor(out=mx[:, fc, 0:n], in0=ta[:, 0:n], in1=tc_[:, 0:n], op=ALU.add), reads=[ta, tc_], writes=[mx])
            for dc in range(8):
                po = k.next_ps()
                for fc in range(8):
                    k.mm(po, po[:, 0:n], ow, ow[:, fc, dc * 128:(dc + 1) * 128], mx, mx[:, fc, 0:n], fc == 0, fc == 7)
                xo = xo_p()
                if dbg:
                    mo = xo_p()
                    k.op("act", lambda e, mo=mo, po=po: e.copy(mo[:, 0:n], po[:, 0:n]), reads=[po], writes=[mo])
                    k.dma(MIX[dc * 128:(dc + 1) * 128, c0:c0 + n], mo[:, 0:n], reads=[mo], writes=[MIX])
                k.op("dve", lambda e, xo=xo, po=po, xb=xb, dc=dc: e.scalar_tensor_tensor(out=xo[:, 0:n], in0=po[:, 0:n], scalar=m3[:, 16 + dc, j:j + 1], in1=xb[:, dc, 0:n], op0=ALU.mult, op1=ALU.add),
                     reads=[po, modt, xb], writes=[xo])
                k.dma(XT[dc * 128:(dc + 1) * 128, c0:c0 + n], xo[:, 0:n], reads=[xo], writes=[XT])
                if dbg:
                    k.dma(XDBG[dc * 128:(dc + 1) * 128, c0:c0 + n], xo[:, 0:n], reads=[xo], writes=[XDBG])
        k.phase_end()
        if stop_after == "F":
            break

        k.phase_begin()
        modt = k.sb("modtG", [128, 96])
        k.dma(modt[:], MODT[:, :], reads=[MODT], writes=[modt])
        m3 = modt.t[:, :].rearrange("p (f j) -> p f j", j=2)
        g2n = k.sb("g2n", [128, 8])
        k.dma(g2n[:], w["n2g"][:, :], reads=[w["n2g"]], writes=[g2n])
        A2 = k.sb("A2", [128, 8, 2])
        for j in range(2):
            k.op("dve", lambda e, j=j: e.scalar_tensor_tensor(out=A2[:, :, j], in0=m3[:, 32:40, j], scalar=1.0, in1=g2n[:], op0=ALU.add, op1=ALU.mult), reads=[modt, g2n], writes=[A2])
        wq = k.sb("peer_wq", [128, 8, 2048])
        wqv = w["peer_wq"].t.rearrange("(kc p) n -> p kc n", p=128)
        for kc in range(8):
            k.dma(wq[:, kc, :], wqv[:, kc, :], reads=[w["peer_wq"]], writes=[wq])
        kT = k.sb("peer_kT", [128, 16, 128])
        k.dma(kT[:], w["peer_kT"].t.rearrange("j k n -> k j n"), reads=[w["peer_kT"]], writes=[kT])
        csel = k.sb("csel", [128, 255])
        k.dma(csel[:], csel_in[:, :], reads=[csel_in], writes=[csel])
        io16 = k.sb("io16", [128, 16])
        k.dma(io16[:], iota16_in[:, :], reads=[iota16_in], writes=[io16])
        xb_p = k.pool("xbG", [128, 8, 128], 1)
        hb_p = k.pool("hbG", [128, 8, 128], 1)
        sq_p = k.pool("sqG", [128, 8, 128], 1)
        sm_p = k.pool("smG", [128, 128], 6)
        qT_p = k.pool("qTG", [128, 16, 128], 1)
        S_p = k.pool("SG", [128, 16, 128], 1)
        SW_p = k.pool("SWG", [128, 128], 2)
        TV_p = k.pool("TVG", [128, 16, 16], 1)
        TI_p = k.pool("TIG", [128, 16, 16], 1, U32)
        TF_p = k.pool("TFG", [128, 16, 16], 1)
        cand_p = k.pool("candG", [128, 8, 256], 1)
        cw_p = k.pool("cwG", [128, 256], 2)
        BV_p = k.pool("BVG", [128, 8, 16], 1)
        BP_p = k.pool("BPG", [128, 8, 16], 1, U32)
        BI_p = k.pool("BIG", [128, 8, 16], 2, I32)
        oh_p = k.pool("ohG", [128, 16, 16], 2)
        IDF_p = k.pool("IDFG", [128, 128], 1)
        GT_p = k.pool("GTG", [128, 128], 1)
        idT_p = k.pool("idTG", [128, 128], 1, I32)
        gT_p = k.pool("gTG", [128, 128], 1)
        htok_p = k.pool("htokG", [128, D], 1)
        ug_p = k.pool("ugG", [128, D], 6)
        vg_p = k.pool("vgG", [128, D], 6)
        dots_p = k.pool("dotsG", [128, 128], 1)
        wts_p = k.pool("wtsG", [128, 128], 1)
        wt_p = k.pool("wtG", [128, 128], 3)
        otok_p = k.pool("otokG", [128, D], 1)
        xo_p = k.pool("xoG", [128, 128], 3)
        GELU_C = 2.0 * math.sqrt(2.0 / math.pi)

        def bc3(t_ap_base, strides):
            return bass.AP(tensor=t_ap_base.tensor, offset=t_ap_base.offset, ap=[list(t_ap_base.ap[0]), [strides[0], 8], [strides[1], 16], [strides[2], 16]])

        tiles = [(t0, 1 if t0 < C else 0) for t0 in range(0 if need_ctx else C, NT, 128)]
        for ti_, (t0, j) in enumerate(tiles):
            if ti_ > 0 and ti_ % 8 == 0:
                k.barrier()
            xb = xb_p()
            k.dma(xb[:], XT[:, t0:t0 + 128].rearrange("(kc p) t -> p kc t", p=128), reads=[XT], writes=[xb])
            sq = sq_p()
            k.op("act", lambda e, sq=sq, xb=xb: e.activation(out=sq[:], in_=xb[:], func=AF.Square), reads=[xb], writes=[sq])
            pss = k.next_ps()
            for kc in range(8):
                k.mm(pss, pss[:, 0:128], ones, ones[:], sq, sq[:, kc, :], kc == 0, kc == 7)
            rstd = sm_p()
            k.op("act", lambda e, rstd=rstd, pss=pss: e.activation(out=rstd[:], in_=pss[:, 0:128], func=AF.Sqrt, bias=eps_t[:, 0:1], scale=1.0 / D), reads=[pss, eps_t], writes=[rstd])
            k.op("dve", lambda e, rstd=rstd: e.reciprocal(rstd[:], rstd[:]), reads=[rstd], writes=[rstd])
            hb = hb_p()
            for kc in range(8):
                k.op("dve", lambda e, hb=hb, xb=xb, rstd=rstd, kc=kc: e.tensor_tensor(out=hb[:, kc, :], in0=xb[:, kc, :], in1=rstd[:], op=ALU.mult), reads=[xb, rstd], writes=[hb])
                k.op("pool", lambda e, hb=hb, kc=kc: e.tensor_scalar(hb[:, kc, :], hb[:, kc, :], A2[:, kc, j:j + 1], m3[:, 24 + kc, j:j + 1], op0=ALU.mult, op1=ALU.add), reads=[hb, A2, modt], writes=[hb])
            htok = htok_p()
            for kc in range(8):
                pst = k.next_ps()
                k.op("pe", lambda e, pst=pst, hb=hb, kc=kc: e.transpose(pst[:, 0:128], hb[:, kc, :], ident[:]), reads=[hb, ident], writes=[pst])
                k.op("act", lambda e, htok=htok, pst=pst, kc=kc: e.copy(htok[:, kc * 128:(kc + 1) * 128], pst[:, 0:128]), reads=[pst], writes=[htok])
            qT = qT_p()
            for jc in range(16):
                psq = k.next_ps()
                for kc in range(8):
                    k.mm(psq, psq[:, 0:128], wq, wq[:, kc, jc * 128:(jc + 1) * 128], hb, hb[:, kc, :], kc == 0, kc == 7)
                if jc % 2 == 0:
                    k.op("act", lambda e, qT=qT, psq=psq, jc=jc: e.copy(qT[:, jc, :], psq[:, 0:128]), reads=[psq], writes=[qT])
                else:
                    k.op("dve", lambda e, qT=qT, psq=psq, jc=jc: e.tensor_copy(qT[:, jc, :], psq[:, 0:128]), reads=[psq], writes=[qT])
            S = S_p()
            for jc in range(16):
                pss2 = k.next_ps()
                k.mm(pss2, pss2[:, 0:128], qT, qT[:, jc, :], kT, kT[:, jc, :], True, True)
                if jc % 2 == 0:
                    k.op("act", lambda e, S=S, pss2=pss2, jc=jc: e.copy(S[:, jc, :], pss2[:, 0:128]), reads=[pss2], writes=[S])
                else:
                    k.op("dve", lambda e, S=S, pss2=pss2, jc=jc: e.tensor_copy(S[:, jc, :], pss2[:, 0:128]), reads=[pss2], writes=[S])
            TV = TV_p(); TI = TI_p()
            for jc in range(16):
                SW = SW_p()
                k.op("dve", lambda e, TV=TV, S=S, jc=jc: e.max(out=TV[:, jc, 0:8], in_=S[:, jc, :]), reads=[S], writes=[TV])
                k.op("dve", lambda e, TI=TI, TV=TV, S=S, jc=jc: e.max_index(out=TI[:, jc, 0:8], in_max=TV[:, jc, 0:8], in_values=S[:, jc, :]), reads=[S, TV], writes=[TI])
                k.op("dve", lambda e, SW=SW, TV=TV, S=S, jc=jc: e.match_replace(out=SW[:], in_to_replace=TV[:, jc, 0:8], in_values=S[:, jc, :], imm_value=-1e30), reads=[S, TV], writes=[SW])
                k.op("dve", lambda e, TV=TV, SW=SW, jc=jc: e.max(out=TV[:, jc, 8:16], in_=SW[:]), reads=[SW], writes=[TV])
                k.op("dve", lambda e, TI=TI, TV=TV, SW=SW, jc=jc: e.max_index(out=TI[:, jc, 8:16], in_max=TV[:, jc, 8:16], in_values=SW[:]), reads=[SW, TV], writes=[TI])
            TF = TF_p()
            k.op("dve", lambda e, TF=TF, TI=TI: e.tensor_copy(TF[:], TI[:]), reads=[TI], writes=[TF])
            cand = cand_p()
            tvb = TV.t[:, 0:1, 0:1]
            cv4 = cand.t[:, :, :].rearrange("p h (a b) -> p h a b", b=16)
            in0 = bc3(TV.t[:, 0, 0:1], (32, 1, 0))
            in1 = bc3(TV.t[:, 1, 0:1], (32, 0, 1))
            k.op("dve", lambda e, cv4=cv4, in0=in0, in1=in1: e.tensor_tensor(out=cv4, in0=in0, in1=in1, op=ALU.add), reads=[TV], writes=[cand])
            BV = BV_p(); BP = BP_p()
            for h in range(8):
                cw = cw_p()
                k.op("dve", lambda e, BV=BV, cand=cand, h=h: e.max(out=BV[:, h, 0:8], in_=cand[:, h, :]), reads=[cand], writes=[BV])
                k.op("dve", lambda e, BP=BP, BV=BV, cand=cand, h=h: e.max_index(out=BP[:, h, 0:8], in_max=BV[:, h, 0:8], in_values=cand[:, h, :]), reads=[cand, BV], writes=[BP])
                k.op("dve", lambda e, cw=cw, BV=BV, cand=cand, h=h: e.match_replace(out=cw[:], in_to_replace=BV[:, h, 0:8], in_values=cand[:, h, :], imm_value=-1e30), reads=[cand, BV], writes=[cw])
                k.op("dve", lambda e, BV=BV, cw=cw, h=h: e.max(out=BV[:, h, 8:16], in_=cw[:]), reads=[cw], writes=[BV])
                k.op("dve", lambda e, BP=BP, BV=BV, cw=cw, h=h: e.max_index(out=BP[:, h, 8:16], in_max=BV[:, h, 8:16], in_values=cw[:]), reads=[cw, BV], writes=[BP])
            bpi = BP.t[:, :, :].bitcast(I32)
            Ai = BI_p(); Bi = BI_p()
            k.op("dve", lambda e, Ai=Ai, bpi=bpi: e.tensor_single_scalar(Ai[:], bpi, 4, op=ALU.arith_shift_right), reads=[BP], writes=[Ai])
            k.op("dve", lambda e, Bi=Bi, bpi=bpi: e.tensor_single_scalar(Bi[:], bpi, 15, op=ALU.bitwise_and), reads=[BP], writes=[Bi])
            Af = sm_p(); Bf = sm_p()
            k.op("dve", lambda e, Af=Af, Ai=Ai: e.tensor_copy(Af[:], Ai.t[:, :, :].rearrange("p h k -> p (h k)")), reads=[Ai], writes=[Af])
            k.op("dve", lambda e, Bf=Bf, Bi=Bi: e.tensor_copy(Bf[:], Bi.t[:, :, :].rearrange("p h k -> p (h k)")), reads=[Bi], writes=[Bf])
            IDF = IDF_p()
            isel = sm_p(); jsel = sm_p()
            for h in range(8):
                for (posf, seg, dst) in ((Af, 2 * h, isel), (Bf, 2 * h + 1, jsel)):
                    oh = oh_p()
                    pk = posf.t[:, h * 16:(h + 1) * 16]
                    pk3 = bass.AP(tensor=pk.tensor, offset=pk.offset, ap=[list(pk.ap[0]), [1, 16], [0, 16]])
                    io3 = bass.AP(tensor=io16.t[:, 0:16].tensor, offset=io16.t[:, 0:16].offset, ap=[list(io16.t[:, 0:16].ap[0]), [0, 16], [1, 16]])
                    tf_ = TF.t[:, seg, 0:16]
                    tf3 = bass.AP(tensor=tf_.tensor, offset=tf_.offset, ap=[list(tf_.ap[0]), [0, 16], [1, 16]])
                    k.op("dve", lambda e, oh=oh, pk3=pk3, io3=io3: e.tensor_tensor(out=oh[:], in0=pk3, in1=io3, op=ALU.is_equal), reads=[posf, io16], writes=[oh])
                    k.op("dve", lambda e, oh=oh, tf3=tf3: e.tensor_tensor(out=oh[:], in0=oh[:], in1=tf3, op=ALU.mult), reads=[oh, TF], writes=[oh])
                    k.op("dve", lambda e, oh=oh, dst=dst, h=h: e.tensor_reduce(out=dst[:, h * 16:(h + 1) * 16], in_=oh[:], axis=AX.X, op=ALU.add), reads=[oh], writes=[dst])
            k.op("dve", lambda e, IDF=IDF, isel=isel, jsel=jsel: e.scalar_tensor_tensor(out=IDF[:], in0=isel[:], scalar=128.0, in1=jsel[:], op0=ALU.mult, op1=ALU.add), reads=[isel, jsel], writes=[IDF])
            k.op("dve", lambda e, IDF=IDF: e.tensor_scalar(IDF[:], IDF[:], 16383.0, 0.0, op0=ALU.min, op1=ALU.max), reads=[IDF], writes=[IDF])
            GT = GT_p()
            gst = sm_p()
            for h in range(8):
                k.op("dve", lambda e, gst=gst, BV=BV, h=h: e.tensor_scalar(gst[:, h:h + 1], BV[:, h, 0:1], -1.0, None, op0=ALU.mult), reads=[BV], writes=[gst])
                k.op("act", lambda e, GT=GT, BV=BV, gst=gst, h=h: e.activation(out=GT[:, h * 16:(h + 1) * 16], in_=BV[:, h, :], func=AF.Exp, bias=gst[:, h:h + 1], accum_out=gst[:, 8 + h:9 + h]),
                     reads=[BV, gst], writes=[GT, gst])
            k.op("dve", lambda e, gst=gst: e.reciprocal(gst[:, 16:24], gst[:, 8:16]), reads=[gst], writes=[gst])
            for h in range(8):
                k.op("dve", lambda e, GT=GT, gst=gst, h=h: e.tensor_scalar(GT[:, h * 16:(h + 1) * 16], GT[:, h * 16:(h + 1) * 16], gst[:, 16 + h:17 + h], None, op0=ALU.mult), reads=[GT, gst], writes=[GT])
            idT = idT_p(); gT = gT_p()
            pst = k.next_ps()
            k.op("pe", lambda e, pst=pst, IDF=IDF: e.transpose(pst[:, 0:128], IDF[:], ident[:]), reads=[IDF, ident], writes=[pst])
            k.op("dve", lambda e, idT=idT, pst=pst: e.tensor_copy(idT[:], pst[:, 0:128]), reads=[pst], writes=[idT])
            pst2 = k.next_ps()
            k.op("pe", lambda e, pst2=pst2, GT=GT: e.transpose(pst2[:, 0:128], GT[:], ident[:]), reads=[GT, ident], writes=[pst2])
            k.op("act", lambda e, gT=gT, pst2=pst2: e.copy(gT[:], pst2[:, 0:128]), reads=[pst2], writes=[gT])
            dots = dots_p()
            for t in range(128):
                ug = ug_p()
                k.dma(None, None, reads=[idT, w["peer_u"]], writes=[ug], q="pool",
                      fn=lambda e, ug=ug, t=t: e.indirect_dma_start(out=ug[:], out_offset=None, in_=w["peer_u"][:, :],
                                                                  in_offset=bass.IndirectOffsetOnAxis(ap=idT[:, t:t + 1], axis=0)))
                pb0 = k.next_ps(); pb1 = k.next_ps()
                sel = ident[:, t:t + 1].to_broadcast([128, 128])
                k.mm(pb0, pb0[:, :], ident, sel, htok, htok[:, 0:512], True, True)
                k.mm(pb1, pb1[:, :], ident, sel, htok, htok[:, 512:1024], True, True)
                hrow = vg_p()
                k.op("act", lambda e, hrow=hrow, pb0=pb0: e.copy(hrow[:, 0:512], pb0[:, :]), reads=[pb0], writes=[hrow])
                k.op("act", lambda e, hrow=hrow, pb1=pb1: e.copy(hrow[:, 512:1024], pb1[:, :]), reads=[pb1], writes=[hrow])
                k.op("dve", lambda e, ug=ug, hrow=hrow, t=t: e.scalar_tensor_tensor(out=hrow[:], in0=ug[:], scalar=1.0, in1=hrow[:], op0=ALU.mult, op1=ALU.mult, accum_out=dots[:, t:t + 1]),
                     reads=[ug, hrow], writes=[hrow, dots])
            wts = wts_p()
            g1_ = sm_p()
            k.op("dve", lambda e, g1_=g1_: e.tensor_tensor(out=g1_[:], in0=dots[:], in1=dots[:], op=ALU.mult), reads=[dots], writes=[g1_])
            k.op("dve", lambda e, g1_=g1_: e.tensor_scalar(g1_[:], g1_[:], 0.044715, 1.0, op0=ALU.mult, op1=ALU.add), reads=[g1_], writes=[g1_])
            k.op("dve", lambda e, g1_=g1_: e.tensor_tensor(out=g1_[:], in0=g1_[:], in1=dots[:], op=ALU.mult), reads=[g1_, dots], writes=[g1_])
            k.op("act", lambda e, g1_=g1_: e.activation(out=g1_[:], in_=g1_[:], func=AF.Sigmoid, scale=GELU_C), reads=[g1_], writes=[g1_])
            k.op("dve", lambda e, g1_=g1_: e.tensor_tensor(out=g1_[:], in0=g1_[:], in1=dots[:], op=ALU.mult), reads=[g1_, dots], writes=[g1_])
            k.op("dve", lambda e, wts=wts, g1_=g1_, gT=gT: e.tensor_tensor(out=wts[:], in0=g1_[:], in1=gT[:], op=ALU.mult), reads=[g1_, gT], writes=[wts])
            po0 = k.next_ps(); po1 = k.next_ps()
            for t in range(128):
                vg = vg_p()
                k.dma(None, None, reads=[idT, w["peer_v"]], writes=[vg], q="pool",
                      fn=lambda e, vg=vg, t=t: e.indirect_dma_start(out=vg[:], out_offset=None, in_=w["peer_v"][:, :],
                                                                  in_offset=bass.IndirectOffsetOnAxis(ap=idT[:, t:t + 1], axis=0)))
                wt_ = wt_p()
                k.op("dve", lambda e, wt_=wt_, t=t: e.tensor_scalar(wt_[:], csel[:, 127 - t:255 - t], wts[:, t:t + 1], None, op0=ALU.mult), reads=[csel, wts], writes=[wt_])
                k.mm(po0, po0[:, :], wt_, wt_[:], vg, vg[:, 0:512], t == 0, t == 127)
                k.mm(po1, po1[:, :], wt_, wt_[:], vg, vg[:, 512:1024], t == 0, t == 127)
            otok = otok_p()
            k.op("act", lambda e, otok=otok, po0=po0: e.copy(otok[:, 0:512], po0[:, :]), reads=[po0], writes=[otok])
            k.op("dve", lambda e, otok=otok, po1=po1: e.tensor_copy(otok[:, 512:1024], po1[:, :]), reads=[po1], writes=[otok])
            for dc in range(8):
                pst = k.next_ps()
                k.op("pe", lambda e, pst=pst, otok=otok, dc=dc: e.transpose(pst[:, 0:128], otok[:, dc * 128:(dc + 1) * 128], ident[:]), reads=[otok, ident], writes=[pst])
                xo = xo_p()
                k.op("dve", lambda e, xo=xo, pst=pst, xb=xb, dc=dc: e.scalar_tensor_tensor(out=xo[:], in0=pst[:, 0:128], scalar=m3[:, 40 + dc, j:j + 1], in1=xb[:, dc, :], op0=ALU.mult, op1=ALU.add),
                     reads=[pst, modt, xb], writes=[xo])
                k.dma(XT[dc * 128:(dc + 1) * 128, t0:t0 + 128], xo[:], reads=[xo], writes=[XT])
        k.phase_end()
        if stop_after == "G":
            break

    if stop_after is None:
        k.phase_begin()
        fg = k.sb("final_g", [128, 8])
        k.dma(fg[:], finalg_in[:, :], reads=[finalg_in], writes=[fg])
        xb_p = k.pool("xbN", [128, 8, 512], 2)
        sq_p = k.pool("sqN", [128, 8, 512], 1)
        rs_p = k.pool("rsN", [128, 512], 2)
        yo_p = k.pool("yoN", [128, 512], 4)
        for bi in range(8):
            t0 = C + bi * 512
            xb = xb_p()
            k.dma(xb[:], XT[:, t0:t0 + 512].rearrange("(kc p) t -> p kc t", p=128), reads=[XT], writes=[xb])
            sq = sq_p()
            k.op("act", lambda e, sq=sq, xb=xb: e.activation(out=sq[:], in_=xb[:], func=AF.Square), reads=[xb], writes=[sq])
            pss = k.next_ps()
            for kc in range(8):
                k.mm(pss, pss[:, :], ones, ones[:], sq, sq[:, kc, :], kc == 0, kc == 7)
            rstd = rs_p()
            k.op("act", lambda e, rstd=rstd, pss=pss: e.activation(out=rstd[:], in_=pss[:, :], func=AF.Sqrt, bias=eps_t[:, 0:1], scale=1.0 / D), reads=[pss, eps_t], writes=[rstd])
            k.op("dve", lambda e, rstd=rstd: e.reciprocal(rstd[:], rstd[:]), reads=[rstd], writes=[rstd])
            for kc in range(8):
                yo = yo_p()
                k.op("dve", lambda e, yo=yo, xb=xb, rstd=rstd, kc=kc: e.scalar_tensor_tensor(out=yo[:], in0=xb[:, kc, :], scalar=fg[:, kc:kc + 1], in1=rstd[:], op0=ALU.mult, op1=ALU.mult),
                     reads=[xb, fg, rstd], writes=[yo])
                k.dma(out[kc * 128:(kc + 1) * 128, bi * 512:(bi + 1) * 512], yo[:], reads=[yo], writes=[out])
        k.phase_end()
    else:
        k.phase_begin()
        fin_p = k.pool("fin", [128, L], 2)
        for kc in range(8):
            st = fin_p()
            k.dma(st[:], XT[kc * 128:(kc + 1) * 128, C:NT], reads=[XT], writes=[st])
            k.dma(out[kc * 128:(kc + 1) * 128, :], st[:], reads=[st], writes=[out])
        k.phase_end()
    k.finish()
    return nc, k


def kernel(**inputs):
    inp = {kk: np.asarray(v) for kk, v in inputs.items()}
    nc, _ = build()
    in_maps = [_prep(inp, b) for b in range(4)]
    res = run_bass_kernel_spmd(nc, in_maps, core_ids=list(range(4)))
    return np.stack([np.ascontiguousarray(res.results[b]["out"].T) for b in range(4)], 0).astype(np.float32)
```

```python
import math
import numpy as np
from contextlib import ExitStack
import concourse.bass as bass
import concourse.mybir as mybir
from concourse.bass_utils import run_bass_kernel_spmd

F32 = mybir.dt.float32
I32 = mybir.dt.int32
U32 = mybir.dt.uint32
AF = mybir.ActivationFunctionType
ALU = mybir.AluOpType
AX = mybir.AxisListType

D = 1024
L = 4096
C = 256
NT = L + C
DEPTH = 2
IN_W = 5888
NBLK = [(0, 256)] + [(256 + 512 * i, 512) for i in range(8)]


class Res:
    __slots__ = ("name", "w", "r", "ep")

    def __init__(self, name):
        self.name = name
        self.w = None
        self.r = {}
        self.ep = 0


class Tile(Res):
    __slots__ = ("t",)

    def __init__(self, name, t):
        Res.__init__(self, name)
        self.t = t

    def __getitem__(self, k):
        return self.t[k]


class Eng:
    def __init__(self, name, e, sem):
        self.name = name
        self.e = e
        self.sem = sem
        self.cnt = 0
        self.known = {}


class K:
    SAME_ENGINE_SYNC = True
    ROTATE_AT = 20000

    def __init__(self, nc, n_dma_slots=24):
        self.nc = nc
        self._sems = []
        self.es = ExitStack()
        self.epoch = 1
        self.uid = 0
        self.eng = {}
        for nm, e in (("pe", nc.tensor), ("act", nc.scalar), ("dve", nc.vector),
                      ("pool", nc.gpsimd), ("sp", nc.sync)):
            self.eng[nm] = Eng(nm, e, self.es.enter_context(nc.semaphore("sem_" + nm)))
        self.slots = []
        for i in range(n_dma_slots):
            s = self.es.enter_context(nc.semaphore("dsem%d" % i))
            self.slots.append([s, 0])
        self.slot_i = 0
        self.phase_stack = None
        self.psum = []
        for i in range(8):
            t = self.es.enter_context(nc.psum_tensor("ps%d" % i, [128, 512], F32))
            self.psum.append(Tile("ps%d" % i, t))
        self.ps_i = 0
        self.n_ins = 0
        self.n_rot = 0
        self._sems = []

    def next_ps(self):
        p = self.psum[self.ps_i]
        self.ps_i = (self.ps_i + 1) % 8
        return p

    def sb(self, name, shape, dtype=F32, persistent=False):
        self.uid += 1
        st = self.es if (persistent or self.phase_stack is None) else self.phase_stack
        t = st.enter_context(self.nc.sbuf_tensor("%s_%d" % (name, self.uid), list(shape), dtype))
        return Tile(name, t)

    def pool(self, name, shape, n, dtype=F32):
        tiles = [self.sb("%s%d" % (name, i), shape, dtype) for i in range(n)]
        st = {"i": 0}

        def nxt():
            t = tiles[st["i"]]
            st["i"] = (st["i"] + 1) % n
            return t
        return nxt

    def dram(self, name, shape, dtype=F32, kind="Internal"):
        t = self.nc.dram_tensor(name, list(shape), dtype, kind=kind)
        return Tile(name, t.ap())

    def phase_begin(self):
        assert self.phase_stack is None
        self.phase_stack = ExitStack()

    def phase_end(self):
        self.barrier()
        self.phase_stack.close()
        self.phase_stack = None

    def _key(self, sem):
        for i, s_ in enumerate(self._sems):
            if s_ is sem:
                return i
        self._sems.append(sem)
        return len(self._sems) - 1

    def _wait(self, E, ev):
        sem, val = ev
        k = self._key(sem)
        if E.known.get(k, 0) >= val:
            return
        E.e.wait_ge(sem, val)
        E.known[k] = val
        self.n_ins += 1

    def _deps(self, E, reads, writes, is_pe=False):
        evs = []
        for r in reads:
            if r.ep == self.epoch and r.w is not None:
                evs.append(r.w)
        for w in writes:
            if w.ep == self.epoch:
                if w.w is not None:
                    evs.append(w.w)
                evs.extend(w.r.values())
        for ev in evs:
            if ev[0] is E.sem and (is_pe or not self.SAME_ENGINE_SYNC):
                continue
            self._wait(E, ev)

    def _record(self, ev, reads, writes):
        for r in reads:
            if r.ep != self.epoch:
                r.ep = self.epoch
                r.w = None
                r.r = {}
            r.r[self._key(ev[0])] = ev
        for w in writes:
            w.ep = self.epoch
            w.w = ev
            w.r = {}

    def op(self, eng, fn, reads=(), writes=()):
        E = self.eng[eng]
        self._deps(E, reads, writes, is_pe=(eng == "pe"))
        ins = fn(E.e)
        E.cnt += 1
        ins.then_inc(E.sem, 1)
        self.n_ins += 1
        self._record((E.sem, E.cnt), reads, writes)
        return ins

    def dma(self, out_ap, in_ap, reads=(), writes=(), q="sp", fn=None):
        E = self.eng[q]
        self._deps(E, reads, writes)
        slot = self.slots[self.slot_i]
        self.slot_i = (self.slot_i + 1) % len(self.slots)
        if slot[1] > 0:
            self._wait(E, (slot[0], slot[1]))
        ins = E.e.dma_start(out=out_ap, in_=in_ap) if fn is None else fn(E.e)
        slot[1] += 16
        ins.then_inc(slot[0], 16)
        self.n_ins += 1
        self._record((slot[0], slot[1]), reads, writes)
        return ins

    def barrier(self):
        evs = []
        for E in self.eng.values():
            if E.cnt > 0:
                evs.append((E.sem, E.cnt))
        for s in self.slots:
            if s[1] > 0:
                evs.append((s[0], s[1]))
        for E in self.eng.values():
            for ev in evs:
                if ev[0] is E.sem:
                    continue
                self._wait(E, ev)
        self.epoch += 1
        for E in self.eng.values():
            if E.cnt > self.ROTATE_AT:
                self.n_rot += 1
                E.sem = self.es.enter_context(self.nc.semaphore("sem_%s_r%d" % (E.name, self.n_rot)))
                E.cnt = 0
        for s in self.slots:
            if s[1] > self.ROTATE_AT:
                self.n_rot += 1
                s[0] = self.es.enter_context(self.nc.semaphore("dsem_r%d" % self.n_rot))
                s[1] = 0

    def finish(self):
        self.barrier()
        self.es.close()

    def mm(self, ps, out_ap, lt, lhsT_ap, rt, rhs_ap, start, stop):
        return self.op("pe", lambda e: e.matmul(out_ap, lhsT_ap, rhs_ap, start=start, stop=stop),
                       reads=[lt, rt], writes=[ps])


def _rot_perm():
    perm = []
    for hd in range(10):
        for i in range(64):
            perm.append(hd * 64 + (i + 32) % 64)
    return np.array(perm)


def _rope_tables():
    row = np.repeat(np.arange(L // 64), 64).astype(np.float32)
    col = np.tile(np.arange(64), L // 64).astype(np.float32)
    inv = np.power(np.float32(10000.0), -np.arange(16, dtype=np.float32) / 16).astype(np.float32)
    ang = np.concatenate([row[:, None] * inv, col[:, None] * inv], axis=-1)
    cos = np.cos(ang).astype(np.float32)
    sin = np.sin(ang).astype(np.float32)
    cosT = np.ones((64, NT), np.float32)
    sinT = np.zeros((64, NT), np.float32)
    cosT[:32, C:] = cos.T
    cosT[32:, C:] = cos.T
    sinT[:32, C:] = -sin.T
    sinT[32:, C:] = sin.T
    return np.concatenate([cosT, cosT], 0), np.concatenate([sinT, sinT], 0)


def _pk(v, n):
    return np.ascontiguousarray(np.asarray(v, np.float32).reshape(n, 128).T)


def _prep(inp, b):
    m = {}
    m["xT"] = np.ascontiguousarray(inp["x"][b].T)
    m["ctxT"] = np.ascontiguousarray(inp["ctx"][b].T)
    cv = np.stack([_pk(inp["c"][b], 8), _pk(inp["c_ctx"], 8)], axis=-1)
    m["cvec"] = np.ascontiguousarray(cv.reshape(128, 16))
    perm = _rot_perm()
    cosT, sinT = _rope_tables()
    m["ropec"] = cosT
    qq = np.arange(128)[:, None]
    kk = np.arange(128)[None, :]
    mk = np.zeros((128, 256), np.float32)
    mk[:, :128] = np.where(kk >= qq, 0.0, -1e30)
    mk[:, 128:] = np.where(kk <= qq, 0.0, -1e30)
    m["amask"] = mk
    m["final_g"] = _pk(inp["final_g"], 8)
    csel = np.zeros((128, 255), np.float32)
    csel[:, 127] = 1.0
    m["csel"] = csel
    m["iota16"] = np.ascontiguousarray(np.broadcast_to(np.arange(16, dtype=np.float32)[None, :], (128, 16)))
    for nm, Ls in (("L", L), ("C", C)):
        t = np.arange(Ls, dtype=np.float32)
        tn = t / np.float32(Ls)
        bands = np.arange(1, 17, dtype=np.float32)
        ang = np.float32(2.0 * math.pi) * tn[:, None] * bands[None, :]
        zf = np.concatenate([tn[:, None], np.cos(ang), np.sin(ang)], axis=-1).astype(np.float32)
        m["hyz" + nm] = np.ascontiguousarray(zf.T)
        tw = np.linspace(0.0, 1.0, Ls, dtype=np.float32)
        m["hytw" + nm] = np.ascontiguousarray((-tw).reshape(Ls // 128, 128).T)
    fast = -math.log(1e-2) / 0.3
    slow = -math.log(1e-2) / 1.5
    rate = np.linspace(fast, slow, 512, dtype=np.float32)
    m["hyrate"] = np.ascontiguousarray(np.broadcast_to(rate[None, :], (128, 512)))
    m["ropes"] = sinT
    for l in range(DEPTH):
        m["mod_w%d" % l] = np.ascontiguousarray(inp["mod_w"][l])
        m["mod_b%d" % l] = _pk(inp["mod_b"][l], 48)
        m["n1g%d" % l] = _pk(inp["norm1_g"][l], 8)
        m["in_w%d" % l] = np.ascontiguousarray(inp["in_w"][l])
        m["in_wr%d" % l] = np.ascontiguousarray(inp["in_w"][l][:, perm])
        m["gate_b%d" % l] = _pk(inp["gate_b"][l], 24)
        m["hy_sw%d" % l] = np.ascontiguousarray(np.asarray(inp["hy_short_w"][l], np.float32).reshape(3, 12, 128).transpose(2, 1, 0))
        m["hy_sb%d" % l] = _pk(inp["hy_short_b"][l], 12)
        m["hy_w1%d" % l] = np.ascontiguousarray(inp["hy_w1"][l])
        m["hy_w2%d" % l] = np.ascontiguousarray(inp["hy_w2"][l])
        m["hy_w3%d" % l] = np.ascontiguousarray(inp["hy_w3"][l])
        m["hy_fb%d" % l] = np.ascontiguousarray(np.stack([inp["hy_b1"][l], inp["hy_freq1"][l], inp["hy_b2"][l], inp["hy_freq2"][l]], 1).astype(np.float32))
        m["hy_bias%d" % l] = _pk(inp["hy_bias"][l], 4)
        for nm_ in ("s5_a_re", "s5_a_im"):
            m["%s%d" % (nm_, l)] = np.ascontiguousarray(np.asarray(inp[nm_][l], np.float32).reshape(2, 16, 128).transpose(2, 0, 1))
        m["s5_ldt%d" % l] = np.ascontiguousarray(np.repeat(np.asarray(inp["s5_log_dt"][l], np.float32), 64, axis=1).reshape(2, 16, 128).transpose(2, 0, 1))
        for nm_ in ("s5_b_re", "s5_b_im"):
            m["%s%d" % (nm_, l)] = np.ascontiguousarray(np.asarray(inp[nm_][l], np.float32).reshape(2, 2048, 16))
        for nm_ in ("s5_c_re", "s5_c_im"):
            m["%s%d" % (nm_, l)] = np.ascontiguousarray(np.asarray(inp[nm_][l], np.float32).transpose(0, 1, 3, 2).reshape(2, 2048, 16))
        m["s5_d%d" % l] = np.ascontiguousarray(np.asarray(inp["s5_d"][l], np.float32).reshape(16, 32).T)
        m["s5_gw%d" % l] = np.ascontiguousarray(inp["s5_glu_w"][l])
        m["s5_gb%d" % l] = _pk(inp["s5_glu_b"][l], 4)
        for nm_ in ("br_attn_w", "br_hyena_w", "br_s5_w", "out_w"):
            m["%s%d" % (nm_, l)] = np.ascontiguousarray(inp[nm_][l])
        m["n2g%d" % l] = _pk(inp["norm2_g"][l], 8)
        m["peer_wq%d" % l] = np.ascontiguousarray(inp["peer_wq"][l])
        m["peer_kT%d" % l] = np.ascontiguousarray(np.asarray(inp["peer_keys"][l], np.float32).transpose(0, 1, 3, 2).reshape(16, 128, 128))
        m["peer_u%d" % l] = np.ascontiguousarray(inp["peer_u"][l])
        m["peer_v%d" % l] = np.ascontiguousarray(inp["peer_v"][l])
        m["sink%d" % l] = np.ascontiguousarray(np.broadcast_to(np.asarray(inp["attn_sink"][l], np.float32)[None, :], (128, 8)))
    return m


def build(dbg=False, n_layers=DEPTH, stop_after=None, start_at=None):
    nc = bass.Bass("TRN2", target_bir_lowering=False)
    k = K(nc)
    okind = "ExternalOutput" if dbg else "Internal"
    ein = lambda name, shape, dt=F32: k.dram(name, shape, dt, kind="ExternalInput")
    xT_in = ein("xT", [D, L])
    ctxT_in = ein("ctxT", [D, C])
    cvec_in = ein("cvec", [128, 16])
    ropec_in = ein("ropec", [128, NT])
    ropes_in = ein("ropes", [128, NT])
    amask_in = ein("amask", [128, 256])
    finalg_in = ein("final_g", [128, 8])
    csel_in = ein("csel", [128, 255])
    iota16_in = ein("iota16", [128, 16])
    hyz_in = {"L": ein("hyzL", [33, L]), "C": ein("hyzC", [33, C])}
    hytw_in = {"L": ein("hytwL", [128, L // 128]), "C": ein("hytwC", [128, C // 128])}
    hyrate_in = ein("hyrate", [128, 512])
    W = []
    for l in range(DEPTH):
        W.append(dict(mod_w=ein("mod_w%d" % l, [D, 6 * D]), mod_b=ein("mod_b%d" % l, [128, 48]),
                      n1g=ein("n1g%d" % l, [128, 8]), hy_sw=ein("hy_sw%d" % l, [128, 12, 3]), hy_sb=ein("hy_sb%d" % l, [128, 12]),
                      hy_w1=ein("hy_w1%d" % l, [33, 64]), hy_w2=ein("hy_w2%d" % l, [64, 64]), hy_w3=ein("hy_w3%d" % l, [64, 1024]),
                      hy_fb=ein("hy_fb%d" % l, [64, 4]),
                      n2g=ein("n2g%d" % l, [128, 8]), peer_wq=ein("peer_wq%d" % l, [D, 2048]), peer_kT=ein("peer_kT%d" % l, [16, 128, 128]),
                      peer_u=ein("peer_u%d" % l, [16384, D]), peer_v=ein("peer_v%d" % l, [16384, D]),
                      br_attn_w=ein("br_attn_w%d" % l, [512, D]), br_hyena_w=ein("br_hyena_w%d" % l, [512, D]), br_s5_w=ein("br_s5_w%d" % l, [512, D]),
                      out_w=ein("out_w%d" % l, [D, D]),
                      s5_a_re=ein("s5_a_re%d" % l, [128, 2, 16]), s5_a_im=ein("s5_a_im%d" % l, [128, 2, 16]), s5_ldt=ein("s5_ldt%d" % l, [128, 2, 16]),
                      s5_b_re=ein("s5_b_re%d" % l, [2, 2048, 16]), s5_b_im=ein("s5_b_im%d" % l, [2, 2048, 16]),
                      s5_c_re=ein("s5_c_re%d" % l, [2, 2048, 16]), s5_c_im=ein("s5_c_im%d" % l, [2, 2048, 16]),
                      s5_d=ein("s5_d%d" % l, [32, 16]), s5_gw=ein("s5_gw%d" % l, [512, 512]), s5_gb=ein("s5_gb%d" % l, [128, 4]), hy_bias=ein("hy_bias%d" % l, [128, 4]), sink=ein("sink%d" % l, [128, 8]), in_w=ein("in_w%d" % l, [D, IN_W]),
                      in_wr=ein("in_wr%d" % l, [D, 640]), gate_b=ein("gate_b%d" % l, [128, 24])))
    XT = k.dram("XT", [D, NT])
    MODT = k.dram("MODT", [128, 96], kind=okind)
    QT = k.dram("QT", [512, NT], kind=okind)
    KT = k.dram("KT", [128, NT], kind=okind)
    VTOK = k.dram("VTOK", [NT, 128], kind=okind)
    US5 = k.dram("US5", [512, NT], kind=okind)
    ZHY = k.dram("ZHY", [1536, NT], kind=okind)
    GATE = k.dram("GATE", [3072, NT], kind=okind)
    HT = k.dram("HT", [D, NT], kind=okind)
    MIX = k.dram("MIX", [D, NT], kind=okind)
    XDBG = k.dram("XDBG", [D, NT], kind=okind)
    S5Y = k.dram("S5Y", [512, NT], kind=okind)
    S5O = k.dram("S5O", [512, NT], kind=okind)
    HY = k.dram("HY", [512, NT], kind=okind)
    DFT = {}
    for nm, Ls in (("L", L), ("C", C)):
        DFT[nm] = {m_: k.dram("DFT_%s_%s" % (nm, m_), [Ls, Ls]) for m_ in ("CK", "SK", "CS", "SS")}
    HSDd, KRId, VXTd, VXFd, X0Fd = {}, {}, {}, {}, {}
    for nm, Ls in (("L", L), ("C", C)):
        HSDd[nm] = k.dram("HSD" + nm, [2, Ls, 512])
        KRId[nm] = k.dram("KRI" + nm, [2, Ls, 512])
        VXTd[nm] = k.dram("VXT" + nm, [Ls, 512])
        VXFd[nm] = k.dram("VXF" + nm, [512, Ls])
        X0Fd[nm] = k.dram("X0F" + nm, [512, Ls])
    ATT = k.dram("ATT", [512, NT], kind=okind)
    out = k.dram("out", [D, L], kind="ExternalOutput")

    ones = k.sb("ones", [128, 128], persistent=True)
    k.op("pool", lambda e: e.memset(ones[:], 1.0), writes=[ones])
    eps_t = k.sb("eps", [128, 1], persistent=True)
    k.op("pool", lambda e: e.memset(eps_t[:], 1e-6), writes=[eps_t])
    ident = k.sb("ident", [128, 128], persistent=True)
    k.op("pool", lambda e: e.memset(ident[:], 0.0), writes=[ident])
    k.op("pool", lambda e: e.affine_select(out=ident[:], in_=ident[:], pattern=[[-1, 128]],
                                           compare_op=ALU.not_equal, fill=1.0, base=0, channel_multiplier=1),
         reads=[ident], writes=[ident])

    k.phase_begin()
    stage_p = k.pool("stage", [128, NT], 2)
    for kc in range(8):
        st = stage_p()
        k.dma(st[:, 0:C], ctxT_in[kc * 128:(kc + 1) * 128, :], reads=[ctxT_in], writes=[st])
        k.dma(st[:, C:NT], xT_in[kc * 128:(kc + 1) * 128, :], reads=[xT_in], writes=[st])
        k.dma(XT[kc * 128:(kc + 1) * 128, :], st[:], reads=[st], writes=[XT])
    k.phase_end()


    k.phase_begin()
    negpi = k.sb("negpi", [128, 1])
    k.op("pool", lambda e: e.memset(negpi[:], -math.pi), writes=[negpi])
    colv_p = k.pool("colv", [128, 4096], 1, I32)
    pm_p = k.pool("pm", [128, 1], 2, I32)
    ri_p = k.pool("ri", [128, 4096], 2, I32)
    rj_p = k.pool("rj", [128, 4096], 2, I32)
    rf_p = k.pool("rf", [128, 4096], 2)
    go_p = k.pool("go", [128, 4096], 3)
    for nm, Ls in (("L", L), ("C", C)):
        N4 = 8 * Ls
        colv = colv_p()
        k.op("pool", lambda e, colv=colv: e.iota(colv[:, 0:Ls], pattern=[[2, Ls]], base=1, channel_multiplier=0), writes=[colv])
        for tt in range(Ls // 128):
            for shifted in (0, 1):
                pm = pm_p()
                k.op("pool", lambda e, pm=pm: e.iota(pm[:], pattern=[[0, 1]], base=2 * tt * 128 + shifted, channel_multiplier=2), writes=[pm])
                ri = ri_p()
                k.op("dve", lambda e, ri=ri, pm=pm: e.tensor_tensor(out=ri[:, 0:Ls], in0=colv[:, 0:Ls], in1=pm[:, 0:1].to_broadcast([128, Ls]), op=ALU.mult),
                     reads=[colv, pm], writes=[ri])
                for trig in ("S", "C"):
                    rj = rj_p()
                    if trig == "S":
                        k.op("dve", lambda e, ri=ri, rj=rj: e.tensor_single_scalar(rj[:, 0:Ls], ri[:, 0:Ls], N4 - 1, op=ALU.bitwise_and), reads=[ri], writes=[rj])
                    else:
                        k.op("dve", lambda e, ri=ri, rj=rj: e.tensor_single_scalar(rj[:, 0:Ls], ri[:, 0:Ls], N4 // 4, op=ALU.add), reads=[ri], writes=[rj])
                        k.op("dve", lambda e, rj=rj: e.tensor_single_scalar(rj[:, 0:Ls], rj[:, 0:Ls], N4 - 1, op=ALU.bitwise_and), reads=[rj], writes=[rj])
                    rf = rf_p()
                    k.op("pool", lambda e, rf=rf, rj=rj: e.tensor_copy(rf[:, 0:Ls], rj[:, 0:Ls]), reads=[rj], writes=[rf])
                    go = go_p()
                    k.op("act", lambda e, go=go, rf=rf: e.activation(out=go[:, 0:Ls], in_=rf[:, 0:Ls], func=AF.Sin, bias=negpi[:, 0:1], scale=2.0 * math.pi / N4),
                         reads=[rf, negpi], writes=[go])
                    dst = DFT[nm][trig + ("S" if shifted else "K")]
                    k.dma(dst[tt * 128:(tt + 1) * 128, :], go[:, 0:Ls], reads=[go], writes=[dst])
    k.phase_end()

    ORDER = ["0", "A", "B", "C", "D", "E", "F", "G"]
    skip = (lambda ph: start_at is not None and ORDER.index(ph) < ORDER.index(start_at))
    for l in range(n_layers):
        w = W[l]
        need_ctx = l < DEPTH - 1
        k.phase_begin()
        cv = k.sb("cv", [128, 16])
        k.dma(cv[:], cvec_in[:, :], reads=[cvec_in], writes=[cv])
        sl = k.sb("silu", [128, 16])
        k.op("act", lambda e: e.activation(out=sl[:], in_=cv[:], func=AF.Silu), reads=[cv], writes=[sl])
        mb = k.sb("mb", [128, 48])
        k.dma(mb[:], w["mod_b"][:, :], reads=[w["mod_b"]], writes=[mb])
        modt = k.sb("modt", [128, 96])
        mwv = w["mod_w"].t.rearrange("(kc p) n -> p kc n", p=128)
        mw_p = k.pool("mw", [128, 8, 1024], 2)
        for fg in range(6):
            mw = mw_p()
            for kc in range(8):
                k.dma(mw[:, kc, :], mwv[:, kc, fg * 1024:(fg + 1) * 1024], reads=[w["mod_w"]], writes=[mw])
            ps = k.next_ps()
            for fc in range(8):
                for kc in range(8):
                    k.mm(ps, ps[:, fc * 2:fc * 2 + 2], mw, mw[:, kc, fc * 128:(fc + 1) * 128],
                         sl, sl[:, kc * 2:kc * 2 + 2], kc == 0, kc == 7)
            for j in range(2):
                pv = ps.t[:, 0:16].rearrange("p (f j) -> p f j", j=2)[:, :, j]
                ov = modt.t[:, fg * 16:(fg + 1) * 16].rearrange("p (f j) -> p f j", j=2)[:, :, j]
                k.op("dve", lambda e, pv=pv, ov=ov: e.tensor_tensor(out=ov, in0=pv, in1=mb[:, fg * 8:(fg + 1) * 8], op=ALU.add),
                     reads=[ps, mb], writes=[modt])
        k.dma(MODT[:, :], modt[:], reads=[modt], writes=[MODT])
        k.phase_end()
        if stop_after == "A":
            break

        k.phase_begin()
        modt = k.sb("modt", [128, 96])
        k.dma(modt[:], MODT[:, :], reads=[MODT], writes=[modt])
        g1n = k.sb("g1n", [128, 8])
        k.dma(g1n[:], w["n1g"][:, :], reads=[w["n1g"]], writes=[g1n])
        gb = k.sb("gb", [128, 24])
        k.dma(gb[:], w["gate_b"][:, :], reads=[w["gate_b"]], writes=[gb])
        m3 = modt.t[:, :].rearrange("p (f j) -> p f j", j=2)
        Acoef = k.sb("Acoef", [128, 8, 2])
        for j in range(2):
            k.op("dve", lambda e, j=j: e.scalar_tensor_tensor(out=Acoef[:, :, j], in0=m3[:, 8:16, j], scalar=1.0, in1=g1n[:],
                                                             op0=ALU.add, op1=ALU.mult), reads=[modt, g1n], writes=[Acoef])
        iwv = w["in_w"].t.rearrange("(kc p) n -> p kc n", p=128)
        irv = w["in_wr"].t.rearrange("(kc p) n -> p kc n", p=128)
        groups = [("q", 0, 512), ("k", 512, 128), ("v", 640, 128), ("s5", 768, 512)]
        groups += [("hy", 1280 + 512 * i, 512) for i in range(3)] + [("gate", 2816 + 512 * i, 512) for i in range(6)]
        xb_p = k.pool("xb", [128, 8, 512], 1)
        sq_p = k.pool("sq", [128, 8, 512], 1)
        hb_p = k.pool("hb", [128, 8, 512], 1)
        wt_p = k.pool("wt", [128, 8, 512], 4)
        wr_p = k.pool("wr", [128, 8, 512], 2)
        rstd_p = k.pool("rstd", [128, 512], 1)
        rc_p = k.pool("rc", [128, 512], 2)
        rs_p = k.pool("rs", [128, 512], 2)
        ev_p = k.pool("ev", [128, 512], 4)
        e2_p = k.pool("e2", [128, 512], 2)
        vt_p = k.pool("vt", [128, 128], 2)
        for bi, (t0, nt) in enumerate(NBLK):
            j = 1 if bi == 0 else 0
            xb = xb_p()
            for kc in range(8):
                k.dma(xb[:, kc, 0:nt], XT[kc * 128:(kc + 1) * 128, t0:t0 + nt], reads=[XT], writes=[xb])
            sq = sq_p()
            k.op("act", lambda e: e.activation(out=sq[:, :, 0:nt], in_=xb[:, :, 0:nt], func=AF.Square), reads=[xb], writes=[sq])
            pss = k.next_ps()
            for kc in range(8):
                k.mm(pss, pss[:, 0:nt], ones, ones[:], sq, sq[:, kc, 0:nt], kc == 0, kc == 7)
            rstd = rstd_p()
            k.op("act", lambda e: e.activation(out=rstd[:, 0:nt], in_=pss[:, 0:nt], func=AF.Sqrt, bias=eps_t[:, 0:1], scale=1.0 / D),
                 reads=[pss, eps_t], writes=[rstd])
            k.op("dve", lambda e: e.reciprocal(rstd[:, 0:nt], rstd[:, 0:nt]), reads=[rstd], writes=[rstd])
            hb = hb_p()
            for kc in range(8):
                k.op("dve", lambda e, kc=kc: e.tensor_tensor(out=hb[:, kc, 0:nt], in0=xb[:, kc, 0:nt], in1=rstd[:, 0:nt], op=ALU.mult),
                     reads=[xb, rstd], writes=[hb])
                k.op("pool", lambda e, kc=kc: e.tensor_scalar(hb[:, kc, 0:nt], hb[:, kc, 0:nt], Acoef[:, kc, j:j + 1], m3[:, kc, j:j + 1],
                                                              op0=ALU.mult, op1=ALU.add), reads=[hb, Acoef, modt], writes=[hb])
            if dbg:
                for kc in range(8):
                    k.dma(HT[kc * 128:(kc + 1) * 128, t0:t0 + nt], hb[:, kc, 0:nt], reads=[hb], writes=[HT])
            rc = rc_p()
            rs = rs_p()
            k.dma(rc[:, 0:nt], ropec_in[:, t0:t0 + nt], reads=[ropec_in], writes=[rc])
            k.dma(rs[:, 0:nt], ropes_in[:, t0:t0 + nt], reads=[ropes_in], writes=[rs])
            for (kind, c0, ncol) in groups:
                wt = wt_p()
                for kc in range(8):
                    k.dma(wt[:, kc, 0:ncol], iwv[:, kc, c0:c0 + ncol], reads=[w["in_w"]], writes=[wt])
                if kind in ("q", "k"):
                    wr = wr_p()
                    for kc in range(8):
                        k.dma(wr[:, kc, 0:ncol], irv[:, kc, c0:c0 + ncol], reads=[w["in_wr"]], writes=[wr])
                if kind == "v":
                    for ts in range(nt // 128):
                        ps = k.next_ps()
                        for kc in range(8):
                            k.mm(ps, ps[:, 0:128], hb, hb[:, kc, ts * 128:(ts + 1) * 128], wt, wt[:, kc, 0:128], kc == 0, kc == 7)
                        vt = vt_p()
                        k.op("act", lambda e, ps=ps, vt=vt: e.copy(vt[:], ps[:, 0:128]), reads=[ps], writes=[vt])
                        k.dma(VTOK[t0 + ts * 128:t0 + (ts + 1) * 128, :], vt[:], reads=[vt], writes=[VTOK], q="act")
                    continue
                for cc in range(ncol // 128):
                    ps = k.next_ps()
                    for kc in range(8):
                        k.mm(ps, ps[:, 0:nt], wt, wt[:, kc, cc * 128:(cc + 1) * 128], hb, hb[:, kc, 0:nt], kc == 0, kc == 7)
                    ev = ev_p()
                    col = c0 + cc * 128
                    if kind in ("q", "k"):
                        ps2 = k.next_ps()
                        for kc in range(8):
                            k.mm(ps2, ps2[:, 0:nt], wr, wr[:, kc, cc * 128:(cc + 1) * 128], hb, hb[:, kc, 0:nt], kc == 0, kc == 7)
                        e2 = e2_p()
                        k.op("dve", lambda e, ps=ps, ev=ev: e.tensor_tensor(out=ev[:, 0:nt], in0=ps[:, 0:nt], in1=rc[:, 0:nt], op=ALU.mult),
                             reads=[ps, rc], writes=[ev])
                        k.op("dve", lambda e, ps2=ps2, e2=e2: e.tensor_tensor(out=e2[:, 0:nt], in0=ps2[:, 0:nt], in1=rs[:, 0:nt], op=ALU.mult),
                             reads=[ps2, rs], writes=[e2])
                        k.op("pool", lambda e, ev=ev, e2=e2: e.tensor_tensor(out=ev[:, 0:nt], in0=ev[:, 0:nt], in1=e2[:, 0:nt], op=ALU.add),
                             reads=[ev, e2], writes=[ev])
                        if kind == "q":
                            k.op("act", lambda e, ev=ev: e.mul(ev[:, 0:nt], ev[:, 0:nt], 0.125), reads=[ev], writes=[ev])
                            dst = QT[col:col + 128, t0:t0 + nt]
                            dres = QT
                        else:
                            dst = KT[0:128, t0:t0 + nt]
                            dres = KT
                    elif kind == "gate":
                        gi = (col - 2816) // 128
                        k.op("act", lambda e, ps=ps, ev=ev, gi=gi: e.activation(out=ev[:, 0:nt], in_=ps[:, 0:nt], func=AF.Sigmoid, bias=gb[:, gi:gi + 1]),
                             reads=[ps, gb], writes=[ev])
                        dst = GATE[col - 2816:col - 2816 + 128, t0:t0 + nt]
                        dres = GATE
                    else:
                        if cc % 2 == 0:
                            k.op("act", lambda e, ps=ps, ev=ev: e.copy(ev[:, 0:nt], ps[:, 0:nt]), reads=[ps], writes=[ev])
                        else:
                            k.op("dve", lambda e, ps=ps, ev=ev: e.tensor_copy(ev[:, 0:nt], ps[:, 0:nt]), reads=[ps], writes=[ev])
                        if kind == "s5":
                            dst = US5[col - 768:col - 768 + 128, t0:t0 + nt]
                            dres = US5
                        else:
                            dst = ZHY[col - 1280:col - 1280 + 128, t0:t0 + nt]
                            dres = ZHY
                    k.dma(dst, ev[:, 0:nt], reads=[ev], writes=[dres], q="act")
        k.phase_end()
        if stop_after == "B":
            break

        need_ctx = l < DEPTH - 1
        k.phase_begin()
        amask = k.sb("amask", [128, 256])
        k.dma(amask[:], amask_in[:, :], reads=[amask_in], writes=[amask])
        sinkb = k.sb("sinkb", [128, 8])
        k.dma(sinkb[:], w["sink"][:, :], reads=[w["sink"]], writes=[sinkb])
        kT_p = k.pool("kT", [64, NT], 1)
        v_p = k.pool("vtk", [128, NT // 128, 64], 1)
        qT_p = k.pool("qT", [64, NT], 2)
        ao_p = k.pool("ao", [64, NT], 2)
        S_p = k.pool("S", [128, 640], 4)
        PT_p = k.pool("PT", [128, 5, 128], 4)
        st_p = k.pool("stat", [128, 8], 6)
        vview = VTOK.t.rearrange("(n p) c -> p n c", p=128)
        for kv in range(2):
            kT = kT_p()
            k.dma(kT[:], KT[kv * 64:(kv + 1) * 64, :], reads=[KT], writes=[kT])
            vt = v_p()
            k.dma(vt[:], vview[:, :, kv * 64:(kv + 1) * 64], reads=[VTOK], writes=[vt])
            for hg in range(4):
                hd = kv * 4 + hg
                qT = qT_p()
                k.dma(qT[:], QT[hd * 64:(hd + 1) * 64, :], reads=[QT], writes=[qT])
                ao = ao_p()
                blocks = ([("c", 0), ("c", 1)] if need_ctx else []) + [("l", n) for n in range(32)]
                def unit_gen(bt, n):
                    q0 = n * 128 if bt == "c" else C + n * 128
                    if bt == "c":
                        kts = []
                    else:
                        kts = [j for j in (n - 1, n, n + 1) if 0 <= j < 32]
                    nloc = len(kts) * 128
                    wtot = 256 + nloc
                    S = S_p()
                    psc = k.next_ps()
                    k.mm(psc, psc[:, 0:256], qT, qT[:, q0:q0 + 128], kT, kT[:, 0:256], True, True)
                    yield
                    k.op("act", lambda e, S=S, psc=psc: e.copy(S[:, 0:256], psc[:, 0:256]), reads=[psc], writes=[S])
                    yield
                    if nloc:
                        psl = k.next_ps()
                        k0 = C + kts[0] * 128
                        k.mm(psl, psl[:, 0:nloc], qT, qT[:, q0:q0 + 128], kT, kT[:, k0:k0 + nloc], True, True)
                        yield
                        for ji, j in enumerate(kts):
                            dst = S[:, 256 + ji * 128:256 + (ji + 1) * 128]
                            src = psl[:, ji * 128:(ji + 1) * 128]
                            if j == n:
                                k.op("dve", lambda e, dst=dst, src=src: e.tensor_copy(dst, src), reads=[psl], writes=[S])
                                yield
                            else:
                                mko = 0 if j < n else 128
                                k.op("dve", lambda e, dst=dst, src=src, mko=mko: e.tensor_tensor(out=dst, in0=src, in1=amask[:, mko:mko + 128], op=ALU.add),
                                     reads=[psl, amask], writes=[S])
                                yield
                    stt = st_p()
                    k.op("dve", lambda e, S=S, stt=stt: e.tensor_reduce(out=stt[:, 0:1], in_=S[:, 0:wtot], axis=AX.X, op=ALU.max), reads=[S], writes=[stt])
                    yield
                    k.op("dve", lambda e, stt=stt: e.tensor_tensor(out=stt[:, 0:1], in0=stt[:, 0:1], in1=sinkb[:, hd:hd + 1], op=ALU.max), reads=[stt, sinkb], writes=[stt])
                    yield
                    k.op("dve", lambda e, stt=stt: e.tensor_scalar(stt[:, 1:2], stt[:, 0:1], -1.0, None, op0=ALU.mult), reads=[stt], writes=[stt])
                    yield
                    k.op("act", lambda e, S=S, stt=stt: e.activation(out=S[:, 0:wtot], in_=S[:, 0:wtot], func=AF.Exp, bias=stt[:, 1:2], accum_out=stt[:, 2:3]),
                         reads=[S, stt], writes=[S, stt])
                    yield
                    k.op("act", lambda e, stt=stt: e.activation(out=stt[:, 3:4], in_=sinkb[:, hd:hd + 1], func=AF.Exp, bias=stt[:, 1:2]), reads=[stt, sinkb], writes=[stt])
                    yield
                    k.op("dve", lambda e, stt=stt: e.tensor_tensor(out=stt[:, 4:5], in0=stt[:, 2:3], in1=stt[:, 3:4], op=ALU.add), reads=[stt], writes=[stt])
                    yield
                    k.op("dve", lambda e, stt=stt: e.reciprocal(stt[:, 5:6], stt[:, 4:5]), reads=[stt], writes=[stt])
                    yield
                    k.op("dve", lambda e, S=S, stt=stt: e.tensor_scalar(S[:, 0:wtot], S[:, 0:wtot], stt[:, 5:6], None, op0=ALU.mult), reads=[S, stt], writes=[S])
                    yield
                    nkt = wtot // 128
                    PT = PT_p()
                    for ti in range(nkt):
                        pst = k.next_ps()
                        k.op("pe", lambda e, pst=pst, S=S, ti=ti: e.transpose(pst[:, 0:128], S[:, ti * 128:(ti + 1) * 128], ident[:]),
                             reads=[S, ident], writes=[pst])
                        yield
                        if ti % 2 == 0:
                            k.op("act", lambda e, PT=PT, pst=pst, ti=ti: e.copy(PT[:, ti, :], pst[:, 0:128]), reads=[pst], writes=[PT])
                            yield
                        else:
                            k.op("dve", lambda e, PT=PT, pst=pst, ti=ti: e.tensor_copy(PT[:, ti, :], pst[:, 0:128]), reads=[pst], writes=[PT])
                            yield
                    pso = k.next_ps()
                    vtiles = [0, 1] + [2 + j for j in kts]
                    for ti, vti in enumerate(vtiles):
                        k.mm(pso, pso[0:64, 0:128], vt, vt[:, vti, :], PT, PT[:, ti, :], ti == 0, ti == nkt - 1)
                        yield
                    k.op("act", lambda e, ao=ao, pso=pso, q0=q0: e.copy(ao[:, q0:q0 + 128], pso[0:64, 0:128]), reads=[pso], writes=[ao])
                    yield
                for bi_ in range(0, len(blocks), 2):
                    gens = [unit_gen(*blk) for blk in blocks[bi_:bi_ + 2]]
                    while gens:
                        for g_ in list(gens):
                            try:
                                next(g_)
                            except StopIteration:
                                gens.remove(g_)
                lo = 0 if need_ctx else C
                k.dma(ATT[hd * 64:(hd + 1) * 64, lo:NT], ao[:, lo:NT], reads=[ao], writes=[ATT])
        k.phase_end()
        if stop_after == "C":
            break

        k.phase_begin()
        fb = k.sb("hy_fb", [64, 4])
        k.dma(fb[:], w["hy_fb"][:, :], reads=[w["hy_fb"]], writes=[fb])
        fbb = k.sb("hy_fbb", [64, 2])
        k.op("dve", lambda e: e.tensor_tensor(out=fbb[:, 0:1], in0=fb[:, 0:1], in1=fb[:, 1:2], op=ALU.mult), reads=[fb], writes=[fbb])
        k.op("dve", lambda e: e.tensor_tensor(out=fbb[:, 1:2], in0=fb[:, 2:3], in1=fb[:, 3:4], op=ALU.mult), reads=[fb], writes=[fbb])
        w1 = k.sb("hy_w1", [33, 64])
        k.dma(w1[:], w["hy_w1"][:, :], reads=[w["hy_w1"]], writes=[w1])
        w2 = k.sb("hy_w2", [64, 64])
        k.dma(w2[:], w["hy_w2"][:, :], reads=[w["hy_w2"]], writes=[w2])
        w3 = k.sb("hy_w3", [64, 1024])
        k.dma(w3[:], w["hy_w3"][:, :], reads=[w["hy_w3"]], writes=[w3])
        rate = k.sb("hy_rate", [128, 512])
        k.dma(rate[:], hyrate_in[:, :], reads=[hyrate_in], writes=[rate])
        sw = k.sb("hy_sw", [128, 12, 3])
        k.dma(sw[:], w["hy_sw"][:, :, :], reads=[w["hy_sw"]], writes=[sw])
        sbias = k.sb("hy_sb", [128, 12])
        k.dma(sbias[:], w["hy_sb"][:, :], reads=[w["hy_sb"]], writes=[sbias])
        hbias = k.sb("hy_bias", [128, 4])
        k.dma(hbias[:], w["hy_bias"][:, :], reads=[w["hy_bias"]], writes=[hbias])
        m0 = k.sb("m0", [128, 1])
        k.op("pool", lambda e: e.memset(m0[:], 1.0), writes=[m0])
        k.op("pool", lambda e: e.affine_select(out=m0[:], in_=m0[:], pattern=[[0, 1]], compare_op=ALU.not_equal, fill=0.0, base=0, channel_multiplier=1),
             reads=[m0], writes=[m0])
        negpi = k.sb("negpi2", [128, 1])
        k.op("pool", lambda e: e.memset(negpi[:], -math.pi), writes=[negpi])

        def sin_mlp(dst_t, dst, ps, fcol, bcol, n, tmp_p, tmpi_p):
            a = tmp_p()
            k.op("act", lambda e: e.activation(out=a[0:64, 0:n], in_=ps[0:64, 0:n], func=AF.Identity, bias=fbb[:, bcol:bcol + 1], scale=fb[:, fcol:fcol + 1]),
                 reads=[ps, fb, fbb], writes=[a])
            ki = tmpi_p()
            k.op("dve", lambda e: e.tensor_scalar(ki[0:64, 0:n], a[0:64, 0:n], 1.0 / (2.0 * math.pi), None, op0=ALU.mult), reads=[a], writes=[ki])
            kf = tmp_p()
            k.op("dve", lambda e: e.tensor_copy(kf[0:64, 0:n], ki[0:64, 0:n]), reads=[ki], writes=[kf])
            k.op("dve", lambda e: e.scalar_tensor_tensor(out=a[0:64, 0:n], in0=kf[0:64, 0:n], scalar=-2.0 * math.pi, in1=a[0:64, 0:n], op0=ALU.mult, op1=ALU.add),
                 reads=[kf, a], writes=[a])
            k.op("dve", lambda e: e.tensor_scalar(a[0:64, 0:n], a[0:64, 0:n], 3.1415925, -3.1415925, op0=ALU.min, op1=ALU.max), reads=[a], writes=[a])
            k.op("act", lambda e: e.activation(out=dst, in_=a[0:64, 0:n], func=AF.Sin), reads=[a], writes=[dst_t])

        seqs = [("L", L, C)] + ([("C", C, 0)] if need_ctx else [])
        tmp_p = k.pool("hy_tmp", [128, 512], 3)
        tmpi_p = k.pool("hy_tmpi", [128, 512], 2, I32)
        h2T_p = k.pool("hy_h2T", [64, 512], 2)
        h1T_p = k.pool("hy_h1T", [64, 512], 2)
        zf_p = k.pool("hy_zf", [33, 512], 2)
        tw_p = k.pool("hy_tw", [128, 32], 1)
        win_p = k.pool("hy_win", [128, 512], 2)
        hfb_p = k.pool("hy_hfb", [128, 2, 512], 2)
        hsd_p = k.pool("hy_hsd", [128, 2, 512], 2)
        zc_p = k.pool("hy_zc", [128, 3, 512 + 2], 2)
        zo_p = k.pool("hy_zo", [128, 3, 512], 2)
        vxt_p = k.pool("hy_vxt", [128, 512], 2)
        for (nm, Ls, toff) in seqs:
            HSD, VXT, VXF, X0F = HSDd[nm], VXTd[nm], VXFd[nm], X0Fd[nm]
            ntile = Ls // 128
            nb = max(1, Ls // 512)
            bw = min(512, Ls)
            tw = tw_p()
            k.dma(tw[:, 0:ntile], hytw_in[nm][:, :], reads=[hytw_in[nm]], writes=[tw])
            for bi in range(nb):
                zf = zf_p()
                k.dma(zf[:, 0:bw], hyz_in[nm][:, bi * bw:(bi + 1) * bw], reads=[hyz_in[nm]], writes=[zf])
                ps1 = k.next_ps()
                k.mm(ps1, ps1[0:64, 0:bw], w1, w1[:, :], zf, zf[:, 0:bw], True, True)
                h1T = h1T_p()
                sin_mlp(h1T, h1T[:, 0:bw], ps1, 1, 0, bw, tmp_p, tmpi_p)
                ps2 = k.next_ps()
                k.mm(ps2, ps2[0:64, 0:bw], w2, w2[:, :], h1T, h1T[:, 0:bw], True, True)
                h2T = h2T_p()
                sin_mlp(h2T, h2T[:, 0:bw], ps2, 3, 1, bw, tmp_p, tmpi_p)
                for ts in range(bw // 128):
                    tt = bi * (bw // 128) + ts
                    win = win_p()
                    k.op("act", lambda e, win=win, tt=tt: e.activation(out=win[:], in_=rate[:], func=AF.Exp, scale=tw[:, tt:tt + 1]), reads=[rate, tw], writes=[win])
                    hfb = hfb_p()
                    for d in range(2):
                        psf = k.next_ps()
                        k.mm(psf, psf[:, :], h2T, h2T[:, ts * 128:(ts + 1) * 128], w3, w3[:, d * 512:(d + 1) * 512], True, True)
                        k.op("dve", lambda e, hfb=hfb, psf=psf, win=win, d=d: e.tensor_tensor(out=hfb[:, d, :], in0=psf[:, :], in1=win[:], op=ALU.mult),
                             reads=[psf, win], writes=[hfb])
                    if tt == 0:
                        k.op("dve", lambda e, hfb=hfb: e.tensor_scalar(hfb[:, 1, :], hfb[:, 1, :], m0[:, 0:1], None, op0=ALU.mult), reads=[hfb, m0], writes=[hfb])
                    hsd = hsd_p()
                    k.op("dve", lambda e, hsd=hsd, hfb=hfb: e.tensor_tensor(out=hsd[:, 0, :], in0=hfb[:, 0, :], in1=hfb[:, 1, :], op=ALU.add), reads=[hfb], writes=[hsd])
                    k.op("pool", lambda e, hsd=hsd, hfb=hfb: e.tensor_tensor(out=hsd[:, 1, :], in0=hfb[:, 0, :], in1=hfb[:, 1, :], op=ALU.subtract), reads=[hfb], writes=[hsd])
                    for d in range(2):
                        k.dma(HSD[d, tt * 128:(tt + 1) * 128, :], hsd[:, d, :], reads=[hsd], writes=[HSD])
            for cc in range(4):
                for bi in range(nb):
                    zc = zc_p()
                    k.op("pool", lambda e, zc=zc: e.memset(zc[:], 0.0), writes=[zc])
                    lo = bi * bw
                    a0 = max(lo - 1, 0)
                    a1 = min(lo + bw + 1, Ls)
                    for pj in range(3):
                        r0 = pj * 512 + cc * 128
                        k.dma(zc[:, pj, (a0 - (lo - 1)):(a1 - (lo - 1))], ZHY[r0:r0 + 128, toff + a0:toff + a1], reads=[ZHY], writes=[zc])
                    zo = zo_p()
                    for pj in range(3):
                        ci = pj * 4 + cc
                        k.op("dve", lambda e, zo=zo, zc=zc, pj=pj, ci=ci: e.tensor_scalar(zo[:, pj, 0:bw], zc[:, pj, 0:bw], sw[:, ci, 0:1], sbias[:, ci:ci + 1], op0=ALU.mult, op1=ALU.add),
                             reads=[zc, sw, sbias], writes=[zo])
                        for tap in (1, 2):
                            k.op("dve", lambda e, zo=zo, zc=zc, pj=pj, ci=ci, tap=tap: e.scalar_tensor_tensor(out=zo[:, pj, 0:bw], in0=zc[:, pj, tap:tap + bw], scalar=sw[:, ci, tap:tap + 1],
                                                                                                                in1=zo[:, pj, 0:bw], op0=ALU.mult, op1=ALU.add), reads=[zc, sw, zo], writes=[zo])
                    k.op("pool", lambda e, zo=zo: e.tensor_tensor(out=zo[:, 2, 0:bw], in0=zo[:, 2, 0:bw], in1=zo[:, 1, 0:bw], op=ALU.mult), reads=[zo], writes=[zo])
                    k.dma(X0F[cc * 128:(cc + 1) * 128, lo:lo + bw], zo[:, 0, 0:bw], reads=[zo], writes=[X0F])
                    k.dma(VXF[cc * 128:(cc + 1) * 128, lo:lo + bw], zo[:, 2, 0:bw], reads=[zo], writes=[VXF])
                    for ts in range(bw // 128):
                        pst = k.next_ps()
                        k.op("pe", lambda e, pst=pst, zo=zo, ts=ts: e.transpose(pst[:, 0:128], zo[:, 2, ts * 128:(ts + 1) * 128], ident[:]), reads=[zo, ident], writes=[pst])
                        vxt = vxt_p()
                        k.op("act", lambda e, vxt=vxt, pst=pst: e.copy(vxt[:, 0:128], pst[:, 0:128]), reads=[pst], writes=[vxt])
                        t0_ = lo + ts * 128
                        k.dma(VXT[t0_:t0_ + 128, cc * 128:(cc + 1) * 128], vxt[:, 0:128], reads=[vxt], writes=[VXT])
        k.phase_end()
        for (nm, Ls, toff) in seqs:
            k.phase_begin()
            HSD, KRI, VXT, VXF, X0F = HSDd[nm], KRId[nm], VXTd[nm], VXFd[nm], X0Fd[nm]
            ntile = Ls // 128
            nb = max(1, Ls // 512)
            bw = min(512, Ls)
            hbias = k.sb("hy_bias", [128, 4])
            k.dma(hbias[:], w["hy_bias"][:, :], reads=[w["hy_bias"]], writes=[hbias])
            Mx = DFT[nm]
            src_p = k.pool("hy_src_" + nm, [128, ntile, 256], 1)
            Y_p = k.pool("hy_Y_" + nm, [128, ntile, 2, 256], 1)
            slab_p = k.pool("hy_slab_" + nm, [128, min(8, ntile), 2, 128], 2)
            kri_p = k.pool("hy_kri_" + nm, [128, 2, 256], 2)
            pw_p = k.pool("hy_pw_" + nm, [128, 4, 256], 2)
            mt_p = k.pool("hy_mt_" + nm, [128, 2, 512], 3)
            ep_p = k.pool("hy_ep_" + nm, [128, 3, 512], 2)
            tg = min(8, ntile)
            for stage in ("kern", "conv"):
                for hh in range(2):
                    cs = slice(hh * 256, (hh + 1) * 256)
                    if stage == "kern":
                        mats = (Mx["CK"], Mx["SK"])
                        for d in range(2):
                            src = src_p()
                            k.dma(src[:, :, :], HSD[d, 0:Ls, cs].rearrange("(n p) c -> p n c", p=128), reads=[HSD], writes=[src])
                            for ft in range(ntile):
                                psK = k.next_ps()
                                for g in range(ntile // tg):
                                    slab = slab_p()
                                    k.dma(slab[:, :, 0, :], mats[d][g * tg * 128:(g + 1) * tg * 128, ft * 128:(ft + 1) * 128].rearrange("(n p) f -> p n f", p=128),
                                          reads=[mats[d]], writes=[slab])
                                    for ti in range(tg):
                                        tt = g * tg + ti
                                        k.mm(psK, psK[:, 0:256], slab, slab[:, ti, 0, :], src, src[:, tt, :], tt == 0, tt == ntile - 1)
                                kri = kri_p()
                                k.op("act", lambda e, kri=kri, psK=psK: e.copy(kri[:, 0, :], psK[:, 0:256]), reads=[psK], writes=[kri])
                                k.dma(KRI[d, ft * 128:(ft + 1) * 128, cs], kri[:, 0, :], reads=[kri], writes=[KRI], q="act")
                        continue
                    mats = (Mx["CS"], Mx["SS"])
                    src = src_p()
                    k.dma(src[:, :, :], VXT[0:Ls, cs].rearrange("(n p) c -> p n c", p=128), reads=[VXT], writes=[src])
                    Y = Y_p()
                    for ft in range(ntile):
                        psR = k.next_ps()
                        psI = k.next_ps()
                        for g in range(ntile // tg):
                            slab = slab_p()
                            for d in range(2):
                                k.dma(slab[:, :, d, :], mats[d][g * tg * 128:(g + 1) * tg * 128, ft * 128:(ft + 1) * 128].rearrange("(n p) f -> p n f", p=128),
                                      reads=[mats[d]], writes=[slab])
                            for ti in range(tg):
                                tt = g * tg + ti
                                k.mm(psR, psR[:, 0:256], slab, slab[:, ti, 0, :], src, src[:, tt, :], tt == 0, tt == ntile - 1)
                                k.mm(psI, psI[:, 0:256], slab, slab[:, ti, 1, :], src, src[:, tt, :], tt == 0, tt == ntile - 1)
                        kri = kri_p()
                        for d in range(2):
                            k.dma(kri[:, d, :], KRI[d, ft * 128:(ft + 1) * 128, cs], reads=[KRI], writes=[kri])
                        pw = pw_p()
                        k.op("dve", lambda e, pw=pw, psR=psR, kri=kri: e.tensor_tensor(out=pw[:, 0, :], in0=psR[:, 0:256], in1=kri[:, 0, :], op=ALU.mult), reads=[psR, kri], writes=[pw])
                        k.op("dve", lambda e, pw=pw, psI=psI, kri=kri: e.tensor_tensor(out=pw[:, 1, :], in0=psI[:, 0:256], in1=kri[:, 1, :], op=ALU.mult), reads=[psI, kri], writes=[pw])
                        k.op("dve", lambda e, pw=pw, psR=psR, kri=kri: e.tensor_tensor(out=pw[:, 2, :], in0=psR[:, 0:256], in1=kri[:, 1, :], op=ALU.mult), reads=[psR, kri], writes=[pw])
                        k.op("dve", lambda e, pw=pw, psI=psI, kri=kri: e.tensor_tensor(out=pw[:, 3, :], in0=psI[:, 0:256], in1=kri[:, 0, :], op=ALU.mult), reads=[psI, kri], writes=[pw])
                        k.op("pool", lambda e, Y=Y, pw=pw, ft=ft: e.tensor_tensor(out=Y[:, ft, 0, :], in0=pw[:, 0, :], in1=pw[:, 1, :], op=ALU.subtract), reads=[pw], writes=[Y])
                        k.op("pool", lambda e, Y=Y, pw=pw, ft=ft: e.tensor_tensor(out=Y[:, ft, 1, :], in0=pw[:, 2, :], in1=pw[:, 3, :], op=ALU.add), reads=[pw], writes=[Y])
                    if stage == "conv":
                        Nfft = 2 * Ls
                        for nbk in range(nb):
                            pso = [k.next_ps(), k.next_ps()]
                            for ft in range(ntile):
                                mt = mt_p()
                                for d in range(2):
                                    k.dma(mt[:, d, 0:bw], mats[d][ft * 128:(ft + 1) * 128, nbk * bw:(nbk + 1) * bw], reads=[mats[d]], writes=[mt])
                                for c2 in range(2):
                                    for d in range(2):
                                        k.mm(pso[c2], pso[c2][:, 0:bw], Y, Y[:, ft, d, c2 * 128:(c2 + 1) * 128], mt, mt[:, d, 0:bw], ft == 0 and d == 0, ft == ntile - 1 and d == 1)
                            for c2 in range(2):
                                ch = hh * 2 + c2
                                ep = ep_p()
                                k.dma(ep[:, 0, 0:bw], VXF[ch * 128:(ch + 1) * 128, nbk * bw:(nbk + 1) * bw], reads=[VXF], writes=[ep])
                                k.dma(ep[:, 1, 0:bw], X0F[ch * 128:(ch + 1) * 128, nbk * bw:(nbk + 1) * bw], reads=[X0F], writes=[ep])
                                k.op("dve", lambda e, ep=ep, ch=ch: e.tensor_scalar(ep[:, 0, 0:bw], ep[:, 0, 0:bw], hbias[:, ch:ch + 1], None, op0=ALU.mult), reads=[ep, hbias], writes=[ep])
                                k.op("dve", lambda e, ep=ep, c2=c2: e.scalar_tensor_tensor(out=ep[:, 2, 0:bw], in0=pso[c2][:, 0:bw], scalar=-2.0 / Nfft, in1=ep[:, 0, 0:bw], op0=ALU.mult, op1=ALU.add),
                                     reads=[pso[c2], ep], writes=[ep])
                                k.op("pool", lambda e, ep=ep: e.tensor_tensor(out=ep[:, 2, 0:bw], in0=ep[:, 2, 0:bw], in1=ep[:, 1, 0:bw], op=ALU.mult), reads=[ep], writes=[ep])
                                k.dma(HY[ch * 128:(ch + 1) * 128, toff + nbk * bw:toff + (nbk + 1) * bw], ep[:, 2, 0:bw], reads=[ep], writes=[HY], q="act")
                k.barrier()
            k.phase_end()
        if stop_after == "D":
            break

        k.phase_begin()
        TWO_PI = 2.0 * math.pi
        P32 = [128, 2, 16]
        a_re = k.sb("a_re", P32); a_im = k.sb("a_im", P32); ldt = k.sb("ldt", P32)
        for t_, src_ in ((a_re, w["s5_a_re"]), (a_im, w["s5_a_im"]), (ldt, w["s5_ldt"])):
            k.dma(t_[:], src_[:, :, :], reads=[src_], writes=[t_])
        prm = k.sb("s5prm", [128, 12, 32])
        fl = lambda t_: t_.t[:, :, :].rearrange("p a b -> p (a b)")
        R_ = lambda i: prm[:, i, :]
        ki32 = k.sb("ki32", [128, 32], I32)
        kf32 = k.sb("kf32", [128, 32])
        hpi = k.sb("hpi", [128, 1])
        k.op("pool", lambda e: e.memset(hpi[:], 0.0), writes=[hpi])

        def reduce_angle(dst, src, shift):
            k.op("dve", lambda e: e.tensor_scalar(R_(8), src, shift, None, op0=ALU.add), reads=[prm], writes=[prm])
            k.op("dve", lambda e: e.tensor_scalar(ki32[:], R_(8), 1.0 / TWO_PI, None, op0=ALU.mult), reads=[prm], writes=[ki32])
            k.op("dve", lambda e: e.tensor_copy(kf32[:], ki32[:]), reads=[ki32], writes=[kf32])
            k.op("dve", lambda e: e.scalar_tensor_tensor(out=dst, in0=kf32[:], scalar=-TWO_PI, in1=R_(8), op0=ALU.mult, op1=ALU.add), reads=[kf32, prm], writes=[prm])
            k.op("dve", lambda e: e.tensor_scalar(dst, dst, 3.1415925, -3.1415925, op0=ALU.min, op1=ALU.max), reads=[prm], writes=[prm])

        k.op("dve", lambda e: e.tensor_scalar(R_(0), fl(a_re), -1e-4, None, op0=ALU.min), reads=[a_re], writes=[prm])
        k.op("act", lambda e: e.activation(out=R_(1), in_=fl(ldt), func=AF.Exp), reads=[ldt], writes=[prm])
        k.op("dve", lambda e: e.tensor_tensor(out=R_(9), in0=R_(0), in1=R_(1), op=ALU.mult), reads=[prm], writes=[prm])
        k.op("act", lambda e: e.activation(out=R_(3), in_=R_(9), func=AF.Exp), reads=[prm], writes=[prm])
        k.op("dve", lambda e: e.tensor_tensor(out=R_(10), in0=fl(a_im), in1=R_(1), op=ALU.mult), reads=[prm, a_im], writes=[prm])
        reduce_angle(R_(2), R_(10), 0.0)
        k.op("act", lambda e: e.activation(out=R_(5), in_=R_(2), func=AF.Sin), reads=[prm], writes=[prm])
        reduce_angle(R_(11), R_(10), math.pi / 2.0)
        k.op("act", lambda e: e.activation(out=R_(4), in_=R_(11), func=AF.Sin), reads=[prm], writes=[prm])
        k.op("dve", lambda e: e.tensor_tensor(out=R_(8), in0=R_(3), in1=R_(4), op=ALU.mult), reads=[prm], writes=[prm])
        k.op("dve", lambda e: e.tensor_scalar(R_(8), R_(8), -1.0, None, op0=ALU.add), reads=[prm], writes=[prm])
        k.op("dve", lambda e: e.tensor_tensor(out=R_(9), in0=R_(3), in1=R_(5), op=ALU.mult), reads=[prm], writes=[prm])
        k.op("dve", lambda e: e.tensor_tensor(out=R_(10), in0=R_(0), in1=R_(0), op=ALU.mult), reads=[prm], writes=[prm])
        k.op("dve", lambda e: e.tensor_tensor(out=R_(11), in0=fl(a_im), in1=fl(a_im), op=ALU.mult), reads=[a_im], writes=[prm])
        k.op("dve", lambda e: e.tensor_tensor(out=R_(10), in0=R_(10), in1=R_(11), op=ALU.add), reads=[prm], writes=[prm])
        k.op("dve", lambda e: e.reciprocal(R_(10), R_(10)), reads=[prm], writes=[prm])
        k.op("dve", lambda e: e.tensor_tensor(out=R_(6), in0=R_(8), in1=R_(0), op=ALU.mult), reads=[prm], writes=[prm])
        k.op("dve", lambda e: e.tensor_tensor(out=R_(11), in0=R_(9), in1=fl(a_im), op=ALU.mult), reads=[prm, a_im], writes=[prm])
        k.op("dve", lambda e: e.tensor_tensor(out=R_(6), in0=R_(6), in1=R_(11), op=ALU.add), reads=[prm], writes=[prm])
        k.op("dve", lambda e: e.tensor_tensor(out=R_(6), in0=R_(6), in1=R_(10), op=ALU.mult), reads=[prm], writes=[prm])
        k.op("dve", lambda e: e.tensor_tensor(out=R_(7), in0=R_(9), in1=R_(0), op=ALU.mult), reads=[prm], writes=[prm])
        k.op("dve", lambda e: e.tensor_tensor(out=R_(11), in0=R_(8), in1=fl(a_im), op=ALU.mult), reads=[prm, a_im], writes=[prm])
        k.op("dve", lambda e: e.tensor_tensor(out=R_(7), in0=R_(7), in1=R_(11), op=ALU.subtract), reads=[prm], writes=[prm])
        k.op("dve", lambda e: e.tensor_tensor(out=R_(7), in0=R_(7), in1=R_(10), op=ALU.mult), reads=[prm], writes=[prm])

        tio = k.sb("tio", [128, 513])
        k.op("pool", lambda e: e.iota(tio[:], pattern=[[1, 513]], base=0, channel_multiplier=0, allow_small_or_imprecise_dtypes=True), writes=[tio])
        s5d = k.sb("s5d", [32, 16])
        k.dma(s5d[:], w["s5_d"][:, :], reads=[w["s5_d"]], writes=[s5d])
        chunks = [(0, 256)] + [(256 + 512 * i, 512) for i in range(8)]
        order = {0: list(range(9)), 1: [0] + list(range(8, 0, -1))}
        u_p = k.pool("s5u", [32, NT], 2)
        ya_p = k.pool("s5ya", [32, NT], 4)
        bc_p = k.pool("s5bc", [128, 4, 16], 2)
        B2_p = k.pool("s5B2", [128, 2, 32], 2)
        BT_p = k.pool("s5BT", [32, 2, 128], 2)
        CL_p = k.pool("s5CL", [128, 2, 32], 2)
        tb_p = k.pool("s5tb", [128, 2, 513], 2)
        ang_p = k.pool("s5ang", [128, 513], 4)
        angi_p = k.pool("s5angi", [128, 513], 2, I32)
        wk_p = k.pool("s5wk", [128, 512], 18)
        g_p = k.pool("s5g", [128, 2, 512], 4)
        h_p = k.pool("s5h", [128, 2, 512], 4)
        ini_p = k.pool("s5ini", [128, 4], 6)
        yo_p = k.pool("s5yo", [32, 512], 3)

        def rev(t_, p0, p1, a, n):
            b_ = t_.t[p0:p1, a + n - 1:a + n]
            return bass.AP(tensor=b_.tensor, offset=b_.offset, ap=[list(b_.ap[0]), [-1, n]])

        for st in range(16):
            u = u_p()
            k.dma(u[:], US5[st * 32:(st + 1) * 32, :], reads=[US5], writes=[u])
            yad = [ya_p(), ya_p()]
            ctxd = {}
            for d in range(2):
                col = d * 16 + st
                bc = bc_p()
                for i_, nm_ in enumerate(("s5_b_re", "s5_b_im", "s5_c_re", "s5_c_im")):
                    k.dma(bc[:, i_, :], w[nm_][d, st * 128:(st + 1) * 128, :], reads=[w[nm_]], writes=[bc])
                B2 = B2_p()
                CL = CL_p()
                k.op("pool", lambda e, B2=B2: e.memset(B2[:], 0.0), writes=[B2])
                k.op("pool", lambda e, CL=CL: e.memset(CL[:], 0.0), writes=[CL])
                wk = wk_p()
                for gl in range(2):
                    ps_ = slice(gl * 64, (gl + 1) * 64)
                    cs_ = slice(gl * 16, (gl + 1) * 16)
                    kr = prm[ps_, 6, col:col + 1]
                    kim = prm[ps_, 7, col:col + 1]
                    k.op("dve", lambda e, wk=wk, bc=bc, ps_=ps_, kim=kim: e.tensor_scalar(wk[ps_, 0:16], bc[ps_, 1, :], kim, None, op0=ALU.mult), reads=[bc, prm], writes=[wk])
                    k.op("dve", lambda e, wk=wk, bc=bc, ps_=ps_, kim=kim: e.tensor_scalar(wk[ps_, 16:32], bc[ps_, 0, :], kim, None, op0=ALU.mult), reads=[bc, prm], writes=[wk])
                    k.op("dve", lambda e, B2=B2, wk=wk, bc=bc, ps_=ps_, cs_=cs_, kr=kr: e.scalar_tensor_tensor(out=B2[ps_, 0, cs_], in0=bc[ps_, 0, :], scalar=kr, in1=wk[ps_, 0:16], op0=ALU.mult, op1=ALU.subtract),
                         reads=[bc, prm, wk], writes=[B2])
                    k.op("dve", lambda e, B2=B2, wk=wk, bc=bc, ps_=ps_, cs_=cs_, kr=kr: e.scalar_tensor_tensor(out=B2[ps_, 1, cs_], in0=bc[ps_, 1, :], scalar=kr, in1=wk[ps_, 16:32], op0=ALU.mult, op1=ALU.add),
                         reads=[bc, prm, wk], writes=[B2])
                    k.op("dve", lambda e, CL=CL, bc=bc, ps_=ps_, cs_=cs_: e.tensor_copy(CL[ps_, 0, cs_], bc[ps_, 2, :]), reads=[bc], writes=[CL])
                    k.op("dve", lambda e, CL=CL, bc=bc, ps_=ps_, cs_=cs_: e.tensor_scalar(CL[ps_, 1, cs_], bc[ps_, 3, :], -1.0, None, op0=ALU.mult), reads=[bc], writes=[CL])
                BT = BT_p()
                for ri in range(2):
                    pst = k.next_ps()
                    k.op("pe", lambda e, pst=pst, B2=B2, ri=ri: e.transpose(pst[0:32, 0:128], B2[:, ri, :], ident[:]), reads=[B2, ident], writes=[pst])
                    k.op("act", lambda e, BT=BT, pst=pst, ri=ri: e.copy(BT[:, ri, :], pst[0:32, 0:128]), reads=[pst], writes=[BT])
                tb = tb_p()
                for ti_, shift in ((1, 0.0), (0, math.pi / 2.0)):
                    ang = ang_p()
                    k.op("dve", lambda e, ang=ang, shift=shift: e.tensor_scalar(ang[:], tio[:], prm[:, 2, col:col + 1], shift, op0=ALU.mult, op1=ALU.add), reads=[tio, prm], writes=[ang])
                    angi = angi_p()
                    k.op("dve", lambda e, ang=ang, angi=angi: e.tensor_scalar(angi[:], ang[:], 1.0 / TWO_PI, None, op0=ALU.mult), reads=[ang], writes=[angi])
                    angf = ang_p()
                    k.op("pool", lambda e, angf=angf, angi=angi: e.tensor_copy(angf[:], angi[:]), reads=[angi], writes=[angf])
                    k.op("dve", lambda e, ang=ang, angf=angf: e.scalar_tensor_tensor(out=ang[:], in0=angf[:], scalar=-TWO_PI, in1=ang[:], op0=ALU.mult, op1=ALU.add), reads=[angf, ang], writes=[ang])
                    k.op("dve", lambda e, ang=ang: e.tensor_scalar(ang[:], ang[:], 3.1415925, -3.1415925, op0=ALU.min, op1=ALU.max), reads=[ang], writes=[ang])
                    k.op("act", lambda e, tb=tb, ang=ang, ti_=ti_: e.activation(out=tb[:, ti_, :], in_=ang[:], func=AF.Sin), reads=[ang], writes=[tb])
                ctxd[d] = dict(col=col, BT=BT, CL=CL, tb=tb, rho_b=prm[:, 3, col:col + 1], prev=None)
            def chunk_gen(d, step):
                cx = ctxd[d]
                col, BT, CL, tb, rho_b, prev, ya = cx["col"], cx["BT"], cx["CL"], cx["tb"], cx["rho_b"], cx["prev"], yad[d]
                ci = order[d][step]
                c0, n = chunks[ci]
                rhs_u = u[:, c0:c0 + n] if d == 0 else rev(u, 0, 32, c0, n)
                pbr = k.next_ps()
                pbi = k.next_ps()
                k.mm(pbr, pbr[:, 0:n], BT, BT[:, 0, :], u, rhs_u, True, True)
                yield
                k.mm(pbi, pbi[:, 0:n], BT, BT[:, 1, :], u, rhs_u, True, True)
                yield
                cosT = tb[:, 0, 0:n]
                sinT = tb[:, 1, 0:n]
                w0, w1, w2, w3_ = wk_p(), wk_p(), wk_p(), wk_p()
                k.op("dve", lambda e, w0=w0, pbr=pbr, cosT=cosT: e.tensor_tensor(out=w0[:, 0:n], in0=pbr[:, 0:n], in1=cosT, op=ALU.mult), reads=[pbr, tb], writes=[w0])
                yield
                k.op("dve", lambda e, w1=w1, pbi=pbi, sinT=sinT: e.tensor_tensor(out=w1[:, 0:n], in0=pbi[:, 0:n], in1=sinT, op=ALU.mult), reads=[pbi, tb], writes=[w1])
                yield
                k.op("dve", lambda e, w2=w2, pbi=pbi, cosT=cosT: e.tensor_tensor(out=w2[:, 0:n], in0=pbi[:, 0:n], in1=cosT, op=ALU.mult), reads=[pbi, tb], writes=[w2])
                yield
                k.op("dve", lambda e, w3_=w3_, pbr=pbr, sinT=sinT: e.tensor_tensor(out=w3_[:, 0:n], in0=pbr[:, 0:n], in1=sinT, op=ALU.mult), reads=[pbr, tb], writes=[w3_])
                yield
                k.op("pool", lambda e, w0=w0, w1=w1: e.tensor_tensor(out=w0[:, 0:n], in0=w0[:, 0:n], in1=w1[:, 0:n], op=ALU.add), reads=[w0, w1], writes=[w0])
                yield
                k.op("pool", lambda e, w2=w2, w3_=w3_: e.tensor_tensor(out=w2[:, 0:n], in0=w2[:, 0:n], in1=w3_[:, 0:n], op=ALU.subtract), reads=[w2, w3_], writes=[w2])
                yield
                ini = ini_p()
                if prev is None:
                    k.op("pool", lambda e, ini=ini: e.memset(ini[:], 0.0), writes=[ini])
                    yield
                else:
                    gp, npv = prev
                    cr = tb[:, 0, npv:npv + 1]
                    sr = tb[:, 1, npv:npv + 1]
                    k.op("dve", lambda e, ini=ini, gp=gp, npv=npv, cr=cr: e.tensor_tensor(out=ini[:, 2:3], in0=gp[:, 0, npv - 1:npv], in1=cr, op=ALU.mult), reads=[gp, tb], writes=[ini])
                    yield
                    k.op("dve", lambda e, ini=ini, gp=gp, npv=npv, sr=sr: e.tensor_tensor(out=ini[:, 3:4], in0=gp[:, 1, npv - 1:npv], in1=sr, op=ALU.mult), reads=[gp, tb], writes=[ini])
                    yield
                    k.op("dve", lambda e, ini=ini: e.tensor_tensor(out=ini[:, 0:1], in0=ini[:, 2:3], in1=ini[:, 3:4], op=ALU.subtract), reads=[ini], writes=[ini])
                    yield
                    k.op("dve", lambda e, ini=ini, gp=gp, npv=npv, sr=sr: e.tensor_tensor(out=ini[:, 2:3], in0=gp[:, 0, npv - 1:npv], in1=sr, op=ALU.mult), reads=[gp, tb], writes=[ini])
                    yield
                    k.op("dve", lambda e, ini=ini, gp=gp, npv=npv, cr=cr: e.tensor_tensor(out=ini[:, 3:4], in0=gp[:, 1, npv - 1:npv], in1=cr, op=ALU.mult), reads=[gp, tb], writes=[ini])
                    yield
                    k.op("dve", lambda e, ini=ini: e.tensor_tensor(out=ini[:, 1:2], in0=ini[:, 2:3], in1=ini[:, 3:4], op=ALU.add), reads=[ini], writes=[ini])
                    yield
                g = g_p()
                k.op("dve", lambda e, g=g, w0=w0, ini=ini: e.tensor_tensor_scan(g[:, 0, 0:n], rho_b.to_broadcast([128, n]), w0[:, 0:n], ini[:, 0:1], op0=ALU.mult, op1=ALU.add),
                     reads=[prm, w0, ini], writes=[g])
                yield
                k.op("dve", lambda e, g=g, w2=w2, ini=ini: e.tensor_tensor_scan(g[:, 1, 0:n], rho_b.to_broadcast([128, n]), w2[:, 0:n], ini[:, 1:2], op0=ALU.mult, op1=ALU.add),
                     reads=[prm, w2, ini], writes=[g])
                yield
                prev = (g, n)
                cx["prev"] = prev
                h = h_p()
                x0_, x1_, x2_, x3_ = wk_p(), wk_p(), wk_p(), wk_p()
                k.op("pool", lambda e, x0_=x0_, g=g, cosT=cosT: e.tensor_tensor(out=x0_[:, 0:n], in0=g[:, 0, 0:n], in1=cosT, op=ALU.mult), reads=[g, tb], writes=[x0_])
                yield
                k.op("pool", lambda e, x1_=x1_, g=g, sinT=sinT: e.tensor_tensor(out=x1_[:, 0:n], in0=g[:, 1, 0:n], in1=sinT, op=ALU.mult), reads=[g, tb], writes=[x1_])
                yield
                k.op("dve", lambda e, x2_=x2_, g=g, sinT=sinT: e.tensor_tensor(out=x2_[:, 0:n], in0=g[:, 0, 0:n], in1=sinT, op=ALU.mult), reads=[g, tb], writes=[x2_])
                yield
                k.op("dve", lambda e, x3_=x3_, g=g, cosT=cosT: e.tensor_tensor(out=x3_[:, 0:n], in0=g[:, 1, 0:n], in1=cosT, op=ALU.mult), reads=[g, tb], writes=[x3_])
                yield
                k.op("pool", lambda e, h=h, x0_=x0_, x1_=x1_: e.tensor_tensor(out=h[:, 0, 0:n], in0=x0_[:, 0:n], in1=x1_[:, 0:n], op=ALU.subtract), reads=[x0_, x1_], writes=[h])
                yield
                k.op("dve", lambda e, h=h, x2_=x2_, x3_=x3_: e.tensor_tensor(out=h[:, 1, 0:n], in0=x2_[:, 0:n], in1=x3_[:, 0:n], op=ALU.add), reads=[x2_, x3_], writes=[h])
                yield
                py = k.next_ps()
                for ri in range(2):
                    hb_ = h.t[:, ri, :]
                    if d == 0:
                        rhs_h = h[:, ri, 0:n]
                    else:
                        b_ = h.t[:, ri, n - 1:n]
                        rhs_h = bass.AP(tensor=b_.tensor, offset=b_.offset, ap=[list(b_.ap[0]), [-1, n]])
                    k.mm(py, py[0:32, 0:n], CL, CL[:, ri, :], h, rhs_h, ri == 0, ri == 1)
                    yield
                if d == 0:
                    k.op("act", lambda e, ya=ya, py=py, c0=c0, n=n: e.copy(ya[:, c0:c0 + n], py[0:32, 0:n]), reads=[py], writes=[ya])
                    yield
                else:
                    k.op("dve", lambda e, ya=ya, py=py, c0=c0, n=n: e.tensor_copy(ya[:, c0:c0 + n], py[0:32, 0:n]), reads=[py], writes=[ya])
                    yield
            for step in range(9):
                gens = [chunk_gen(0, step), chunk_gen(1, step)]
                while gens:
                    for g_ in list(gens):
                        try:
                            next(g_)
                        except StopIteration:
                            gens.remove(g_)
            ya = yad[0]
            for (c0, n) in chunks:
                k.op("pool", lambda e, c0=c0, n=n: e.tensor_tensor(out=yad[0][:, c0:c0 + n], in0=yad[1][:, c0:c0 + n], in1=yad[0][:, c0:c0 + n], op=ALU.add), reads=[yad[0], yad[1]], writes=[yad[0]])
                yo = yo_p()
                k.op("dve", lambda e, yo=yo, c0=c0, n=n: e.scalar_tensor_tensor(out=yo[:, 0:n], in0=u[:, c0:c0 + n], scalar=s5d[:, st:st + 1], in1=ya[:, c0:c0 + n], op0=ALU.mult, op1=ALU.add),
                     reads=[u, s5d, ya], writes=[yo])
                t1 = yo_p()
                k.op("dve", lambda e, yo=yo, t1=t1, n=n: e.tensor_tensor(out=t1[:, 0:n], in0=yo[:, 0:n], in1=yo[:, 0:n], op=ALU.mult), reads=[yo], writes=[t1])
                k.op("dve", lambda e, t1=t1, n=n: e.tensor_scalar(t1[:, 0:n], t1[:, 0:n], 0.044715, 1.0, op0=ALU.mult, op1=ALU.add), reads=[t1], writes=[t1])
                k.op("dve", lambda e, yo=yo, t1=t1, n=n: e.tensor_tensor(out=t1[:, 0:n], in0=t1[:, 0:n], in1=yo[:, 0:n], op=ALU.mult), reads=[t1, yo], writes=[t1])
                k.op("act", lambda e, t1=t1, n=n: e.activation(out=t1[:, 0:n], in_=t1[:, 0:n], func=AF.Sigmoid, scale=2.0 * math.sqrt(2.0 / math.pi)), reads=[t1], writes=[t1])
                k.op("dve", lambda e, yo=yo, t1=t1, n=n: e.tensor_tensor(out=yo[:, 0:n], in0=yo[:, 0:n], in1=t1[:, 0:n], op=ALU.mult), reads=[t1, yo], writes=[yo])
                k.dma(S5Y[st * 32:(st + 1) * 32, c0:c0 + n], yo[:, 0:n], reads=[yo], writes=[S5Y])
        k.phase_end()
        k.phase_begin()
        gw = k.sb("s5gw", [128, 4, 512])
        k.dma(gw[:], w["s5_gw"].t.rearrange("(kc p) n -> p kc n", p=128), reads=[w["s5_gw"]], writes=[gw])
        ggb = k.sb("s5gb", [128, 4])
        k.dma(ggb[:], w["s5_gb"][:, :], reads=[w["s5_gb"]], writes=[ggb])
        yb_p = k.pool("s5yb", [128, 4, 512], 2)
        go_p2 = k.pool("s5go", [128, 512], 3)
        for (c0, n) in NBLK:
            yb = yb_p()
            k.dma(yb[:, :, 0:n], S5Y[:, c0:c0 + n].rearrange("(kc p) t -> p kc t", p=128), reads=[S5Y], writes=[yb])
            for mc_ in range(4):
                ps = k.next_ps()
                for kc in range(4):
                    k.mm(ps, ps[:, 0:n], gw, gw[:, kc, mc_ * 128:(mc_ + 1) * 128], yb, yb[:, kc, 0:n], kc == 0, kc == 3)
                go = go_p2()
                k.op("act", lambda e, go=go, ps=ps, mc_=mc_: e.activation(out=go[:, 0:n], in_=ps[:, 0:n], func=AF.Sigmoid, bias=ggb[:, mc_:mc_ + 1]), reads=[ps, ggb], writes=[go])
                k.op("dve", lambda e, go=go, yb=yb, mc_=mc_: e.tensor_tensor(out=go[:, 0:n], in0=go[:, 0:n], in1=yb[:, mc_, 0:n], op=ALU.mult), reads=[go, yb], writes=[go])
                k.dma(S5O[mc_ * 128:(mc_ + 1) * 128, c0:c0 + n], go[:, 0:n], reads=[go], writes=[S5O])
        k.phase_end()
        if stop_after == "E":
            break

        k.phase_begin()
        modt = k.sb("modtF", [128, 96])
        k.dma(modt[:], MODT[:, :], reads=[MODT], writes=[modt])
        m3 = modt.t[:, :].rearrange("p (f j) -> p f j", j=2)
        brw = []
        for nm_ in ("br_attn_w", "br_hyena_w", "br_s5_w"):
            t_ = k.sb(nm_, [128, 4, D])
            k.dma(t_[:], w[nm_].t.rearrange("(kc p) n -> p kc n", p=128), reads=[w[nm_]], writes=[t_])
            brw.append(t_)
        ow = k.sb("out_w", [128, 8, D])
        k.dma(ow[:], w["out_w"].t.rearrange("(kc p) n -> p kc n", p=128), reads=[w["out_w"]], writes=[ow])
        br_p = k.pool("brin", [128, 3, 4, 512], 1)
        gt_p = k.pool("gt", [128, 3, 512], 2)
        mm_p = k.pool("mmix", [128, 8, 512], 1)
        xb_p = k.pool("xbF", [128, 8, 512], 1)
        t_p = k.pool("tF", [128, 512], 4)
        xo_p = k.pool("xoF", [128, 512], 3)
        srcs = (ATT, HY, S5O)
        for bi, (c0, n) in enumerate(NBLK):
            if bi == 0 and not need_ctx:
                continue
            j = 1 if bi == 0 else 0
            br = br_p()
            for b_ in range(3):
                k.dma(br[:, b_, :, 0:n], srcs[b_][:, c0:c0 + n].rearrange("(kc p) t -> p kc t", p=128), reads=[srcs[b_]], writes=[br])
            xb = xb_p()
            k.dma(xb[:, :, 0:n], XT[:, c0:c0 + n].rearrange("(kc p) t -> p kc t", p=128), reads=[XT], writes=[xb])
            mx = mm_p()
            for fc in range(8):
                gt = gt_p()
                for b_ in range(3):
                    k.dma(gt[:, b_, 0:n], GATE[b_ * D + fc * 128:b_ * D + (fc + 1) * 128, c0:c0 + n], reads=[GATE], writes=[gt])
                pb = [k.next_ps() for _ in range(3)]
                for b_ in range(3):
                    for kc in range(4):
                        k.mm(pb[b_], pb[b_][:, 0:n], brw[b_], brw[b_][:, kc, fc * 128:(fc + 1) * 128], br, br[:, b_, kc, 0:n], kc == 0, kc == 3)
                ta, tb_, tc_ = t_p(), t_p(), t_p()
                k.op("dve", lambda e, ta=ta, pb=pb, gt=gt: e.tensor_tensor(out=ta[:, 0:n], in0=pb[0][:, 0:n], in1=gt[:, 0, 0:n], op=ALU.mult), reads=[pb[0], gt], writes=[ta])
                k.op("dve", lambda e, tb_=tb_, pb=pb, gt=gt: e.tensor_tensor(out=tb_[:, 0:n], in0=pb[1][:, 0:n], in1=gt[:, 1, 0:n], op=ALU.mult), reads=[pb[1], gt], writes=[tb_])
                k.op("dve", lambda e, tc_=tc_, pb=pb, gt=gt: e.tensor_tensor(out=tc_[:, 0:n], in0=pb[2][:, 0:n], in1=gt[:, 2, 0:n], op=ALU.mult), reads=[pb[2], gt], writes=[tc_])
                k.op("pool", lambda e, ta=ta, tb_=tb_: e.tensor_tensor(out=ta[:, 0:n], in0=ta[:, 0:n], in1=tb_[:, 0:n], op=ALU.add), reads=[ta, tb_], writes=[ta])
                k.op("pool", lambda e, mx=mx, ta=ta, tc_=tc_, fc=fc: e.tensor_tensor(out=mx[:, fc, 0:n], in0=ta[:, 0:n], in1=tc_[:, 0:n], op=ALU.add), reads=[ta, tc_], writes=[mx])
            for dc in range(8):
                po = k.next_ps()
                for fc in range(8):
                    k.mm(po, po[:, 0:n], ow, ow[:, fc, dc * 128:(dc + 1) * 128], mx, mx[:, fc, 0:n], fc == 0, fc == 7)
                xo = xo_p()
                if dbg:
                    mo = xo_p()
                    k.op("act", lambda e, mo=mo, po=po: e.copy(mo[:, 0:n], po[:, 0:n]), reads=[po], writes=[mo])
                    k.dma(MIX[dc * 128:(dc + 1) * 128, c0:c0 + n], mo[:, 0:n], reads=[mo], writes=[MIX])
                k.op("dve", lambda e, xo=xo, po=po, xb=xb, dc=dc: e.scalar_tensor_tensor(out=xo[:, 0:n], in0=po[:, 0:n], scalar=m3[:, 16 + dc, j:j + 1], in1=xb[:, dc, 0:n], op0=ALU.mult, op1=ALU.add),
                     reads=[po, modt, xb], writes=[xo])
                k.dma(XT[dc * 128:(dc + 1) * 128, c0:c0 + n], xo[:, 0:n], reads=[xo], writes=[XT], q="act")
                if dbg:
                    k.dma(XDBG[dc * 128:(dc + 1) * 128, c0:c0 + n], xo[:, 0:n], reads=[xo], writes=[XDBG])
        k.phase_end()
        if stop_after == "F":
            break

        k.phase_begin()
        modt = k.sb("modtG", [128, 96])
        k.dma(modt[:], MODT[:, :], reads=[MODT], writes=[modt])
        m3 = modt.t[:, :].rearrange("p (f j) -> p f j", j=2)
        g2n = k.sb("g2n", [128, 8])
        k.dma(g2n[:], w["n2g"][:, :], reads=[w["n2g"]], writes=[g2n])
        A2 = k.sb("A2", [128, 8, 2])
        for j in range(2):
            k.op("dve", lambda e, j=j: e.scalar_tensor_tensor(out=A2[:, :, j], in0=m3[:, 32:40, j], scalar=1.0, in1=g2n[:], op0=ALU.add, op1=ALU.mult), reads=[modt, g2n], writes=[A2])
        wq = k.sb("peer_wq", [128, 8, 2048])
        wqv = w["peer_wq"].t.rearrange("(kc p) n -> p kc n", p=128)
        for kc in range(8):
            k.dma(wq[:, kc, :], wqv[:, kc, :], reads=[w["peer_wq"]], writes=[wq])
        kT = k.sb("peer_kT", [128, 16, 128])
        k.dma(kT[:], w["peer_kT"].t.rearrange("j k n -> k j n"), reads=[w["peer_kT"]], writes=[kT])
        csel = k.sb("csel", [128, 255])
        k.dma(csel[:], csel_in[:, :], reads=[csel_in], writes=[csel])
        io16 = k.sb("io16", [128, 16])
        k.dma(io16[:], iota16_in[:, :], reads=[iota16_in], writes=[io16])
        xb_p = k.pool("xbG", [128, 8, 128], 1)
        hb_p = k.pool("hbG", [128, 8, 128], 1)
        sq_p = k.pool("sqG", [128, 8, 128], 1)
        sm_p = k.pool("smG", [128, 128], 6)
        qT_p = k.pool("qTG", [128, 16, 128], 1)
        S_p = k.pool("SG", [128, 16, 128], 1)
        SW_p = k.pool("SWG", [128, 128], 2)
        TV_p = k.pool("TVG", [128, 16, 16], 1)
        TI_p = k.pool("TIG", [128, 16, 16], 1, U32)
        TF_p = k.pool("TFG", [128, 16, 16], 1)
        cand_p = k.pool("candG", [128, 8, 256], 1)
        cw_p = k.pool("cwG", [128, 256], 2)
        BV_p = k.pool("BVG", [128, 8, 16], 1)
        BP_p = k.pool("BPG", [128, 8, 16], 1, U32)
        BI_p = k.pool("BIG", [128, 8, 16], 2, I32)
        oh_p = k.pool("ohG", [128, 16, 16], 2)
        IDF_p = k.pool("IDFG", [128, 128], 1)
        GT_p = k.pool("GTG", [128, 128], 1)
        idT_p = k.pool("idTG", [128, 128], 1, I32)
        gT_p = k.pool("gTG", [128, 128], 1)
        htok_p = k.pool("htokG", [128, D], 1)
        ug_p = k.pool("ugG", [128, D], 6)
        vg_p = k.pool("vgG", [128, D], 6)
        dots_p = k.pool("dotsG", [128, 128], 1)
        wts_p = k.pool("wtsG", [128, 128], 1)
        wt_p = k.pool("wtG", [128, 128], 3)
        otok_p = k.pool("otokG", [128, D], 1)
        xo_p = k.pool("xoG", [128, 128], 3)
        GELU_C = 2.0 * math.sqrt(2.0 / math.pi)

        def bc3(t_ap_base, strides):
            return bass.AP(tensor=t_ap_base.tensor, offset=t_ap_base.offset, ap=[list(t_ap_base.ap[0]), [strides[0], 8], [strides[1], 16], [strides[2], 16]])

        tiles = [(t0, 1 if t0 < C else 0) for t0 in range(0 if need_ctx else C, NT, 128)]
        for ti_, (t0, j) in enumerate(tiles):
            if ti_ > 0 and ti_ % 8 == 0:
                k.barrier()
            xb = xb_p()
            k.dma(xb[:], XT[:, t0:t0 + 128].rearrange("(kc p) t -> p kc t", p=128), reads=[XT], writes=[xb])
            sq = sq_p()
            k.op("act", lambda e, sq=sq, xb=xb: e.activation(out=sq[:], in_=xb[:], func=AF.Square), reads=[xb], writes=[sq])
            pss = k.next_ps()
            for kc in range(8):
                k.mm(pss, pss[:, 0:128], ones, ones[:], sq, sq[:, kc, :], kc == 0, kc == 7)
            rstd = sm_p()
            k.op("act", lambda e, rstd=rstd, pss=pss: e.activation(out=rstd[:], in_=pss[:, 0:128], func=AF.Sqrt, bias=eps_t[:, 0:1], scale=1.0 / D), reads=[pss, eps_t], writes=[rstd])
            k.op("dve", lambda e, rstd=rstd: e.reciprocal(rstd[:], rstd[:]), reads=[rstd], writes=[rstd])
            hb = hb_p()
            for kc in range(8):
                k.op("dve", lambda e, hb=hb, xb=xb, rstd=rstd, kc=kc: e.tensor_tensor(out=hb[:, kc, :], in0=xb[:, kc, :], in1=rstd[:], op=ALU.mult), reads=[xb, rstd], writes=[hb])
                k.op("pool", lambda e, hb=hb, kc=kc: e.tensor_scalar(hb[:, kc, :], hb[:, kc, :], A2[:, kc, j:j + 1], m3[:, 24 + kc, j:j + 1], op0=ALU.mult, op1=ALU.add), reads=[hb, A2, modt], writes=[hb])
            htok = htok_p()
            for kc in range(8):
                pst = k.next_ps()
                k.op("pe", lambda e, pst=pst, hb=hb, kc=kc: e.transpose(pst[:, 0:128], hb[:, kc, :], ident[:]), reads=[hb, ident], writes=[pst])
                k.op("act", lambda e, htok=htok, pst=pst, kc=kc: e.copy(htok[:, kc * 128:(kc + 1) * 128], pst[:, 0:128]), reads=[pst], writes=[htok])
            qT = qT_p()
            for jc in range(16):
                psq = k.next_ps()
                for kc in range(8):
                    k.mm(psq, psq[:, 0:128], wq, wq[:, kc, jc * 128:(jc + 1) * 128], hb, hb[:, kc, :], kc == 0, kc == 7)
                if jc % 2 == 0:
                    k.op("act", lambda e, qT=qT, psq=psq, jc=jc: e.copy(qT[:, jc, :], psq[:, 0:128]), reads=[psq], writes=[qT])
                else:
                    k.op("dve", lambda e, qT=qT, psq=psq, jc=jc: e.tensor_copy(qT[:, jc, :], psq[:, 0:128]), reads=[psq], writes=[qT])
            S = S_p()
            for jc in range(16):
                pss2 = k.next_ps()
                k.mm(pss2, pss2[:, 0:128], qT, qT[:, jc, :], kT, kT[:, jc, :], True, True)
                if jc % 2 == 0:
                    k.op("act", lambda e, S=S, pss2=pss2, jc=jc: e.copy(S[:, jc, :], pss2[:, 0:128]), reads=[pss2], writes=[S])
                else:
                    k.op("dve", lambda e, S=S, pss2=pss2, jc=jc: e.tensor_copy(S[:, jc, :], pss2[:, 0:128]), reads=[pss2], writes=[S])
            TV = TV_p(); TI = TI_p()
            for jc in range(16):
                SW = SW_p()
                k.op("dve", lambda e, TV=TV, S=S, jc=jc: e.max(out=TV[:, jc, 0:8], in_=S[:, jc, :]), reads=[S], writes=[TV])
                k.op("dve", lambda e, TI=TI, TV=TV, S=S, jc=jc: e.max_index(out=TI[:, jc, 0:8], in_max=TV[:, jc, 0:8], in_values=S[:, jc, :]), reads=[S, TV], writes=[TI])
                k.op("dve", lambda e, SW=SW, TV=TV, S=S, jc=jc: e.match_replace(out=SW[:], in_to_replace=TV[:, jc, 0:8], in_values=S[:, jc, :], imm_value=-1e30), reads=[S, TV], writes=[SW])
                k.op("dve", lambda e, TV=TV, SW=SW, jc=jc: e.max(out=TV[:, jc, 8:16], in_=SW[:]), reads=[SW], writes=[TV])
                k.op("dve", lambda e, TI=TI, TV=TV, SW=SW, jc=jc: e.max_index(out=TI[:, jc, 8:16], in_max=TV[:, jc, 8:16], in_values=SW[:]), reads=[SW, TV], writes=[TI])
            TF = TF_p()
            k.op("dve", lambda e, TF=TF, TI=TI: e.tensor_copy(TF[:], TI[:]), reads=[TI], writes=[TF])
            cand = cand_p()
            tvb = TV.t[:, 0:1, 0:1]
            cv4 = cand.t[:, :, :].rearrange("p h (a b) -> p h a b", b=16)
            in0 = bc3(TV.t[:, 0, 0:1], (32, 1, 0))
            in1 = bc3(TV.t[:, 1, 0:1], (32, 0, 1))
            k.op("dve", lambda e, cv4=cv4, in0=in0, in1=in1: e.tensor_tensor(out=cv4, in0=in0, in1=in1, op=ALU.add), reads=[TV], writes=[cand])
            BV = BV_p(); BP = BP_p()
            for h in range(8):
                cw = cw_p()
                k.op("dve", lambda e, BV=BV, cand=cand, h=h: e.max(out=BV[:, h, 0:8], in_=cand[:, h, :]), reads=[cand], writes=[BV])
                k.op("dve", lambda e, BP=BP, BV=BV, cand=cand, h=h: e.max_index(out=BP[:, h, 0:8], in_max=BV[:, h, 0:8], in_values=cand[:, h, :]), reads=[cand, BV], writes=[BP])
                k.op("dve", lambda e, cw=cw, BV=BV, cand=cand, h=h: e.match_replace(out=cw[:], in_to_replace=BV[:, h, 0:8], in_values=cand[:, h, :], imm_value=-1e30), reads=[cand, BV], writes=[cw])
                k.op("dve", lambda e, BV=BV, cw=cw, h=h: e.max(out=BV[:, h, 8:16], in_=cw[:]), reads=[cw], writes=[BV])
                k.op("dve", lambda e, BP=BP, BV=BV, cw=cw, h=h: e.max_index(out=BP[:, h, 8:16], in_max=BV[:, h, 8:16], in_values=cw[:]), reads=[cw, BV], writes=[BP])
            bpi = BP.t[:, :, :].bitcast(I32)
            Ai = BI_p(); Bi = BI_p()
            k.op("dve", lambda e, Ai=Ai, bpi=bpi: e.tensor_single_scalar(Ai[:], bpi, 4, op=ALU.arith_shift_right), reads=[BP], writes=[Ai])
            k.op("dve", lambda e, Bi=Bi, bpi=bpi: e.tensor_single_scalar(Bi[:], bpi, 15, op=ALU.bitwise_and), reads=[BP], writes=[Bi])
            Af = sm_p(); Bf = sm_p()
            k.op("dve", lambda e, Af=Af, Ai=Ai: e.tensor_copy(Af[:], Ai.t[:, :, :].rearrange("p h k -> p (h k)")), reads=[Ai], writes=[Af])
            k.op("dve", lambda e, Bf=Bf, Bi=Bi: e.tensor_copy(Bf[:], Bi.t[:, :, :].rearrange("p h k -> p (h k)")), reads=[Bi], writes=[Bf])
            IDF = IDF_p()
            isel = sm_p(); jsel = sm_p()
            for h in range(8):
                for (posf, seg, dst) in ((Af, 2 * h, isel), (Bf, 2 * h + 1, jsel)):
                    oh = oh_p()
                    pk = posf.t[:, h * 16:(h + 1) * 16]
                    pk3 = bass.AP(tensor=pk.tensor, offset=pk.offset, ap=[list(pk.ap[0]), [1, 16], [0, 16]])
                    io3 = bass.AP(tensor=io16.t[:, 0:16].tensor, offset=io16.t[:, 0:16].offset, ap=[list(io16.t[:, 0:16].ap[0]), [0, 16], [1, 16]])
                    tf_ = TF.t[:, seg, 0:16]
                    tf3 = bass.AP(tensor=tf_.tensor, offset=tf_.offset, ap=[list(tf_.ap[0]), [0, 16], [1, 16]])
                    k.op("dve", lambda e, oh=oh, pk3=pk3, io3=io3: e.tensor_tensor(out=oh[:], in0=pk3, in1=io3, op=ALU.is_equal), reads=[posf, io16], writes=[oh])
                    k.op("dve", lambda e, oh=oh, tf3=tf3: e.tensor_tensor(out=oh[:], in0=oh[:], in1=tf3, op=ALU.mult), reads=[oh, TF], writes=[oh])
                    k.op("dve", lambda e, oh=oh, dst=dst, h=h: e.tensor_reduce(out=dst[:, h * 16:(h + 1) * 16], in_=oh[:], axis=AX.X, op=ALU.add), reads=[oh], writes=[dst])
            k.op("dve", lambda e, IDF=IDF, isel=isel, jsel=jsel: e.scalar_tensor_tensor(out=IDF[:], in0=isel[:], scalar=128.0, in1=jsel[:], op0=ALU.mult, op1=ALU.add), reads=[isel, jsel], writes=[IDF])
            k.op("dve", lambda e, IDF=IDF: e.tensor_scalar(IDF[:], IDF[:], 16383.0, 0.0, op0=ALU.min, op1=ALU.max), reads=[IDF], writes=[IDF])
            GT = GT_p()
            gst = sm_p()
            for h in range(8):
                k.op("dve", lambda e, gst=gst, BV=BV, h=h: e.tensor_scalar(gst[:, h:h + 1], BV[:, h, 0:1], -1.0, None, op0=ALU.mult), reads=[BV], writes=[gst])
                k.op("act", lambda e, GT=GT, BV=BV, gst=gst, h=h: e.activation(out=GT[:, h * 16:(h + 1) * 16], in_=BV[:, h, :], func=AF.Exp, bias=gst[:, h:h + 1], accum_out=gst[:, 8 + h:9 + h]),
                     reads=[BV, gst], writes=[GT, gst])
            k.op("dve", lambda e, gst=gst: e.reciprocal(gst[:, 16:24], gst[:, 8:16]), reads=[gst], writes=[gst])
            for h in range(8):
                k.op("dve", lambda e, GT=GT, gst=gst, h=h: e.tensor_scalar(GT[:, h * 16:(h + 1) * 16], GT[:, h * 16:(h + 1) * 16], gst[:, 16 + h:17 + h], None, op0=ALU.mult), reads=[GT, gst], writes=[GT])
            idT = idT_p(); gT = gT_p()
            pst = k.next_ps()
            k.op("pe", lambda e, pst=pst, IDF=IDF: e.transpose(pst[:, 0:128], IDF[:], ident[:]), reads=[IDF, ident], writes=[pst])
            k.op("dve", lambda e, idT=idT, pst=pst: e.tensor_copy(idT[:], pst[:, 0:128]), reads=[pst], writes=[idT])
            pst2 = k.next_ps()
            k.op("pe", lambda e, pst2=pst2, GT=GT: e.transpose(pst2[:, 0:128], GT[:], ident[:]), reads=[GT, ident], writes=[pst2])
            k.op("act", lambda e, gT=gT, pst2=pst2: e.copy(gT[:], pst2[:, 0:128]), reads=[pst2], writes=[gT])
            dots = dots_p()
            for t in range(128):
                ug = ug_p()
                k.dma(None, None, reads=[idT, w["peer_u"]], writes=[ug], q="pool",
                      fn=lambda e, ug=ug, t=t: e.indirect_dma_start(out=ug[:], out_offset=None, in_=w["peer_u"][:, :],
                                                                  in_offset=bass.IndirectOffsetOnAxis(ap=idT[:, t:t + 1], axis=0)))
                pb0 = k.next_ps(); pb1 = k.next_ps()
                sel = ident[:, t:t + 1].to_broadcast([128, 128])
                k.mm(pb0, pb0[:, :], ident, sel, htok, htok[:, 0:512], True, True)
                k.mm(pb1, pb1[:, :], ident, sel, htok, htok[:, 512:1024], True, True)
                hrow = vg_p()
                k.op("act", lambda e, hrow=hrow, pb0=pb0: e.copy(hrow[:, 0:512], pb0[:, :]), reads=[pb0], writes=[hrow])
                k.op("act", lambda e, hrow=hrow, pb1=pb1: e.copy(hrow[:, 512:1024], pb1[:, :]), reads=[pb1], writes=[hrow])
                k.op("dve", lambda e, ug=ug, hrow=hrow, t=t: e.scalar_tensor_tensor(out=hrow[:], in0=ug[:], scalar=1.0, in1=hrow[:], op0=ALU.mult, op1=ALU.mult, accum_out=dots[:, t:t + 1]),
                     reads=[ug, hrow], writes=[hrow, dots])
            wts = wts_p()
            g1_ = sm_p()
            k.op("dve", lambda e, g1_=g1_: e.tensor_tensor(out=g1_[:], in0=dots[:], in1=dots[:], op=ALU.mult), reads=[dots], writes=[g1_])
            k.op("dve", lambda e, g1_=g1_: e.tensor_scalar(g1_[:], g1_[:], 0.044715, 1.0, op0=ALU.mult, op1=ALU.add), reads=[g1_], writes=[g1_])
            k.op("dve", lambda e, g1_=g1_: e.tensor_tensor(out=g1_[:], in0=g1_[:], in1=dots[:], op=ALU.mult), reads=[g1_, dots], writes=[g1_])
            k.op("act", lambda e, g1_=g1_: e.activation(out=g1_[:], in_=g1_[:], func=AF.Sigmoid, scale=GELU_C), reads=[g1_], writes=[g1_])
            k.op("dve", lambda e, g1_=g1_: e.tensor_tensor(out=g1_[:], in0=g1_[:], in1=dots[:], op=ALU.mult), reads=[g1_, dots], writes=[g1_])
            k.op("dve", lambda e, wts=wts, g1_=g1_, gT=gT: e.tensor_tensor(out=wts[:], in0=g1_[:], in1=gT[:], op=ALU.mult), reads=[g1_, gT], writes=[wts])
            po0 = k.next_ps(); po1 = k.next_ps()
            for t in range(128):
                vg = vg_p()
                k.dma(None, None, reads=[idT, w["peer_v"]], writes=[vg], q="pool",
                      fn=lambda e, vg=vg, t=t: e.indirect_dma_start(out=vg[:], out_offset=None, in_=w["peer_v"][:, :],
                                                                  in_offset=bass.IndirectOffsetOnAxis(ap=idT[:, t:t + 1], axis=0)))
                wt_ = wt_p()
                k.op("dve", lambda e, wt_=wt_, t=t: e.tensor_scalar(wt_[:], csel[:, 127 - t:255 - t], wts[:, t:t + 1], None, op0=ALU.mult), reads=[csel, wts], writes=[wt_])
                k.mm(po0, po0[:, :], wt_, wt_[:], vg, vg[:, 0:512], t == 0, t == 127)
                k.mm(po1, po1[:, :], wt_, wt_[:], vg, vg[:, 512:1024], t == 0, t == 127)
            otok = otok_p()
            k.op("act", lambda e, otok=otok, po0=po0: e.copy(otok[:, 0:512], po0[:, :]), reads=[po0], writes=[otok])
            k.op("dve", lambda e, otok=otok, po1=po1: e.tensor_copy(otok[:, 512:1024], po1[:, :]), reads=[po1], writes=[otok])
            for dc in range(8):
                pst = k.next_ps()
                k.op("pe", lambda e, pst=pst, otok=otok, dc=dc: e.transpose(pst[:, 0:128], otok[:, dc * 128:(dc + 1) * 128], ident[:]), reads=[otok, ident], writes=[pst])
                xo = xo_p()
                k.op("dve", lambda e, xo=xo, pst=pst, xb=xb, dc=dc: e.scalar_tensor_tensor(out=xo[:], in0=pst[:, 0:128], scalar=m3[:, 40 + dc, j:j + 1], in1=xb[:, dc, :], op0=ALU.mult, op1=ALU.add),
                     reads=[pst, modt, xb], writes=[xo])
                k.dma(XT[dc * 128:(dc + 1) * 128, t0:t0 + 128], xo[:], reads=[xo], writes=[XT])
        k.phase_end()
        if stop_after == "G":
            break

    if stop_after is None:
        k.phase_begin()
        fg = k.sb("final_g", [128, 8])
        k.dma(fg[:], finalg_in[:, :], reads=[finalg_in], writes=[fg])
        xb_p = k.pool("xbN", [128, 8, 512], 2)
        sq_p = k.pool("sqN", [128, 8, 512], 1)
        rs_p = k.pool("rsN", [128, 512], 2)
        yo_p = k.pool("yoN", [128, 512], 4)
        for bi in range(8):
            t0 = C + bi * 512
            xb = xb_p()
            k.dma(xb[:], XT[:, t0:t0 + 512].rearrange("(kc p) t -> p kc t", p=128), reads=[XT], writes=[xb])
            sq = sq_p()
            k.op("act", lambda e, sq=sq, xb=xb: e.activation(out=sq[:], in_=xb[:], func=AF.Square), reads=[xb], writes=[sq])
            pss = k.next_ps()
            for kc in range(8):
                k.mm(pss, pss[:, :], ones, ones[:], sq, sq[:, kc, :], kc == 0, kc == 7)
            rstd = rs_p()
            k.op("act", lambda e, rstd=rstd, pss=pss: e.activation(out=rstd[:], in_=pss[:, :], func=AF.Sqrt, bias=eps_t[:, 0:1], scale=1.0 / D), reads=[pss, eps_t], writes=[rstd])
            k.op("dve", lambda e, rstd=rstd: e.reciprocal(rstd[:], rstd[:]), reads=[rstd], writes=[rstd])
            for kc in range(8):
                yo = yo_p()
                k.op("dve", lambda e, yo=yo, xb=xb, rstd=rstd, kc=kc: e.scalar_tensor_tensor(out=yo[:], in0=xb[:, kc, :], scalar=fg[:, kc:kc + 1], in1=rstd[:], op0=ALU.mult, op1=ALU.mult),
                     reads=[xb, fg, rstd], writes=[yo])
                k.dma(out[kc * 128:(kc + 1) * 128, bi * 512:(bi + 1) * 512], yo[:], reads=[yo], writes=[out])
        k.phase_end()
    else:
        k.phase_begin()
        fin_p = k.pool("fin", [128, L], 2)
        for kc in range(8):
            st = fin_p()
            k.dma(st[:], XT[kc * 128:(kc + 1) * 128, C:NT], reads=[XT], writes=[st])
            k.dma(out[kc * 128:(kc + 1) * 128, :], st[:], reads=[st], writes=[out])
        k.phase_end()
    k.finish()
    return nc, k


def kernel(**inputs):
    inp = {kk: np.asarray(v) for kk, v in inputs.items()}
    nc, _ = build()
    in_maps = [_prep(inp, b) for b in range(4)]
    res = run_bass_kernel_spmd(nc, in_maps, core_ids=list(range(4)))
    return np.stack([np.ascontiguousarray(res.results[b]["out"].T) for b in range(4)], 0).astype(np.float32)
```

```python
import math
import numpy as np
from contextlib import ExitStack
import concourse.bass as bass
import concourse.mybir as mybir
from concourse.bass_utils import run_bass_kernel_spmd

F32 = mybir.dt.float32
I32 = mybir.dt.int32
U32 = mybir.dt.uint32
AF = mybir.ActivationFunctionType
ALU = mybir.AluOpType
AX = mybir.AxisListType

D = 1024
L = 4096
C = 256
NT = L + C
DEPTH = 2
IN_W = 5888
NBLK = [(0, 256)] + [(256 + 512 * i, 512) for i in range(8)]


class Res:
    __slots__ = ("name", "w", "r", "ep")

    def __init__(self, name):
        self.name = name
        self.w = None
        self.r = {}
        self.ep = 0


class Tile(Res):
    __slots__ = ("t",)

    def __init__(self, name, t):
        Res.__init__(self, name)
        self.t = t

    def __getitem__(self, k):
        return self.t[k]


class Eng:
    def __init__(self, name, e, sem):
        self.name = name
        self.e = e
        self.sem = sem
        self.cnt = 0
        self.known = {}


class K:
    SAME_ENGINE_SYNC = True
    ROTATE_AT = 20000

    def __init__(self, nc, n_dma_slots=24):
        self.nc = nc
        self._sems = []
        self.es = ExitStack()
        self.epoch = 1
        self.uid = 0
        self.eng = {}
        for nm, e in (("pe", nc.tensor), ("act", nc.scalar), ("dve", nc.vector),
                      ("pool", nc.gpsimd), ("sp", nc.sync)):
            self.eng[nm] = Eng(nm, e, self.es.enter_context(nc.semaphore("sem_" + nm)))
        self.slots = []
        for i in range(n_dma_slots):
            s = self.es.enter_context(nc.semaphore("dsem%d" % i))
            self.slots.append([s, 0])
        self.slot_i = 0
        self.phase_stack = None
        self.psum = []
        for i in range(8):
            t = self.es.enter_context(nc.psum_tensor("ps%d" % i, [128, 512], F32))
            self.psum.append(Tile("ps%d" % i, t))
        self.ps_i = 0
        self.n_ins = 0
        self.n_rot = 0
        self._sems = []

    def next_ps(self):
        p = self.psum[self.ps_i]
        self.ps_i = (self.ps_i + 1) % 8
        return p

    def sb(self, name, shape, dtype=F32, persistent=False):
        self.uid += 1
        st = self.es if (persistent or self.phase_stack is None) else self.phase_stack
        t = st.enter_context(self.nc.sbuf_tensor("%s_%d" % (name, self.uid), list(shape), dtype))
        return Tile(name, t)

    def pool(self, name, shape, n, dtype=F32):
        tiles = [self.sb("%s%d" % (name, i), shape, dtype) for i in range(n)]
        st = {"i": 0}

        def nxt():
            t = tiles[st["i"]]
            st["i"] = (st["i"] + 1) % n
            return t
        return nxt

    def dram(self, name, shape, dtype=F32, kind="Internal"):
        t = self.nc.dram_tensor(name, list(shape), dtype, kind=kind)
        return Tile(name, t.ap())

    def phase_begin(self):
        assert self.phase_stack is None
        self.phase_stack = ExitStack()

    def phase_end(self):
        self.barrier()
        self.phase_stack.close()
        self.phase_stack = None

    def _key(self, sem):
        for i, s_ in enumerate(self._sems):
            if s_ is sem:
                return i
        self._sems.append(sem)
        return len(self._sems) - 1

    def _wait(self, E, ev):
        sem, val = ev
        k = self._key(sem)
        if E.known.get(k, 0) >= val:
            return
        E.e.wait_ge(sem, val)
        E.known[k] = val
        self.n_ins += 1

    def _deps(self, E, reads, writes, is_pe=False):
        evs = []
        for r in reads:
            if r.ep == self.epoch and r.w is not None:
                evs.append(r.w)
        for w in writes:
            if w.ep == self.epoch:
                if w.w is not None:
                    evs.append(w.w)
                evs.extend(w.r.values())
        for ev in evs:
            if ev[0] is E.sem and (is_pe or not self.SAME_ENGINE_SYNC):
                continue
            self._wait(E, ev)

    def _record(self, ev, reads, writes):
        for r in reads:
            if r.ep != self.epoch:
                r.ep = self.epoch
                r.w = None
                r.r = {}
            r.r[self._key(ev[0])] = ev
        for w in writes:
            w.ep = self.epoch
            w.w = ev
            w.r = {}

    def op(self, eng, fn, reads=(), writes=()):
        E = self.eng[eng]
        self._deps(E, reads, writes, is_pe=(eng == "pe"))
        ins = fn(E.e)
        E.cnt += 1
        ins.then_inc(E.sem, 1)
        self.n_ins += 1
        self._record((E.sem, E.cnt), reads, writes)
        return ins

    def dma(self, out_ap, in_ap, reads=(), writes=(), q="sp", fn=None):
        E = self.eng[q]
        self._deps(E, reads, writes)
        slot = self.slots[self.slot_i]
        self.slot_i = (self.slot_i + 1) % len(self.slots)
        if slot[1] > 0:
            self._wait(E, (slot[0], slot[1]))
        ins = E.e.dma_start(out=out_ap, in_=in_ap) if fn is None else fn(E.e)
        slot[1] += 16
        ins.then_inc(slot[0], 16)
        self.n_ins += 1
        self._record((slot[0], slot[1]), reads, writes)
        return ins

    def barrier(self):
        evs = []
        for E in self.eng.values():
            if E.cnt > 0:
                evs.append((E.sem, E.cnt))
        for s in self.slots:
            if s[1] > 0:
                evs.append((s[0], s[1]))
        for E in self.eng.values():
            for ev in evs:
                if ev[0] is E.sem:
                    continue
                self._wait(E, ev)
        self.epoch += 1
        for E in self.eng.values():
            if E.cnt > self.ROTATE_AT:
                self.n_rot += 1
                E.sem = self.es.enter_context(self.nc.semaphore("sem_%s_r%d" % (E.name, self.n_rot)))
                E.cnt = 0
        for s in self.slots:
            if s[1] > self.ROTATE_AT:
                self.n_rot += 1
                s[0] = self.es.enter_context(self.nc.semaphore("dsem_r%d" % self.n_rot))
                s[1] = 0

    def finish(self):
        self.barrier()
        self.es.close()

    def mm(self, ps, out_ap, lt, lhsT_ap, rt, rhs_ap, start, stop):
        return self.op("pe", lambda e: e.matmul(out_ap, lhsT_ap, rhs_ap, start=start, stop=stop),
                       reads=[lt, rt], writes=[ps])


def _rot_perm():
    perm = []
    for hd in range(10):
        for i in range(64):
            perm.append(hd * 64 + (i + 32) % 64)
    return np.array(perm)


def _rope_tables():
    row = np.repeat(np.arange(L // 64), 64).astype(np.float32)
    col = np.tile(np.arange(64), L // 64).astype(np.float32)
    inv = np.power(np.float32(10000.0), -np.arange(16, dtype=np.float32) / 16).astype(np.float32)
    ang = np.concatenate([row[:, None] * inv, col[:, None] * inv], axis=-1)
    cos = np.cos(ang).astype(np.float32)
    sin = np.sin(ang).astype(np.float32)
    cosT = np.ones((64, NT), np.float32)
    sinT = np.zeros((64, NT), np.float32)
    cosT[:32, C:] = cos.T
    cosT[32:, C:] = cos.T
    sinT[:32, C:] = -sin.T
    sinT[32:, C:] = sin.T
    return np.concatenate([cosT, cosT], 0), np.concatenate([sinT, sinT], 0)


def _pk(v, n):
    return np.ascontiguousarray(np.asarray(v, np.float32).reshape(n, 128).T)


def _prep(inp, b):
    m = {}
    m["xT"] = np.ascontiguousarray(inp["x"][b].T)
    m["ctxT"] = np.ascontiguousarray(inp["ctx"][b].T)
    cv = np.stack([_pk(inp["c"][b], 8), _pk(inp["c_ctx"], 8)], axis=-1)
    m["cvec"] = np.ascontiguousarray(cv.reshape(128, 16))
    perm = _rot_perm()
    cosT, sinT = _rope_tables()
    m["ropec"] = cosT
    qq = np.arange(128)[:, None]
    kk = np.arange(128)[None, :]
    mk = np.zeros((128, 256), np.float32)
    mk[:, :128] = np.where(kk >= qq, 0.0, -1e30)
    mk[:, 128:] = np.where(kk <= qq, 0.0, -1e30)
    m["amask"] = mk
    m["final_g"] = _pk(inp["final_g"], 8)
    csel = np.zeros((128, 255), np.float32)
    csel[:, 127] = 1.0
    m["csel"] = csel
    m["iota16"] = np.ascontiguousarray(np.broadcast_to(np.arange(16, dtype=np.float32)[None, :], (128, 16)))
    for nm, Ls in (("L", L), ("C", C)):
        t = np.arange(Ls, dtype=np.float32)
        tn = t / np.float32(Ls)
        bands = np.arange(1, 17, dtype=np.float32)
        ang = np.float32(2.0 * math.pi) * tn[:, None] * bands[None, :]
        zf = np.concatenate([tn[:, None], np.cos(ang), np.sin(ang)], axis=-1).astype(np.float32)
        m["hyz" + nm] = np.ascontiguousarray(zf.T)
        tw = np.linspace(0.0, 1.0, Ls, dtype=np.float32)
        m["hytw" + nm] = np.ascontiguousarray((-tw).reshape(Ls // 128, 128).T)
    fast = -math.log(1e-2) / 0.3
    slow = -math.log(1e-2) / 1.5
    rate = np.linspace(fast, slow, 512, dtype=np.float32)
    m["hyrate"] = np.ascontiguousarray(np.broadcast_to(rate[None, :], (128, 512)))
    m["ropes"] = sinT
    for l in range(DEPTH):
        m["mod_w%d" % l] = np.ascontiguousarray(inp["mod_w"][l])
        m["mod_b%d" % l] = _pk(inp["mod_b"][l], 48)
        m["n1g%d" % l] = _pk(inp["norm1_g"][l], 8)
        m["in_w%d" % l] = np.ascontiguousarray(inp["in_w"][l])
        m["in_wr%d" % l] = np.ascontiguousarray(inp["in_w"][l][:, perm])
        m["gate_b%d" % l] = _pk(inp["gate_b"][l], 24)
        m["hy_sw%d" % l] = np.ascontiguousarray(np.asarray(inp["hy_short_w"][l], np.float32).reshape(3, 12, 128).transpose(2, 1, 0))
        m["hy_sb%d" % l] = _pk(inp["hy_short_b"][l], 12)
        m["hy_w1%d" % l] = np.ascontiguousarray(inp["hy_w1"][l])
        m["hy_w2%d" % l] = np.ascontiguousarray(inp["hy_w2"][l])
        m["hy_w3%d" % l] = np.ascontiguousarray(inp["hy_w3"][l])
        m["hy_fb%d" % l] = np.ascontiguousarray(np.stack([inp["hy_b1"][l], inp["hy_freq1"][l], inp["hy_b2"][l], inp["hy_freq2"][l]], 1).astype(np.float32))
        m["hy_bias%d" % l] = _pk(inp["hy_bias"][l], 4)
        for nm_ in ("s5_a_re", "s5_a_im"):
            m["%s%d" % (nm_, l)] = np.ascontiguousarray(np.asarray(inp[nm_][l], np.float32).reshape(2, 16, 128).transpose(2, 0, 1))
        m["s5_ldt%d" % l] = np.ascontiguousarray(np.repeat(np.asarray(inp["s5_log_dt"][l], np.float32), 64, axis=1).reshape(2, 16, 128).transpose(2, 0, 1))
        for nm_ in ("s5_b_re", "s5_b_im"):
            m["%s%d" % (nm_, l)] = np.ascontiguousarray(np.asarray(inp[nm_][l], np.float32).reshape(2, 2048, 16))
        for nm_ in ("s5_c_re", "s5_c_im"):
            m["%s%d" % (nm_, l)] = np.ascontiguousarray(np.asarray(inp[nm_][l], np.float32).transpose(0, 1, 3, 2).reshape(2, 2048, 16))
        m["s5_d%d" % l] = np.ascontiguousarray(np.asarray(inp["s5_d"][l], np.float32).reshape(16, 32).T)
        m["s5_gw%d" % l] = np.ascontiguousarray(inp["s5_glu_w"][l])
        m["s5_gb%d" % l] = _pk(inp["s5_glu_b"][l], 4)
        for nm_ in ("br_attn_w", "br_hyena_w", "br_s5_w", "out_w"):
            m["%s%d" % (nm_, l)] = np.ascontiguousarray(inp[nm_][l])
        m["n2g%d" % l] = _pk(inp["norm2_g"][l], 8)
        m["peer_wq%d" % l] = np.ascontiguousarray(inp["peer_wq"][l])
        m["peer_kT%d" % l] = np.ascontiguousarray(np.asarray(inp["peer_keys"][l], np.float32).transpose(0, 1, 3, 2).reshape(16, 128, 128))
        m["peer_u%d" % l] = np.ascontiguousarray(inp["peer_u"][l])
        m["peer_v%d" % l] = np.ascontiguousarray(inp["peer_v"][l])
        m["sink%d" % l] = np.ascontiguousarray(np.broadcast_to(np.asarray(inp["attn_sink"][l], np.float32)[None, :], (128, 8)))
    return m


def build(dbg=False, n_layers=DEPTH, stop_after=None, start_at=None):
    nc = bass.Bass("TRN2", target_bir_lowering=False)
    k = K(nc)
    okind = "ExternalOutput" if dbg else "Internal"
    ein = lambda name, shape, dt=F32: k.dram(name, shape, dt, kind="ExternalInput")
    xT_in = ein("xT", [D, L])
    ctxT_in = ein("ctxT", [D, C])
    cvec_in = ein("cvec", [128, 16])
    ropec_in = ein("ropec", [128, NT])
    ropes_in = ein("ropes", [128, NT])
    amask_in = ein("amask", [128, 256])
    finalg_in = ein("final_g", [128, 8])
    csel_in = ein("csel", [128, 255])
    iota16_in = ein("iota16", [128, 16])
    hyz_in = {"L": ein("hyzL", [33, L]), "C": ein("hyzC", [33, C])}
    hytw_in = {"L": ein("hytwL", [128, L // 128]), "C": ein("hytwC", [128, C // 128])}
    hyrate_in = ein("hyrate", [128, 512])
    W = []
    for l in range(DEPTH):
        W.append(dict(mod_w=ein("mod_w%d" % l, [D, 6 * D]), mod_b=ein("mod_b%d" % l, [128, 48]),
                      n1g=ein("n1g%d" % l, [128, 8]), hy_sw=ein("hy_sw%d" % l, [128, 12, 3]), hy_sb=ein("hy_sb%d" % l, [128, 12]),
                      hy_w1=ein("hy_w1%d" % l, [33, 64]), hy_w2=ein("hy_w2%d" % l, [64, 64]), hy_w3=ein("hy_w3%d" % l, [64, 1024]),
                      hy_fb=ein("hy_fb%d" % l, [64, 4]),
                      n2g=ein("n2g%d" % l, [128, 8]), peer_wq=ein("peer_wq%d" % l, [D, 2048]), peer_kT=ein("peer_kT%d" % l, [16, 128, 128]),
                      peer_u=ein("peer_u%d" % l, [16384, D]), peer_v=ein("peer_v%d" % l, [16384, D]),
                      br_attn_w=ein("br_attn_w%d" % l, [512, D]), br_hyena_w=ein("br_hyena_w%d" % l, [512, D]), br_s5_w=ein("br_s5_w%d" % l, [512, D]),
                      out_w=ein("out_w%d" % l, [D, D]),
                      s5_a_re=ein("s5_a_re%d" % l, [128, 2, 16]), s5_a_im=ein("s5_a_im%d" % l, [128, 2, 16]), s5_ldt=ein("s5_ldt%d" % l, [128, 2, 16]),
                      s5_b_re=ein("s5_b_re%d" % l, [2, 2048, 16]), s5_b_im=ein("s5_b_im%d" % l, [2, 2048, 16]),
                      s5_c_re=ein("s5_c_re%d" % l, [2, 2048, 16]), s5_c_im=ein("s5_c_im%d" % l, [2, 2048, 16]),
                      s5_d=ein("s5_d%d" % l, [32, 16]), s5_gw=ein("s5_gw%d" % l, [512, 512]), s5_gb=ein("s5_gb%d" % l, [128, 4]), hy_bias=ein("hy_bias%d" % l, [128, 4]), sink=ein("sink%d" % l, [128, 8]), in_w=ein("in_w%d" % l, [D, IN_W]),
                      in_wr=ein("in_wr%d" % l, [D, 640]), gate_b=ein("gate_b%d" % l, [128, 24])))
    XT = k.dram("XT", [D, NT])
    MODT = k.dram("MODT", [128, 96], kind=okind)
    QT = k.dram("QT", [512, NT], kind=okind)
    KT = k.dram("KT", [128, NT], kind=okind)
    VTOK = k.dram("VTOK", [NT, 128], kind=okind)
    US5 = k.dram("US5", [512, NT], kind=okind)
    ZHY = k.dram("ZHY", [1536, NT], kind=okind)
    GATE = k.dram("GATE", [3072, NT], kind=okind)
    HT = k.dram("HT", [D, NT], kind=okind)
    MIX = k.dram("MIX", [D, NT], kind=okind)
    XDBG = k.dram("XDBG", [D, NT], kind=okind)
    S5Y = k.dram("S5Y", [512, NT], kind=okind)
    S5O = k.dram("S5O", [512, NT], kind=okind)
    HY = k.dram("HY", [512, NT], kind=okind)
    DFT = {}
    for nm, Ls in (("L", L), ("C", C)):
        DFT[nm] = {m_: k.dram("DFT_%s_%s" % (nm, m_), [Ls, Ls]) for m_ in ("CK", "SK", "CS", "SS")}
    HSDd, KRId, VXTd, VXFd, X0Fd = {}, {}, {}, {}, {}
    for nm, Ls in (("L", L), ("C", C)):
        HSDd[nm] = k.dram("HSD" + nm, [2, Ls, 512])
        KRId[nm] = k.dram("KRI" + nm, [2, Ls, 512])
        VXTd[nm] = k.dram("VXT" + nm, [Ls, 512])
        VXFd[nm] = k.dram("VXF" + nm, [512, Ls])
        X0Fd[nm] = k.dram("X0F" + nm, [512, Ls])
    ATT = k.dram("ATT", [512, NT], kind=okind)
    out = k.dram("out", [D, L], kind="ExternalOutput")

    ones = k.sb("ones", [128, 128], persistent=True)
    k.op("pool", lambda e: e.memset(ones[:], 1.0), writes=[ones])
    eps_t = k.sb("eps", [128, 1], persistent=True)
    k.op("pool", lambda e: e.memset(eps_t[:], 1e-6), writes=[eps_t])
    ident = k.sb("ident", [128, 128], persistent=True)
    k.op("pool", lambda e: e.memset(ident[:], 0.0), writes=[ident])
    k.op("pool", lambda e: e.affine_select(out=ident[:], in_=ident[:], pattern=[[-1, 128]],
                                           compare_op=ALU.not_equal, fill=1.0, base=0, channel_multiplier=1),
         reads=[ident], writes=[ident])

    k.phase_begin()
    stage_p = k.pool("stage", [128, NT], 2)
    for kc in range(8):
        st = stage_p()
        k.dma(st[:, 0:C], ctxT_in[kc * 128:(kc + 1) * 128, :], reads=[ctxT_in], writes=[st])
        k.dma(st[:, C:NT], xT_in[kc * 128:(kc + 1) * 128, :], reads=[xT_in], writes=[st])
        k.dma(XT[kc * 128:(kc + 1) * 128, :], st[:], reads=[st], writes=[XT])
    k.phase_end()


    k.phase_begin()
    negpi = k.sb("negpi", [128, 1])
    k.op("pool", lambda e: e.memset(negpi[:], -math.pi), writes=[negpi])
    colv_p = k.pool("colv", [128, 4096], 1, I32)
    pm_p = k.pool("pm", [128, 1], 2, I32)
    ri_p = k.pool("ri", [128, 4096], 2, I32)
    rj_p = k.pool("rj", [128, 4096], 2, I32)
    rf_p = k.pool("rf", [128, 4096], 2)
    go_p = k.pool("go", [128, 4096], 3)
    for nm, Ls in (("L", L), ("C", C)):
        N4 = 8 * Ls
        colv = colv_p()
        k.op("pool", lambda e, colv=colv: e.iota(colv[:, 0:Ls], pattern=[[2, Ls]], base=1, channel_multiplier=0), writes=[colv])
        for tt in range(Ls // 128):
            for shifted in (0, 1):
                pm = pm_p()
                k.op("pool", lambda e, pm=pm: e.iota(pm[:], pattern=[[0, 1]], base=2 * tt * 128 + shifted, channel_multiplier=2), writes=[pm])
                ri = ri_p()
                k.op("dve", lambda e, ri=ri, pm=pm: e.tensor_tensor(out=ri[:, 0:Ls], in0=colv[:, 0:Ls], in1=pm[:, 0:1].to_broadcast([128, Ls]), op=ALU.mult),
                     reads=[colv, pm], writes=[ri])
                for trig in ("S", "C"):
                    rj = rj_p()
                    if trig == "S":
                        k.op("dve", lambda e, ri=ri, rj=rj: e.tensor_single_scalar(rj[:, 0:Ls], ri[:, 0:Ls], N4 - 1, op=ALU.bitwise_and), reads=[ri], writes=[rj])
                    else:
                        k.op("dve", lambda e, ri=ri, rj=rj: e.tensor_single_scalar(rj[:, 0:Ls], ri[:, 0:Ls], N4 // 4, op=ALU.add), reads=[ri], writes=[rj])
                        k.op("dve", lambda e, rj=rj: e.tensor_single_scalar(rj[:, 0:Ls], rj[:, 0:Ls], N4 - 1, op=ALU.bitwise_and), reads=[rj], writes=[rj])
                    go = go_p()
                    k.op("act", lambda e, go=go, rj=rj: e.activation(out=go[:, 0:Ls], in_=rj[:, 0:Ls], func=AF.Sin, bias=negpi[:, 0:1], scale=2.0 * math.pi / N4),
                         reads=[rj, negpi], writes=[go])
                    dst = DFT[nm][trig + ("S" if shifted else "K")]
                    k.dma(dst[tt * 128:(tt + 1) * 128, :], go[:, 0:Ls], reads=[go], writes=[dst])
    k.phase_end()

    ORDER = ["0", "A", "B", "C", "D", "E", "F", "G"]
    skip = (lambda ph: start_at is not None and ORDER.index(ph) < ORDER.index(start_at))
    for l in range(n_layers):
        w = W[l]
        need_ctx = l < DEPTH - 1
        k.phase_begin()
        cv = k.sb("cv", [128, 16])
        k.dma(cv[:], cvec_in[:, :], reads=[cvec_in], writes=[cv])
        sl = k.sb("silu", [128, 16])
        k.op("act", lambda e: e.activation(out=sl[:], in_=cv[:], func=AF.Silu), reads=[cv], writes=[sl])
        mb = k.sb("mb", [128, 48])
        k.dma(mb[:], w["mod_b"][:, :], reads=[w["mod_b"]], writes=[mb])
        modt = k.sb("modt", [128, 96])
        mwv = w["mod_w"].t.rearrange("(kc p) n -> p kc n", p=128)
        mw_p = k.pool("mw", [128, 8, 1024], 2)
        for fg in range(6):
            mw = mw_p()
            for kc in range(8):
                k.dma(mw[:, kc, :], mwv[:, kc, fg * 1024:(fg + 1) * 1024], reads=[w["mod_w"]], writes=[mw])
            ps = k.next_ps()
            for fc in range(8):
                for kc in range(8):
                    k.mm(ps, ps[:, fc * 2:fc * 2 + 2], mw, mw[:, kc, fc * 128:(fc + 1) * 128],
                         sl, sl[:, kc * 2:kc * 2 + 2], kc == 0, kc == 7)
            for j in range(2):
                pv = ps.t[:, 0:16].rearrange("p (f j) -> p f j", j=2)[:, :, j]
                ov = modt.t[:, fg * 16:(fg + 1) * 16].rearrange("p (f j) -> p f j", j=2)[:, :, j]
                k.op("dve", lambda e, pv=pv, ov=ov: e.tensor_tensor(out=ov, in0=pv, in1=mb[:, fg * 8:(fg + 1) * 8], op=ALU.add),
                     reads=[ps, mb], writes=[modt])
        k.dma(MODT[:, :], modt[:], reads=[modt], writes=[MODT])
        k.phase_end()
        if stop_after == "A":
            break

        k.phase_begin()
        modt = k.sb("modt", [128, 96])
        k.dma(modt[:], MODT[:, :], reads=[MODT], writes=[modt])
        g1n = k.sb("g1n", [128, 8])
        k.dma(g1n[:], w["n1g"][:, :], reads=[w["n1g"]], writes=[g1n])
        gb = k.sb("gb", [128, 24])
        k.dma(gb[:], w["gate_b"][:, :], reads=[w["gate_b"]], writes=[gb])
        m3 = modt.t[:, :].rearrange("p (f j) -> p f j", j=2)
        Acoef = k.sb("Acoef", [128, 8, 2])
        for j in range(2):
            k.op("dve", lambda e, j=j: e.scalar_tensor_tensor(out=Acoef[:, :, j], in0=m3[:, 8:16, j], scalar=1.0, in1=g1n[:],
                                                             op0=ALU.add, op1=ALU.mult), reads=[modt, g1n], writes=[Acoef])
        iwv = w["in_w"].t.rearrange("(kc p) n -> p kc n", p=128)
        irv = w["in_wr"].t.rearrange("(kc p) n -> p kc n", p=128)
        groups = [("q", 0, 512), ("k", 512, 128), ("v", 640, 128), ("s5", 768, 512)]
        groups += [("hy", 1280 + 512 * i, 512) for i in range(3)] + [("gate", 2816 + 512 * i, 512) for i in range(6)]
        xb_p = k.pool("xb", [128, 8, 512], 1)
        sq_p = k.pool("sq", [128, 8, 512], 1)
        hb_p = k.pool("hb", [128, 8, 512], 1)
        wt_p = k.pool("wt", [128, 8, 512], 4)
        wr_p = k.pool("wr", [128, 8, 512], 2)
        rstd_p = k.pool("rstd", [128, 512], 1)
        rc_p = k.pool("rc", [128, 512], 2)
        rs_p = k.pool("rs", [128, 512], 2)
        ev_p = k.pool("ev", [128, 512], 4)
        e2_p = k.pool("e2", [128, 512], 2)
        vt_p = k.pool("vt", [128, 128], 2)
        for bi, (t0, nt) in enumerate(NBLK):
            j = 1 if bi == 0 else 0
            xb = xb_p()
            for kc in range(8):
                k.dma(xb[:, kc, 0:nt], XT[kc * 128:(kc + 1) * 128, t0:t0 + nt], reads=[XT], writes=[xb])
            sq = sq_p()
            k.op("act", lambda e: e.activation(out=sq[:, :, 0:nt], in_=xb[:, :, 0:nt], func=AF.Square), reads=[xb], writes=[sq])
            pss = k.next_ps()
            for kc in range(8):
                k.mm(pss, pss[:, 0:nt], ones, ones[:], sq, sq[:, kc, 0:nt], kc == 0, kc == 7)
            rstd = rstd_p()
            k.op("act", lambda e: e.activation(out=rstd[:, 0:nt], in_=pss[:, 0:nt], func=AF.Sqrt, bias=eps_t[:, 0:1], scale=1.0 / D),
                 reads=[pss, eps_t], writes=[rstd])
            k.op("dve", lambda e: e.reciprocal(rstd[:, 0:nt], rstd[:, 0:nt]), reads=[rstd], writes=[rstd])
            hb = hb_p()
            for kc in range(8):
                k.op("dve", lambda e, kc=kc: e.tensor_tensor(out=hb[:, kc, 0:nt], in0=xb[:, kc, 0:nt], in1=rstd[:, 0:nt], op=ALU.mult),
                     reads=[xb, rstd], writes=[hb])
                k.op("pool", lambda e, kc=kc: e.tensor_scalar(hb[:, kc, 0:nt], hb[:, kc, 0:nt], Acoef[:, kc, j:j + 1], m3[:, kc, j:j + 1],
                                                              op0=ALU.mult, op1=ALU.add), reads=[hb, Acoef, modt], writes=[hb])
            if dbg:
                for kc in range(8):
                    k.dma(HT[kc * 128:(kc + 1) * 128, t0:t0 + nt], hb[:, kc, 0:nt], reads=[hb], writes=[HT])
            rc = rc_p()
            rs = rs_p()
            k.dma(rc[:, 0:nt], ropec_in[:, t0:t0 + nt], reads=[ropec_in], writes=[rc])
            k.dma(rs[:, 0:nt], ropes_in[:, t0:t0 + nt], reads=[ropes_in], writes=[rs])
            for (kind, c0, ncol) in groups:
                wt = wt_p()
                for kc in range(8):
                    k.dma(wt[:, kc, 0:ncol], iwv[:, kc, c0:c0 + ncol], reads=[w["in_w"]], writes=[wt])
                if kind in ("q", "k"):
                    wr = wr_p()
                    for kc in range(8):
                        k.dma(wr[:, kc, 0:ncol], irv[:, kc, c0:c0 + ncol], reads=[w["in_wr"]], writes=[wr])
                if kind == "v":
                    for ts in range(nt // 128):
                        ps = k.next_ps()
                        for kc in range(8):
                            k.mm(ps, ps[:, 0:128], hb, hb[:, kc, ts * 128:(ts + 1) * 128], wt, wt[:, kc, 0:128], kc == 0, kc == 7)
                        vt = vt_p()
                        k.op("act", lambda e, ps=ps, vt=vt: e.copy(vt[:], ps[:, 0:128]), reads=[ps], writes=[vt])
                        k.dma(VTOK[t0 + ts * 128:t0 + (ts + 1) * 128, :], vt[:], reads=[vt], writes=[VTOK], q="act")
                    continue
                for cc in range(ncol // 128):
                    ps = k.next_ps()
                    for kc in range(8):
                        k.mm(ps, ps[:, 0:nt], wt, wt[:, kc, cc * 128:(cc + 1) * 128], hb, hb[:, kc, 0:nt], kc == 0, kc == 7)
                    ev = ev_p()
                    col = c0 + cc * 128
                    if kind in ("q", "k"):
                        ps2 = k.next_ps()
                        for kc in range(8):
                            k.mm(ps2, ps2[:, 0:nt], wr, wr[:, kc, cc * 128:(cc + 1) * 128], hb, hb[:, kc, 0:nt], kc == 0, kc == 7)
                        e2 = e2_p()
                        k.op("dve", lambda e, ps=ps, ev=ev: e.tensor_tensor(out=ev[:, 0:nt], in0=ps[:, 0:nt], in1=rc[:, 0:nt], op=ALU.mult),
                             reads=[ps, rc], writes=[ev])
                        k.op("dve", lambda e, ps2=ps2, e2=e2: e.tensor_tensor(out=e2[:, 0:nt], in0=ps2[:, 0:nt], in1=rs[:, 0:nt], op=ALU.mult),
                             reads=[ps2, rs], writes=[e2])
                        k.op("pool", lambda e, ev=ev, e2=e2: e.tensor_tensor(out=ev[:, 0:nt], in0=ev[:, 0:nt], in1=e2[:, 0:nt], op=ALU.add),
                             reads=[ev, e2], writes=[ev])
                        if kind == "q":
                            k.op("act", lambda e, ev=ev: e.mul(ev[:, 0:nt], ev[:, 0:nt], 0.125), reads=[ev], writes=[ev])
                            dst = QT[col:col + 128, t0:t0 + nt]
                            dres = QT
                        else:
                            dst = KT[0:128, t0:t0 + nt]
                            dres = KT
                    elif kind == "gate":
                        gi = (col - 2816) // 128
                        k.op("act", lambda e, ps=ps, ev=ev, gi=gi: e.activation(out=ev[:, 0:nt], in_=ps[:, 0:nt], func=AF.Sigmoid, bias=gb[:, gi:gi + 1]),
                             reads=[ps, gb], writes=[ev])
                        dst = GATE[col - 2816:col - 2816 + 128, t0:t0 + nt]
                        dres = GATE
                    else:
                        if cc % 2 == 0:
                            k.op("act", lambda e, ps=ps, ev=ev: e.copy(ev[:, 0:nt], ps[:, 0:nt]), reads=[ps], writes=[ev])
                        else:
                            k.op("dve", lambda e, ps=ps, ev=ev: e.tensor_copy(ev[:, 0:nt], ps[:, 0:nt]), reads=[ps], writes=[ev])
                        if kind == "s5":
                            dst = US5[col - 768:col - 768 + 128, t0:t0 + nt]
                            dres = US5
                        else:
                            dst = ZHY[col - 1280:col - 1280 + 128, t0:t0 + nt]
                            dres = ZHY
                    k.dma(dst, ev[:, 0:nt], reads=[ev], writes=[dres], q="act")
        k.phase_end()
        if stop_after == "B":
            break

        need_ctx = l < DEPTH - 1
        k.phase_begin()
        amask = k.sb("amask", [128, 256])
        k.dma(amask[:], amask_in[:, :], reads=[amask_in], writes=[amask])
        sinkb = k.sb("sinkb", [128, 8])
        k.dma(sinkb[:], w["sink"][:, :], reads=[w["sink"]], writes=[sinkb])
        kT_p = k.pool("kT", [64, NT], 1)
        v_p = k.pool("vtk", [128, NT // 128, 64], 1)
        qT_p = k.pool("qT", [64, NT], 2)
        ao_p = k.pool("ao", [64, NT], 2)
        S_p = k.pool("S", [128, 640], 4)
        PT_p = k.pool("PT", [128, 5, 128], 4)
        st_p = k.pool("stat", [128, 8], 6)
        vview = VTOK.t.rearrange("(n p) c -> p n c", p=128)
        for kv in range(2):
            kT = kT_p()
            k.dma(kT[:], KT[kv * 64:(kv + 1) * 64, :], reads=[KT], writes=[kT])
            vt = v_p()
            k.dma(vt[:], vview[:, :, kv * 64:(kv + 1) * 64], reads=[VTOK], writes=[vt])
            for hg in range(4):
                hd = kv * 4 + hg
                qT = qT_p()
                k.dma(qT[:], QT[hd * 64:(hd + 1) * 64, :], reads=[QT], writes=[qT])
                ao = ao_p()
                blocks = ([("c", 0), ("c", 1)] if need_ctx else []) + [("l", n) for n in range(32)]
                def unit_gen(bt, n):
                    q0 = n * 128 if bt == "c" else C + n * 128
                    if bt == "c":
                        kts = []
                    else:
                        kts = [j for j in (n - 1, n, n + 1) if 0 <= j < 32]
                    nloc = len(kts) * 128
                    wtot = 256 + nloc
                    S = S_p()
                    psc = k.next_ps()
                    k.mm(psc, psc[:, 0:256], qT, qT[:, q0:q0 + 128], kT, kT[:, 0:256], True, True)
                    yield
                    k.op("act", lambda e, S=S, psc=psc: e.copy(S[:, 0:256], psc[:, 0:256]), reads=[psc], writes=[S])
                    yield
                    if nloc:
                        psl = k.next_ps()
                        k0 = C + kts[0] * 128
                        k.mm(psl, psl[:, 0:nloc], qT, qT[:, q0:q0 + 128], kT, kT[:, k0:k0 + nloc], True, True)
                        yield
                        for ji, j in enumerate(kts):
                            dst = S[:, 256 + ji * 128:256 + (ji + 1) * 128]
                            src = psl[:, ji * 128:(ji + 1) * 128]
                            if j == n:
                                k.op("dve", lambda e, dst=dst, src=src: e.tensor_copy(dst, src), reads=[psl], writes=[S])
                                yield
                            else:
                                mko = 0 if j < n else 128
                                k.op("dve", lambda e, dst=dst, src=src, mko=mko: e.tensor_tensor(out=dst, in0=src, in1=amask[:, mko:mko + 128], op=ALU.add),
                                     reads=[psl, amask], writes=[S])
                                yield
                    stt = st_p()
                    k.op("dve", lambda e, S=S, stt=stt: e.tensor_reduce(out=stt[:, 0:1], in_=S[:, 0:wtot], axis=AX.X, op=ALU.max), reads=[S], writes=[stt])
                    yield
                    k.op("dve", lambda e, stt=stt: e.tensor_tensor(out=stt[:, 0:1], in0=stt[:, 0:1], in1=sinkb[:, hd:hd + 1], op=ALU.max), reads=[stt, sinkb], writes=[stt])
                    yield
                    k.op("dve", lambda e, stt=stt: e.tensor_scalar(stt[:, 1:2], stt[:, 0:1], -1.0, None, op0=ALU.mult), reads=[stt], writes=[stt])
                    yield
                    k.op("act", lambda e, S=S, stt=stt: e.activation(out=S[:, 0:wtot], in_=S[:, 0:wtot], func=AF.Exp, bias=stt[:, 1:2], accum_out=stt[:, 2:3]),
                         reads=[S, stt], writes=[S, stt])
                    yield
                    k.op("act", lambda e, stt=stt: e.activation(out=stt[:, 3:4], in_=sinkb[:, hd:hd + 1], func=AF.Exp, bias=stt[:, 1:2]), reads=[stt, sinkb], writes=[stt])
                    yield
                    k.op("dve", lambda e, stt=stt: e.tensor_tensor(out=stt[:, 4:5], in0=stt[:, 2:3], in1=stt[:, 3:4], op=ALU.add), reads=[stt], writes=[stt])
                    yield
                    k.op("dve", lambda e, stt=stt: e.reciprocal(stt[:, 5:6], stt[:, 4:5]), reads=[stt], writes=[stt])
                    yield
                    k.op("dve", lambda e, S=S, stt=stt: e.tensor_scalar(S[:, 0:wtot], S[:, 0:wtot], stt[:, 5:6], None, op0=ALU.mult), reads=[S, stt], writes=[S])
                    yield
                    nkt = wtot // 128
                    PT = PT_p()
                    for ti in range(nkt):
                        pst = k.next_ps()
                        k.op("pe", lambda e, pst=pst, S=S, ti=ti: e.transpose(pst[:, 0:128], S[:, ti * 128:(ti + 1) * 128], ident[:]),
                             reads=[S, ident], writes=[pst])
                        yield
                        if ti % 2 == 0:
                            k.op("act", lambda e, PT=PT, pst=pst, ti=ti: e.copy(PT[:, ti, :], pst[:, 0:128]), reads=[pst], writes=[PT])
                            yield
                        else:
                            k.op("dve", lambda e, PT=PT, pst=pst, ti=ti: e.tensor_copy(PT[:, ti, :], pst[:, 0:128]), reads=[pst], writes=[PT])
                            yield
                    pso = k.next_ps()
                    vtiles = [0, 1] + [2 + j for j in kts]
                    for ti, vti in enumerate(vtiles):
                        k.mm(pso, pso[0:64, 0:128], vt, vt[:, vti, :], PT, PT[:, ti, :], ti == 0, ti == nkt - 1)
                        yield
                    k.op("act", lambda e, ao=ao, pso=pso, q0=q0: e.copy(ao[:, q0:q0 + 128], pso[0:64, 0:128]), reads=[pso], writes=[ao])
                    yield
                for bi_ in range(0, len(blocks), 2):
                    gens = [unit_gen(*blk) for blk in blocks[bi_:bi_ + 2]]
                    while gens:
                        for g_ in list(gens):
                            try:
                                next(g_)
                            except StopIteration:
                                gens.remove(g_)
                lo = 0 if need_ctx else C
                k.dma(ATT[hd * 64:(hd + 1) * 64, lo:NT], ao[:, lo:NT], reads=[ao], writes=[ATT], q="act")
        k.phase_end()
        if stop_after == "C":
            break

        k.phase_begin()
        fb = k.sb("hy_fb", [64, 4])
        k.dma(fb[:], w["hy_fb"][:, :], reads=[w["hy_fb"]], writes=[fb])
        fbb = k.sb("hy_fbb", [64, 2])
        k.op("dve", lambda e: e.tensor_tensor(out=fbb[:, 0:1], in0=fb[:, 0:1], in1=fb[:, 1:2], op=ALU.mult), reads=[fb], writes=[fbb])
        k.op("dve", lambda e: e.tensor_tensor(out=fbb[:, 1:2], in0=fb[:, 2:3], in1=fb[:, 3:4], op=ALU.mult), reads=[fb], writes=[fbb])
        w1 = k.sb("hy_w1", [33, 64])
        k.dma(w1[:], w["hy_w1"][:, :], reads=[w["hy_w1"]], writes=[w1])
        w2 = k.sb("hy_w2", [64, 64])
        k.dma(w2[:], w["hy_w2"][:, :], reads=[w["hy_w2"]], writes=[w2])
        w3 = k.sb("hy_w3", [64, 1024])
        k.dma(w3[:], w["hy_w3"][:, :], reads=[w["hy_w3"]], writes=[w3])
        rate = k.sb("hy_rate", [128, 512])
        k.dma(rate[:], hyrate_in[:, :], reads=[hyrate_in], writes=[rate])
        sw = k.sb("hy_sw", [128, 12, 3])
        k.dma(sw[:], w["hy_sw"][:, :, :], reads=[w["hy_sw"]], writes=[sw])
        sbias = k.sb("hy_sb", [128, 12])
        k.dma(sbias[:], w["hy_sb"][:, :], reads=[w["hy_sb"]], writes=[sbias])
        hbias = k.sb("hy_bias", [128, 4])
        k.dma(hbias[:], w["hy_bias"][:, :], reads=[w["hy_bias"]], writes=[hbias])
        m0 = k.sb("m0", [128, 1])
        k.op("pool", lambda e: e.memset(m0[:], 1.0), writes=[m0])
        k.op("pool", lambda e: e.affine_select(out=m0[:], in_=m0[:], pattern=[[0, 1]], compare_op=ALU.not_equal, fill=0.0, base=0, channel_multiplier=1),
             reads=[m0], writes=[m0])
        negpi = k.sb("negpi2", [128, 1])
        k.op("pool", lambda e: e.memset(negpi[:], -math.pi), writes=[negpi])

        def sin_mlp(dst_t, dst, ps, fcol, bcol, n, tmp_p, tmpi_p):
            a = tmp_p()
            k.op("act", lambda e: e.activation(out=a[0:64, 0:n], in_=ps[0:64, 0:n], func=AF.Identity, bias=fbb[:, bcol:bcol + 1], scale=fb[:, fcol:fcol + 1]),
                 reads=[ps, fb, fbb], writes=[a])
            ki = tmpi_p()
            k.op("dve", lambda e: e.tensor_scalar(ki[0:64, 0:n], a[0:64, 0:n], 1.0 / (2.0 * math.pi), None, op0=ALU.mult), reads=[a], writes=[ki])
            kf = tmp_p()
            k.op("dve", lambda e: e.tensor_copy(kf[0:64, 0:n], ki[0:64, 0:n]), reads=[ki], writes=[kf])
            k.op("dve", lambda e: e.scalar_tensor_tensor(out=a[0:64, 0:n], in0=kf[0:64, 0:n], scalar=-2.0 * math.pi, in1=a[0:64, 0:n], op0=ALU.mult, op1=ALU.add),
                 reads=[kf, a], writes=[a])
            k.op("dve", lambda e: e.tensor_scalar(a[0:64, 0:n], a[0:64, 0:n], 3.1415925, -3.1415925, op0=ALU.min, op1=ALU.max), reads=[a], writes=[a])
            k.op("act", lambda e: e.activation(out=dst, in_=a[0:64, 0:n], func=AF.Sin), reads=[a], writes=[dst_t])

        seqs = [("L", L, C)] + ([("C", C, 0)] if need_ctx else [])
        tmp_p = k.pool("hy_tmp", [128, 512], 3)
        tmpi_p = k.pool("hy_tmpi", [128, 512], 2, I32)
        h2T_p = k.pool("hy_h2T", [64, 512], 2)
        h1T_p = k.pool("hy_h1T", [64, 512], 2)
        zf_p = k.pool("hy_zf", [33, 512], 2)
        tw_p = k.pool("hy_tw", [128, 32], 1)
        win_p = k.pool("hy_win", [128, 512], 2)
        hfb_p = k.pool("hy_hfb", [128, 2, 512], 2)
        hsd_p = k.pool("hy_hsd", [128, 2, 512], 2)
        zc_p = k.pool("hy_zc", [128, 3, 512 + 2], 2)
        zo_p = k.pool("hy_zo", [128, 3, 512], 2)
        vxt_p = k.pool("hy_vxt", [128, 512], 2)
        for (nm, Ls, toff) in seqs:
            HSD, VXT, VXF, X0F = HSDd[nm], VXTd[nm], VXFd[nm], X0Fd[nm]
            ntile = Ls // 128
            nb = max(1, Ls // 512)
            bw = min(512, Ls)
            tw = tw_p()
            k.dma(tw[:, 0:ntile], hytw_in[nm][:, :], reads=[hytw_in[nm]], writes=[tw])
            for bi in range(nb):
                zf = zf_p()
                k.dma(zf[:, 0:bw], hyz_in[nm][:, bi * bw:(bi + 1) * bw], reads=[hyz_in[nm]], writes=[zf])
                ps1 = k.next_ps()
                k.mm(ps1, ps1[0:64, 0:bw], w1, w1[:, :], zf, zf[:, 0:bw], True, True)
                h1T = h1T_p()
                sin_mlp(h1T, h1T[:, 0:bw], ps1, 1, 0, bw, tmp_p, tmpi_p)
                ps2 = k.next_ps()
                k.mm(ps2, ps2[0:64, 0:bw], w2, w2[:, :], h1T, h1T[:, 0:bw], True, True)
                h2T = h2T_p()
                sin_mlp(h2T, h2T[:, 0:bw], ps2, 3, 1, bw, tmp_p, tmpi_p)
                for ts in range(bw // 128):
                    tt = bi * (bw // 128) + ts
                    win = win_p()
                    k.op("act", lambda e, win=win, tt=tt: e.activation(out=win[:], in_=rate[:], func=AF.Exp, scale=tw[:, tt:tt + 1]), reads=[rate, tw], writes=[win])
                    hfb = hfb_p()
                    for d in range(2):
                        psf = k.next_ps()
                        k.mm(psf, psf[:, :], h2T, h2T[:, ts * 128:(ts + 1) * 128], w3, w3[:, d * 512:(d + 1) * 512], True, True)
                        k.op("dve", lambda e, hfb=hfb, psf=psf, win=win, d=d: e.tensor_tensor(out=hfb[:, d, :], in0=psf[:, :], in1=win[:], op=ALU.mult),
                             reads=[psf, win], writes=[hfb])
                    if tt == 0:
                        k.op("dve", lambda e, hfb=hfb: e.tensor_scalar(hfb[:, 1, :], hfb[:, 1, :], m0[:, 0:1], None, op0=ALU.mult), reads=[hfb, m0], writes=[hfb])
                    hsd = hsd_p()
                    k.op("dve", lambda e, hsd=hsd, hfb=hfb: e.tensor_tensor(out=hsd[:, 0, :], in0=hfb[:, 0, :], in1=hfb[:, 1, :], op=ALU.add), reads=[hfb], writes=[hsd])
                    k.op("pool", lambda e, hsd=hsd, hfb=hfb: e.tensor_tensor(out=hsd[:, 1, :], in0=hfb[:, 0, :], in1=hfb[:, 1, :], op=ALU.subtract), reads=[hfb], writes=[hsd])
                    for d in range(2):
                        k.dma(HSD[d, tt * 128:(tt + 1) * 128, :], hsd[:, d, :], reads=[hsd], writes=[HSD], q="act")
            for cc in range(4):
                for bi in range(nb):
                    zc = zc_p()
                    k.op("pool", lambda e, zc=zc: e.memset(zc[:], 0.0), writes=[zc])
                    lo = bi * bw
                    a0 = max(lo - 1, 0)
                    a1 = min(lo + bw + 1, Ls)
                    for pj in range(3):
                        r0 = pj * 512 + cc * 128
                        k.dma(zc[:, pj, (a0 - (lo - 1)):(a1 - (lo - 1))], ZHY[r0:r0 + 128, toff + a0:toff + a1], reads=[ZHY], writes=[zc])
                    zo = zo_p()
                    for pj in range(3):
                        ci = pj * 4 + cc
                        k.op("dve", lambda e, zo=zo, zc=zc, pj=pj, ci=ci: e.tensor_scalar(zo[:, pj, 0:bw], zc[:, pj, 0:bw], sw[:, ci, 0:1], sbias[:, ci:ci + 1], op0=ALU.mult, op1=ALU.add),
                             reads=[zc, sw, sbias], writes=[zo])
                        for tap in (1, 2):
                            k.op("dve", lambda e, zo=zo, zc=zc, pj=pj, ci=ci, tap=tap: e.scalar_tensor_tensor(out=zo[:, pj, 0:bw], in0=zc[:, pj, tap:tap + bw], scalar=sw[:, ci, tap:tap + 1],
                                                                                                                in1=zo[:, pj, 0:bw], op0=ALU.mult, op1=ALU.add), reads=[zc, sw, zo], writes=[zo])
                    k.op("pool", lambda e, zo=zo: e.tensor_tensor(out=zo[:, 2, 0:bw], in0=zo[:, 2, 0:bw], in1=zo[:, 1, 0:bw], op=ALU.mult), reads=[zo], writes=[zo])
                    k.dma(X0F[cc * 128:(cc + 1) * 128, lo:lo + bw], zo[:, 0, 0:bw], reads=[zo], writes=[X0F], q="act")
                    k.dma(VXF[cc * 128:(cc + 1) * 128, lo:lo + bw], zo[:, 2, 0:bw], reads=[zo], writes=[VXF], q="act")
                    for ts in range(bw // 128):
                        pst = k.next_ps()
                        k.op("pe", lambda e, pst=pst, zo=zo, ts=ts: e.transpose(pst[:, 0:128], zo[:, 2, ts * 128:(ts + 1) * 128], ident[:]), reads=[zo, ident], writes=[pst])
                        vxt = vxt_p()
                        k.op("act", lambda e, vxt=vxt, pst=pst: e.copy(vxt[:, 0:128], pst[:, 0:128]), reads=[pst], writes=[vxt])
                        t0_ = lo + ts * 128
                        k.dma(VXT[t0_:t0_ + 128, cc * 128:(cc + 1) * 128], vxt[:, 0:128], reads=[vxt], writes=[VXT], q="act")
        k.phase_end()
        for (nm, Ls, toff) in seqs:
            k.phase_begin()
            HSD, KRI, VXT, VXF, X0F = HSDd[nm], KRId[nm], VXTd[nm], VXFd[nm], X0Fd[nm]
            ntile = Ls // 128
            nb = max(1, Ls // 512)
            bw = min(512, Ls)
            hbias = k.sb("hy_bias", [128, 4])
            k.dma(hbias[:], w["hy_bias"][:, :], reads=[w["hy_bias"]], writes=[hbias])
            Mx = DFT[nm]
            src_p = k.pool("hy_src_" + nm, [128, ntile, 256], 1)
            Y_p = k.pool("hy_Y_" + nm, [128, ntile, 2, 256], 1)
            slab_p = k.pool("hy_slab_" + nm, [128, min(8, ntile), 2, 128], 2)
            kri_p = k.pool("hy_kri_" + nm, [128, 2, 256], 2)
            pw_p = k.pool("hy_pw_" + nm, [128, 4, 256], 2)
            mt_p = k.pool("hy_mt_" + nm, [128, 2, 512], 3)
            ep_p = k.pool("hy_ep_" + nm, [128, 3, 512], 2)
            tg = min(8, ntile)
            for stage in ("kern", "conv"):
                for hh in range(2):
                    cs = slice(hh * 256, (hh + 1) * 256)
                    if stage == "kern":
                        mats = (Mx["CK"], Mx["SK"])
                        for d in range(2):
                            src = src_p()
                            k.dma(src[:, :, :], HSD[d, 0:Ls, cs].rearrange("(n p) c -> p n c", p=128), reads=[HSD], writes=[src])
                            for ft in range(ntile):
                                psK = k.next_ps()
                                for g in range(ntile // tg):
                                    slab = slab_p()
                                    k.dma(slab[:, :, 0, :], mats[d][g * tg * 128:(g + 1) * tg * 128, ft * 128:(ft + 1) * 128].rearrange("(n p) f -> p n f", p=128),
                                          reads=[mats[d]], writes=[slab])
                                    for ti in range(tg):
                                        tt = g * tg + ti
                                        k.mm(psK, psK[:, 0:256], slab, slab[:, ti, 0, :], src, src[:, tt, :], tt == 0, tt == ntile - 1)
                                kri = kri_p()
                                k.op("act", lambda e, kri=kri, psK=psK: e.copy(kri[:, 0, :], psK[:, 0:256]), reads=[psK], writes=[kri])
                                k.dma(KRI[d, ft * 128:(ft + 1) * 128, cs], kri[:, 0, :], reads=[kri], writes=[KRI], q="act")
                        continue
                    mats = (Mx["CS"], Mx["SS"])
                    src = src_p()
                    k.dma(src[:, :, :], VXT[0:Ls, cs].rearrange("(n p) c -> p n c", p=128), reads=[VXT], writes=[src])
                    Y = Y_p()
                    for ft in range(ntile):
                        psR = k.next_ps()
                        psI = k.next_ps()
                        for g in range(ntile // tg):
                            slab = slab_p()
                            for d in range(2):
                                k.dma(slab[:, :, d, :], mats[d][g * tg * 128:(g + 1) * tg * 128, ft * 128:(ft + 1) * 128].rearrange("(n p) f -> p n f", p=128),
                                      reads=[mats[d]], writes=[slab])
                            for ti in range(tg):
                                tt = g * tg + ti
                                k.mm(psR, psR[:, 0:256], slab, slab[:, ti, 0, :], src, src[:, tt, :], tt == 0, tt == ntile - 1)
                                k.mm(psI, psI[:, 0:256], slab, slab[:, ti, 1, :], src, src[:, tt, :], tt == 0, tt == ntile - 1)
                        kri = kri_p()
                        for d in range(2):
                            k.dma(kri[:, d, :], KRI[d, ft * 128:(ft + 1) * 128, cs], reads=[KRI], writes=[kri])
                        pw = pw_p()
                        k.op("dve", lambda e, pw=pw, psR=psR, kri=kri: e.tensor_tensor(out=pw[:, 0, :], in0=psR[:, 0:256], in1=kri[:, 0, :], op=ALU.mult), reads=[psR, kri], writes=[pw])
                        k.op("dve", lambda e, pw=pw, psI=psI, kri=kri: e.tensor_tensor(out=pw[:, 1, :], in0=psI[:, 0:256], in1=kri[:, 1, :], op=ALU.mult), reads=[psI, kri], writes=[pw])
                        k.op("dve", lambda e, pw=pw, psR=psR, kri=kri: e.tensor_tensor(out=pw[:, 2, :], in0=psR[:, 0:256], in1=kri[:, 1, :], op=ALU.mult), reads=[psR, kri], writes=[pw])
                        k.op("dve", lambda e, pw=pw, psI=psI, kri=kri: e.tensor_tensor(out=pw[:, 3, :], in0=psI[:, 0:256], in1=kri[:, 0, :], op=ALU.mult), reads=[psI, kri], writes=[pw])
                        k.op("pool", lambda e, Y=Y, pw=pw, ft=ft: e.tensor_tensor(out=Y[:, ft, 0, :], in0=pw[:, 0, :], in1=pw[:, 1, :], op=ALU.subtract), reads=[pw], writes=[Y])
                        k.op("pool", lambda e, Y=Y, pw=pw, ft=ft: e.tensor_tensor(out=Y[:, ft, 1, :], in0=pw[:, 2, :], in1=pw[:, 3, :], op=ALU.add), reads=[pw], writes=[Y])
                    if stage == "conv":
                        Nfft = 2 * Ls
                        for nbk in range(nb):
                            pso = [k.next_ps(), k.next_ps()]
                            for ft in range(ntile):
                                mt = mt_p()
                                for d in range(2):
                                    k.dma(mt[:, d, 0:bw], mats[d][ft * 128:(ft + 1) * 128, nbk * bw:(nbk + 1) * bw], reads=[mats[d]], writes=[mt])
                                for c2 in range(2):
                                    for d in range(2):
                                        k.mm(pso[c2], pso[c2][:, 0:bw], Y, Y[:, ft, d, c2 * 128:(c2 + 1) * 128], mt, mt[:, d, 0:bw], ft == 0 and d == 0, ft == ntile - 1 and d == 1)
                            for c2 in range(2):
                                ch = hh * 2 + c2
                                ep = ep_p()
                                k.dma(ep[:, 0, 0:bw], VXF[ch * 128:(ch + 1) * 128, nbk * bw:(nbk + 1) * bw], reads=[VXF], writes=[ep])
                                k.dma(ep[:, 1, 0:bw], X0F[ch * 128:(ch + 1) * 128, nbk * bw:(nbk + 1) * bw], reads=[X0F], writes=[ep])
                                k.op("dve", lambda e, ep=ep, ch=ch: e.tensor_scalar(ep[:, 0, 0:bw], ep[:, 0, 0:bw], hbias[:, ch:ch + 1], None, op0=ALU.mult), reads=[ep, hbias], writes=[ep])
                                k.op("dve", lambda e, ep=ep, c2=c2: e.scalar_tensor_tensor(out=ep[:, 2, 0:bw], in0=pso[c2][:, 0:bw], scalar=-2.0 / Nfft, in1=ep[:, 0, 0:bw], op0=ALU.mult, op1=ALU.add),
                                     reads=[pso[c2], ep], writes=[ep])
                                k.op("pool", lambda e, ep=ep: e.tensor_tensor(out=ep[:, 2, 0:bw], in0=ep[:, 2, 0:bw], in1=ep[:, 1, 0:bw], op=ALU.mult), reads=[ep], writes=[ep])
                                k.dma(HY[ch * 128:(ch + 1) * 128, toff + nbk * bw:toff + (nbk + 1) * bw], ep[:, 2, 0:bw], reads=[ep], writes=[HY], q="act")
                k.barrier()
            k.phase_end()
        if stop_after == "D":
            break

        k.phase_begin()
        TWO_PI = 2.0 * math.pi
        P32 = [128, 2, 16]
        a_re = k.sb("a_re", P32); a_im = k.sb("a_im", P32); ldt = k.sb("ldt", P32)
        for t_, src_ in ((a_re, w["s5_a_re"]), (a_im, w["s5_a_im"]), (ldt, w["s5_ldt"])):
            k.dma(t_[:], src_[:, :, :], reads=[src_], writes=[t_])
        prm = k.sb("s5prm", [128, 12, 32])
        fl = lambda t_: t_.t[:, :, :].rearrange("p a b -> p (a b)")
        R_ = lambda i: prm[:, i, :]
        ki32 = k.sb("ki32", [128, 32], I32)
        kf32 = k.sb("kf32", [128, 32])
        hpi = k.sb("hpi", [128, 1])
        k.op("pool", lambda e: e.memset(hpi[:], 0.0), writes=[hpi])

        def reduce_angle(dst, src, shift):
            k.op("dve", lambda e: e.tensor_scalar(R_(8), src, shift, None, op0=ALU.add), reads=[prm], writes=[prm])
            k.op("dve", lambda e: e.tensor_scalar(ki32[:], R_(8), 1.0 / TWO_PI, None, op0=ALU.mult), reads=[prm], writes=[ki32])
            k.op("dve", lambda e: e.tensor_copy(kf32[:], ki32[:]), reads=[ki32], writes=[kf32])
            k.op("dve", lambda e: e.scalar_tensor_tensor(out=dst, in0=kf32[:], scalar=-TWO_PI, in1=R_(8), op0=ALU.mult, op1=ALU.add), reads=[kf32, prm], writes=[prm])
            k.op("dve", lambda e: e.tensor_scalar(dst, dst, 3.1415925, -3.1415925, op0=ALU.min, op1=ALU.max), reads=[prm], writes=[prm])

        k.op("dve", lambda e: e.tensor_scalar(R_(0), fl(a_re), -1e-4, None, op0=ALU.min), reads=[a_re], writes=[prm])
        k.op("act", lambda e: e.activation(out=R_(1), in_=fl(ldt), func=AF.Exp), reads=[ldt], writes=[prm])
        k.op("dve", lambda e: e.tensor_tensor(out=R_(9), in0=R_(0), in1=R_(1), op=ALU.mult), reads=[prm], writes=[prm])
        k.op("act", lambda e: e.activation(out=R_(3), in_=R_(9), func=AF.Exp), reads=[prm], writes=[prm])
        k.op("dve", lambda e: e.tensor_tensor(out=R_(10), in0=fl(a_im), in1=R_(1), op=ALU.mult), reads=[prm, a_im], writes=[prm])
        reduce_angle(R_(2), R_(10), 0.0)
        k.op("act", lambda e: e.activation(out=R_(5), in_=R_(2), func=AF.Sin), reads=[prm], writes=[prm])
        reduce_angle(R_(11), R_(10), math.pi / 2.0)
        k.op("act", lambda e: e.activation(out=R_(4), in_=R_(11), func=AF.Sin), reads=[prm], writes=[prm])
        k.op("dve", lambda e: e.tensor_tensor(out=R_(8), in0=R_(3), in1=R_(4), op=ALU.mult), reads=[prm], writes=[prm])
        k.op("dve", lambda e: e.tensor_scalar(R_(8), R_(8), -1.0, None, op0=ALU.add), reads=[prm], writes=[prm])
        k.op("dve", lambda e: e.tensor_tensor(out=R_(9), in0=R_(3), in1=R_(5), op=ALU.mult), reads=[prm], writes=[prm])
        k.op("dve", lambda e: e.tensor_tensor(out=R_(10), in0=R_(0), in1=R_(0), op=ALU.mult), reads=[prm], writes=[prm])
        k.op("dve", lambda e: e.tensor_tensor(out=R_(11), in0=fl(a_im), in1=fl(a_im), op=ALU.mult), reads=[a_im], writes=[prm])
        k.op("dve", lambda e: e.tensor_tensor(out=R_(10), in0=R_(10), in1=R_(11), op=ALU.add), reads=[prm], writes=[prm])
        k.op("dve", lambda e: e.reciprocal(R_(10), R_(10)), reads=[prm], writes=[prm])
        k.op("dve", lambda e: e.tensor_tensor(out=R_(6), in0=R_(8), in1=R_(0), op=ALU.mult), reads=[prm], writes=[prm])
        k.op("dve", lambda e: e.tensor_tensor(out=R_(11), in0=R_(9), in1=fl(a_im), op=ALU.mult), reads=[prm, a_im], writes=[prm])
        k.op("dve", lambda e: e.tensor_tensor(out=R_(6), in0=R_(6), in1=R_(11), op=ALU.add), reads=[prm], writes=[prm])
        k.op("dve", lambda e: e.tensor_tensor(out=R_(6), in0=R_(6), in1=R_(10), op=ALU.mult), reads=[prm], writes=[prm])
        k.op("dve", lambda e: e.tensor_tensor(out=R_(7), in0=R_(9), in1=R_(0), op=ALU.mult), reads=[prm], writes=[prm])
        k.op("dve", lambda e: e.tensor_tensor(out=R_(11), in0=R_(8), in1=fl(a_im), op=ALU.mult), reads=[prm, a_im], writes=[prm])
        k.op("dve", lambda e: e.tensor_tensor(out=R_(7), in0=R_(7), in1=R_(11), op=ALU.subtract), reads=[prm], writes=[prm])
        k.op("dve", lambda e: e.tensor_tensor(out=R_(7), in0=R_(7), in1=R_(10), op=ALU.mult), reads=[prm], writes=[prm])

        tio = k.sb("tio", [128, 513])
        k.op("pool", lambda e: e.iota(tio[:], pattern=[[1, 513]], base=0, channel_multiplier=0, allow_small_or_imprecise_dtypes=True), writes=[tio])
        s5d = k.sb("s5d", [32, 16])
        k.dma(s5d[:], w["s5_d"][:, :], reads=[w["s5_d"]], writes=[s5d])
        chunks = [(0, 256)] + [(256 + 512 * i, 512) for i in range(8)]
        order = {0: list(range(9)), 1: [0] + list(range(8, 0, -1))}
        u_p = k.pool("s5u", [32, NT], 2)
        ya_p = k.pool("s5ya", [32, NT], 4)
        bc_p = k.pool("s5bc", [128, 4, 16], 2)
        B2_p = k.pool("s5B2", [128, 2, 32], 2)
        BT_p = k.pool("s5BT", [32, 2, 128], 2)
        CL_p = k.pool("s5CL", [128, 2, 32], 2)
        tb_p = k.pool("s5tb", [128, 2, 513], 2)
        ang_p = k.pool("s5ang", [128, 513], 4)
        angi_p = k.pool("s5angi", [128, 513], 2, I32)
        wk_p = k.pool("s5wk", [128, 512], 18)
        g_p = k.pool("s5g", [128, 2, 512], 4)
        h_p = k.pool("s5h", [128, 2, 512], 4)
        ini_p = k.pool("s5ini", [128, 4], 6)
        yo_p = k.pool("s5yo", [32, 512], 3)

        def rev(t_, p0, p1, a, n):
            b_ = t_.t[p0:p1, a + n - 1:a + n]
            return bass.AP(tensor=b_.tensor, offset=b_.offset, ap=[list(b_.ap[0]), [-1, n]])

        for st in range(16):
            u = u_p()
            k.dma(u[:], US5[st * 32:(st + 1) * 32, :], reads=[US5], writes=[u])
            yad = [ya_p(), ya_p()]
            ctxd = {}
            for d in range(2):
                col = d * 16 + st
                bc = bc_p()
                for i_, nm_ in enumerate(("s5_b_re", "s5_b_im", "s5_c_re", "s5_c_im")):
                    k.dma(bc[:, i_, :], w[nm_][d, st * 128:(st + 1) * 128, :], reads=[w[nm_]], writes=[bc])
                B2 = B2_p()
                CL = CL_p()
                k.op("pool", lambda e, B2=B2: e.memset(B2[:], 0.0), writes=[B2])
                k.op("pool", lambda e, CL=CL: e.memset(CL[:], 0.0), writes=[CL])
                wk = wk_p()
                for gl in range(2):
                    ps_ = slice(gl * 64, (gl + 1) * 64)
                    cs_ = slice(gl * 16, (gl + 1) * 16)
                    kr = prm[ps_, 6, col:col + 1]
                    kim = prm[ps_, 7, col:col + 1]
                    k.op("dve", lambda e, wk=wk, bc=bc, ps_=ps_, kim=kim: e.tensor_scalar(wk[ps_, 0:16], bc[ps_, 1, :], kim, None, op0=ALU.mult), reads=[bc, prm], writes=[wk])
                    k.op("dve", lambda e, wk=wk, bc=bc, ps_=ps_, kim=kim: e.tensor_scalar(wk[ps_, 16:32], bc[ps_, 0, :], kim, None, op0=ALU.mult), reads=[bc, prm], writes=[wk])
                    k.op("dve", lambda e, B2=B2, wk=wk, bc=bc, ps_=ps_, cs_=cs_, kr=kr: e.scalar_tensor_tensor(out=B2[ps_, 0, cs_], in0=bc[ps_, 0, :], scalar=kr, in1=wk[ps_, 0:16], op0=ALU.mult, op1=ALU.subtract),
                         reads=[bc, prm, wk], writes=[B2])
                    k.op("dve", lambda e, B2=B2, wk=wk, bc=bc, ps_=ps_, cs_=cs_, kr=kr: e.scalar_tensor_tensor(out=B2[ps_, 1, cs_], in0=bc[ps_, 1, :], scalar=kr, in1=wk[ps_, 16:32], op0=ALU.mult, op1=ALU.add),
                         reads=[bc, prm, wk], writes=[B2])
                    k.op("dve", lambda e, CL=CL, bc=bc, ps_=ps_, cs_=cs_: e.tensor_copy(CL[ps_, 0, cs_], bc[ps_, 2, :]), reads=[bc], writes=[CL])
                    k.op("dve", lambda e, CL=CL, bc=bc, ps_=ps_, cs_=cs_: e.tensor_scalar(CL[ps_, 1, cs_], bc[ps_, 3, :], -1.0, None, op0=ALU.mult), reads=[bc], writes=[CL])
                BT = BT_p()
                for ri in range(2):
                    pst = k.next_ps()
                    k.op("pe", lambda e, pst=pst, B2=B2, ri=ri: e.transpose(pst[0:32, 0:128], B2[:, ri, :], ident[:]), reads=[B2, ident], writes=[pst])
                    k.op("act", lambda e, BT=BT, pst=pst, ri=ri: e.copy(BT[:, ri, :], pst[0:32, 0:128]), reads=[pst], writes=[BT])
                tb = tb_p()
                for ti_, shift in ((1, 0.0), (0, math.pi / 2.0)):
                    ang = ang_p()
                    k.op("dve", lambda e, ang=ang, shift=shift: e.tensor_scalar(ang[:], tio[:], prm[:, 2, col:col + 1], shift, op0=ALU.mult, op1=ALU.add), reads=[tio, prm], writes=[ang])
                    angi = angi_p()
                    k.op("dve", lambda e, ang=ang, angi=angi: e.tensor_scalar(angi[:], ang[:], 1.0 / TWO_PI, None, op0=ALU.mult), reads=[ang], writes=[angi])
                    angf = ang_p()
                    k.op("pool", lambda e, angf=angf, angi=angi: e.tensor_copy(angf[:], angi[:]), reads=[angi], writes=[angf])
                    k.op("dve", lambda e, ang=ang, angf=angf: e.scalar_tensor_tensor(out=ang[:], in0=angf[:], scalar=-TWO_PI, in1=ang[:], op0=ALU.mult, op1=ALU.add), reads=[angf, ang], writes=[ang])
                    k.op("dve", lambda e, ang=ang: e.tensor_scalar(ang[:], ang[:], 3.1415925, -3.1415925, op0=ALU.min, op1=ALU.max), reads=[ang], writes=[ang])
                    k.op("act", lambda e, tb=tb, ang=ang, ti_=ti_: e.activation(out=tb[:, ti_, :], in_=ang[:], func=AF.Sin), reads=[ang], writes=[tb])
                ctxd[d] = dict(col=col, BT=BT, CL=CL, tb=tb, rho_b=prm[:, 3, col:col + 1], prev=None)
            def chunk_gen(d, step):
                cx = ctxd[d]
                col, BT, CL, tb, rho_b, prev, ya = cx["col"], cx["BT"], cx["CL"], cx["tb"], cx["rho_b"], cx["prev"], yad[d]
                ci = order[d][step]
                c0, n = chunks[ci]
                rhs_u = u[:, c0:c0 + n] if d == 0 else rev(u, 0, 32, c0, n)
                pbr = k.next_ps()
                pbi = k.next_ps()
                k.mm(pbr, pbr[:, 0:n], BT, BT[:, 0, :], u, rhs_u, True, True)
                yield
                k.mm(pbi, pbi[:, 0:n], BT, BT[:, 1, :], u, rhs_u, True, True)
                yield
                cosT = tb[:, 0, 0:n]
                sinT = tb[:, 1, 0:n]
                w0, w1, w2, w3_ = wk_p(), wk_p(), wk_p(), wk_p()
                k.op("dve", lambda e, w0=w0, pbr=pbr, cosT=cosT: e.tensor_tensor(out=w0[:, 0:n], in0=pbr[:, 0:n], in1=cosT, op=ALU.mult), reads=[pbr, tb], writes=[w0])
                yield
                k.op("dve", lambda e, w1=w1, pbi=pbi, sinT=sinT: e.tensor_tensor(out=w1[:, 0:n], in0=pbi[:, 0:n], in1=sinT, op=ALU.mult), reads=[pbi, tb], writes=[w1])
                yield
                k.op("dve", lambda e, w2=w2, pbi=pbi, cosT=cosT: e.tensor_tensor(out=w2[:, 0:n], in0=pbi[:, 0:n], in1=cosT, op=ALU.mult), reads=[pbi, tb], writes=[w2])
                yield
                k.op("dve", lambda e, w3_=w3_, pbr=pbr, sinT=sinT: e.tensor_tensor(out=w3_[:, 0:n], in0=pbr[:, 0:n], in1=sinT, op=ALU.mult), reads=[pbr, tb], writes=[w3_])
                yield
                k.op("pool", lambda e, w0=w0, w1=w1: e.tensor_tensor(out=w0[:, 0:n], in0=w0[:, 0:n], in1=w1[:, 0:n], op=ALU.add), reads=[w0, w1], writes=[w0])
                yield
                k.op("pool", lambda e, w2=w2, w3_=w3_: e.tensor_tensor(out=w2[:, 0:n], in0=w2[:, 0:n], in1=w3_[:, 0:n], op=ALU.subtract), reads=[w2, w3_], writes=[w2])
                yield
                ini = ini_p()
                if prev is None:
                    k.op("pool", lambda e, ini=ini: e.memset(ini[:], 0.0), writes=[ini])
                    yield
                else:
                    gp, npv = prev
                    cr = tb[:, 0, npv:npv + 1]
                    sr = tb[:, 1, npv:npv + 1]
                    k.op("dve", lambda e, ini=ini, gp=gp, npv=npv, cr=cr: e.tensor_tensor(out=ini[:, 2:3], in0=gp[:, 0, npv - 1:npv], in1=cr, op=ALU.mult), reads=[gp, tb], writes=[ini])
                    yield
                    k.op("dve", lambda e, ini=ini, gp=gp, npv=npv, sr=sr: e.tensor_tensor(out=ini[:, 3:4], in0=gp[:, 1, npv - 1:npv], in1=sr, op=ALU.mult), reads=[gp, tb], writes=[ini])
                    yield
                    k.op("dve", lambda e, ini=ini: e.tensor_tensor(out=ini[:, 0:1], in0=ini[:, 2:3], in1=ini[:, 3:4], op=ALU.subtract), reads=[ini], writes=[ini])
                    yield
                    k.op("dve", lambda e, ini=ini, gp=gp, npv=npv, sr=sr: e.tensor_tensor(out=ini[:, 2:3], in0=gp[:, 0, npv - 1:npv], in1=sr, op=ALU.mult), reads=[gp, tb], writes=[ini])
                    yield
                    k.op("dve", lambda e, ini=ini, gp=gp, npv=npv, cr=cr: e.tensor_tensor(out=ini[:, 3:4], in0=gp[:, 1, npv - 1:npv], in1=cr, op=ALU.mult), reads=[gp, tb], writes=[ini])
                    yield
                    k.op("dve", lambda e, ini=ini: e.tensor_tensor(out=ini[:, 1:2], in0=ini[:, 2:3], in1=ini[:, 3:4], op=ALU.add), reads=[ini], writes=[ini])
                    yield
                g = g_p()
                k.op("dve", lambda e, g=g, w0=w0, ini=ini: e.tensor_tensor_scan(g[:, 0, 0:n], rho_b.to_broadcast([128, n]), w0[:, 0:n], ini[:, 0:1], op0=ALU.mult, op1=ALU.add),
                     reads=[prm, w0, ini], writes=[g])
                yield
                k.op("dve", lambda e, g=g, w2=w2, ini=ini: e.tensor_tensor_scan(g[:, 1, 0:n], rho_b.to_broadcast([128, n]), w2[:, 0:n], ini[:, 1:2], op0=ALU.mult, op1=ALU.add),
                     reads=[prm, w2, ini], writes=[g])
                yield
                prev = (g, n)
                cx["prev"] = prev
                h = h_p()
                x0_, x1_, x2_, x3_ = wk_p(), wk_p(), wk_p(), wk_p()
                k.op("pool", lambda e, x0_=x0_, g=g, cosT=cosT: e.tensor_tensor(out=x0_[:, 0:n], in0=g[:, 0, 0:n], in1=cosT, op=ALU.mult), reads=[g, tb], writes=[x0_])
                yield
                k.op("pool", lambda e, x1_=x1_, g=g, sinT=sinT: e.tensor_tensor(out=x1_[:, 0:n], in0=g[:, 1, 0:n], in1=sinT, op=ALU.mult), reads=[g, tb], writes=[x1_])
                yield
                k.op("dve", lambda e, x2_=x2_, g=g, sinT=sinT: e.tensor_tensor(out=x2_[:, 0:n], in0=g[:, 0, 0:n], in1=sinT, op=ALU.mult), reads=[g, tb], writes=[x2_])
                yield
                k.op("dve", lambda e, x3_=x3_, g=g, cosT=cosT: e.tensor_tensor(out=x3_[:, 0:n], in0=g[:, 1, 0:n], in1=cosT, op=ALU.mult), reads=[g, tb], writes=[x3_])
                yield
                k.op("pool", lambda e, h=h, x0_=x0_, x1_=x1_: e.tensor_tensor(out=h[:, 0, 0:n], in0=x0_[:, 0:n], in1=x1_[:, 0:n], op=ALU.subtract), reads=[x0_, x1_], writes=[h])
                yield
                k.op("dve", lambda e, h=h, x2_=x2_, x3_=x3_: e.tensor_tensor(out=h[:, 1, 0:n], in0=x2_[:, 0:n], in1=x3_[:, 0:n], op=ALU.add), reads=[x2_, x3_], writes=[h])
                yield
                py = k.next_ps()
                for ri in range(2):
                    hb_ = h.t[:, ri, :]
                    if d == 0:
                        rhs_h = h[:, ri, 0:n]
                    else:
                        b_ = h.t[:, ri, n - 1:n]
                        rhs_h = bass.AP(tensor=b_.tensor, offset=b_.offset, ap=[list(b_.ap[0]), [-1, n]])
                    k.mm(py, py[0:32, 0:n], CL, CL[:, ri, :], h, rhs_h, ri == 0, ri == 1)
                    yield
                if d == 0:
                    k.op("act", lambda e, ya=ya, py=py, c0=c0, n=n: e.copy(ya[:, c0:c0 + n], py[0:32, 0:n]), reads=[py], writes=[ya])
                    yield
                else:
                    k.op("dve", lambda e, ya=ya, py=py, c0=c0, n=n: e.tensor_copy(ya[:, c0:c0 + n], py[0:32, 0:n]), reads=[py], writes=[ya])
                    yield
            for step in range(9):
                gens = [chunk_gen(0, step), chunk_gen(1, step)]
                while gens:
                    for g_ in list(gens):
                        try:
                            next(g_)
                        except StopIteration:
                            gens.remove(g_)
            ya = yad[0]
            for (c0, n) in chunks:
                k.op("pool", lambda e, c0=c0, n=n: e.tensor_tensor(out=yad[0][:, c0:c0 + n], in0=yad[1][:, c0:c0 + n], in1=yad[0][:, c0:c0 + n], op=ALU.add), reads=[yad[0], yad[1]], writes=[yad[0]])
                yo = yo_p()
                k.op("dve", lambda e, yo=yo, c0=c0, n=n: e.scalar_tensor_tensor(out=yo[:, 0:n], in0=u[:, c0:c0 + n], scalar=s5d[:, st:st + 1], in1=ya[:, c0:c0 + n], op0=ALU.mult, op1=ALU.add),
                     reads=[u, s5d, ya], writes=[yo])
                t1 = yo_p()
                k.op("dve", lambda e, yo=yo, t1=t1, n=n: e.tensor_tensor(out=t1[:, 0:n], in0=yo[:, 0:n], in1=yo[:, 0:n], op=ALU.mult), reads=[yo], writes=[t1])
                k.op("dve", lambda e, t1=t1, n=n: e.tensor_scalar(t1[:, 0:n], t1[:, 0:n], 0.044715, 1.0, op0=ALU.mult, op1=ALU.add), reads=[t1], writes=[t1])
                k.op("dve", lambda e, yo=yo, t1=t1, n=n: e.tensor_tensor(out=t1[:, 0:n], in0=t1[:, 0:n], in1=yo[:, 0:n], op=ALU.mult), reads=[t1, yo], writes=[t1])
                k.op("act", lambda e, t1=t1, n=n: e.activation(out=t1[:, 0:n], in_=t1[:, 0:n], func=AF.Sigmoid, scale=2.0 * math.sqrt(2.0 / math.pi)), reads=[t1], writes=[t1])
                k.op("dve", lambda e, yo=yo, t1=t1, n=n: e.tensor_tensor(out=yo[:, 0:n], in0=yo[:, 0:n], in1=t1[:, 0:n], op=ALU.mult), reads=[t1, yo], writes=[yo])
                k.dma(S5Y[st * 32:(st + 1) * 32, c0:c0 + n], yo[:, 0:n], reads=[yo], writes=[S5Y], q="act")
        k.phase_end()
        k.phase_begin()
        gw = k.sb("s5gw", [128, 4, 512])
        k.dma(gw[:], w["s5_gw"].t.rearrange("(kc p) n -> p kc n", p=128), reads=[w["s5_gw"]], writes=[gw])
        ggb = k.sb("s5gb", [128, 4])
        k.dma(ggb[:], w["s5_gb"][:, :], reads=[w["s5_gb"]], writes=[ggb])
        yb_p = k.pool("s5yb", [128, 4, 512], 2)
        go_p2 = k.pool("s5go", [128, 512], 3)
        for (c0, n) in NBLK:
            yb = yb_p()
            k.dma(yb[:, :, 0:n], S5Y[:, c0:c0 + n].rearrange("(kc p) t -> p kc t", p=128), reads=[S5Y], writes=[yb])
            for mc_ in range(4):
                ps = k.next_ps()
                for kc in range(4):
                    k.mm(ps, ps[:, 0:n], gw, gw[:, kc, mc_ * 128:(mc_ + 1) * 128], yb, yb[:, kc, 0:n], kc == 0, kc == 3)
                go = go_p2()
                k.op("act", lambda e, go=go, ps=ps, mc_=mc_: e.activation(out=go[:, 0:n], in_=ps[:, 0:n], func=AF.Sigmoid, bias=ggb[:, mc_:mc_ + 1]), reads=[ps, ggb], writes=[go])
                k.op("dve", lambda e, go=go, yb=yb, mc_=mc_: e.tensor_tensor(out=go[:, 0:n], in0=go[:, 0:n], in1=yb[:, mc_, 0:n], op=ALU.mult), reads=[go, yb], writes=[go])
                k.dma(S5O[mc_ * 128:(mc_ + 1) * 128, c0:c0 + n], go[:, 0:n], reads=[go], writes=[S5O], q="act")
        k.phase_end()
        if stop_after == "E":
            break

        k.phase_begin()
        modt = k.sb("modtF", [128, 96])
        k.dma(modt[:], MODT[:, :], reads=[MODT], writes=[modt])
        m3 = modt.t[:, :].rearrange("p (f j) -> p f j", j=2)
        brw = []
        for nm_ in ("br_attn_w", "br_hyena_w", "br_s5_w"):
            t_ = k.sb(nm_, [128, 4, D])
            k.dma(t_[:], w[nm_].t.rearrange("(kc p) n -> p kc n", p=128), reads=[w[nm_]], writes=[t_])
            brw.append(t_)
        ow = k.sb("out_w", [128, 8, D])
        k.dma(ow[:], w["out_w"].t.rearrange("(kc p) n -> p kc n", p=128), reads=[w["out_w"]], writes=[ow])
        br_p = k.pool("brin", [128, 3, 4, 512], 1)
        gt_p = k.pool("gt", [128, 3, 512], 2)
        mm_p = k.pool("mmix", [128, 8, 512], 1)
        xb_p = k.pool("xbF", [128, 8, 512], 1)
        t_p = k.pool("tF", [128, 512], 4)
        xo_p = k.pool("xoF", [128, 512], 3)
        srcs = (ATT, HY, S5O)
        for bi, (c0, n) in enumerate(NBLK):
            if bi == 0 and not need_ctx:
                continue
            j = 1 if bi == 0 else 0
            br = br_p()
            for b_ in range(3):
                k.dma(br[:, b_, :, 0:n], srcs[b_][:, c0:c0 + n].rearrange("(kc p) t -> p kc t", p=128), reads=[srcs[b_]], writes=[br])
            xb = xb_p()
            k.dma(xb[:, :, 0:n], XT[:, c0:c0 + n].rearrange("(kc p) t -> p kc t", p=128), reads=[XT], writes=[xb])
            mx = mm_p()
            for fc in range(8):
                gt = gt_p()
                for b_ in range(3):
                    k.dma(gt[:, b_, 0:n], GATE[b_ * D + fc * 128:b_ * D + (fc + 1) * 128, c0:c0 + n], reads=[GATE], writes=[gt])
                pb = [k.next_ps() for _ in range(3)]
                for b_ in range(3):
                    for kc in range(4):
                        k.mm(pb[b_], pb[b_][:, 0:n], brw[b_], brw[b_][:, kc, fc * 128:(fc + 1) * 128], br, br[:, b_, kc, 0:n], kc == 0, kc == 3)
                ta, tb_, tc_ = t_p(), t_p(), t_p()
                k.op("dve", lambda e, ta=ta, pb=pb, gt=gt: e.tensor_tensor(out=ta[:, 0:n], in0=pb[0][:, 0:n], in1=gt[:, 0, 0:n], op=ALU.mult), reads=[pb[0], gt], writes=[ta])
                k.op("dve", lambda e, tb_=tb_, pb=pb, gt=gt: e.tensor_tensor(out=tb_[:, 0:n], in0=pb[1][:, 0:n], in1=gt[:, 1, 0:n], op=ALU.mult), reads=[pb[1], gt], writes=[tb_])
                k.op("dve", lambda e, tc_=tc_, pb=pb, gt=gt: e.tensor_tensor(out=tc_[:, 0:n], in0=pb[2][:, 0:n], in1=gt[:, 2, 0:n], op=ALU.mult), reads=[pb[2], gt], writes=[tc_])
                k.op("pool", lambda e, ta=ta, tb_=tb_: e.tensor_tensor(out=ta[:, 0:n], in0=ta[:, 0:n], in1=tb_[:, 0:n], op=ALU.add), reads=[ta, tb_], writes=[ta])
                k.op("pool", lambda e, mx=mx, ta=ta, tc_=tc_, fc=fc: e.tensor_tensor(out=mx[:, fc, 0:n], in0=ta[:, 0:n], in1=tc_[:, 0:n], op=ALU.add), reads=[ta, tc_], writes=[mx])
            for dc in range(8):
                po = k.next_ps()
                for fc in range(8):
                    k.mm(po, po[:, 0:n], ow, ow[:, fc, dc * 128:(dc + 1) * 128], mx, mx[:, fc, 0:n], fc == 0, fc == 7)
                xo = xo_p()
                if dbg:
                    mo = xo_p()
                    k.op("act", lambda e, mo=mo, po=po: e.copy(mo[:, 0:n], po[:, 0:n]), reads=[po], writes=[mo])
                    k.dma(MIX[dc * 128:(dc + 1) * 128, c0:c0 + n], mo[:, 0:n], reads=[mo], writes=[MIX])
                k.op("dve", lambda e, xo=xo, po=po, xb=xb, dc=dc: e.scalar_tensor_tensor(out=xo[:, 0:n], in0=po[:, 0:n], scalar=m3[:, 16 + dc, j:j + 1], in1=xb[:, dc, 0:n], op0=ALU.mult, op1=ALU.add),
                     reads=[po, modt, xb], writes=[xo])
                k.dma(XT[dc * 128:(dc + 1) * 128, c0:c0 + n], xo[:, 0:n], reads=[xo], writes=[XT], q="act")
                if dbg:
                    k.dma(XDBG[dc * 128:(dc + 1) * 128, c0:c0 + n], xo[:, 0:n], reads=[xo], writes=[XDBG])
        k.phase_end()
        if stop_after == "F":
            break

        k.phase_begin()
        modt = k.sb("modtG", [128, 96])
        k.dma(modt[:], MODT[:, :], reads=[MODT], writes=[modt])
        m3 = modt.t[:, :].rearrange("p (f j) -> p f j", j=2)
        g2n = k.sb("g2n", [128, 8])
        k.dma(g2n[:], w["n2g"][:, :], reads=[w["n2g"]], writes=[g2n])
        A2 = k.sb("A2", [128, 8, 2])
        for j in range(2):
            k.op("dve", lambda e, j=j: e.scalar_tensor_tensor(out=A2[:, :, j], in0=m3[:, 32:40, j], scalar=1.0, in1=g2n[:], op0=ALU.add, op1=ALU.mult), reads=[modt, g2n], writes=[A2])
        wq = k.sb("peer_wq", [128, 8, 2048])
        wqv = w["peer_wq"].t.rearrange("(kc p) n -> p kc n", p=128)
        for kc in range(8):
            k.dma(wq[:, kc, :], wqv[:, kc, :], reads=[w["peer_wq"]], writes=[wq])
        kT = k.sb("peer_kT", [128, 16, 128])
        k.dma(kT[:], w["peer_kT"].t.rearrange("j k n -> k j n"), reads=[w["peer_kT"]], writes=[kT])
        csel = k.sb("csel", [128, 255])
        k.dma(csel[:], csel_in[:, :], reads=[csel_in], writes=[csel])
        io16 = k.sb("io16", [128, 16])
        k.dma(io16[:], iota16_in[:, :], reads=[iota16_in], writes=[io16])
        xb_p = k.pool("xbG", [128, 8, 128], 1)
        hb_p = k.pool("hbG", [128, 8, 128], 1)
        sq_p = k.pool("sqG", [128, 8, 128], 1)
        sm_p = k.pool("smG", [128, 128], 6)
        qT_p = k.pool("qTG", [128, 16, 128], 1)
        S_p = k.pool("SG", [128, 16, 128], 1)
        SW_p = k.pool("SWG", [128, 128], 2)
        TV_p = k.pool("TVG", [128, 16, 16], 1)
        TI_p = k.pool("TIG", [128, 16, 16], 1, U32)
        TF_p = k.pool("TFG", [128, 16, 16], 1)
        cand_p = k.pool("candG", [128, 8, 256], 1)
        cw_p = k.pool("cwG", [128, 256], 2)
        BV_p = k.pool("BVG", [128, 8, 16], 1)
        BP_p = k.pool("BPG", [128, 8, 16], 1, U32)
        BI_p = k.pool("BIG", [128, 8, 16], 2, I32)
        oh_p = k.pool("ohG", [128, 16, 16], 2)
        IDF_p = k.pool("IDFG", [128, 128], 1)
        GT_p = k.pool("GTG", [128, 128], 1)
        idT_p = k.pool("idTG", [128, 128], 1, I32)
        gT_p = k.pool("gTG", [128, 128], 1)
        htok_p = k.pool("htokG", [128, D], 1)
        ug_p = k.pool("ugG", [128, D], 6)
        vg_p = k.pool("vgG", [128, D], 6)
        dots_p = k.pool("dotsG", [128, 128], 1)
        wts_p = k.pool("wtsG", [128, 128], 1)
        wt_p = k.pool("wtG", [128, 128], 3)
        otok_p = k.pool("otokG", [128, D], 1)
        xo_p = k.pool("xoG", [128, 128], 3)
        GELU_C = 2.0 * math.sqrt(2.0 / math.pi)

        def bc3(t_ap_base, strides):
            return bass.AP(tensor=t_ap_base.tensor, offset=t_ap_base.offset, ap=[list(t_ap_base.ap[0]), [strides[0], 8], [strides[1], 16], [strides[2], 16]])

        tiles = [(t0, 1 if t0 < C else 0) for t0 in range(0 if need_ctx else C, NT, 128)]
        for ti_, (t0, j) in enumerate(tiles):
            if ti_ > 0 and ti_ % 8 == 0:
                k.barrier()
            xb = xb_p()
            k.dma(xb[:], XT[:, t0:t0 + 128].rearrange("(kc p) t -> p kc t", p=128), reads=[XT], writes=[xb])
            sq = sq_p()
            k.op("act", lambda e, sq=sq, xb=xb: e.activation(out=sq[:], in_=xb[:], func=AF.Square), reads=[xb], writes=[sq])
            pss = k.next_ps()
            for kc in range(8):
                k.mm(pss, pss[:, 0:128], ones, ones[:], sq, sq[:, kc, :], kc == 0, kc == 7)
            rstd = sm_p()
            k.op("act", lambda e, rstd=rstd, pss=pss: e.activation(out=rstd[:], in_=pss[:, 0:128], func=AF.Sqrt, bias=eps_t[:, 0:1], scale=1.0 / D), reads=[pss, eps_t], writes=[rstd])
            k.op("dve", lambda e, rstd=rstd: e.reciprocal(rstd[:], rstd[:]), reads=[rstd], writes=[rstd])
            hb = hb_p()
            for kc in range(8):
                k.op("dve", lambda e, hb=hb, xb=xb, rstd=rstd, kc=kc: e.tensor_tensor(out=hb[:, kc, :], in0=xb[:, kc, :], in1=rstd[:], op=ALU.mult), reads=[xb, rstd], writes=[hb])
                k.op("pool", lambda e, hb=hb, kc=kc: e.tensor_scalar(hb[:, kc, :], hb[:, kc, :], A2[:, kc, j:j + 1], m3[:, 24 + kc, j:j + 1], op0=ALU.mult, op1=ALU.add), reads=[hb, A2, modt], writes=[hb])
            htok = htok_p()
            for kc in range(8):
                pst = k.next_ps()
                k.op("pe", lambda e, pst=pst, hb=hb, kc=kc: e.transpose(pst[:, 0:128], hb[:, kc, :], ident[:]), reads=[hb, ident], writes=[pst])
                k.op("act", lambda e, htok=htok, pst=pst, kc=kc: e.copy(htok[:, kc * 128:(kc + 1) * 128], pst[:, 0:128]), reads=[pst], writes=[htok])
            qT = qT_p()
            for jc in range(16):
                psq = k.next_ps()
                for kc in range(8):
                    k.mm(psq, psq[:, 0:128], wq, wq[:, kc, jc * 128:(jc + 1) * 128], hb, hb[:, kc, :], kc == 0, kc == 7)
                if jc % 2 == 0:
                    k.op("act", lambda e, qT=qT, psq=psq, jc=jc: e.copy(qT[:, jc, :], psq[:, 0:128]), reads=[psq], writes=[qT])
                else:
                    k.op("dve", lambda e, qT=qT, psq=psq, jc=jc: e.tensor_copy(qT[:, jc, :], psq[:, 0:128]), reads=[psq], writes=[qT])
            S = S_p()
            for jc in range(16):
                pss2 = k.next_ps()
                k.mm(pss2, pss2[:, 0:128], qT, qT[:, jc, :], kT, kT[:, jc, :], True, True)
                if jc % 2 == 0:
                    k.op("act", lambda e, S=S, pss2=pss2, jc=jc: e.copy(S[:, jc, :], pss2[:, 0:128]), reads=[pss2], writes=[S])
                else:
                    k.op("dve", lambda e, S=S, pss2=pss2, jc=jc: e.tensor_copy(S[:, jc, :], pss2[:, 0:128]), reads=[pss2], writes=[S])
            TV = TV_p(); TI = TI_p()
            for jc in range(16):
                SW = SW_p()
                k.op("dve", lambda e, TV=TV, S=S, jc=jc: e.max(out=TV[:, jc, 0:8], in_=S[:, jc, :]), reads=[S], writes=[TV])
                k.op("dve", lambda e, TI=TI, TV=TV, S=S, jc=jc: e.max_index(out=TI[:, jc, 0:8], in_max=TV[:, jc, 0:8], in_values=S[:, jc, :]), reads=[S, TV], writes=[TI])
                k.op("dve", lambda e, SW=SW, TV=TV, S=S, jc=jc: e.match_replace(out=SW[:], in_to_replace=TV[:, jc, 0:8], in_values=S[:, jc, :], imm_value=-1e30), reads=[S, TV], writes=[SW])
                k.op("dve", lambda e, TV=TV, SW=SW, jc=jc: e.max(out=TV[:, jc, 8:16], in_=SW[:]), reads=[SW], writes=[TV])
                k.op("dve", lambda e, TI=TI, TV=TV, SW=SW, jc=jc: e.max_index(out=TI[:, jc, 8:16], in_max=TV[:, jc, 8:16], in_values=SW[:]), reads=[SW, TV], writes=[TI])
            TF = TF_p()
            k.op("dve", lambda e, TF=TF, TI=TI: e.tensor_copy(TF[:], TI[:]), reads=[TI], writes=[TF])
            cand = cand_p()
            tvb = TV.t[:, 0:1, 0:1]
            cv4 = cand.t[:, :, :].rearrange("p h (a b) -> p h a b", b=16)
            in0 = bc3(TV.t[:, 0, 0:1], (32, 1, 0))
            in1 = bc3(TV.t[:, 1, 0:1], (32, 0, 1))
            k.op("dve", lambda e, cv4=cv4, in0=in0, in1=in1: e.tensor_tensor(out=cv4, in0=in0, in1=in1, op=ALU.add), reads=[TV], writes=[cand])
            BV = BV_p(); BP = BP_p()
            for h in range(8):
                cw = cw_p()
                k.op("dve", lambda e, BV=BV, cand=cand, h=h: e.max(out=BV[:, h, 0:8], in_=cand[:, h, :]), reads=[cand], writes=[BV])
                k.op("dve", lambda e, BP=BP, BV=BV, cand=cand, h=h: e.max_index(out=BP[:, h, 0:8], in_max=BV[:, h, 0:8], in_values=cand[:, h, :]), reads=[cand, BV], writes=[BP])
                k.op("dve", lambda e, cw=cw, BV=BV, cand=cand, h=h: e.match_replace(out=cw[:], in_to_replace=BV[:, h, 0:8], in_values=cand[:, h, :], imm_value=-1e30), reads=[cand, BV], writes=[cw])
                k.op("dve", lambda e, BV=BV, cw=cw, h=h: e.max(out=BV[:, h, 8:16], in_=cw[:]), reads=[cw], writes=[BV])
                k.op("dve", lambda e, BP=BP, BV=BV, cw=cw, h=h: e.max_index(out=BP[:, h, 8:16], in_max=BV[:, h, 8:16], in_values=cw[:]), reads=[cw, BV], writes=[BP])
            bpi = BP.t[:, :, :].bitcast(I32)
            Ai = BI_p(); Bi = BI_p()
            k.op("dve", lambda e, Ai=Ai, bpi=bpi: e.tensor_single_scalar(Ai[:], bpi, 4, op=ALU.arith_shift_right), reads=[BP], writes=[Ai])
            k.op("dve", lambda e, Bi=Bi, bpi=bpi: e.tensor_single_scalar(Bi[:], bpi, 15, op=ALU.bitwise_and), reads=[BP], writes=[Bi])
            Af = sm_p(); Bf = sm_p()
            k.op("dve", lambda e, Af=Af, Ai=Ai: e.tensor_copy(Af[:], Ai.t[:, :, :].rearrange("p h k -> p (h k)")), reads=[Ai], writes=[Af])
            k.op("dve", lambda e, Bf=Bf, Bi=Bi: e.tensor_copy(Bf[:], Bi.t[:, :, :].rearrange("p h k -> p (h k)")), reads=[Bi], writes=[Bf])
            IDF = IDF_p()
            isel = sm_p(); jsel = sm_p()
            for h in range(8):
                for (posf, seg, dst) in ((Af, 2 * h, isel), (Bf, 2 * h + 1, jsel)):
                    oh = oh_p()
                    pk = posf.t[:, h * 16:(h + 1) * 16]
                    pk3 = bass.AP(tensor=pk.tensor, offset=pk.offset, ap=[list(pk.ap[0]), [1, 16], [0, 16]])
                    io3 = bass.AP(tensor=io16.t[:, 0:16].tensor, offset=io16.t[:, 0:16].offset, ap=[list(io16.t[:, 0:16].ap[0]), [0, 16], [1, 16]])
                    tf_ = TF.t[:, seg, 0:16]
                    tf3 = bass.AP(tensor=tf_.tensor, offset=tf_.offset, ap=[list(tf_.ap[0]), [0, 16], [1, 16]])
                    k.op("dve", lambda e, oh=oh, pk3=pk3, io3=io3: e.tensor_tensor(out=oh[:], in0=pk3, in1=io3, op=ALU.is_equal), reads=[posf, io16], writes=[oh])
                    k.op("dve", lambda e, oh=oh, tf3=tf3: e.tensor_tensor(out=oh[:], in0=oh[:], in1=tf3, op=ALU.mult), reads=[oh, TF], writes=[oh])
                    k.op("dve", lambda e, oh=oh, dst=dst, h=h: e.tensor_reduce(out=dst[:, h * 16:(h + 1) * 16], in_=oh[:], axis=AX.X, op=ALU.add), reads=[oh], writes=[dst])
            k.op("dve", lambda e, IDF=IDF, isel=isel, jsel=jsel: e.scalar_tensor_tensor(out=IDF[:], in0=isel[:], scalar=128.0, in1=jsel[:], op0=ALU.mult, op1=ALU.add), reads=[isel, jsel], writes=[IDF])
            k.op("dve", lambda e, IDF=IDF: e.tensor_scalar(IDF[:], IDF[:], 16383.0, 0.0, op0=ALU.min, op1=ALU.max), reads=[IDF], writes=[IDF])
            GT = GT_p()
            gst = sm_p()
            for h in range(8):
                k.op("dve", lambda e, gst=gst, BV=BV, h=h: e.tensor_scalar(gst[:, h:h + 1], BV[:, h, 0:1], -1.0, None, op0=ALU.mult), reads=[BV], writes=[gst])
                k.op("act", lambda e, GT=GT, BV=BV, gst=gst, h=h: e.activation(out=GT[:, h * 16:(h + 1) * 16], in_=BV[:, h, :], func=AF.Exp, bias=gst[:, h:h + 1], accum_out=gst[:, 8 + h:9 + h]),
                     reads=[BV, gst], writes=[GT, gst])
            k.op("dve", lambda e, gst=gst: e.reciprocal(gst[:, 16:24], gst[:, 8:16]), reads=[gst], writes=[gst])
            for h in range(8):
                k.op("dve", lambda e, GT=GT, gst=gst, h=h: e.tensor_scalar(GT[:, h * 16:(h + 1) * 16], GT[:, h * 16:(h + 1) * 16], gst[:, 16 + h:17 + h], None, op0=ALU.mult), reads=[GT, gst], writes=[GT])
            idT = idT_p(); gT = gT_p()
            pst = k.next_ps()
            k.op("pe", lambda e, pst=pst, IDF=IDF: e.transpose(pst[:, 0:128], IDF[:], ident[:]), reads=[IDF, ident], writes=[pst])
            k.op("dve", lambda e, idT=idT, pst=pst: e.tensor_copy(idT[:], pst[:, 0:128]), reads=[pst], writes=[idT])
            pst2 = k.next_ps()
            k.op("pe", lambda e, pst2=pst2, GT=GT: e.transpose(pst2[:, 0:128], GT[:], ident[:]), reads=[GT, ident], writes=[pst2])
            k.op("act", lambda e, gT=gT, pst2=pst2: e.copy(gT[:], pst2[:, 0:128]), reads=[pst2], writes=[gT])
            dots = dots_p()
            for t in range(128):
                ug = ug_p()
                k.dma(None, None, reads=[idT, w["peer_u"]], writes=[ug], q="pool",
                      fn=lambda e, ug=ug, t=t: e.indirect_dma_start(out=ug[:], out_offset=None, in_=w["peer_u"][:, :],
                                                                  in_offset=bass.IndirectOffsetOnAxis(ap=idT[:, t:t + 1], axis=0)))
                pb0 = k.next_ps(); pb1 = k.next_ps()
                sel = ident[:, t:t + 1].to_broadcast([128, 128])
                k.mm(pb0, pb0[:, :], ident, sel, htok, htok[:, 0:512], True, True)
                k.mm(pb1, pb1[:, :], ident, sel, htok, htok[:, 512:1024], True, True)
                hrow = vg_p()
                k.op("act", lambda e, hrow=hrow, pb0=pb0: e.copy(hrow[:, 0:512], pb0[:, :]), reads=[pb0], writes=[hrow])
                k.op("act", lambda e, hrow=hrow, pb1=pb1: e.copy(hrow[:, 512:1024], pb1[:, :]), reads=[pb1], writes=[hrow])
                k.op("dve", lambda e, ug=ug, hrow=hrow, t=t: e.scalar_tensor_tensor(out=hrow[:], in0=ug[:], scalar=1.0, in1=hrow[:], op0=ALU.mult, op1=ALU.mult, accum_out=dots[:, t:t + 1]),
                     reads=[ug, hrow], writes=[hrow, dots])
            wts = wts_p()
            g1_ = sm_p()
            k.op("dve", lambda e, g1_=g1_: e.tensor_tensor(out=g1_[:], in0=dots[:], in1=dots[:], op=ALU.mult), reads=[dots], writes=[g1_])
            k.op("dve", lambda e, g1_=g1_: e.tensor_scalar(g1_[:], g1_[:], 0.044715, 1.0, op0=ALU.mult, op1=ALU.add), reads=[g1_], writes=[g1_])
            k.op("dve", lambda e, g1_=g1_: e.tensor_tensor(out=g1_[:], in0=g1_[:], in1=dots[:], op=ALU.mult), reads=[g1_, dots], writes=[g1_])
            k.op("act", lambda e, g1_=g1_: e.activation(out=g1_[:], in_=g1_[:], func=AF.Sigmoid, scale=GELU_C), reads=[g1_], writes=[g1_])
            k.op("dve", lambda e, g1_=g1_: e.tensor_tensor(out=g1_[:], in0=g1_[:], in1=dots[:], op=ALU.mult), reads=[g1_, dots], writes=[g1_])
            k.op("dve", lambda e, wts=wts, g1_=g1_, gT=gT: e.tensor_tensor(out=wts[:], in0=g1_[:], in1=gT[:], op=ALU.mult), reads=[g1_, gT], writes=[wts])
            po0 = k.next_ps(); po1 = k.next_ps()
            for t in range(128):
                vg = vg_p()
                k.dma(None, None, reads=[idT, w["peer_v"]], writes=[vg], q="pool",
                      fn=lambda e, vg=vg, t=t: e.indirect_dma_start(out=vg[:], out_offset=None, in_=w["peer_v"][:, :],
                                                                  in_offset=bass.IndirectOffsetOnAxis(ap=idT[:, t:t + 1], axis=0)))
                wt_ = wt_p()
                k.op("dve", lambda e, wt_=wt_, t=t: e.tensor_scalar(wt_[:], csel[:, 127 - t:255 - t], wts[:, t:t + 1], None, op0=ALU.mult), reads=[csel, wts], writes=[wt_])
                k.mm(po0, po0[:, :], wt_, wt_[:], vg, vg[:, 0:512], t == 0, t == 127)
                k.mm(po1, po1[:, :], wt_, wt_[:], vg, vg[:, 512:1024], t == 0, t == 127)
            otok = otok_p()
            k.op("act", lambda e, otok=otok, po0=po0: e.copy(otok[:, 0:512], po0[:, :]), reads=[po0], writes=[otok])
            k.op("dve", lambda e, otok=otok, po1=po1: e.tensor_copy(otok[:, 512:1024], po1[:, :]), reads=[po1], writes=[otok])
            for dc in range(8):
                pst = k.next_ps()
                k.op("pe", lambda e, pst=pst, otok=otok, dc=dc: e.transpose(pst[:, 0:128], otok[:, dc * 128:(dc + 1) * 128], ident[:]), reads=[otok, ident], writes=[pst])
                xo = xo_p()
                k.op("dve", lambda e, xo=xo, pst=pst, xb=xb, dc=dc: e.scalar_tensor_tensor(out=xo[:], in0=pst[:, 0:128], scalar=m3[:, 40 + dc, j:j + 1], in1=xb[:, dc, :], op0=ALU.mult, op1=ALU.add),
                     reads=[pst, modt, xb], writes=[xo])
                k.dma(XT[dc * 128:(dc + 1) * 128, t0:t0 + 128], xo[:], reads=[xo], writes=[XT])
        k.phase_end()
        if stop_after == "G":
            break

    if stop_after is None:
        k.phase_begin()
        fg = k.sb("final_g", [128, 8])
        k.dma(fg[:], finalg_in[:, :], reads=[finalg_in], writes=[fg])
        xb_p = k.pool("xbN", [128, 8, 512], 2)
        sq_p = k.pool("sqN", [128, 8, 512], 1)
        rs_p = k.pool("rsN", [128, 512], 2)
        yo_p = k.pool("yoN", [128, 512], 4)
        for bi in range(8):
            t0 = C + bi * 512
            xb = xb_p()
            k.dma(xb[:], XT[:, t0:t0 + 512].rearrange("(kc p) t -> p kc t", p=128), reads=[XT], writes=[xb])
            sq = sq_p()
            k.op("act", lambda e, sq=sq, xb=xb: e.activation(out=sq[:], in_=xb[:], func=AF.Square), reads=[xb], writes=[sq])
            pss = k.next_ps()
            for kc in range(8):
                k.mm(pss, pss[:, :], ones, ones[:], sq, sq[:, kc, :], kc == 0, kc == 7)
            rstd = rs_p()
            k.op("act", lambda e, rstd=rstd, pss=pss: e.activation(out=rstd[:], in_=pss[:, :], func=AF.Sqrt, bias=eps_t[:, 0:1], scale=1.0 / D), reads=[pss, eps_t], writes=[rstd])
            k.op("dve", lambda e, rstd=rstd: e.reciprocal(rstd[:], rstd[:]), reads=[rstd], writes=[rstd])
            for kc in range(8):
                yo = yo_p()
                k.op("dve", lambda e, yo=yo, xb=xb, rstd=rstd, kc=kc: e.scalar_tensor_tensor(out=yo[:], in0=xb[:, kc, :], scalar=fg[:, kc:kc + 1], in1=rstd[:], op0=ALU.mult, op1=ALU.mult),
                     reads=[xb, fg, rstd], writes=[yo])
                k.dma(out[kc * 128:(kc + 1) * 128, bi * 512:(bi + 1) * 512], yo[:], reads=[yo], writes=[out], q="act")
        k.phase_end()
    else:
        k.phase_begin()
        fin_p = k.pool("fin", [128, L], 2)
        for kc in range(8):
            st = fin_p()
            k.dma(st[:], XT[kc * 128:(kc + 1) * 128, C:NT], reads=[XT], writes=[st])
            k.dma(out[kc * 128:(kc + 1) * 128, :], st[:], reads=[st], writes=[out])
        k.phase_end()
    k.finish()
    return nc, k


def kernel(**inputs):
    inp = {kk: np.asarray(v) for kk, v in inputs.items()}
    nc, _ = build()
    in_maps = [_prep(inp, b) for b in range(4)]
    res = run_bass_kernel_spmd(nc, in_maps, core_ids=list(range(4)))
    return np.stack([np.ascontiguousarray(res.results[b]["out"].T) for b in range(4)], 0).astype(np.float32)
```
